# Optimizing a Trainium2 kernel written in Bass

```python
import jax, jax.numpy as jnp
from jax import lax
import numpy as np


D_MODEL = 2048
BATCH = 2
SEQ = 8192
DEPTH = 1

CHUNK = 64
HG_HEADS = 8
HG_DK = 128
HG_DV = 128
HG_WIDTH = HG_HEADS * HG_DK
SA_HEADS = 8
SA_HEAD_DIM = 128
SA_WIDTH = SA_HEADS * SA_HEAD_DIM
IDX_HEADS = 8
IDX_DIM = 64
MAX_TOPK = 256
N_BRANCH = 2
N_GROUPS = 4
EXPERTS_PER_GROUP = 8
N_EXPERTS = N_GROUPS * EXPERTS_PER_GROUP
TOPK_IN_GROUP = 2
D_FF_EXPERT = 1024
MOE_BLOCK = 128
DN_ALPHA = (2.0 * DEPTH) ** 0.25
DN_BETA = (8.0 * DEPTH) ** -0.25
LN_EPS = 1e-5
RMS_EPS = 1e-6
PROJ_SIZES = (HG_WIDTH, HG_WIDTH, HG_WIDTH, HG_WIDTH,
              SA_WIDTH, SA_WIDTH, SA_WIDTH,
              IDX_HEADS * IDX_DIM, IDX_DIM, IDX_HEADS,
              N_BRANCH * D_MODEL)
PROJ_TOTAL = sum(PROJ_SIZES)

kernel_name = 'hybrid_hgrn2_dsa_hmoe_block'


def _layer_norm(x, g, b):
    xf = x.astype(jnp.float32)
    mu = jnp.mean(xf, -1, keepdims=True)
    var = jnp.mean(jnp.square(xf - mu), -1, keepdims=True)
    y = (xf - mu) * lax.rsqrt(var + LN_EPS) * g.astype(jnp.float32) + b.astype(jnp.float32)
    return y.astype(x.dtype)


def _hgrn2(q, f_logit, i, g, lb, norm_g):
    bsz, seq, _ = q.shape
    n_chunks = seq // CHUNK
    f32 = jnp.float32
    shp_k = (bsz, n_chunks, CHUNK, HG_HEADS, HG_DK)
    shp_v = (bsz, n_chunks, CHUNK, HG_HEADS, HG_DV)
    f = lb + (1.0 - lb) * jax.nn.sigmoid(f_logit.astype(f32))
    qh = jax.nn.silu(q.astype(f32)).reshape(shp_k)
    kh = (1.0 - f).reshape(shp_k)
    vh = i.astype(f32).reshape(shp_v)
    cum = jnp.cumsum(jnp.log(f).reshape(shp_k), axis=2)
    cum_last = cum[:, :, -1:]
    q_dec = qh * jnp.exp(cum)
    k_dec = kh * jnp.exp(-cum)
    k_end = kh * jnp.exp(cum_last - cum)
    causal = jnp.tril(jnp.ones((CHUNK, CHUNK), dtype=bool))
    att = jnp.einsum('bnthd,bnshd->bnhts', q_dec, k_dec)
    att = jnp.where(causal, att, 0.0)
    o = jnp.einsum('bnhts,bnshv->bnthv', att, vh)
    chunk_kv = jnp.einsum('bnshd,bnshv->bnhdv', k_end, vh)
    chunk_decay = jnp.exp(cum_last[:, :, 0])

    def step(state, inp):
        dec, kv = inp
        return dec[..., None] * state + kv, state

    s0 = jnp.zeros((bsz, HG_HEADS, HG_DK, HG_DV), f32)
    _, s_prev = lax.scan(step, s0, (jnp.moveaxis(chunk_decay, 1, 0), jnp.moveaxis(chunk_kv, 1, 0)))
    s_prev = jnp.moveaxis(s_prev, 0, 1)
    o = o + jnp.einsum('bnthd,bnhdv->bnthv', q_dec, s_prev)
    o = o * lax.rsqrt(jnp.mean(jnp.square(o), -1, keepdims=True) + RMS_EPS)
    o = o.reshape(bsz, seq, HG_HEADS * HG_DV) * norm_g.astype(f32) * jax.nn.silu(g.astype(f32))
    return o.astype(q.dtype)


def _dsa(q, k, v, q_idx, k_idx, w_idx):
    bsz, seq, _ = q.shape
    n_chunks = seq // CHUNK
    topk = min(MAX_TOPK, seq // 4)
    f32 = jnp.float32
    q = q.reshape(bsz, seq, SA_HEADS, SA_HEAD_DIM)
    k = k.reshape(bsz, seq, SA_HEADS, SA_HEAD_DIM)
    v = v.reshape(bsz, seq, SA_HEADS, SA_HEAD_DIM)
    q_idx = q_idx.reshape(bsz, seq, IDX_HEADS, IDX_DIM)
    w_idx = w_idx * (IDX_HEADS ** -0.5 * IDX_DIM ** -0.5)
    scale = SA_HEAD_DIM ** -0.5
    slopes = 2.0 ** (-8.0 * jnp.arange(1, SA_HEADS + 1, dtype=f32) / SA_HEADS)
    key_chunk = jnp.arange(seq) // CHUNK
    gather = jax.vmap(lambda arr, idx: arr[idx])

    def one_chunk(c):
        start = c * CHUNK
        qc = lax.dynamic_slice_in_dim(q, start, CHUNK, axis=1)
        qic = lax.dynamic_slice_in_dim(q_idx, start, CHUNK, axis=1)
        wc = lax.dynamic_slice_in_dim(w_idx, start, CHUNK, axis=1)
        rel = jax.nn.relu(jnp.einsum('bqhd,bsd->bqhs', qic, k_idx))
        score = jnp.einsum('bqh,bqhs->bqs', wc, rel).astype(f32)
        score = jnp.where(key_chunk <= c, score, -jnp.inf)
        top_val, top_idx = lax.top_k(score, topk)
        valid = top_val > -jnp.inf
        ks = gather(k, top_idx)
        vs = gather(v, top_idx)
        qpos = start + jnp.arange(CHUNK)
        dist = jnp.abs(qpos[None, :, None] - top_idx).astype(f32)
        logits = jnp.einsum('bqhd,bqkhd->bqhk', qc, ks).astype(f32) * scale
        logits = logits - slopes[None, None, :, None] * dist[:, :, None, :]
        logits = jnp.where(valid[:, :, None, :], logits, -jnp.inf)
        p = jax.nn.softmax(logits, axis=-1)
        return jnp.einsum('bqhk,bqkhd->bqhd', p.astype(v.dtype), vs)

    out = lax.map(one_chunk, jnp.arange(n_chunks))
    return jnp.transpose(out, (1, 0, 2, 3, 4)).reshape(bsz, seq, SA_WIDTH)


def _token_mixer(x, w_in, b_in, lb, hg_norm_g, w_branch_a, w_branch_b, w_out):
    proj = x @ w_in + b_in
    points = [int(p) for p in np.cumsum(PROJ_SIZES)[:-1]]
    aq, af, ai, ag, bq, bk, bv, iq, ik, iw, gates = jnp.split(proj, points, axis=-1)
    branch_a = _hgrn2(aq, af, ai, ag, lb, hg_norm_g)
    branch_b = _dsa(bq, bk, bv, iq, ik, iw)
    gate = jax.nn.sigmoid(gates.astype(jnp.float32)).astype(x.dtype)
    gate_a, gate_b = jnp.split(gate, N_BRANCH, axis=-1)
    merged = gate_a * (branch_a @ w_branch_a) + gate_b * (branch_b @ w_branch_b)
    return merged @ w_out


def _routed_experts(xf, e_ids, wts, w_gate, w_up, w_down):
    n_tok, d = xf.shape
    n_assign = n_tok * TOPK_IN_GROUP
    e = e_ids.reshape(-1)
    tok = jnp.repeat(jnp.arange(n_tok, dtype=jnp.int32), TOPK_IN_GROUP)
    w = wts.reshape(-1)
    order = jnp.argsort(e)
    e_s, tok_s, w_s = e[order], tok[order], w[order]
    counts = jnp.zeros((N_EXPERTS,), jnp.int32).at[e].add(1)
    padded = ((counts + MOE_BLOCK - 1) // MOE_BLOCK) * MOE_BLOCK
    start = jnp.cumsum(counts) - counts
    pend = jnp.cumsum(padded)
    pstart = pend - padded
    dest = pstart[e_s] + (jnp.arange(n_assign, dtype=jnp.int32) - start[e_s])
    n_blocks = -(-(n_assign + N_EXPERTS * (MOE_BLOCK - 1)) // MOE_BLOCK)
    n_rows = n_blocks * MOE_BLOCK
    row_tok = jnp.zeros((n_rows,), jnp.int32).at[dest].set(tok_s)
    row_w = jnp.zeros((n_rows,), xf.dtype).at[dest].set(w_s)
    block_pos = jnp.arange(n_blocks, dtype=jnp.int32) * MOE_BLOCK
    block_e = jnp.minimum(jnp.searchsorted(pend, block_pos, side='right'), N_EXPERTS - 1)

    def one_block(args):
        rows, ex = args
        xb = xf[rows]
        hb = jax.nn.silu(xb @ w_gate[ex]) * (xb @ w_up[ex])
        return hb @ w_down[ex]

    ys = lax.map(one_block, (row_tok.reshape(n_blocks, MOE_BLOCK), block_e))
    ys = ys.reshape(n_rows, d) * row_w[:, None]
    return jnp.zeros_like(xf).at[row_tok].add(ys)


def _hier_moe(x, w_group, b_group, w_router, b_router, w_gate, w_up, w_down):
    bsz, seq, d = x.shape
    xf = x.reshape(-1, d)
    n_tok = xf.shape[0]
    g_prob = jax.nn.softmax((xf @ w_group + b_group).astype(jnp.float32), axis=-1)
    g_top_p, g_top = lax.top_k(g_prob, 1)
    e_logits = (xf @ w_router + b_router).astype(jnp.float32).reshape(n_tok, N_GROUPS, EXPERTS_PER_GROUP)
    e_in_group = jnp.take_along_axis(e_logits, g_top[:, :, None], axis=1)[:, 0]
    e_val, e_idx = lax.top_k(e_in_group, TOPK_IN_GROUP)
    wts = jax.nn.softmax(e_val, axis=-1) * g_top_p
    e_ids = g_top * EXPERTS_PER_GROUP + e_idx
    y = _routed_experts(xf, e_ids, wts.astype(x.dtype), w_gate, w_up, w_down)
    return y.reshape(bsz, seq, d)


def setup_inputs(seed: int = 0) -> dict:
    key = jax.random.key(seed)
    ks = jax.random.split(key, 20)
    f32 = jnp.float32
    d = D_MODEL
    nrm = lambda k, shp, s: jax.random.normal(k, shp, f32) * s
    x = nrm(ks[0], (BATCH, SEQ, d), 1.0)
    w_in = nrm(ks[1], (DEPTH, d, PROJ_TOTAL), d ** -0.5)
    v_lo = 4 * HG_WIDTH + 2 * SA_WIDTH
    w_in = w_in.at[:, :, v_lo:v_lo + SA_WIDTH].multiply(DN_BETA)
    b_in = nrm(ks[2], (DEPTH, PROJ_TOTAL), 0.02)
    hg_lb_logits = nrm(ks[3], (DEPTH + 1, HG_WIDTH), 0.1)
    hg_norm_g = 1.0 + nrm(ks[4], (DEPTH, HG_WIDTH), 0.02)
    w_branch_a = nrm(ks[5], (DEPTH, HG_WIDTH, d), HG_WIDTH ** -0.5)
    w_branch_b = nrm(ks[6], (DEPTH, SA_WIDTH, d), SA_WIDTH ** -0.5)
    w_out = nrm(ks[7], (DEPTH, d, d), d ** -0.5 * DN_BETA)
    ln1_g = 1.0 + nrm(ks[8], (DEPTH, d), 0.02)
    ln1_b = nrm(ks[9], (DEPTH, d), 0.02)
    w_group = nrm(ks[10], (DEPTH, d, N_GROUPS), d ** -0.5)
    b_group = nrm(ks[11], (DEPTH, N_GROUPS), 0.01)
    w_router = nrm(ks[12], (DEPTH, d, N_EXPERTS), d ** -0.5)
    b_router = nrm(ks[13], (DEPTH, N_EXPERTS), 0.01)
    w_gate = nrm(ks[14], (DEPTH, N_EXPERTS, d, D_FF_EXPERT), d ** -0.5)
    w_up = nrm(ks[15], (DEPTH, N_EXPERTS, d, D_FF_EXPERT), d ** -0.5)
    w_down = nrm(ks[16], (DEPTH, N_EXPERTS, D_FF_EXPERT, d), D_FF_EXPERT ** -0.5 * DN_BETA)
    ln2_g = 1.0 + nrm(ks[17], (DEPTH, d), 0.02)
    ln2_b = nrm(ks[18], (DEPTH, d), 0.02)
    return {'x': x, 'w_in': w_in, 'b_in': b_in, 'hg_lb_logits': hg_lb_logits, 'hg_norm_g': hg_norm_g,
            'w_branch_a': w_branch_a, 'w_branch_b': w_branch_b, 'w_out': w_out,
            'ln1_g': ln1_g, 'ln1_b': ln1_b, 'w_group': w_group, 'b_group': b_group,
            'w_router': w_router, 'b_router': b_router, 'w_gate': w_gate, 'w_up': w_up,
            'w_down': w_down, 'ln2_g': ln2_g, 'ln2_b': ln2_b}


def reference(x, w_in, b_in, hg_lb_logits, hg_norm_g, w_branch_a, w_branch_b, w_out,
              ln1_g, ln1_b, w_group, b_group, w_router, b_router, w_gate, w_up,
              w_down, ln2_g, ln2_b):
    lower_bounds = jnp.cumsum(jax.nn.softmax(hg_lb_logits.astype(jnp.float32), axis=0), axis=0)
    h = x
    for l in range(DEPTH):
        mix = _token_mixer(h, w_in[l], b_in[l], lower_bounds[l], hg_norm_g[l],
                           w_branch_a[l], w_branch_b[l], w_out[l])
        h = _layer_norm(DN_ALPHA * h + mix, ln1_g[l], ln1_b[l])
        ffn = _hier_moe(h, w_group[l], b_group[l], w_router[l], b_router[l],
                        w_gate[l], w_up[l], w_down[l])
        h = _layer_norm(DN_ALPHA * h + ffn, ln2_g[l], ln2_b[l])
    return h
```

```python
import numpy as np
import concourse.bass as bass
import concourse.mybir as mybir

F32 = mybir.dt.float32
BF16 = mybir.dt.bfloat16
U32 = mybir.dt.uint32
I32 = mybir.dt.int32
U8 = mybir.dt.uint8
AF = mybir.ActivationFunctionType
ALU = mybir.AluOpType
AX = mybir.AxisListType


class Buf:
    __slots__ = ("name", "lastw", "readers", "dsem", "dcount", "glob", "dkey")

    def __init__(self, name, glob=False):
        self.name = name
        self.glob = glob
        self.dkey = None
        self.lastw = None
        self.readers = {}
        self.dsem = None
        self.dcount = 0


class Sched:
    ENGS = ("pe", "act", "dve", "pool", "sp")
    SEM_LIMIT = 30000

    def __init__(self, nc, es):
        self.nc = nc
        self.es = es
        self.es_sem = es
        self.dbufs = []
        self.dstate = {}
        self.free_dsems = []
        self.local_dbufs = []
        self.sem = {}
        self.count = {}
        self.known = {}
        self.ops = {}
        self.epoch = {}
        for n in self.ENGS:
            self.sem[n] = es.enter_context(nc.semaphore("se_" + n))
            self.count[n] = 0
            self.epoch[n] = 0
            self.known[n] = {}
            self.ops[n] = []
        self.nbuf = 0
        self.ninst = 0

    def sbuf(self, name, shape, dtype):
        self.nbuf += 1
        name = "%s_u%d" % (name, self.nbuf)
        return self.es.enter_context(self.nc.sbuf_tensor(name, list(shape), dtype))

    def psum(self, name, shape, dtype):
        return self.es.enter_context(self.nc.psum_tensor(name, list(shape), dtype))

    def buf(self, name=None, glob=False):
        self.nbuf += 1
        return Buf("%s_b%d" % (name or "b", self.nbuf), glob)

    def bufs(self, n, name="b"):
        return [self.buf("%s%d" % (name, i)) for i in range(n)]

    def _waits(self, eng, reads, writes):
        need = {}

        def add(ev, skip_same):
            if ev is None:
                return
            key, sem, val, prod = ev
            if skip_same and prod == eng:
                return
            if self.known[eng].get(key, 0) >= val:
                return
            if key not in need or need[key][1] < val:
                need[key] = (sem, val)

        for b in reads:
            add(b.lastw, False)
        for b in writes:
            add(b.lastw, True)
            for ev in b.readers.values():
                add(ev, True)
        for key, (sem, val) in need.items():
            self.known[eng][key] = val
        return list(need.values())

    def op(self, eng, fn, reads=(), writes=()):
        waits = self._waits(eng, reads, writes)
        if self.count[eng] >= self.SEM_LIMIT:
            self.epoch[eng] += 1
            self.count[eng] = 0
            self.sem[eng] = self.es_sem.enter_context(self.nc.semaphore("se_%s_%d" % (eng, self.epoch[eng])))
        self.count[eng] += 1
        seq = self.count[eng]
        sem = self.sem[eng]
        key = "e_%s_%d" % (eng, self.epoch[eng])
        ev = (key, sem, seq, eng)
        for b in writes:
            b.lastw = ev
            b.readers = {}
        for b in reads:
            b.readers[key] = ev
        self.ninst += 1 + len(waits)

        def emit(e, fn=fn, waits=waits, sem=sem):
            for (s, v) in waits:
                e.wait_ge(s, v)
            fn(e).then_inc(sem, 1)

        self.ops[eng].append(emit)
        return ev

    def dma(self, q, out_ap, in_ap, reads=(), writes=(), nowaw=False, builder=None, **kw):
        waits = self._waits(q, reads, [] if nowaw else writes)
        tb = writes[0]
        if tb.dsem is None:
            if (not tb.glob) and self.free_dsems:
                tb.dsem, tb.dcount, tb.dkey = self.free_dsems.pop()
            else:
                tb.dsem = self.es_sem.enter_context(self.nc.semaphore("sd_" + tb.name))
                tb.dkey = "d_" + tb.name
            if not tb.glob:
                self.local_dbufs.append(tb)
        tb.dcount += 16
        self.dstate[tb.dkey] = (tb.dsem, tb.dcount)
        ev = (tb.dkey, tb.dsem, tb.dcount, None)
        for b in writes:
            b.lastw = ev
            if not nowaw:
                b.readers = {}
        for b in reads:
            b.readers[ev[0]] = ev
        self.ninst += 1 + len(waits)

        def emit(e, waits=waits, sem=tb.dsem, out_ap=out_ap, in_ap=in_ap, kw=kw, builder=builder):
            for (s, v) in waits:
                e.wait_ge(s, v)
            if builder is not None:
                builder(e).then_inc(sem, 16)
            else:
                e.dma_start(out=out_ap, in_=in_ap, **kw).then_inc(sem, 16)

        self.ops[q].append(emit)
        return ev

    def phase_end(self):
        for b in self.local_dbufs:
            self.free_dsems.append((b.dsem, b.dcount, b.dkey))
            b.dsem = None
        self.local_dbufs = []

    def raw(self, eng, fn):
        self.ops[eng].append(lambda e, fn=fn: fn(e))

    def wait_all(self, eng, bufs):
        waits = self._waits(eng, list(bufs), [])

        def emit(e, waits=waits):
            for (s, v) in waits:
                e.wait_ge(s, v)

        self.ops[eng].append(emit)

    def barrier(self):
        evs = []
        for n in self.ENGS:
            if self.count[n] > 0:
                evs.append(("e_%s_%d" % (n, self.epoch[n]), self.sem[n], self.count[n], n))
        for key, (sem, cnt) in self.dstate.items():
            evs.append((key, sem, cnt, None))
        for eng in self.ENGS:
            waits = []
            for (key, sem, val, prod) in evs:
                if prod == eng:
                    continue
                if self.known[eng].get(key, 0) >= val:
                    continue
                self.known[eng][key] = val
                waits.append((sem, val))

            def emit(e, waits=waits):
                for (s, v) in waits:
                    e.wait_ge(s, v)

            self.ops[eng].append(emit)

    def run(self):
        nc = self.nc
        ops = self.ops
        with nc.Block() as block:
            @block.tensor
            def _(e):
                for f in ops["pe"]:
                    f(e)

            @block.scalar
            def _(e):
                for f in ops["act"]:
                    f(e)

            @block.vector
            def _(e):
                for f in ops["dve"]:
                    f(e)

            @block.gpsimd
            def _(e):
                for f in ops["pool"]:
                    f(e)

            @block.sync
            def _(e):
                for f in ops["sp"]:
                    f(e)

NSLOT = 8192
NOWN = 2048
OWN0 = NSLOT - NOWN
D = 2048
KC = 16
C_AQ, C_AF, C_AI, C_AG, C_BQ, C_BK, C_BV, C_IQ, C_IK, C_IW, C_G = 0, 1024, 2048, 3072, 4096, 5120, 6144, 7168, 7680, 7744, 7752
W_SCALE = (8 ** -0.5) * (64 ** -0.5)


class Rot:
    def __init__(self, S, name, shape, dtype, n):
        self.t = [S.sbuf("%s%d" % (name, i), shape, dtype) for i in range(n)]
        self.b = [S.buf("%s%d" % (name, i)) for i in range(n)]
        self.i = 0
        self.n = n

    def next(self):
        k = self.i % self.n
        self.i += 1
        return self.t[k], self.b[k]


class PBanks:
    def __init__(self, S):
        self.t = [S.psum("pb%d" % i, [128, 512], F32) for i in range(8)]
        self.b = [S.buf("pb%d" % i) for i in range(8)]
        self.i = 0

    def next(self, lo=0, hi=8):
        n = hi - lo
        k = lo + (self.i % n)
        self.i += 1
        return self.t[k], self.b[k]


def phase1a(S, G, blocks=(0, 1, 2, 3)):
    A = G["ap"]
    PB = G["pb"]
    ident = G["ident_bf"]
    Bident = G["Bident"]
    w_in = A["w_in"]
    xT = S.sbuf("xT", [128, KC, 2048], BF16)
    BxT = S.bufs(16, "xT")
    xin = Rot(S, "xin", [128, 2048], BF16, 2)
    wt = Rot(S, "wt", [128, KC, 512], BF16, 2)
    f32t = Rot(S, "hgf", [128, 512], F32, 12)
    st16 = Rot(S, "st16", [128, 512], BF16, 6)
    stkT = Rot(S, "stkT", [128, 4, 128], BF16, 2)
    decst = S.sbuf("decst", [128, 8, 32], F32)
    Bdec = S.buf("decst")
    wist = Rot(S, "wist", [128, 8], F32, 2)
    cst = G["cst"]
    Bc = G["Bcst"]
    evq = [0]

    def evac_engine():
        evq[0] += 1
        return "act" if evq[0] % 2 else "dve"

    def copy_op(eng, out_ap, in_ap, reads, writes):
        if eng == "act":
            S.op("act", lambda e, out_ap=out_ap, in_ap=in_ap: e.activation(out_ap, in_ap, AF.Copy), reads=reads, writes=writes)
        else:
            S.op("dve", lambda e, out_ap=out_ap, in_ap=in_ap: e.tensor_copy(out_ap, in_ap), reads=reads, writes=writes)

    for blk in blocks:
        own = (blk == 3)
        s0 = blk * 2048
        for t in range(16):
            xi, Bxi = xin.next()
            S.dma("pool", xi[:], A["xs"][s0 + t * 128: s0 + (t + 1) * 128, :], writes=[Bxi])
            for q4 in range(4):
                pb, Bpb = PB.next()
                for j in range(4):
                    kc = q4 * 4 + j
                    S.op("pe", lambda e, pb=pb, xi=xi, kc=kc, j=j: e.matmul(
                        pb[:, j * 128:(j + 1) * 128], xi[:, kc * 128:(kc + 1) * 128], ident[:], start=True, stop=True),
                        reads=[Bxi, Bident], writes=[Bpb])
                copy_op(evac_engine(), xT[:, q4 * 4:(q4 + 1) * 4, t * 128:(t + 1) * 128],
                        pb[:].rearrange("p (a b) -> p a b", a=4), [Bpb], [BxT[t]])

        def load_w(cols):
            w, Bw = wt.next()
            off = 0
            for (c0, n) in cols:
                S.dma("pool", w[:, :, off:off + n], w_in[:, c0:c0 + n].rearrange("(kc p) n -> p kc n", p=128), writes=[Bw])
                off += n
            return w, Bw

        def fm_mm(w, Bw, m, g):
            pb, Bpb = PB.next()
            for kc in range(KC):
                S.op("pe", lambda e, pb=pb, w=w, kc=kc, m=m, g=g: e.matmul(
                    pb[:], w[:, kc, m * 128:(m + 1) * 128], xT[:, kc, g * 512:(g + 1) * 512], start=(kc == 0), stop=(kc == KC - 1)),
                    reads=[Bw] + BxT[g * 4:(g + 1) * 4], writes=[Bpb])
            return pb, Bpb

        def simple_fm(w, Bw, m, g, bias_ap, func, dst_ap, Bd, rows=128):
            pb, Bpb = fm_mm(w, Bw, m, g)
            st, Bst = st16.next()
            S.op("act", lambda e, st=st, pb=pb, func=func, bias_ap=bias_ap: e.activation(st[:], pb[:], func, bias=bias_ap), reads=[Bpb, Bc], writes=[Bst])
            S.dma("sp", dst_ap, st[0:rows, :], reads=[Bst], writes=[Bd], nowaw=True)

        tm_units = [("ai", C_AI), ("ai", C_AI + 512), ("bv", C_BV), ("bv", C_BV + 512)]
        for (kind, c0) in tm_units:
            w, Bw = load_w([(c0, 512)])
            for t in range(16):
                pb, Bpb = PB.next()
                for kc in range(KC):
                    S.op("pe", lambda e, pb=pb, w=w, kc=kc, t=t: e.matmul(
                        pb[:], xT[:, kc, t * 128:(t + 1) * 128], w[:, kc, :], start=(kc == 0), stop=False),
                        reads=[Bw, BxT[t]], writes=[Bpb])
                boff = (c0 - C_AI) if kind == "ai" else (1024 + c0 - C_BV)
                S.op("pe", lambda e, pb=pb, boff=boff: e.matmul(pb[:], cst["ones_row"][0:1, :], cst["brow"][0:1, boff:boff + 512],
                                                            start=False, stop=True), reads=[Bc], writes=[Bpb])
                st, Bst = st16.next()
                tt = blk * 16 + t
                if kind == "ai":
                    S.op("act", lambda e, st=st, pb=pb, tt=tt: e.activation(st[:], pb[:], AF.Identity, scale=cst["valid_tm"][:, tt:tt + 1]),
                         reads=[Bpb, Bc], writes=[Bst])
                    dst = A["HG_v"][s0 + t * 128:s0 + (t + 1) * 128, c0 - C_AI:c0 - C_AI + 512]
                else:
                    S.op("dve", lambda e, st=st, pb=pb: e.tensor_copy(st[:], pb[:]), reads=[Bpb], writes=[Bst])
                    h0 = (c0 - C_BV) // 128
                    dst = A["VH"][h0:h0 + 4, :, tt, :].rearrange("h p d -> p h d")
                if kind == "ai":
                    S.dma("sp", dst, st[:], reads=[Bst], writes=[G["B"]["HG_v"]], nowaw=True)
                else:
                    S.dma("sp", dst, st[:].rearrange("p (h d) -> p h d", h=4), reads=[Bst], writes=[G["B"]["VH"]], nowaw=True)
        if own:
            w, Bw = load_w([(C_IW, 8)])
            for t in range(16):
                pb, Bpb = PB.next()
                for kc in range(KC):
                    S.op("pe", lambda e, pb=pb, w=w, kc=kc, t=t: e.matmul(
                        pb[:, 0:8], xT[:, kc, t * 128:(t + 1) * 128], w[:, kc, 0:8], start=(kc == 0), stop=False),
                        reads=[Bw, BxT[t]], writes=[Bpb])
                S.op("pe", lambda e, pb=pb: e.matmul(pb[:, 0:8], cst["ones_row"][0:1, :], cst["brow"][0:1, 2048:2056],
                                                     start=False, stop=True), reads=[Bc], writes=[Bpb])
                st, Bst = wist.next()
                S.op("dve", lambda e, st=st, pb=pb: e.tensor_scalar(st[:], pb[:, 0:8], W_SCALE, None, op0=ALU.mult), reads=[Bpb], writes=[Bst])
                S.dma("sp", A["WI"][t * 128:(t + 1) * 128, :], st[:], reads=[Bst], writes=[G["B"]["WI"]], nowaw=True)

        for u in range(2):
            w, Bw = load_w([(C_BK + u * 512, 512)])
            for m in range(4):
                h = u * 4 + m
                for g in range(4):
                    simple_fm(w, Bw, m, g, cst["bfm"][:, G["bfm_idx"]["bk"] + h: G["bfm_idx"]["bk"] + h + 1], AF.Identity,
                              A["KT"][h, :, s0 + g * 512:s0 + (g + 1) * 512], G["B"]["KT"])
        w, Bw = load_w([(C_IK, 64), (C_IK, 64)])
        for g in range(4):
            simple_fm(w, Bw, 0, g, cst["bfm"][:, G["bfm_idx"]["ik"]: G["bfm_idx"]["ik"] + 1], AF.Identity,
                      A["KIT"][:, s0 + g * 512:s0 + (g + 1) * 512], G["B"]["KIT"])
        if own:
            for u in range(2):
                w, Bw = load_w([(C_BQ + u * 512, 512)])
                for m in range(4):
                    h = u * 4 + m
                    for g in range(4):
                        simple_fm(w, Bw, m, g, cst["bfm"][:, G["bfm_idx"]["bq"] + h: G["bfm_idx"]["bq"] + h + 1], AF.Identity,
                                  A["QT"][h, :, g * 512:(g + 1) * 512], G["B"]["QT"])
            w, Bw = load_w([(C_IQ, 512)])
            for m in range(4):
                for g in range(4):
                    simple_fm(w, Bw, m, g, cst["bfm"][:, G["bfm_idx"]["iq"] + m: G["bfm_idx"]["iq"] + m + 1], AF.Identity,
                              A["QIT"][m, :, g * 512:(g + 1) * 512], G["B"]["QIT"])
            for u in range(8):
                w, Bw = load_w([(C_G + u * 512, 512)])
                for m in range(4):
                    c = u * 4 + m
                    for g in range(4):
                        simple_fm(w, Bw, m, g, cst["bfm"][:, G["bfm_idx"]["g"] + c: G["bfm_idx"]["g"] + c + 1], AF.Sigmoid,
                                  A["GT"][c, :, g * 512:(g + 1) * 512], G["B"]["GT"])

        for h in range(8):
            if own:
                w, Bw = load_w([(C_AF + h * 128, 128), (C_AQ + h * 128, 128), (C_AG + h * 128, 128)])
            else:
                if h % 4 == 0:
                    w4, Bw4 = load_w([(C_AF + h * 128, 512)])
                w, Bw = w4, Bw4
            mf = 0 if own else (h % 4)
            bi = G["bfm_idx"]
            oml_h = cst["oml"][:, h:h + 1]
            noml_h = cst["noml"][:, h:h + 1]
            lb_h = cst["lb"][:, h:h + 1]
            b_af = cst["bfm"][:, bi["af"] + h: bi["af"] + h + 1]
            b_aq = cst["bfm"][:, bi["aq"] + h: bi["aq"] + h + 1]
            b_ag = cst["bfm"][:, bi["ag"] + h: bi["ag"] + h + 1]
            for g in range(4):
                pb, Bpb = fm_mm(w, Bw, mf, g)
                sg, Bsg = f32t.next()
                S.op("act", lambda e, sg=sg, pb=pb, b_af=b_af: e.activation(sg[:], pb[:], AF.Sigmoid, bias=b_af),
                     reads=[Bpb, Bc], writes=[Bsg])
                lf, Blf = f32t.next()
                S.op("act", lambda e, lf=lf, sg=sg, oml_h=oml_h, lb_h=lb_h: e.activation(lf[:], sg[:], AF.Ln, scale=oml_h, bias=lb_h),
                     reads=[Bsg, Bc], writes=[Blf])
                cum, Bcum = f32t.next()
                S.op("dve", lambda e, cum=cum, lf=lf: e.tensor_tensor_scan(cum[:], cst["rmask"][:], lf[:], 0.0, ALU.mult, ALU.add),
                     reads=[Blf, Bc], writes=[Bcum])
                en, Ben = f32t.next()
                S.op("act", lambda e, en=en, cum=cum: e.activation(en[:], cum[:], AF.Exp, scale=-1.0), reads=[Bcum], writes=[Ben])
                kk, Bkk = f32t.next()
                S.op("dve", lambda e, kk=kk, sg=sg, noml_h=noml_h, oml_h=oml_h: e.tensor_scalar(kk[:], sg[:], noml_h, oml_h, op0=ALU.mult, op1=ALU.add),
                     reads=[Bsg, Bc], writes=[Bkk])
                kd, Bkd = st16.next()
                S.op("dve", lambda e, kd=kd, kk=kk, en=en: e.tensor_tensor(kd[:], kk[:], en[:], ALU.mult), reads=[Bkk, Ben], writes=[Bkd])
                S.dma("sp", A["HG_kdec"][h, :, s0 + g * 512:s0 + (g + 1) * 512], kd[:], reads=[Bkd], writes=[G["B"]["HG_kdec"]], nowaw=True)
                ex2, Bex2 = f32t.next()
                for c in range(8):
                    S.op("act", lambda e, ex2=ex2, cum=cum, c=c: e.activation(ex2[:, c * 64:(c + 1) * 64], cum[:, c * 64:(c + 1) * 64], AF.Exp,
                                                                           scale=-1.0, bias=cum[:, c * 64 + 63:c * 64 + 64]),
                         reads=[Bcum], writes=[Bex2])
                ke, Bke = st16.next()
                S.op("dve", lambda e, ke=ke, kk=kk, ex2=ex2: e.tensor_tensor(ke[:], kk[:], ex2[:], ALU.mult), reads=[Bkk, Bex2], writes=[Bke])
                dec_ap = decst[:, h, g * 8:(g + 1) * 8]
                S.op("act", lambda e, cum=cum, dec_ap=dec_ap: e.activation(dec_ap, cum[:].rearrange("p (c s) -> p c s", s=64)[:, :, 63], AF.Exp),
                     reads=[Bcum], writes=[Bdec])
                pbt, Bpbt = PB.next()
                for j in range(4):
                    S.op("pe", lambda e, pbt=pbt, ke=ke, j=j: e.matmul(pbt[:, j * 128:(j + 1) * 128], ke[:, j * 128:(j + 1) * 128], ident[:],
                                                                       start=True, stop=True), reads=[Bke, Bident], writes=[Bpbt])
                kT, BkT = stkT.next()
                S.op("dve", lambda e, kT=kT, pbt=pbt: e.tensor_copy(kT[:], pbt[:].rearrange("p (a b) -> p a b", a=4)), reads=[Bpbt], writes=[BkT])
                S.dma("sp", A["HG_kend"][s0 + g * 512:s0 + (g + 1) * 512, h * 128:(h + 1) * 128].rearrange("(t p) d -> p t d", p=128),
                      kT[:], reads=[BkT], writes=[G["B"]["HG_kend"]], nowaw=True)
                if own:
                    ec, Bec = f32t.next()
                    S.op("act", lambda e, ec=ec, cum=cum: e.activation(ec[:], cum[:], AF.Exp), reads=[Bcum], writes=[Bec])
                    pbq, Bpbq = fm_mm(w, Bw, 1, g)
                    qs, Bqs = f32t.next()
                    S.op("act", lambda e, qs=qs, pbq=pbq, b_aq=b_aq: e.activation(qs[:], pbq[:], AF.Silu, bias=b_aq),
                         reads=[Bpbq, Bc], writes=[Bqs])
                    qd, Bqd = st16.next()
                    S.op("dve", lambda e, qd=qd, qs=qs, ec=ec: e.tensor_tensor(qd[:], qs[:], ec[:], ALU.mult), reads=[Bqs, Bec], writes=[Bqd])
                    S.dma("sp", A["HG_qdec"][h, :, g * 512:(g + 1) * 512], qd[:], reads=[Bqd], writes=[G["B"]["HG_qdec"]], nowaw=True)
                    simple_fm(w, Bw, 2, g, b_ag, AF.Silu, A["HG_gs"][h, :, g * 512:(g + 1) * 512], G["B"]["HG_gs"])
        S.dma("sp", A["HG_dec"][:, :, blk * 32:(blk + 1) * 32], decst[:], reads=[Bdec], writes=[G["B"]["HG_dec"]], nowaw=True)

RMS_EPS = 1e-6


def phase1b(S, G):
    A = G["ap"]
    PB = G["pb"]
    cst = G["cst"]
    Bc = G["Bcst"]
    B = G["B"]
    kend = S.sbuf("hs_kend", [128, 16, 1024], BF16)
    Bkend = S.buf("hs_kend")
    vv = S.sbuf("hs_v", [128, 16, 1024], BF16)
    Bvv = S.buf("hs_v")
    dec = S.sbuf("hs_dec", [128, 8, 128], F32)
    Bdec = S.buf("hs_dec")
    Sf = S.sbuf("hs_S", [128, 8, 128], F32)
    Sb = S.sbuf("hs_Sb", [128, 8, 128], BF16)
    BS = S.bufs(8, "hs_S")
    BSb = S.bufs(8, "hs_Sb")
    S.dma("sp", dec[:], A["HG_dec"], reads=[B["HG_dec"]], writes=[Bdec])
    S.op("dve", lambda e: e.memset(Sf[:], 0.0), writes=BS)
    S.op("dve", lambda e: e.memset(Sb[:], 0.0), writes=BSb)
    onesb = S.sbuf("hs_ones", [128, 128], BF16)
    Bones = S.buf("hs_ones")
    S.op("dve", lambda e: e.memset(onesb[:], 1.0 / 128.0), writes=[Bones])
    kdec = Rot(S, "hs_kdec", [128, 2048], BF16, 2)
    qdec = Rot(S, "hs_qdec", [128, 2048], BF16, 2)
    gs = Rot(S, "hs_gs", [128, 2048], BF16, 2)
    attm = Rot(S, "hs_attm", [128, 128], BF16, 3)
    sq = Rot(S, "hs_sq", [128, 128], BF16, 3)
    rs = Rot(S, "hs_rs", [128, 128], F32, 3)
    yy = Rot(S, "hs_y", [128, 128], F32, 3)
    bast = S.sbuf("hs_bast", [128, 2048], BF16)
    Bbast = S.buf("hs_bast")

    def state_update(t, h, half, ):
        p0 = half * 64
        chunk = None
        pk, Bpk = PB.next()
        S.op("pe", lambda e, pk=pk, t=t, h=h, p0=p0: e.matmul(pk[:, 0:128], kend[p0:p0 + 64, t, h * 128:(h + 1) * 128],
                                                            vv[p0:p0 + 64, t, h * 128:(h + 1) * 128], start=True, stop=True),
             reads=[Bkend, Bvv], writes=[Bpk])
        return pk, Bpk

    for blk in range(4):
        own = (blk == 3)
        s0 = blk * 2048
        S.dma("sp", kend[:], A["HG_kend"][s0:s0 + 2048, :].rearrange("(t p) c -> p t c", p=128), reads=[B["HG_kend"]], writes=[Bkend])
        S.dma("sp", vv[:], A["HG_v"][s0:s0 + 2048, :].rearrange("(t p) c -> p t c", p=128), reads=[B["HG_v"]], writes=[Bvv])
        if not own:
            for t in range(16):
                for half in range(2):
                    ch = blk * 32 + t * 2 + half
                    for h in range(8):
                        pk, Bpk = state_update(t, h, half)
                        S.op("dve", lambda e, pk=pk, h=h, ch=ch: e.scalar_tensor_tensor(Sf[:, h, :], Sf[:, h, :], dec[:, h, ch:ch + 1], pk[:, 0:128],
                                                                                      op0=ALU.mult, op1=ALU.add),
                             reads=[Bpk, Bdec, BS[h]], writes=[BS[h]])
            if blk == 2:
                for h in range(8):
                    S.op("act", lambda e, h=h: e.activation(Sb[:, h, :], Sf[:, h, :], AF.Copy), reads=[BS[h]], writes=[BSb[h]])
            continue
        for h in range(8):
            kd, Bkd = kdec.next()
            qd, Bqd = qdec.next()
            gg, Bgg = gs.next()
            S.dma("sp", kd[:], A["HG_kdec"][h, :, s0:s0 + 2048], reads=[B["HG_kdec"]], writes=[Bkd])
            S.dma("sp", qd[:], A["HG_qdec"][h], reads=[B["HG_qdec"]], writes=[Bqd])
            S.dma("sp", gg[:], A["HG_gs"][h], reads=[B["HG_gs"]], writes=[Bgg])
            for t in range(16):
                tc = slice(t * 128, (t + 1) * 128)
                pa, Bpa = PB.next()
                S.op("pe", lambda e, pa=pa, kd=kd, qd=qd, tc=tc: e.matmul(pa[:, 0:128], kd[:, tc], qd[:, tc], start=True, stop=True),
                     reads=[Bkd, Bqd], writes=[Bpa])
                am, Bam = attm.next()
                S.op("dve", lambda e, am=am, pa=pa: e.tensor_tensor(am[:], pa[:, 0:128], cst["bdmask"][:], ALU.mult), reads=[Bpa, Bc], writes=[Bam])
                po, Bpo = PB.next()
                S.op("pe", lambda e, po=po, am=am, t=t, h=h: e.matmul(po[:, 0:128], vv[:, t, h * 128:(h + 1) * 128], am[:], start=True, stop=False),
                     reads=[Bvv, Bam], writes=[Bpo])
                for half in range(2):
                    ch = blk * 32 + t * 2 + half
                    c0 = t * 128 + half * 64
                    S.op("pe", lambda e, po=po, qd=qd, h=h, c0=c0, half=half: e.matmul(po[:, half * 64:(half + 1) * 64], Sb[:, h, :], qd[:, c0:c0 + 64],
                                                                                  start=False, stop=(half == 1)),
                         reads=[BSb[h], Bqd], writes=[Bpo])
                    pk, Bpk = state_update(t, h, half)
                    S.op("dve", lambda e, pk=pk, h=h, ch=ch: e.scalar_tensor_tensor(Sf[:, h, :], Sf[:, h, :], dec[:, h, ch:ch + 1], pk[:, 0:128],
                                                                                  op0=ALU.mult, op1=ALU.add),
                         reads=[Bpk, Bdec, BS[h]], writes=[BS[h]])
                    S.op("act", lambda e, h=h: e.activation(Sb[:, h, :], Sf[:, h, :], AF.Copy), reads=[BS[h]], writes=[BSb[h]])
                q2, Bq2 = sq.next()
                S.op("act", lambda e, q2=q2, po=po: e.activation(q2[:], po[:, 0:128], AF.Square), reads=[Bpo], writes=[Bq2])
                pm, Bpm = PB.next()
                S.op("pe", lambda e, pm=pm, q2=q2: e.matmul(pm[:, 0:128], onesb[:], q2[:], start=True, stop=True), reads=[Bones, Bq2], writes=[Bpm])
                r1, Br1 = rs.next()
                S.op("act", lambda e, r1=r1, pm=pm: e.activation(r1[:], pm[:, 0:128], AF.Sqrt, bias=cst["eps_rms"][:, 0:1]), reads=[Bpm, Bc], writes=[Br1])
                S.op("dve", lambda e, r1=r1: e.reciprocal(r1[:], r1[:]), reads=[Br1], writes=[Br1])
                y1, By1 = yy.next()
                S.op("dve", lambda e, y1=y1, po=po, r1=r1: e.tensor_tensor(y1[:], po[:, 0:128], r1[:], ALU.mult), reads=[Bpo, Br1], writes=[By1])
                S.op("dve", lambda e, y1=y1, gg=gg, tc=tc, h=h: e.scalar_tensor_tensor(bast[:, tc], y1[:], cst["normg"][:, h:h + 1], gg[:, tc],
                                                                                    op0=ALU.mult, op1=ALU.mult),
                     reads=[By1, Bgg, Bc], writes=[Bbast])
            S.dma("sp", A["BA"][h], bast[:], reads=[Bbast], writes=[B["BA"]], nowaw=True)

SM_SCALE = 128 ** -0.5
TOPK = 256
NBIS = 18
NTER = 12
BIS_WIN = 32.0
NEG_ADM = -30000.0


def phase2_dsa(S, G, groups=(0, 1, 2, 3)):
    A = G["ap"]
    PB = G["pb"]
    cst = G["cst"]
    Bc = G["Bcst"]
    B = G["B"]
    ident = G["ident_bf"]
    Bident = G["Bident"]
    es_outer = S.es
    with ExitStack() as pes:
        S.es = pes
        alb = S.sbuf("ds_alb", [128, 8, 64], F32)
        dtab = S.sbuf("ds_dtab", [128, 8192], BF16)
        qrel = S.sbuf("ds_qrel", [1, 512], F32)
        onesr = S.sbuf("ds_onesr", [1, 128], BF16)
        drow = S.sbuf("ds_drow", [1, 512], F32)
        Bdrow = S.buf("ds_drow")
        shrow = S.sbuf("ds_shrow", [1, 8, 512], BF16)
        Bshrow = S.buf("ds_shrow")
        dmc = S.sbuf("ds_dmc", [128, 1], F32)
        Bdmc = S.buf("ds_dmc")
        corr = S.sbuf("ds_corr", [128, 8, 128], BF16)
        sel2 = S.sbuf("ds_sel2", [2, 128], BF16)
        onesb = S.sbuf("ds_ones", [128, 128], BF16)
        Bk = S.buf("ds_const")
        S.dma("sp", alb[:], A["alb"], writes=[Bk], nowaw=True)
        S.dma("sp", corr[:], A["corr"], writes=[Bk], nowaw=True)
        S.dma("sp", sel2[:], A["sel2"], writes=[Bk], nowaw=True)
        S.op("dve", lambda e: e.memset(onesb[:], 1.0), writes=[Bk])
        S.op("dve", lambda e: e.memset(onesr[:], 1.0), writes=[Bk])
        S.dma("sp", dtab[:], A["dtab"], writes=[Bk], nowaw=True)
        S.dma("sp", qrel[:], A["qrel"], writes=[Bk], nowaw=True)
        kit = S.sbuf("ds_kit", [128, 8192], BF16)
        S.dma("sp", kit[:], A["KIT"], reads=[B["KIT"]], writes=[Bk], nowaw=True)
        wi = S.sbuf("ds_wi", [128, 16, 8], F32)
        S.dma("sp", wi[:], A["WI"].rearrange("(t p) h -> p t h", p=128), reads=[B["WI"]], writes=[Bk], nowaw=True)
        maskT = S.sbuf("ds_maskT", [128, 64, 512], BF16)
        BmT = S.buf("ds_maskT")
        S.barrier()

        def do_group(g):
            Q0 = OWN0 + g * 512
            with ExitStack() as aes:
                S.es = aes
                qit = S.sbuf("ds_qit", [128, 4, 512], BF16)
                Bqit = S.buf("ds_qit")
                S.dma("sp", qit[:], A["QIT"][:, :, g * 512:(g + 1) * 512].rearrange("m p q -> p m q"), reads=[B["QIT"]], writes=[Bqit])
                sc = S.sbuf("ds_sc", [128, 8192], F32)
                Bsc = S.buf("ds_sc")
                mq = S.sbuf("ds_mq", [128, 8192], BF16)
                Bmq = S.buf("ds_mq")
                adm = Rot(S, "ds_adm", [2, 512], BF16, 3)
                junk2 = S.sbuf("ds_junk2", [128, 8192], U8)
                Bj2 = S.buf("ds_junk2")
                relu = Rot(S, "ds_relu", [128, 512], BF16, 9)
                dg = S.sbuf("ds_dg", [128, 8, 128], BF16)
                Bdg = S.buf("ds_dg")
                sm = {n: S.sbuf("ds_s_" + n, [128, 1], F32) for n in ("lo", "hi", "mid", "cnt", "ge", "d", "c0", "d3", "t1", "nt2", "s2", "g2")}
                Bsm = S.buf("ds_small")
                Bth = S.buf("ds_th")
                Bs2 = S.buf("ds_s2")
                for T in range(4):
                    tg = g * 4 + T
                    Qt = Q0 + T * 128
                    nk = Qt + 128
                    nb5 = (nk + 511) // 512
                    nkp = nb5 * 512
                    for h in range(8):
                        S.op("dve", lambda e, h=h, tg=tg: e.tensor_scalar(dg[:, h, :], ident[:], wi[:, tg, h:h + 1], None, op0=ALU.mult),
                             reads=[Bident, Bk], writes=[Bdg])
                    for kb5 in range(nb5):
                        ks = slice(kb5 * 512, (kb5 + 1) * 512)
                        ad, Bad = adm.next()
                        S.dma("sp", ad[:], A["adm"][tg, :, ks], writes=[Bad])
                        rl = []
                        for hp in range(4):
                            for par in range(2):
                                pb, Bpb = PB.next()
                                p0 = par * 64
                                S.op("pe", lambda e, pb=pb, hp=hp, p0=p0, T=T, ks=ks: e.matmul(
                                    pb[:], qit[p0:p0 + 64, hp, T * 128:(T + 1) * 128], kit[p0:p0 + 64, ks], start=True, stop=True),
                                    reads=[Bqit, Bk], writes=[Bpb])
                                r, Br = relu.next()
                                if par == 0:
                                    S.op("act", lambda e, r=r, pb=pb: e.activation(r[:], pb[:], AF.Relu), reads=[Bpb], writes=[Br])
                                else:
                                    S.op("dve", lambda e, r=r, pb=pb: e.tensor_scalar(r[:], pb[:], 0.0, None, op0=ALU.max), reads=[Bpb], writes=[Br])
                                rl.append((r, Br))
                        ps, Bps = PB.next()
                        for h in range(8):
                            r, Br = rl[h]
                            S.op("pe", lambda e, ps=ps, h=h, r=r: e.matmul(ps[:], dg[:, h, :], r[:], start=(h == 0), stop=False),
                                 reads=[Bdg, Br], writes=[Bps])
                        S.op("pe", lambda e, ps=ps, ad=ad, ks=ks: e.matmul(ps[:], sel2[0:2, :], ad[0:2, :], start=False, stop=True),
                             reads=[Bk, Bad], writes=[Bps])
                        S.op("act", lambda e, ps=ps, ks=ks: e.activation(sc[:, ks], ps[:], AF.Copy), reads=[Bps], writes=[Bsc])
                    scv = sc[:, 0:nkp]
                    S.op("dve", lambda e, scv=scv: e.reduce_max(sm["hi"][:], scv, AX.X), reads=[Bsc], writes=[Bsm])
                    S.op("dve", lambda e: e.tensor_scalar(sm["lo"][:], sm["hi"][:], -BIS_WIN, None, op0=ALU.add), reads=[Bsm], writes=[Bsm])
                    S.op("dve", lambda e: e.tensor_scalar(sm["hi"][:], sm["hi"][:], 1e-3, None, op0=ALU.add), reads=[Bsm], writes=[Bsm])
                    S.op("dve", lambda e, scv=scv, nkp=nkp: e.tensor_scalar(mq[:, 0:nkp], scv, sm["lo"][:, 0:1], None, op0=ALU.is_ge, op1=ALU.add,
                                                                        accum_out=sm["c0"][:]), reads=[Bsc, Bsm], writes=[Bmq, Bsm])
                    for it in range(NTER):
                        S.op("dve", lambda e: e.tensor_tensor(sm["d"][:], sm["hi"][:], sm["lo"][:], ALU.subtract), reads=[Bsm], writes=[Bsm])
                        S.op("dve", lambda e: e.tensor_scalar(sm["d3"][:], sm["d"][:], 1.0 / 3.0, None, op0=ALU.mult), reads=[Bsm], writes=[Bsm])
                        S.op("dve", lambda e: e.tensor_tensor(sm["t1"][:], sm["lo"][:], sm["d3"][:], ALU.add), reads=[Bsm], writes=[Bsm])
                        S.op("dve", lambda e: e.tensor_tensor(sm["nt2"][:], sm["d3"][:], sm["hi"][:], ALU.subtract), reads=[Bsm], writes=[Bsm, Bth])
                        S.op("act", lambda e, scv=scv, nkp=nkp: e.activation(junk2[:, 0:nkp], scv, AF.Sign, bias=sm["nt2"][:, 0:1], accum_out=sm["s2"][:]),
                             reads=[Bsc, Bth], writes=[Bj2, Bs2])
                        S.op("dve", lambda e, scv=scv, nkp=nkp: e.tensor_scalar(mq[:, 0:nkp], scv, sm["t1"][:, 0:1], None, op0=ALU.is_ge, op1=ALU.add,
                                                                            accum_out=sm["cnt"][:]), reads=[Bsc, Bsm], writes=[Bmq, Bsm])
                        S.op("dve", lambda e: e.tensor_scalar(sm["ge"][:], sm["cnt"][:], TOPK - 0.5, None, op0=ALU.is_ge), reads=[Bsm], writes=[Bsm])
                        S.op("dve", lambda e, nkp=nkp: e.tensor_scalar(sm["g2"][:], sm["s2"][:], 2.0 * (TOPK - 0.5) - nkp, None, op0=ALU.is_ge), reads=[Bs2], writes=[Bsm])
                        S.op("dve", lambda e: e.tensor_tensor(sm["ge"][:], sm["ge"][:], sm["g2"][:], ALU.add), reads=[Bsm], writes=[Bsm])
                        S.op("dve", lambda e: e.scalar_tensor_tensor(sm["lo"][:], sm["d3"][:], sm["ge"][:, 0:1], sm["lo"][:], op0=ALU.mult, op1=ALU.add),
                             reads=[Bsm], writes=[Bsm])
                        S.op("dve", lambda e: e.tensor_tensor(sm["hi"][:], sm["lo"][:], sm["d3"][:], ALU.add), reads=[Bsm], writes=[Bsm, Bth])
                    S.op("dve", lambda e: e.tensor_scalar(sm["ge"][:], sm["c0"][:], TOPK - 0.5, None, op0=ALU.is_ge), reads=[Bsm], writes=[Bsm])
                    S.op("dve", lambda e: e.tensor_scalar(sm["d"][:], sm["lo"][:], 1000.0, None, op0=ALU.add), reads=[Bsm], writes=[Bsm])
                    S.op("dve", lambda e: e.tensor_scalar(sm["lo"][:], sm["d"][:], sm["ge"][:, 0:1], -1000.0, op0=ALU.mult, op1=ALU.add),
                         reads=[Bsm], writes=[Bsm])
                    S.op("dve", lambda e, scv=scv, nkp=nkp: e.tensor_scalar(mq[:, 0:nkp], scv, sm["lo"][:, 0:1], None, op0=ALU.is_ge),
                         reads=[Bsc, Bsm], writes=[Bmq])
                    S.op("dve", lambda e, nk=nk: e.scalar_tensor_tensor(sc[:, 0:nk], mq[:, 0:nk], -16384.0, dtab[:, 8192 - nk:8192], op0=ALU.mult, op1=ALU.add),
                         reads=[Bmq, Bk], writes=[Bsc])
                    S.op("dve", lambda e, nk=nk: e.tensor_reduce(dmc[:], sc[:, 0:nk], AX.X, ALU.min), reads=[Bsc], writes=[Bdmc])
                    S.op("dve", lambda e: e.tensor_scalar(dmc[:], dmc[:], 16384.0, None, op0=ALU.add), reads=[Bdmc], writes=[Bdmc])
                    pbd, Bpbd = PB.next()
                    S.op("pe", lambda e, pbd=pbd: e.matmul(pbd[0:1, 0:128], dmc[:, 0:1], G["ident_f"][:], start=True, stop=True),
                         reads=[Bdmc, G["Bidf"]], writes=[Bpbd])
                    S.op("dve", lambda e, pbd=pbd, T=T: e.tensor_copy(drow[0:1, T * 128:(T + 1) * 128], pbd[0:1, 0:128]), reads=[Bpbd], writes=[Bdrow])
                    nkb = nk // 128
                    for k4 in range(0, nkb, 4):
                        n4 = min(4, nkb - k4)
                        pb, Bpb = PB.next()
                        for j in range(n4):
                            kb = k4 + j
                            S.op("pe", lambda e, pb=pb, j=j, kb=kb: e.matmul(pb[:, j * 128:(j + 1) * 128], mq[:, kb * 128:(kb + 1) * 128], ident[:],
                                                                           start=True, stop=True), reads=[Bmq, Bident], writes=[Bpb])
                        dst = maskT[:, k4:k4 + n4, T * 128:(T + 1) * 128]
                        src = pb[:, 0:n4 * 128].rearrange("p (a b) -> p a b", a=n4)
                        if (k4 // 4) % 2 == 0:
                            S.op("act", lambda e, dst=dst, src=src: e.activation(dst, src, AF.Copy), reads=[Bpb], writes=[BmT])
                        else:
                            S.op("dve", lambda e, dst=dst, src=src: e.tensor_copy(dst, src), reads=[Bpb], writes=[BmT])
                S.op("dve", lambda e: e.tensor_tensor(drow[:], drow[:], qrel[:], ALU.subtract), reads=[Bdrow, Bk], writes=[Bdrow])
                for h in range(8):
                    S.op("dve", lambda e, h=h: e.tensor_scalar(shrow[0:1, h, :], drow[:], (2.0 ** -(h + 1)) / SM_SCALE, None, op0=ALU.mult),
                         reads=[Bdrow], writes=[Bshrow])
                S.barrier()
                S.phase_end()
            with ExitStack() as bes:
                S.es = bes
                kt = Rot(S, "ds_kt", [128, 8192], BF16, 2)
                vh = Rot(S, "ds_vh", [128, 64, 128], BF16, 2)
                qt = Rot(S, "ds_qt", [128, 512], BF16, 2)
                pT = Rot(S, "ds_pT", [128, 512], BF16, 5)
                mcr = Rot(S, "ds_mc", [128, 128], BF16, 3)
                rec = Rot(S, "ds_rec", [128, 512], F32, 2)
                ob = Rot(S, "ds_ob", [128, 512], BF16, 2)
                nkb = (Q0 + 512) // 128
                kb0 = Q0 // 128
                for h in range(8):
                    k_, Bk_ = kt.next()
                    v_, Bv_ = vh.next()
                    q_, Bq_ = qt.next()
                    S.dma("sp", k_[:, 0:nkb * 128], A["KT"][h, :, 0:nkb * 128], reads=[B["KT"]], writes=[Bk_])
                    S.dma("sp", v_[:, 0:nkb, :], A["VH"][h, :, 0:nkb, :], reads=[B["VH"]], writes=[Bv_])
                    S.dma("sp", q_[:], A["QT"][h, :, g * 512:(g + 1) * 512], reads=[B["QT"]], writes=[Bq_])
                    po, Bpo = PB.t[6], PB.b[6]
                    pd, Bpd = PB.t[7], PB.b[7]
                    pend = []

                    def stage2(kb, p_, Bp_, c0, first, last, po=po, pd=pd, Bpo=Bpo, Bpd=Bpd, v_=v_, Bv_=Bv_):
                        S.op("pe", lambda e, po=po, v_=v_, p_=p_, kb=kb, c0=c0, first=first, last=last: e.matmul(
                            po[:, c0:512], v_[:, kb, :], p_[:, c0:512], start=first, stop=last), reads=[Bv_, Bp_], writes=[Bpo])
                        S.op("pe", lambda e, pd=pd, p_=p_, c0=c0, first=first, last=last: e.matmul(
                            pd[:, c0:512], onesb[:], p_[:, c0:512], start=first, stop=last), reads=[Bk, Bp_], writes=[Bpd])

                    for kb in range(nkb):
                        r = kb - kb0
                        c0 = max(r, 0) * 128
                        first = (kb == 0)
                        last = (kb == nkb - 1)
                        pst, Bpst = PB.next(0, 6)
                        S.op("pe", lambda e, pst=pst, k_=k_, q_=q_, kb=kb, c0=c0: e.matmul(
                            pst[:, c0:512], k_[:, kb * 128:(kb + 1) * 128], q_[:, c0:512], start=True, stop=(h >= 6)),
                            reads=[Bk_, Bq_], writes=[Bpst])
                        if h < 6:
                            S.op("pe", lambda e, pst=pst, h=h, c0=c0: e.matmul(pst[:, c0:512], onesr[0:1, :], shrow[0:1, h, c0:512], start=False, stop=True),
                                 reads=[Bk, Bshrow], writes=[Bpst])
                        p_, Bp_ = pT.next()
                        bias_ap = alb[:, h, r + 60:r + 61]
                        S.op("act", lambda e, p_=p_, pst=pst, c0=c0, bias_ap=bias_ap: e.activation(
                            p_[:, c0:512], pst[:, c0:512], AF.Exp, scale=SM_SCALE, bias=bias_ap), reads=[Bpst, Bk], writes=[Bp_])
                        if r >= 0:
                            mc, Bmc = mcr.next()
                            S.op("dve", lambda e, mc=mc, kb=kb, r=r, h=h: e.tensor_tensor(mc[:], maskT[:, kb, r * 128:(r + 1) * 128], corr[:, h, :], ALU.mult),
                                 reads=[BmT, Bk], writes=[Bmc])
                            S.op("dve", lambda e, p_=p_, mc=mc, r=r: e.scalar_tensor_tensor(p_[:, r * 128:(r + 1) * 128], p_[:, r * 128:(r + 1) * 128], 3.0e38, mc[:],
                                                                                         op0=ALU.min, op1=ALU.mult), reads=[Bmc, Bp_], writes=[Bp_])
                            if c0 + 128 < 512:
                                S.op("dve", lambda e, p_=p_, kb=kb, c0=c0: e.scalar_tensor_tensor(p_[:, c0 + 128:512], p_[:, c0 + 128:512], 3.0e38, maskT[:, kb, c0 + 128:512],
                                                                                               op0=ALU.min, op1=ALU.mult), reads=[BmT, Bp_], writes=[Bp_])
                        else:
                            S.op("dve", lambda e, p_=p_, kb=kb: e.scalar_tensor_tensor(p_[:], p_[:], 3.0e38, maskT[:, kb, :], op0=ALU.min, op1=ALU.mult),
                                 reads=[BmT, Bp_], writes=[Bp_])
                        pend.append((kb, p_, Bp_, c0, first, last))
                        if len(pend) > 2:
                            stage2(*pend.pop(0))
                    while pend:
                        stage2(*pend.pop(0))
                    rc, Brc = rec.next()
                    S.op("dve", lambda e, rc=rc, pd=pd: e.reciprocal(rc[:], pd[:]), reads=[Bpd], writes=[Brc])
                    o_, Bo_ = ob.next()
                    S.op("dve", lambda e, o_=o_, po=po, rc=rc: e.tensor_tensor(o_[:], po[:], rc[:], ALU.mult), reads=[Bpo, Brc], writes=[Bo_])
                    S.dma("sp", A["BB"][h, :, g * 512:(g + 1) * 512], o_[:], reads=[Bo_], writes=[B["BB"]], nowaw=True)
                S.barrier()
                S.phase_end()
        for g in groups:
            do_group(g)
        S.es = es_outer

LN_EPS = 1e-5
DN_ALPHA = 2.0 ** 0.25
CAP = 256
NEXP = 32


def layer_norm_tile(S, G, pre, Bpre, out_t, Bout, gb, bb, Bgb, small, Bsmall, junk, Bjunk):
    s1, s2, mean, var, rstd, nmr = small
    S.op("act", lambda e: e.activation(junk[:], pre[:], AF.Identity, accum_out=s1[:]), reads=[Bpre], writes=[Bjunk, Bsmall])
    S.op("act", lambda e: e.activation(junk[:], pre[:], AF.Square, accum_out=s2[:]), reads=[Bpre], writes=[Bjunk, Bsmall])
    S.op("dve", lambda e: e.tensor_scalar(mean[:], s1[:], 1.0 / 2048.0, None, op0=ALU.mult), reads=[Bsmall], writes=[Bsmall])
    S.op("dve", lambda e: e.tensor_tensor(var[:], mean[:], mean[:], ALU.mult), reads=[Bsmall], writes=[Bsmall])
    S.op("dve", lambda e: e.scalar_tensor_tensor(var[:], s2[:], 1.0 / 2048.0, var[:], op0=ALU.mult, op1=ALU.subtract), reads=[Bsmall], writes=[Bsmall])
    S.op("act", lambda e: e.activation(rstd[:], var[:], AF.Sqrt, bias=G["cst"]["eps_ln"][:, 0:1]), reads=[Bsmall, G["Bcst"]], writes=[Bsmall])
    S.op("dve", lambda e: e.reciprocal(rstd[:], rstd[:]), reads=[Bsmall], writes=[Bsmall])
    S.op("dve", lambda e: e.scalar_tensor_tensor(nmr[:], mean[:], -1.0, rstd[:], op0=ALU.mult, op1=ALU.mult), reads=[Bsmall], writes=[Bsmall])
    S.op("act", lambda e: e.activation(pre[:], pre[:], AF.Identity, scale=rstd[:, 0:1], bias=nmr[:, 0:1]), reads=[Bpre, Bsmall], writes=[Bpre])
    S.op("dve", lambda e: e.tensor_tensor(pre[:], pre[:], gb[:], ALU.mult), reads=[Bpre, Bgb], writes=[Bpre])
    S.op("dve", lambda e: e.tensor_tensor(out_t[:], pre[:], bb[:], ALU.add), reads=[Bpre, Bgb], writes=[Bout])


def phase3a_merge(S, G):
    A = G["ap"]
    PB = G["pb"]
    B = G["B"]
    wa = S.sbuf("o_wa", [128, 8, 2048], BF16)
    wb = S.sbuf("o_wb", [128, 8, 2048], BF16)
    Bw = S.buf("o_w")
    S.dma("pool", wa[:], A["w_branch_a"].rearrange("(kc p) n -> p kc n", p=128), writes=[Bw], nowaw=True)
    S.dma("pool", wb[:], A["w_branch_b"].rearrange("(kc p) n -> p kc n", p=128), writes=[Bw], nowaw=True)
    bag = Rot(S, "o_ba", [128, 8, 512], BF16, 2)
    bbg = Rot(S, "o_bb", [128, 8, 512], BF16, 2)
    gtg = Rot(S, "o_gt", [128, 32, 512], BF16, 2)
    t1r = Rot(S, "o_t1", [128, 512], F32, 2)
    t2r = Rot(S, "o_t2", [128, 512], F32, 2)
    mgs = Rot(S, "o_mgs", [128, 512], BF16, 3)
    for g in range(4):
        ba_, Bba = bag.next()
        bb_, Bbb = bbg.next()
        gt_, Bgt = gtg.next()
        S.dma("sp", ba_[:], A["BA"][:, :, g * 512:(g + 1) * 512].rearrange("h p t -> p h t"), reads=[B["BA"]], writes=[Bba])
        S.dma("sp", bb_[:], A["BB"][:, :, g * 512:(g + 1) * 512].rearrange("h p t -> p h t"), reads=[B["BB"]], writes=[Bbb])
        S.dma("sp", gt_[:], A["GT"][:, :, g * 512:(g + 1) * 512].rearrange("c p t -> p c t"), reads=[B["GT"]], writes=[Bgt])
        for c in range(16):
            pa, Bpa = PB.next()
            for k in range(8):
                S.op("pe", lambda e, pa=pa, k=k, c=c, ba_=ba_: e.matmul(pa[:], wa[:, k, c * 128:(c + 1) * 128], ba_[:, k, :], start=(k == 0), stop=(k == 7)),
                     reads=[Bw, Bba], writes=[Bpa])
            pb2, Bpb2 = PB.next()
            for k in range(8):
                S.op("pe", lambda e, pb2=pb2, k=k, c=c, bb_=bb_: e.matmul(pb2[:], wb[:, k, c * 128:(c + 1) * 128], bb_[:, k, :], start=(k == 0), stop=(k == 7)),
                     reads=[Bw, Bbb], writes=[Bpb2])
            t1, Bt1 = t1r.next()
            t2, Bt2 = t2r.next()
            S.op("dve", lambda e, t1=t1, pa=pa, gt_=gt_, c=c: e.tensor_tensor(t1[:], pa[:], gt_[:, c, :], ALU.mult), reads=[Bpa, Bgt], writes=[Bt1])
            S.op("dve", lambda e, t2=t2, pb2=pb2, gt_=gt_, c=c: e.tensor_tensor(t2[:], pb2[:], gt_[:, 16 + c, :], ALU.mult), reads=[Bpb2, Bgt], writes=[Bt2])
            m_, Bm = mgs.next()
            S.op("dve", lambda e, t1=t1, t2=t2, m_=m_: e.tensor_tensor(m_[:], t1[:], t2[:], ALU.add), reads=[Bt1, Bt2], writes=[Bm])
            S.dma("sp", A["MG"][c, :, g * 512:(g + 1) * 512], m_[:], reads=[Bm], writes=[B["MG"]], nowaw=True)


def phase3b_out(S, G):
    A = G["ap"]
    PB = G["pb"]
    B = G["B"]
    P = G["persist"]
    ident_f = G["ident_f"]
    Bw = S.buf("o_w")
    wr = S.sbuf("o_wr", [128, 16, 36], F32)
    S.dma("sp", wr[:], A["wr"].rearrange("(kc p) n -> p kc n", p=128), writes=[Bw], nowaw=True)
    brr = S.sbuf("o_brr", [1, 36], F32)
    S.dma("sp", brr[:], A["brr"], writes=[Bw], nowaw=True)
    g1 = S.sbuf("o_g1", [128, 2048], F32)
    b1 = S.sbuf("o_b1", [128, 2048], F32)
    S.dma("sp", g1[:], A["ln1_gb"], writes=[Bw], nowaw=True)
    S.dma("sp", b1[:], A["ln1_bb"], writes=[Bw], nowaw=True)
    ltri = S.sbuf("o_ltri", [128, 128], BF16)
    S.dma("sp", ltri[:], A["ltri"], writes=[Bw], nowaw=True)
    e256 = S.sbuf("o_e256", [128, 32], F32)
    S.dma("sp", e256[:], A["e256"], writes=[Bw], nowaw=True)
    onesc = S.sbuf("o_onesc", [128, 1], BF16)
    S.op("dve", lambda e: e.memset(onesc[:], 1.0), writes=[Bw])
    onesr = S.sbuf("o_onesr", [1, 128], F32)
    S.op("dve", lambda e: e.memset(onesr[:], 1.0), writes=[Bw])
    cnt = S.sbuf("o_cnt", [1, 32], F32)
    Bcnt = S.buf("o_cnt")
    S.op("dve", lambda e: e.memset(cnt[:], 0.0), writes=[Bcnt])
    S.barrier()
    wo = Rot(S, "o_wo", [128, 16, 512], BF16, 2)
    mgr = Rot(S, "o_mg", [128, 16, 512], BF16, 2)
    pre = Rot(S, "o_pre", [128, 2048], F32, 5)
    h1t = Rot(S, "o_h1", [128, 2048], F32, 2)
    h1b = Rot(S, "o_h1b", [128, 2048], BF16, 2)
    junk = S.sbuf("o_junk", [128, 2048], BF16)
    Bjunk = S.buf("o_junk")
    hT = Rot(S, "o_hT", [128, 16, 128], F32, 1)
    smalls = [S.sbuf("o_sm%d" % i, [128, 1], F32) for i in range(6)]
    Bsmall = S.buf("o_small")
    rt = {n: S.sbuf("o_r_" + n, shp, F32) for n, shp in (
        ("L", [128, 36]), ("gmax", [128, 1]), ("ngmax", [128, 1]), ("ohg", [128, 4]), ("ge", [128, 4]), ("gsum", [128, 1]), ("gp", [128, 1]),
        ("e8", [128, 8]), ("m1", [128, 1]), ("oh1", [128, 8]), ("e8b", [128, 8]), ("m2", [128, 1]), ("oh2", [128, 8]), ("d", [128, 1]),
        ("sg", [128, 1]), ("A1", [128, 32]), ("A2", [128, 32]), ("At", [128, 32]), ("rk", [128, 32]), ("t", [128, 32]), ("i1", [128, 1]), ("i2", [128, 1]))}
    Abf = S.sbuf("o_Abf", [128, 32], BF16)
    Br = G["Br_persist"]

    def rop(fn, eng="dve"):
        S.op(eng, fn, reads=[Br, Bw], writes=[Br])

    for g in range(4):
        mg, Bmg = mgr.next()
        S.dma("sp", mg[:], A["MG"][:, :, g * 512:(g + 1) * 512].rearrange("c p t -> p c t"), reads=[B["MG"]], writes=[Bmg])
        prs = []
        for t in range(4):
            tt = g * 4 + t
            pr, Bpr = pre.next()
            S.dma("sp", pr[:], A["xs"][OWN0 + tt * 128:OWN0 + (tt + 1) * 128, :], writes=[Bpr])
            prs.append((pr, Bpr))
        for cb in range(4):
            w_, Bwo = wo.next()
            S.dma("pool", w_[:], A["w_out"][:, cb * 512:(cb + 1) * 512].rearrange("(kc p) n -> p kc n", p=128), writes=[Bwo])
            for t in range(4):
                pr, Bpr = prs[t]
                po, Bpo = PB.next()
                for c in range(16):
                    S.op("pe", lambda e, po=po, c=c, t=t, w_=w_, mg=mg: e.matmul(po[:], mg[:, c, t * 128:(t + 1) * 128], w_[:, c, :], start=(c == 0), stop=(c == 15)),
                         reads=[Bmg, Bwo], writes=[Bpo])
                S.op("dve", lambda e, pr=pr, po=po, cb=cb: e.scalar_tensor_tensor(pr[:, cb * 512:(cb + 1) * 512], pr[:, cb * 512:(cb + 1) * 512], DN_ALPHA, po[:],
                                                                                op0=ALU.mult, op1=ALU.add), reads=[Bpo, Bpr], writes=[Bpr])
        for t in range(4):
            tt = g * 4 + t
            pr, Bpr = prs[t]
            h_, Bh = h1t.next()
            layer_norm_tile(S, G, pr, Bpr, h_, Bh, g1, b1, Bw, smalls, Bsmall, junk, Bjunk)
            S.dma("sp", A["H1"][tt * 128:(tt + 1) * 128, :], h_[:], reads=[Bh], writes=[B["H1"]], nowaw=True)
            hb, Bhb = h1b.next()
            S.op("act", lambda e, hb=hb, h_=h_: e.activation(hb[:], h_[:], AF.Copy), reads=[Bh], writes=[Bhb])
            S.dma("sp", A["H1b"][tt * 128:(tt + 1) * 128, :], hb[:], reads=[Bhb], writes=[B["H1b"]], nowaw=True)
            hT_, BhT = hT.next()
            for q4 in range(4):
                pb, Bpb = PB.next()
                for j in range(4):
                    c = q4 * 4 + j
                    S.op("pe", lambda e, pb=pb, h_=h_, c=c, j=j: e.matmul(pb[:, j * 128:(j + 1) * 128], h_[:, c * 128:(c + 1) * 128], ident_f[:], start=True, stop=True),
                         reads=[Bh, G["Bidf"]], writes=[Bpb])
                S.op("act", lambda e, pb=pb, hT_=hT_, q4=q4: e.activation(hT_[:, q4 * 4:(q4 + 1) * 4, :], pb[:].rearrange("p (a b) -> p a b", a=4), AF.Copy),
                     reads=[Bpb], writes=[BhT])
            pl, Bpl = PB.next()
            for c in range(16):
                S.op("pe", lambda e, pl=pl, hT_=hT_, c=c: e.matmul(pl[:, 0:36], hT_[:, c, :], wr[:, c, :], start=(c == 0), stop=False), reads=[BhT, Bw], writes=[Bpl])
            S.op("pe", lambda e, pl=pl: e.matmul(pl[:, 0:36], onesr[0:1, :], brr[0:1, :], start=False, stop=True), reads=[Bw], writes=[Bpl])
            L = rt["L"]
            S.op("dve", lambda e, pl=pl: e.tensor_copy(L[:], pl[:, 0:36]), reads=[Bpl], writes=[Br])
            rop(lambda e: e.reduce_max(rt["gmax"][:], L[:, 0:4], AX.X))
            rop(lambda e: e.tensor_scalar(rt["ohg"][:], L[:, 0:4], rt["gmax"][:, 0:1], None, op0=ALU.is_equal))
            rop(lambda e: e.tensor_scalar(rt["ngmax"][:], rt["gmax"][:], -1.0, None, op0=ALU.mult))
            rop(lambda e: e.activation(rt["ge"][:], L[:, 0:4], AF.Exp, bias=rt["ngmax"][:, 0:1], accum_out=rt["gsum"][:]), "act")
            rop(lambda e: e.reciprocal(rt["gp"][:], rt["gsum"][:]))
            rop(lambda e: e.tensor_scalar(rt["e8"][:], L[:, 4:12], rt["ohg"][:, 0:1], None, op0=ALU.mult))
            for gg in range(1, 4):
                rop(lambda e, gg=gg: e.scalar_tensor_tensor(rt["e8"][:], L[:, 4 + 8 * gg:12 + 8 * gg], rt["ohg"][:, gg:gg + 1], rt["e8"][:], op0=ALU.mult, op1=ALU.add))
            rop(lambda e: e.reduce_max(rt["m1"][:], rt["e8"][:], AX.X))
            rop(lambda e: e.tensor_scalar(rt["oh1"][:], rt["e8"][:], rt["m1"][:, 0:1], None, op0=ALU.is_equal))
            rop(lambda e: e.scalar_tensor_tensor(rt["e8b"][:], rt["oh1"][:], -1.0e30, rt["e8"][:], op0=ALU.mult, op1=ALU.add))
            rop(lambda e: e.reduce_max(rt["m2"][:], rt["e8b"][:], AX.X))
            rop(lambda e: e.tensor_scalar(rt["oh2"][:], rt["e8b"][:], rt["m2"][:, 0:1], None, op0=ALU.is_equal))
            rop(lambda e: e.tensor_tensor(rt["d"][:], rt["m1"][:], rt["m2"][:], ALU.subtract))
            rop(lambda e: e.activation(rt["sg"][:], rt["d"][:], AF.Sigmoid), "act")
            rop(lambda e, tt=tt: e.tensor_tensor(P["wts"][:, tt, 0:1], rt["sg"][:], rt["gp"][:], ALU.mult))
            rop(lambda e, tt=tt: e.tensor_tensor(P["wts"][:, tt, 1:2], rt["gp"][:], P["wts"][:, tt, 0:1], ALU.subtract))
            for gg in range(4):
                rop(lambda e, gg=gg: e.tensor_scalar(rt["A1"][:, gg * 8:(gg + 1) * 8], rt["oh1"][:], rt["ohg"][:, gg:gg + 1], None, op0=ALU.mult))
                rop(lambda e, gg=gg: e.tensor_scalar(rt["A2"][:, gg * 8:(gg + 1) * 8], rt["oh2"][:], rt["ohg"][:, gg:gg + 1], None, op0=ALU.mult))
            rop(lambda e: e.tensor_tensor(rt["At"][:], rt["A1"][:], rt["A2"][:], ALU.add))
            rop(lambda e: e.tensor_copy(Abf[:], rt["At"][:]))
            pk, Bpk = PB.next()
            S.op("pe", lambda e, pk=pk: e.matmul(pk[:, 0:32], ltri[:], Abf[:], start=True, stop=False), reads=[Bw, Br], writes=[Bpk])
            S.op("pe", lambda e, pk=pk: e.matmul(pk[:, 0:32], onesr[0:1, :], cnt[0:1, :], start=False, stop=True), reads=[Bw, Bcnt], writes=[Bpk])
            pc, Bpc = PB.next()
            S.op("pe", lambda e, pc=pc: e.matmul(pc[0:1, 0:32], onesc[:], Abf[:], start=True, stop=True), reads=[Bw, Br], writes=[Bpc])
            S.op("dve", lambda e, pk=pk: e.tensor_copy(rt["rk"][:], pk[:, 0:32]), reads=[Bpk, Br], writes=[Br])
            S.op("dve", lambda e, pc=pc: e.tensor_tensor(cnt[:], cnt[:], pc[0:1, 0:32], ALU.add), reads=[Bpc, Bcnt], writes=[Bcnt])
            rop(lambda e: e.scalar_tensor_tensor(rt["t"][:], rt["rk"][:], 1.0, rt["At"][:], op0=ALU.add, op1=ALU.mult))
            rop(lambda e, tt=tt: e.tensor_scalar(P["RK"][:, tt, :], rt["t"][:], -1.0, None, op0=ALU.add))
            rop(lambda e: e.tensor_tensor(rt["rk"][:], rt["rk"][:], e256[:], ALU.add))
            rop(lambda e: e.tensor_tensor(rt["t"][:], rt["rk"][:], rt["A1"][:], ALU.mult))
            rop(lambda e: e.reduce_sum(rt["i1"][:], rt["t"][:], AX.X))
            rop(lambda e: e.tensor_tensor(rt["t"][:], rt["rk"][:], rt["A2"][:], ALU.mult))
            rop(lambda e: e.reduce_sum(rt["i2"][:], rt["t"][:], AX.X))
            rop(lambda e, tt=tt: e.tensor_copy(P["idx"][:, tt, 0:1], rt["i1"][:]))
            rop(lambda e, tt=tt: e.tensor_copy(P["idx"][:, tt, 1:2], rt["i2"][:]))


def phase4_moe(S, G, experts=range(NEXP)):
    A = G["ap"]
    PB = G["pb"]
    B = G["B"]
    P = G["persist"]
    h1b = S.sbuf("m_h1b", [128, 16, 2048], BF16)
    Bh1b = S.buf("m_h1b")
    S.dma("sp", h1b[:], A["H1b"].rearrange("(t p) d -> p t d", p=128), reads=[B["H1b"]], writes=[Bh1b])
    iot = S.sbuf("m_iota", [128, CAP], F32)
    Biot = S.buf("m_iota")
    S.dma("sp", iot[:], A["iota256"], writes=[Biot])
    wu = Rot(S, "m_w", [128, 16, 512], BF16, 4)
    wd = Rot(S, "m_wd", [128, 4, 2048], BF16, 2)
    sel = Rot(S, "m_sel", [128, 16, CAP], BF16, 1)
    xs_ = Rot(S, "m_xs", [128, 16, CAP], BF16, 1)
    hT = Rot(S, "m_hT", [128, 8, CAP], BF16, 2)
    sg = Rot(S, "m_sg", [128, CAP], F32, 3)
    yst = Rot(S, "m_y", [128, 1024], F32, 1)
    for e_ in experts:
        s_, Bs = sel.next()
        for tt in range(16):
            S.op("dve", lambda e, s_=s_, tt=tt, e_=e_: e.tensor_scalar(s_[:, tt, :], iot[:], P["RK"][:, tt, e_:e_ + 1], None, op0=ALU.is_equal),
                 reads=[Biot, G["Br_persist"]], writes=[Bs])
        x_, Bx = xs_.next()
        for kc in range(16):
            pb, Bpb = PB.next()
            for tt in range(16):
                S.op("pe", lambda e, pb=pb, tt=tt, kc=kc, s_=s_: e.matmul(pb[:, 0:CAP], h1b[:, tt, kc * 128:(kc + 1) * 128], s_[:, tt, :],
                                                                         start=(tt == 0), stop=(tt == 15)), reads=[Bh1b, Bs], writes=[Bpb])
            if kc % 2 == 0:
                S.op("act", lambda e, pb=pb, x_=x_, kc=kc: e.activation(x_[:, kc, :], pb[:, 0:CAP], AF.Copy), reads=[Bpb], writes=[Bx])
            else:
                S.op("dve", lambda e, pb=pb, x_=x_, kc=kc: e.tensor_copy(x_[:, kc, :], pb[:, 0:CAP]), reads=[Bpb], writes=[Bx])
        h_, Bh = hT.next()
        for half in range(2):
            wg_, Bwg = wu.next()
            S.dma("pool", wg_[:], A["w_gate"][e_, :, half * 512:(half + 1) * 512].rearrange("(kc p) n -> p kc n", p=128), writes=[Bwg])
            wu_, Bwu = wu.next()
            S.dma("pool", wu_[:], A["w_up"][e_, :, half * 512:(half + 1) * 512].rearrange("(kc p) n -> p kc n", p=128), writes=[Bwu])
            for f4 in range(4):
                f = half * 4 + f4
                pg, Bpg = PB.next()
                for kc in range(16):
                    S.op("pe", lambda e, pg=pg, kc=kc, f4=f4, wg_=wg_, x_=x_: e.matmul(pg[:, 0:CAP], wg_[:, kc, f4 * 128:(f4 + 1) * 128], x_[:, kc, :],
                                                                                    start=(kc == 0), stop=(kc == 15)), reads=[Bwg, Bx], writes=[Bpg])
                for kc in range(16):
                    S.op("pe", lambda e, pg=pg, kc=kc, f4=f4, wu_=wu_, x_=x_: e.matmul(pg[:, CAP:2 * CAP], wu_[:, kc, f4 * 128:(f4 + 1) * 128], x_[:, kc, :],
                                                                                    start=(kc == 0), stop=(kc == 15)), reads=[Bwu, Bx], writes=[Bpg])
                s1, Bs1 = sg.next()
                S.op("act", lambda e, s1=s1, pg=pg: e.activation(s1[:], pg[:, 0:CAP], AF.Silu), reads=[Bpg], writes=[Bs1])
                S.op("dve", lambda e, h_=h_, f=f, s1=s1, pg=pg: e.tensor_tensor(h_[:, f, :], s1[:], pg[:, CAP:2 * CAP], ALU.mult), reads=[Bs1, Bpg], writes=[Bh])
        wds = []
        for half in range(2):
            wd_, Bwd = wd.next()
            S.dma("pool", wd_[:], A["w_down"][e_, half * 512:(half + 1) * 512, :].rearrange("(fc p) n -> p fc n", p=128), writes=[Bwd])
            wds.append((wd_, Bwd))
        for rh in range(CAP // 128):
            for cbp in range(2):
                y_, By = yst.next()
                for c2 in range(2):
                    cb = cbp * 2 + c2
                    py, Bpy = PB.next()
                    for f in range(8):
                        wd_, Bwd = wds[f // 4]
                        S.op("pe", lambda e, py=py, f=f, rh=rh, cb=cb, wd_=wd_, h_=h_: e.matmul(py[:], h_[:, f, rh * 128:(rh + 1) * 128], wd_[:, f % 4, cb * 512:(cb + 1) * 512],
                                                                                             start=(f == 0), stop=(f == 7)), reads=[Bh, Bwd], writes=[Bpy])
                    if c2 == 0:
                        S.op("act", lambda e, y_=y_, py=py, c2=c2: e.activation(y_[:, c2 * 512:(c2 + 1) * 512], py[:], AF.Copy), reads=[Bpy], writes=[By])
                    else:
                        S.op("dve", lambda e, y_=y_, py=py, c2=c2: e.tensor_copy(y_[:, c2 * 512:(c2 + 1) * 512], py[:]), reads=[Bpy], writes=[By])
                S.dma("sp", A["Y"][e_ * CAP + rh * 128:e_ * CAP + (rh + 1) * 128, cbp * 1024:(cbp + 1) * 1024], y_[:], reads=[By], writes=[B["Y"]], nowaw=True)


def phase5_final(S, G):
    A = G["ap"]
    B = G["B"]
    P = G["persist"]
    g2 = S.sbuf("f_g2", [128, 2048], F32)
    b2 = S.sbuf("f_b2", [128, 2048], F32)
    Bw = S.buf("f_w")
    S.dma("sp", g2[:], A["ln2_gb"], writes=[Bw], nowaw=True)
    S.dma("sp", b2[:], A["ln2_bb"], writes=[Bw], nowaw=True)
    idxi = S.sbuf("f_idx", [128, 16, 2], U32)
    Bidx = S.buf("f_idx")
    S.op("dve", lambda e: e.tensor_copy(idxi[:], P["idx"][:]), reads=[G["Br_persist"]], writes=[Bidx])
    S.barrier()
    h1 = Rot(S, "f_h1", [128, 2048], F32, 2)
    y1 = Rot(S, "f_y1", [128, 2048], F32, 2)
    y2 = Rot(S, "f_y2", [128, 2048], F32, 2)
    ot = Rot(S, "f_ot", [128, 2048], F32, 2)
    junk = S.sbuf("f_junk", [128, 2048], BF16)
    Bjunk = S.buf("f_junk")
    smalls = [S.sbuf("f_sm%d" % i, [128, 1], F32) for i in range(6)]
    Bsmall = S.buf("f_small")
    for tt in range(16):
        h_, Bh = h1.next()
        S.dma("sp", h_[:], A["H1"][tt * 128:(tt + 1) * 128, :], reads=[B["H1"]], writes=[Bh])
        a_, Ba = y1.next()
        b_, Bb = y2.next()
        for (dst, Bd, k) in ((a_, Ba, 0), (b_, Bb, 1)):
            S.dma("pool", None, None, reads=[B["Y"], Bidx], writes=[Bd],
                  builder=lambda e, dst=dst, tt=tt, k=k: e.indirect_dma_start(
                      out=dst[:], out_offset=None, in_=A["Y"], in_offset=bass.IndirectOffsetOnAxis(ap=idxi[:, tt, k:k + 1], axis=0),
                      bounds_check=NEXP * CAP - 1, oob_is_err=False))
        S.op("dve", lambda e, a_=a_, tt=tt: e.tensor_scalar(a_[:], a_[:], P["wts"][:, tt, 0:1], None, op0=ALU.mult), reads=[Ba, G["Br_persist"]], writes=[Ba])
        S.op("dve", lambda e, a_=a_, b_=b_, tt=tt: e.scalar_tensor_tensor(a_[:], b_[:], P["wts"][:, tt, 1:2], a_[:], op0=ALU.mult, op1=ALU.add),
             reads=[Ba, Bb, G["Br_persist"]], writes=[Ba])
        S.op("dve", lambda e, a_=a_, h_=h_: e.scalar_tensor_tensor(a_[:], h_[:], DN_ALPHA, a_[:], op0=ALU.mult, op1=ALU.add), reads=[Ba, Bh], writes=[Ba])
        o_, Bo = ot.next()
        layer_norm_tile(S, G, a_, Ba, o_, Bo, g2, b2, Bw, smalls, Bsmall, junk, Bjunk)
        S.dma("sp", A["out"][tt * 128:(tt + 1) * 128, :], o_[:], reads=[Bo], writes=[B["out"]], nowaw=True)

from contextlib import ExitStack
from concourse.bass_utils import run_bass_kernel_spmd

BFM_IDX = {"af": 0, "aq": 8, "ag": 16, "bk": 24, "bq": 32, "iq": 40, "ik": 44, "g": 45}
NBFM = 77

SCRATCH = {
    "KT": ([8, 128, 8192], "bf16"), "VH": ([8, 128, 64, 128], "bf16"), "KIT": ([128, 8192], "bf16"),
    "HG_kdec": ([8, 128, 8192], "bf16"), "HG_kend": ([8192, 1024], "bf16"), "HG_v": ([8192, 1024], "bf16"),
    "HG_dec": ([128, 8, 128], "f32"), "HG_qdec": ([8, 128, 2048], "bf16"), "HG_gs": ([8, 128, 2048], "bf16"),
    "QT": ([8, 128, 2048], "bf16"), "QIT": ([4, 128, 2048], "bf16"), "WI": ([2048, 8], "f32"),
    "GT": ([32, 128, 2048], "bf16"), "BA": ([8, 128, 2048], "bf16"), "BB": ([8, 128, 2048], "bf16"),
    "MG": ([16, 128, 2048], "bf16"), "H1": ([2048, 2048], "f32"), "H1b": ([2048, 2048], "bf16"), "Y": ([NEXP * CAP, 2048], "f32"),
}

INPUTS = {
    "xs": ([8192, 2048], "f32"), "valid_tm": ([128, 64], "f32"), "w_in": ([2048, 11848], "f32"),
    "ident": ([128, 128], "f32"), "bfm": ([128, NBFM], "f32"), "brow": ([1, 2056], "f32"),
    "lbl": ([128, 2, 8], "f32"), "normg": ([128, 8], "f32"), "rmask": ([128, 512], "f32"), "bdmask": ([128, 128], "f32"),
    "alb": ([128, 8, 64], "f32"), "corr": ([128, 8, 128], "bf16"), "sel2": ([2, 128], "bf16"), "dtab": ([128, 8192], "bf16"),
    "qrel": ([1, 512], "f32"), "adm": ([16, 2, 8192], "bf16"),
    "w_branch_a": ([1024, 2048], "f32"), "w_branch_b": ([1024, 2048], "f32"), "w_out": ([2048, 2048], "f32"),
    "wr": ([2048, 36], "f32"), "brr": ([1, 36], "f32"),
    "ln1_gb": ([128, 2048], "f32"), "ln1_bb": ([128, 2048], "f32"), "ln2_gb": ([128, 2048], "f32"), "ln2_bb": ([128, 2048], "f32"),
    "ltri": ([128, 128], "bf16"), "e256": ([128, 32], "f32"), "iota256": ([128, CAP], "f32"),
    "w_gate": ([NEXP, 2048, 1024], "f32"), "w_up": ([NEXP, 2048, 1024], "f32"), "w_down": ([NEXP, 1024, 2048], "f32"),
}


def _dt(s):
    return {"f32": F32, "bf16": BF16, "u32": U32, "i32": I32}[s]


def host_consts(inputs):
    b_in = np.asarray(inputs["b_in"][0], np.float32)
    bfm = np.zeros((128, NBFM), np.float32)
    def put(idx, c0, n):
        for c in range(n):
            bfm[:, idx + c] = b_in[c0 + c * 128: c0 + (c + 1) * 128]
    put(BFM_IDX["af"], C_AF, 8); put(BFM_IDX["aq"], C_AQ, 8); put(BFM_IDX["ag"], C_AG, 8)
    put(BFM_IDX["bk"], C_BK, 8); put(BFM_IDX["bq"], C_BQ, 8); put(BFM_IDX["iq"], C_IQ, 4)
    put(BFM_IDX["g"], C_G, 32)
    bfm[0:64, BFM_IDX["ik"]] = b_in[C_IK:C_IK + 64]
    bfm[64:128, BFM_IDX["ik"]] = b_in[C_IK:C_IK + 64]
    brow = np.concatenate([b_in[C_AI:C_AI + 1024], b_in[C_BV:C_BV + 1024], b_in[C_IW:C_IW + 8]])[None, :].astype(np.float32)
    lbl = np.ascontiguousarray(np.asarray(inputs["hg_lb_logits"], np.float32).reshape(2, 8, 128).transpose(2, 0, 1))
    normg = np.ascontiguousarray(np.asarray(inputs["hg_norm_g"][0], np.float32).reshape(8, 128).T)
    rmask = np.ones((128, 512), np.float32)
    rmask[:, ::64] = 0.0
    ii = np.arange(128)
    bdmask = ((ii[:, None] // 64 == ii[None, :] // 64) & (ii[:, None] <= ii[None, :])).astype(np.float32)
    import ml_dtypes
    bf = ml_dtypes.bfloat16
    slopes = 2.0 ** -(np.arange(8) + 1.0)
    pp = np.arange(128)
    alb = (slopes[None, :, None] * (pp[:, None, None] + 128.0 * (np.arange(64)[None, None, :] - 60))).astype(np.float32)
    dsq = np.maximum(pp[:, None] - pp[None, :], 0).astype(np.float64)
    corr = np.exp(-2.0 * slopes[None, :, None] * dsq[:, None, :]).astype(bf)
    sel2 = np.zeros((2, 128), np.float32); sel2[0, :64] = 1; sel2[1, 64:] = 1
    dtab = np.abs(8064 + pp[:, None] - np.arange(8192)[None, :]).astype(bf)
    qrel = np.arange(512, dtype=np.float32)[None, :]
    extra = {}
    if "w_out" in inputs:
        extra["w_branch_a"] = np.ascontiguousarray(inputs["w_branch_a"][0]); extra["w_branch_b"] = np.ascontiguousarray(inputs["w_branch_b"][0])
        extra["w_out"] = np.ascontiguousarray(inputs["w_out"][0])
        extra["wr"] = np.ascontiguousarray(np.concatenate([inputs["w_group"][0], inputs["w_router"][0]], axis=1).astype(np.float32))
        extra["brr"] = np.concatenate([inputs["b_group"][0], inputs["b_router"][0]])[None, :].astype(np.float32)
        for nm in ("ln1_g", "ln1_b", "ln2_g", "ln2_b"):
            extra[nm + "b"] = np.ascontiguousarray(np.broadcast_to(np.asarray(inputs[nm][0], np.float32)[None, :], (128, 2048)))
        extra["ltri"] = (pp[:, None] < pp[None, :]).astype(bf)
        extra["e256"] = np.ascontiguousarray(np.broadcast_to((np.arange(32, dtype=np.float32) * CAP)[None, :], (128, 32)))
        extra["iota256"] = np.ascontiguousarray(np.broadcast_to(np.arange(CAP, dtype=np.float32)[None, :], (128, CAP)))
        extra["w_gate"] = np.ascontiguousarray(inputs["w_gate"][0]); extra["w_up"] = np.ascontiguousarray(inputs["w_up"][0])
        extra["w_down"] = np.ascontiguousarray(inputs["w_down"][0])
    return {**extra, "alb": alb, "corr": corr, "sel2": sel2.astype(bf), "dtab": dtab, "qrel": qrel, "bdmask": bdmask, "ident": np.eye(128, dtype=np.float32), "bfm": bfm, "brow": brow, "lbl": lbl, "normg": normg, "rmask": rmask,
            "w_in": np.ascontiguousarray(inputs["w_in"][0])}


def host_core_inputs(inputs, hc, core):
    b, j = core // 4, core % 4
    x = np.asarray(inputs["x"], np.float32)
    xs = np.zeros((8192, 2048), np.float32)
    npre = (3 - j) * 2048
    xs[npre:] = x[b, :(j + 1) * 2048]
    valid = np.zeros(8192, np.float32)
    valid[npre:] = 1.0
    d = dict(hc)
    d["xs"] = xs
    d["valid_tm"] = np.ascontiguousarray(valid.reshape(64, 128).T)
    import ml_dtypes
    chunk = np.arange(8192) // 64
    adm = np.full((16, 2, 8192), NEG_ADM, np.float32)
    for t in range(16):
        c_first = (OWN0 + t * 128) // 64
        adm[t, 0, (valid > 0) & (chunk <= c_first)] = 0.0
        adm[t, 1, (valid > 0) & (chunk <= c_first + 1)] = 0.0
    d["adm"] = adm.astype(ml_dtypes.bfloat16)
    return d


def build_program(phases=("p1a",), dump=(), p1_blocks=(0, 1, 2, 3), p2_groups=(0, 1, 2, 3), in_names=None):
    nc = bass.Bass("TRN2", target_bir_lowering=False)
    A = {}
    used_inputs = in_names if in_names is not None else list(INPUTS)
    for n in used_inputs:
        shp, dt = INPUTS[n]
        A[n] = nc.dram_tensor(n, shp, _dt(dt), kind="ExternalInput").ap()
    for n, (shp, dt) in SCRATCH.items():
        kind = "ExternalOutput" if n in dump else "Internal"
        A[n] = nc.dram_tensor(n, shp, _dt(dt), kind=kind).ap()
    A["out"] = nc.dram_tensor("out", [2048, 2048], F32, kind="ExternalOutput").ap()
    with ExitStack() as es:
        S = Sched(nc, es)
        G = {"ap": A, "pb": PBanks(S), "B": {n: S.buf(n, glob=True) for n in list(SCRATCH) + ["out"]}, "bfm_idx": BFM_IDX}
        G["persist"] = {"wts": S.sbuf("p_wts", [128, 16, 2], F32), "RK": S.sbuf("p_RK", [128, 16, 32], F32), "idx": S.sbuf("p_idx", [128, 16, 2], F32)}
        G["Br_persist"] = S.buf("persist", glob=True)
        cst = {}
        Bc = S.buf("cst", glob=True)
        G["cst"] = cst
        G["Bcst"] = Bc
        idf = S.sbuf("idf", [128, 128], F32)
        Bidf = S.buf("idf", glob=True)
        S.dma("sp", idf[:], A["ident"], writes=[Bidf])
        idb = S.sbuf("idb", [128, 128], BF16)
        Bident = S.buf("idb")
        S.op("dve", lambda e: e.tensor_copy(idb[:], idf[:]), reads=[Bidf], writes=[Bident])
        G["ident_bf"] = idb
        G["Bident"] = Bident
        G["ident_f"] = idf
        G["Bidf"] = Bidf
        for n in ("bfm", "brow", "valid_tm", "normg", "rmask", "bdmask"):
            shp, dt = INPUTS[n]
            cst[n] = S.sbuf("c_" + n, shp, _dt(dt))
            S.dma("sp", cst[n][:], A[n], writes=[Bc], nowaw=True)
        lbl = S.sbuf("c_lbl", [128, 2, 8], F32)
        Blbl = S.buf("lbl", glob=True)
        S.dma("sp", lbl[:], A["lbl"], writes=[Blbl])
        for n in ("lb", "oml", "noml", "lbd"):
            cst[n] = S.sbuf("c_" + n, [128, 8], F32)
        cst["ones_row"] = S.sbuf("c_ones_row", [1, 128], F32)
        S.op("dve", lambda e: e.memset(cst["ones_row"][:], 1.0), writes=[Bc])
        S.op("dve", lambda e: e.tensor_tensor(cst["lbd"][:], lbl[:, 0, :], lbl[:, 1, :], ALU.subtract), reads=[Blbl], writes=[Bc])
        S.op("act", lambda e: e.activation(cst["lb"][:], cst["lbd"][:], AF.Sigmoid), reads=[Bc], writes=[Bc])
        S.op("dve", lambda e: e.tensor_scalar(cst["oml"][:], cst["lb"][:], -1.0, 1.0, op0=ALU.mult, op1=ALU.add), reads=[Bc], writes=[Bc])
        S.op("dve", lambda e: e.tensor_scalar(cst["noml"][:], cst["oml"][:], -1.0, None, op0=ALU.mult), reads=[Bc], writes=[Bc])

        cst["eps_ln"] = S.sbuf("c_eps_ln", [128, 1], F32)
        S.op("dve", lambda e: e.memset(cst["eps_ln"][:], LN_EPS), writes=[Bc])
        cst["eps_rms"] = S.sbuf("c_eps_rms", [128, 1], F32)
        S.op("dve", lambda e: e.memset(cst["eps_rms"][:], RMS_EPS), writes=[Bc])
        S.barrier()
        if "p1a" in phases:
            with ExitStack() as pes:
                S.es = pes
                phase1a(S, G, blocks=p1_blocks)
                S.es = es
            S.barrier()
            S.phase_end()
        if "p1b" in phases:
            with ExitStack() as pes:
                S.es = pes
                phase1b(S, G)
                S.es = es
            S.barrier()
            S.phase_end()
        if "p2" in phases:
            phase2_dsa(S, G, groups=p2_groups)
            S.barrier()
            S.phase_end()
        for nm, fn in (("p3a", phase3a_merge), ("p3b", phase3b_out), ("p4", phase4_moe), ("p5", phase5_final)):
            if nm in phases:
                with ExitStack() as pes:
                    S.es = pes
                    fn(S, G)
                    S.es = es
                S.barrier()
                S.phase_end()
        outs = [G["B"][n] for n in dump] + ([G["B"]["out"]] if "p5" in phases else [])
        S.wait_all("sp", outs)
        print("instructions:", S.ninst, {k: len(v) for k, v in S.ops.items()})
        S.run()
    return nc


ALL_PHASES = ("p1a", "p1b", "p2", "p3a", "p3b", "p4", "p5")
_CACHE = {}


def kernel(**inputs):
    if "nc" not in _CACHE:
        _CACHE["nc"] = build_program(phases=ALL_PHASES)
    nc = _CACHE["nc"]
    hc = host_consts(inputs)
    in_maps = [host_core_inputs(inputs, hc, c) for c in range(8)]
    res = run_bass_kernel_spmd(nc, in_maps, core_ids=list(range(8)))
    out = np.zeros((2, 8192, 2048), np.float32)
    for c in range(8):
        b, j = c // 4, c % 4
        out[b, j * 2048:(j + 1) * 2048] = np.asarray(res.results[c]["out"])
    return out
```

```python
import numpy as np
import concourse.bass as bass
import concourse.mybir as mybir

F32 = mybir.dt.float32
BF16 = mybir.dt.bfloat16
U32 = mybir.dt.uint32
I32 = mybir.dt.int32
U8 = mybir.dt.uint8
AF = mybir.ActivationFunctionType
ALU = mybir.AluOpType
AX = mybir.AxisListType


class Buf:
    __slots__ = ("name", "lastw", "readers", "dsem", "dcount", "glob", "dkey")

    def __init__(self, name, glob=False):
        self.name = name
        self.glob = glob
        self.dkey = None
        self.lastw = None
        self.readers = {}
        self.dsem = None
        self.dcount = 0


class Sched:
    ENGS = ("pe", "act", "dve", "pool", "sp")
    SEM_LIMIT = 30000

    def __init__(self, nc, es):
        self.nc = nc
        self.es = es
        self.es_sem = es
        self.dbufs = []
        self.dstate = {}
        self.free_dsems = []
        self.local_dbufs = []
        self.sem = {}
        self.count = {}
        self.known = {}
        self.ops = {}
        self.epoch = {}
        for n in self.ENGS:
            self.sem[n] = es.enter_context(nc.semaphore("se_" + n))
            self.count[n] = 0
            self.epoch[n] = 0
            self.known[n] = {}
            self.ops[n] = []
        self.nbuf = 0
        self.ninst = 0

    def sbuf(self, name, shape, dtype):
        self.nbuf += 1
        name = "%s_u%d" % (name, self.nbuf)
        return self.es.enter_context(self.nc.sbuf_tensor(name, list(shape), dtype))

    def psum(self, name, shape, dtype):
        return self.es.enter_context(self.nc.psum_tensor(name, list(shape), dtype))

    def buf(self, name=None, glob=False):
        self.nbuf += 1
        return Buf("%s_b%d" % (name or "b", self.nbuf), glob)

    def bufs(self, n, name="b"):
        return [self.buf("%s%d" % (name, i)) for i in range(n)]

    def _waits(self, eng, reads, writes):
        need = {}

        def add(ev, skip_same):
            if ev is None:
                return
            key, sem, val, prod = ev
            if skip_same and prod == eng:
                return
            if self.known[eng].get(key, 0) >= val:
                return
            if key not in need or need[key][1] < val:
                need[key] = (sem, val)

        for b in reads:
            add(b.lastw, False)
        for b in writes:
            add(b.lastw, True)
            for ev in b.readers.values():
                add(ev, True)
        for key, (sem, val) in need.items():
            self.known[eng][key] = val
        return list(need.values())

    def op(self, eng, fn, reads=(), writes=()):
        waits = self._waits(eng, reads, writes)
        if self.count[eng] >= self.SEM_LIMIT:
            self.epoch[eng] += 1
            self.count[eng] = 0
            self.sem[eng] = self.es_sem.enter_context(self.nc.semaphore("se_%s_%d" % (eng, self.epoch[eng])))
        self.count[eng] += 1
        seq = self.count[eng]
        sem = self.sem[eng]
        key = "e_%s_%d" % (eng, self.epoch[eng])
        ev = (key, sem, seq, eng)
        for b in writes:
            b.lastw = ev
            b.readers = {}
        for b in reads:
            b.readers[key] = ev
        self.ninst += 1 + len(waits)

        def emit(e, fn=fn, waits=waits, sem=sem):
            for (s, v) in waits:
                e.wait_ge(s, v)
            fn(e).then_inc(sem, 1)

        self.ops[eng].append(emit)
        return ev

    def dma(self, q, out_ap, in_ap, reads=(), writes=(), nowaw=False, builder=None, **kw):
        waits = self._waits(q, reads, [] if nowaw else writes)
        tb = writes[0]
        if tb.dsem is None:
            if (not tb.glob) and self.free_dsems:
                tb.dsem, tb.dcount, tb.dkey = self.free_dsems.pop()
            else:
                tb.dsem = self.es_sem.enter_context(self.nc.semaphore("sd_" + tb.name))
                tb.dkey = "d_" + tb.name
            if not tb.glob:
                self.local_dbufs.append(tb)
        tb.dcount += 16
        self.dstate[tb.dkey] = (tb.dsem, tb.dcount)
        ev = (tb.dkey, tb.dsem, tb.dcount, None)
        for b in writes:
            b.lastw = ev
            if not nowaw:
                b.readers = {}
        for b in reads:
            b.readers[ev[0]] = ev
        self.ninst += 1 + len(waits)

        def emit(e, waits=waits, sem=tb.dsem, out_ap=out_ap, in_ap=in_ap, kw=kw, builder=builder):
            for (s, v) in waits:
                e.wait_ge(s, v)
            if builder is not None:
                builder(e).then_inc(sem, 16)
            else:
                e.dma_start(out=out_ap, in_=in_ap, **kw).then_inc(sem, 16)

        self.ops[q].append(emit)
        return ev

    def phase_end(self):
        for b in self.local_dbufs:
            self.free_dsems.append((b.dsem, b.dcount, b.dkey))
            b.dsem = None
        self.local_dbufs = []

    def raw(self, eng, fn):
        self.ops[eng].append(lambda e, fn=fn: fn(e))

    def wait_all(self, eng, bufs):
        waits = self._waits(eng, list(bufs), [])

        def emit(e, waits=waits):
            for (s, v) in waits:
                e.wait_ge(s, v)

        self.ops[eng].append(emit)

    def barrier(self):
        evs = []
        for n in self.ENGS:
            if self.count[n] > 0:
                evs.append(("e_%s_%d" % (n, self.epoch[n]), self.sem[n], self.count[n], n))
        for key, (sem, cnt) in self.dstate.items():
            evs.append((key, sem, cnt, None))
        for eng in self.ENGS:
            waits = []
            for (key, sem, val, prod) in evs:
                if prod == eng:
                    continue
                if self.known[eng].get(key, 0) >= val:
                    continue
                self.known[eng][key] = val
                waits.append((sem, val))

            def emit(e, waits=waits):
                for (s, v) in waits:
                    e.wait_ge(s, v)

            self.ops[eng].append(emit)

    def run(self):
        nc = self.nc
        ops = self.ops
        with nc.Block() as block:
            @block.tensor
            def _(e):
                for f in ops["pe"]:
                    f(e)

            @block.scalar
            def _(e):
                for f in ops["act"]:
                    f(e)

            @block.vector
            def _(e):
                for f in ops["dve"]:
                    f(e)

            @block.gpsimd
            def _(e):
                for f in ops["pool"]:
                    f(e)

            @block.sync
            def _(e):
                for f in ops["sp"]:
                    f(e)

NSLOT = 8192
NOWN = 2048
OWN0 = NSLOT - NOWN
D = 2048
KC = 16
C_AQ, C_AF, C_AI, C_AG, C_BQ, C_BK, C_BV, C_IQ, C_IK, C_IW, C_G = 0, 1024, 2048, 3072, 4096, 5120, 6144, 7168, 7680, 7744, 7752
W_SCALE = (8 ** -0.5) * (64 ** -0.5)


class Rot:
    def __init__(self, S, name, shape, dtype, n):
        self.t = [S.sbuf("%s%d" % (name, i), shape, dtype) for i in range(n)]
        self.b = [S.buf("%s%d" % (name, i)) for i in range(n)]
        self.i = 0
        self.n = n

    def next(self):
        k = self.i % self.n
        self.i += 1
        return self.t[k], self.b[k]


class PBanks:
    def __init__(self, S):
        self.t = [S.psum("pb%d" % i, [128, 512], F32) for i in range(8)]
        self.b = [S.buf("pb%d" % i) for i in range(8)]
        self.i = 0

    def next(self, lo=0, hi=8):
        n = hi - lo
        k = lo + (self.i % n)
        self.i += 1
        return self.t[k], self.b[k]


def phase1a(S, G, blocks=(0, 1, 2, 3)):
    A = G["ap"]
    PB = G["pb"]
    ident = G["ident_bf"]
    Bident = G["Bident"]
    w_in = A["w_in"]
    xT = S.sbuf("xT", [128, KC, 2048], BF16)
    BxT = S.bufs(16, "xT")
    xin = Rot(S, "xin", [128, 2048], BF16, 2)
    wt = Rot(S, "wt", [128, KC, 512], BF16, 2)
    f32t = Rot(S, "hgf", [128, 512], F32, 12)
    st16 = Rot(S, "st16", [128, 512], BF16, 8)
    stkT = Rot(S, "stkT", [128, 4, 128], BF16, 2)
    decst = S.sbuf("decst", [128, 8, 32], F32)
    Bdec = S.buf("decst")
    wist = Rot(S, "wist", [128, 8], F32, 2)
    cst = G["cst"]
    Bc = G["Bcst"]
    evq = [0]

    def evac_engine():
        evq[0] += 1
        return "act" if evq[0] % 2 else "dve"

    def copy_op(eng, out_ap, in_ap, reads, writes):
        if eng == "act":
            S.op("act", lambda e, out_ap=out_ap, in_ap=in_ap: e.activation(out_ap, in_ap, AF.Copy), reads=reads, writes=writes)
        else:
            S.op("dve", lambda e, out_ap=out_ap, in_ap=in_ap: e.tensor_copy(out_ap, in_ap), reads=reads, writes=writes)

    for blk in blocks:
        own = (blk == 3)
        s0 = blk * 2048
        for t in range(16):
            xi, Bxi = xin.next()
            S.dma("pool", xi[:], A["xs"][s0 + t * 128: s0 + (t + 1) * 128, :], writes=[Bxi])
            for q4 in range(4):
                pb, Bpb = PB.next()
                for j in range(4):
                    kc = q4 * 4 + j
                    S.op("pe", lambda e, pb=pb, xi=xi, kc=kc, j=j: e.matmul(
                        pb[:, j * 128:(j + 1) * 128], xi[:, kc * 128:(kc + 1) * 128], ident[:], start=True, stop=True),
                        reads=[Bxi, Bident], writes=[Bpb])
                copy_op(evac_engine(), xT[:, q4 * 4:(q4 + 1) * 4, t * 128:(t + 1) * 128],
                        pb[:].rearrange("p (a b) -> p a b", a=4), [Bpb], [BxT[t]])

        def load_w(cols):
            w, Bw = wt.next()
            off = 0
            for (c0, n) in cols:
                S.dma("pool", w[:, :, off:off + n], w_in[:, c0:c0 + n].rearrange("(kc p) n -> p kc n", p=128), writes=[Bw])
                off += n
            return w, Bw

        def fm_mm(w, Bw, m, g):
            pb, Bpb = PB.next()
            for kc in range(KC):
                S.op("pe", lambda e, pb=pb, w=w, kc=kc, m=m, g=g: e.matmul(
                    pb[:], w[:, kc, m * 128:(m + 1) * 128], xT[:, kc, g * 512:(g + 1) * 512], start=(kc == 0), stop=(kc == KC - 1)),
                    reads=[Bw] + BxT[g * 4:(g + 1) * 4], writes=[Bpb])
            return pb, Bpb

        def simple_fm(w, Bw, m, g, bias_ap, func, dst_ap, Bd, rows=128):
            pb, Bpb = fm_mm(w, Bw, m, g)
            st, Bst = st16.next()
            S.op("act", lambda e, st=st, pb=pb, func=func, bias_ap=bias_ap: e.activation(st[:], pb[:], func, bias=bias_ap), reads=[Bpb, Bc], writes=[Bst])
            S.dma("sp", dst_ap, st[0:rows, :], reads=[Bst], writes=[Bd], nowaw=True)

        tm_units = [("ai", C_AI), ("ai", C_AI + 512), ("bv", C_BV), ("bv", C_BV + 512)]
        for (kind, c0) in tm_units:
            w, Bw = load_w([(c0, 512)])
            for t in range(16):
                pb, Bpb = PB.next()
                for kc in range(KC):
                    S.op("pe", lambda e, pb=pb, w=w, kc=kc, t=t: e.matmul(
                        pb[:], xT[:, kc, t * 128:(t + 1) * 128], w[:, kc, :], start=(kc == 0), stop=False),
                        reads=[Bw, BxT[t]], writes=[Bpb])
                boff = (c0 - C_AI) if kind == "ai" else (1024 + c0 - C_BV)
                S.op("pe", lambda e, pb=pb, boff=boff: e.matmul(pb[:], cst["ones_row"][0:1, :], cst["brow"][0:1, boff:boff + 512],
                                                            start=False, stop=True), reads=[Bc], writes=[Bpb])
                st, Bst = st16.next()
                tt = blk * 16 + t
                if kind == "ai":
                    S.op("act", lambda e, st=st, pb=pb, tt=tt: e.activation(st[:], pb[:], AF.Identity, scale=cst["valid_tm"][:, tt:tt + 1]),
                         reads=[Bpb, Bc], writes=[Bst])
                    dst = A["HG_v"][s0 + t * 128:s0 + (t + 1) * 128, c0 - C_AI:c0 - C_AI + 512]
                else:
                    S.op("dve", lambda e, st=st, pb=pb: e.tensor_copy(st[:], pb[:]), reads=[Bpb], writes=[Bst])
                    h0 = (c0 - C_BV) // 128
                    dst = A["VH"][h0:h0 + 4, :, tt, :].rearrange("h p d -> p h d")
                if kind == "ai":
                    S.dma("sp", dst, st[:], reads=[Bst], writes=[G["B"]["HG_v"]], nowaw=True)
                else:
                    S.dma("sp", dst, st[:].rearrange("p (h d) -> p h d", h=4), reads=[Bst], writes=[G["B"]["VH"]], nowaw=True)
        if own:
            w, Bw = load_w([(C_IW, 8)])
            for t in range(16):
                pb, Bpb = PB.next()
                for kc in range(KC):
                    S.op("pe", lambda e, pb=pb, w=w, kc=kc, t=t: e.matmul(
                        pb[:, 0:8], xT[:, kc, t * 128:(t + 1) * 128], w[:, kc, 0:8], start=(kc == 0), stop=False),
                        reads=[Bw, BxT[t]], writes=[Bpb])
                S.op("pe", lambda e, pb=pb: e.matmul(pb[:, 0:8], cst["ones_row"][0:1, :], cst["brow"][0:1, 2048:2056],
                                                     start=False, stop=True), reads=[Bc], writes=[Bpb])
                st, Bst = wist.next()
                S.op("dve", lambda e, st=st, pb=pb: e.tensor_scalar(st[:], pb[:, 0:8], W_SCALE, None, op0=ALU.mult), reads=[Bpb], writes=[Bst])
                S.dma("sp", A["WI"][t * 128:(t + 1) * 128, :], st[:], reads=[Bst], writes=[G["B"]["WI"]], nowaw=True)

        for u in range(2):
            w, Bw = load_w([(C_BK + u * 512, 512)])
            for m in range(4):
                h = u * 4 + m
                for g in range(4):
                    simple_fm(w, Bw, m, g, cst["bfm"][:, G["bfm_idx"]["bk"] + h: G["bfm_idx"]["bk"] + h + 1], AF.Identity,
                              A["KT"][h, :, s0 + g * 512:s0 + (g + 1) * 512], G["B"]["KT"])
        w, Bw = load_w([(C_IK, 64), (C_IK, 64)])
        for g in range(4):
            simple_fm(w, Bw, 0, g, cst["bfm"][:, G["bfm_idx"]["ik"]: G["bfm_idx"]["ik"] + 1], AF.Identity,
                      A["KIT"][:, s0 + g * 512:s0 + (g + 1) * 512], G["B"]["KIT"])
        if own:
            for u in range(2):
                w, Bw = load_w([(C_BQ + u * 512, 512)])
                for m in range(4):
                    h = u * 4 + m
                    for g in range(4):
                        simple_fm(w, Bw, m, g, cst["bfm"][:, G["bfm_idx"]["bq"] + h: G["bfm_idx"]["bq"] + h + 1], AF.Identity,
                                  A["QT"][h, :, g * 512:(g + 1) * 512], G["B"]["QT"])
            w, Bw = load_w([(C_IQ, 512)])
            for m in range(4):
                for g in range(4):
                    simple_fm(w, Bw, m, g, cst["bfm"][:, G["bfm_idx"]["iq"] + m: G["bfm_idx"]["iq"] + m + 1], AF.Identity,
                              A["QIT"][m, :, g * 512:(g + 1) * 512], G["B"]["QIT"])
            for u in range(8):
                w, Bw = load_w([(C_G + u * 512, 512)])
                for m in range(4):
                    c = u * 4 + m
                    for g in range(4):
                        simple_fm(w, Bw, m, g, cst["bfm"][:, G["bfm_idx"]["g"] + c: G["bfm_idx"]["g"] + c + 1], AF.Sigmoid,
                                  A["GT"][c, :, g * 512:(g + 1) * 512], G["B"]["GT"])

        deferred = []
        for h in range(8):
            if own:
                w, Bw = load_w([(C_AF + h * 128, 128), (C_AQ + h * 128, 128), (C_AG + h * 128, 128)])
            else:
                if h % 4 == 0:
                    w4, Bw4 = load_w([(C_AF + h * 128, 512)])
                w, Bw = w4, Bw4
            mf = 0 if own else (h % 4)
            bi = G["bfm_idx"]
            oml_h = cst["oml"][:, h:h + 1]
            noml_h = cst["noml"][:, h:h + 1]
            lb_h = cst["lb"][:, h:h + 1]
            b_af = cst["bfm"][:, bi["af"] + h: bi["af"] + h + 1]
            b_aq = cst["bfm"][:, bi["aq"] + h: bi["aq"] + h + 1]
            b_ag = cst["bfm"][:, bi["ag"] + h: bi["ag"] + h + 1]
            for g in range(4):
                pb, Bpb = fm_mm(w, Bw, mf, g)
                while deferred:
                    deferred.pop(0)()
                sg, Bsg = f32t.next()
                S.op("act", lambda e, sg=sg, pb=pb, b_af=b_af: e.activation(sg[:], pb[:], AF.Sigmoid, bias=b_af),
                     reads=[Bpb, Bc], writes=[Bsg])
                lf, Blf = f32t.next()
                S.op("act", lambda e, lf=lf, sg=sg, oml_h=oml_h, lb_h=lb_h: e.activation(lf[:], sg[:], AF.Ln, scale=oml_h, bias=lb_h),
                     reads=[Bsg, Bc], writes=[Blf])
                cum, Bcum = f32t.next()
                S.op("dve", lambda e, cum=cum, lf=lf: e.tensor_tensor_scan(cum[:], cst["rmask"][:], lf[:], 0.0, ALU.mult, ALU.add),
                     reads=[Blf, Bc], writes=[Bcum])
                en, Ben = f32t.next()
                S.op("act", lambda e, en=en, cum=cum: e.activation(en[:], cum[:], AF.Exp, scale=-1.0), reads=[Bcum], writes=[Ben])
                kk, Bkk = f32t.next()
                S.op("dve", lambda e, kk=kk, sg=sg, noml_h=noml_h, oml_h=oml_h: e.tensor_scalar(kk[:], sg[:], noml_h, oml_h, op0=ALU.mult, op1=ALU.add),
                     reads=[Bsg, Bc], writes=[Bkk])
                kd, Bkd = st16.next()
                S.op("dve", lambda e, kd=kd, kk=kk, en=en: e.tensor_tensor(kd[:], kk[:], en[:], ALU.mult), reads=[Bkk, Ben], writes=[Bkd])
                S.dma("sp", A["HG_kdec"][h, :, s0 + g * 512:s0 + (g + 1) * 512], kd[:], reads=[Bkd], writes=[G["B"]["HG_kdec"]], nowaw=True)
                ex2, Bex2 = f32t.next()
                for c in range(8):
                    S.op("act", lambda e, ex2=ex2, cum=cum, c=c: e.activation(ex2[:, c * 64:(c + 1) * 64], cum[:, c * 64:(c + 1) * 64], AF.Exp,
                                                                           scale=-1.0, bias=cum[:, c * 64 + 63:c * 64 + 64]),
                         reads=[Bcum], writes=[Bex2])
                ke, Bke = st16.next()
                S.op("dve", lambda e, ke=ke, kk=kk, ex2=ex2: e.tensor_tensor(ke[:], kk[:], ex2[:], ALU.mult), reads=[Bkk, Bex2], writes=[Bke])
                dec_ap = decst[:, h, g * 8:(g + 1) * 8]
                S.op("act", lambda e, cum=cum, dec_ap=dec_ap: e.activation(dec_ap, cum[:].rearrange("p (c s) -> p c s", s=64)[:, :, 63], AF.Exp),
                     reads=[Bcum], writes=[Bdec])
                def kend_T(ke=ke, Bke=Bke, g=g, h=h, s0=s0):
                    pbt, Bpbt = PB.next()
                    for j in range(4):
                        S.op("pe", lambda e, pbt=pbt, ke=ke, j=j: e.matmul(pbt[:, j * 128:(j + 1) * 128], ke[:, j * 128:(j + 1) * 128], ident[:],
                                                                           start=True, stop=True), reads=[Bke, Bident], writes=[Bpbt])
                    kT, BkT = stkT.next()
                    S.op("dve", lambda e, kT=kT, pbt=pbt: e.tensor_copy(kT[:], pbt[:].rearrange("p (a b) -> p a b", a=4)), reads=[Bpbt], writes=[BkT])
                    S.dma("sp", A["HG_kend"][s0 + g * 512:s0 + (g + 1) * 512, h * 128:(h + 1) * 128].rearrange("(t p) d -> p t d", p=128),
                          kT[:], reads=[BkT], writes=[G["B"]["HG_kend"]], nowaw=True)
                deferred.append(kend_T)
                if own:
                    ec, Bec = f32t.next()
                    S.op("act", lambda e, ec=ec, cum=cum: e.activation(ec[:], cum[:], AF.Exp), reads=[Bcum], writes=[Bec])
                    pbq, Bpbq = fm_mm(w, Bw, 1, g)
                    qs, Bqs = f32t.next()
                    S.op("act", lambda e, qs=qs, pbq=pbq, b_aq=b_aq: e.activation(qs[:], pbq[:], AF.Silu, bias=b_aq),
                         reads=[Bpbq, Bc], writes=[Bqs])
                    qd, Bqd = st16.next()
                    S.op("dve", lambda e, qd=qd, qs=qs, ec=ec: e.tensor_tensor(qd[:], qs[:], ec[:], ALU.mult), reads=[Bqs, Bec], writes=[Bqd])
                    S.dma("sp", A["HG_qdec"][h, :, g * 512:(g + 1) * 512], qd[:], reads=[Bqd], writes=[G["B"]["HG_qdec"]], nowaw=True)
                    simple_fm(w, Bw, 2, g, b_ag, AF.Silu, A["HG_gs"][h, :, g * 512:(g + 1) * 512], G["B"]["HG_gs"])
        while deferred:
            deferred.pop(0)()
        S.dma("sp", A["HG_dec"][:, :, blk * 32:(blk + 1) * 32], decst[:], reads=[Bdec], writes=[G["B"]["HG_dec"]], nowaw=True)

RMS_EPS = 1e-6


def phase1b(S, G):
    A = G["ap"]
    PB = G["pb"]
    cst = G["cst"]
    Bc = G["Bcst"]
    B = G["B"]
    kend = S.sbuf("hs_kend", [128, 16, 1024], BF16)
    Bkend = S.buf("hs_kend")
    vv = S.sbuf("hs_v", [128, 16, 1024], BF16)
    Bvv = S.buf("hs_v")
    dec = S.sbuf("hs_dec", [128, 8, 128], F32)
    Bdec = S.buf("hs_dec")
    Sf = S.sbuf("hs_S", [128, 8, 128], F32)
    Sb = S.sbuf("hs_Sb", [128, 8, 128], BF16)
    BS = S.bufs(8, "hs_S")
    BSb = S.bufs(8, "hs_Sb")
    S.dma("sp", dec[:], A["HG_dec"], reads=[B["HG_dec"]], writes=[Bdec])
    S.op("dve", lambda e: e.memset(Sf[:], 0.0), writes=BS)
    S.op("dve", lambda e: e.memset(Sb[:], 0.0), writes=BSb)
    onesb = S.sbuf("hs_ones", [128, 128], BF16)
    Bones = S.buf("hs_ones")
    S.op("dve", lambda e: e.memset(onesb[:], 1.0 / 128.0), writes=[Bones])
    kdec = Rot(S, "hs_kdec", [128, 2048], BF16, 2)
    qdec = Rot(S, "hs_qdec", [128, 2048], BF16, 2)
    gs = Rot(S, "hs_gs", [128, 2048], BF16, 2)
    attm = Rot(S, "hs_attm", [128, 128], BF16, 3)
    sq = Rot(S, "hs_sq", [128, 128], BF16, 3)
    rs = Rot(S, "hs_rs", [128, 128], F32, 3)
    yy = Rot(S, "hs_y", [128, 128], F32, 3)
    bast = S.sbuf("hs_bast", [128, 2048], BF16)
    Bbast = S.buf("hs_bast")

    def state_update(t, h, half, ):
        p0 = half * 64
        chunk = None
        pk, Bpk = PB.next()
        S.op("pe", lambda e, pk=pk, t=t, h=h, p0=p0: e.matmul(pk[:, 0:128], kend[p0:p0 + 64, t, h * 128:(h + 1) * 128],
                                                            vv[p0:p0 + 64, t, h * 128:(h + 1) * 128], start=True, stop=True),
             reads=[Bkend, Bvv], writes=[Bpk])
        return pk, Bpk

    for blk in range(4):
        own = (blk == 3)
        s0 = blk * 2048
        S.dma("sp", kend[:], A["HG_kend"][s0:s0 + 2048, :].rearrange("(t p) c -> p t c", p=128), reads=[B["HG_kend"]], writes=[Bkend])
        S.dma("sp", vv[:], A["HG_v"][s0:s0 + 2048, :].rearrange("(t p) c -> p t c", p=128), reads=[B["HG_v"]], writes=[Bvv])
        if not own:
            for t in range(16):
                for half in range(2):
                    ch = blk * 32 + t * 2 + half
                    for h in range(8):
                        pk, Bpk = state_update(t, h, half)
                        S.op("dve", lambda e, pk=pk, h=h, ch=ch: e.scalar_tensor_tensor(Sf[:, h, :], Sf[:, h, :], dec[:, h, ch:ch + 1], pk[:, 0:128],
                                                                                      op0=ALU.mult, op1=ALU.add),
                             reads=[Bpk, Bdec, BS[h]], writes=[BS[h]])
            if blk == 2:
                for h in range(8):
                    S.op("act", lambda e, h=h: e.activation(Sb[:, h, :], Sf[:, h, :], AF.Copy), reads=[BS[h]], writes=[BSb[h]])
            continue
        for h in range(8):
            kd, Bkd = kdec.next()
            qd, Bqd = qdec.next()
            gg, Bgg = gs.next()
            S.dma("sp", kd[:], A["HG_kdec"][h, :, s0:s0 + 2048], reads=[B["HG_kdec"]], writes=[Bkd])
            S.dma("sp", qd[:], A["HG_qdec"][h], reads=[B["HG_qdec"]], writes=[Bqd])
            S.dma("sp", gg[:], A["HG_gs"][h], reads=[B["HG_gs"]], writes=[Bgg])
            for t in range(16):
                tc = slice(t * 128, (t + 1) * 128)
                pa, Bpa = PB.next()
                S.op("pe", lambda e, pa=pa, kd=kd, qd=qd, tc=tc: e.matmul(pa[:, 0:128], kd[:, tc], qd[:, tc], start=True, stop=True),
                     reads=[Bkd, Bqd], writes=[Bpa])
                am, Bam = attm.next()
                S.op("dve", lambda e, am=am, pa=pa: e.tensor_tensor(am[:], pa[:, 0:128], cst["bdmask"][:], ALU.mult), reads=[Bpa, Bc], writes=[Bam])
                po, Bpo = PB.next()
                S.op("pe", lambda e, po=po, am=am, t=t, h=h: e.matmul(po[:, 0:128], vv[:, t, h * 128:(h + 1) * 128], am[:], start=True, stop=False),
                     reads=[Bvv, Bam], writes=[Bpo])
                for half in range(2):
                    ch = blk * 32 + t * 2 + half
                    c0 = t * 128 + half * 64
                    S.op("pe", lambda e, po=po, qd=qd, h=h, c0=c0, half=half: e.matmul(po[:, half * 64:(half + 1) * 64], Sb[:, h, :], qd[:, c0:c0 + 64],
                                                                                  start=False, stop=(half == 1)),
                         reads=[BSb[h], Bqd], writes=[Bpo])
                    pk, Bpk = state_update(t, h, half)
                    S.op("dve", lambda e, pk=pk, h=h, ch=ch: e.scalar_tensor_tensor(Sf[:, h, :], Sf[:, h, :], dec[:, h, ch:ch + 1], pk[:, 0:128],
                                                                                  op0=ALU.mult, op1=ALU.add),
                         reads=[Bpk, Bdec, BS[h]], writes=[BS[h]])
                    S.op("act", lambda e, h=h: e.activation(Sb[:, h, :], Sf[:, h, :], AF.Copy), reads=[BS[h]], writes=[BSb[h]])
                q2, Bq2 = sq.next()
                S.op("act", lambda e, q2=q2, po=po: e.activation(q2[:], po[:, 0:128], AF.Square), reads=[Bpo], writes=[Bq2])
                pm, Bpm = PB.next()
                S.op("pe", lambda e, pm=pm, q2=q2: e.matmul(pm[:, 0:128], onesb[:], q2[:], start=True, stop=True), reads=[Bones, Bq2], writes=[Bpm])
                r1, Br1 = rs.next()
                S.op("act", lambda e, r1=r1, pm=pm: e.activation(r1[:], pm[:, 0:128], AF.Sqrt, bias=cst["eps_rms"][:, 0:1]), reads=[Bpm, Bc], writes=[Br1])
                S.op("dve", lambda e, r1=r1: e.reciprocal(r1[:], r1[:]), reads=[Br1], writes=[Br1])
                y1, By1 = yy.next()
                S.op("dve", lambda e, y1=y1, po=po, r1=r1: e.tensor_tensor(y1[:], po[:, 0:128], r1[:], ALU.mult), reads=[Bpo, Br1], writes=[By1])
                S.op("dve", lambda e, y1=y1, gg=gg, tc=tc, h=h: e.scalar_tensor_tensor(bast[:, tc], y1[:], cst["normg"][:, h:h + 1], gg[:, tc],
                                                                                    op0=ALU.mult, op1=ALU.mult),
                     reads=[By1, Bgg, Bc], writes=[Bbast])
            S.dma("sp", A["BA"][h], bast[:], reads=[Bbast], writes=[B["BA"]], nowaw=True)

SM_SCALE = 128 ** -0.5
TOPK = 256
NBIS = 18
NTER = 11
BIS_WIN = 16.0
NEG_ADM = -30000.0


def phase2_dsa(S, G, groups=(0, 1, 2, 3)):
    A = G["ap"]
    PB = G["pb"]
    cst = G["cst"]
    Bc = G["Bcst"]
    B = G["B"]
    ident = G["ident_bf"]
    Bident = G["Bident"]
    es_outer = S.es
    with ExitStack() as pes:
        S.es = pes
        alb = S.sbuf("ds_alb", [128, 8, 64], F32)
        dtab = S.sbuf("ds_dtab", [128, 8192], BF16)
        qrel = S.sbuf("ds_qrel", [1, 512], F32)
        onesr = S.sbuf("ds_onesr", [1, 128], BF16)
        drow = S.sbuf("ds_drow", [1, 512], F32)
        Bdrow = S.buf("ds_drow")
        shrow = S.sbuf("ds_shrow", [1, 8, 512], BF16)
        Bshrow = S.buf("ds_shrow")
        dmc = S.sbuf("ds_dmc", [128, 1], F32)
        Bdmc = S.buf("ds_dmc")
        corr = S.sbuf("ds_corr", [128, 8, 128], BF16)
        sel2 = S.sbuf("ds_sel2", [2, 128], BF16)
        onesb = S.sbuf("ds_ones", [128, 128], BF16)
        Bk = S.buf("ds_const")
        S.dma("sp", alb[:], A["alb"], writes=[Bk], nowaw=True)
        S.dma("sp", corr[:], A["corr"], writes=[Bk], nowaw=True)
        S.dma("sp", sel2[:], A["sel2"], writes=[Bk], nowaw=True)
        S.op("dve", lambda e: e.memset(onesb[:], 1.0), writes=[Bk])
        S.op("dve", lambda e: e.memset(onesr[:], 1.0), writes=[Bk])
        S.dma("sp", dtab[:], A["dtab"], writes=[Bk], nowaw=True)
        S.dma("sp", qrel[:], A["qrel"], writes=[Bk], nowaw=True)
        kit = S.sbuf("ds_kit", [128, 8192], BF16)
        S.dma("sp", kit[:], A["KIT"], reads=[B["KIT"]], writes=[Bk], nowaw=True)
        wi = S.sbuf("ds_wi", [128, 16, 8], F32)
        S.dma("sp", wi[:], A["WI"].rearrange("(t p) h -> p t h", p=128), reads=[B["WI"]], writes=[Bk], nowaw=True)
        maskT = S.sbuf("ds_maskT", [128, 64, 512], BF16)
        BmT = S.buf("ds_maskT")
        S.barrier()

        def do_group(g):
            Q0 = OWN0 + g * 512
            with ExitStack() as aes:
                S.es = aes
                qit = S.sbuf("ds_qit", [128, 4, 512], BF16)
                Bqit = S.buf("ds_qit")
                S.dma("sp", qit[:], A["QIT"][:, :, g * 512:(g + 1) * 512].rearrange("m p q -> p m q"), reads=[B["QIT"]], writes=[Bqit])
                sc = S.sbuf("ds_sc", [128, 8192], F32)
                Bsc = S.buf("ds_sc")
                mq = S.sbuf("ds_mq", [128, 8192], BF16)
                Bmq = S.buf("ds_mq")
                adm = Rot(S, "ds_adm", [2, 512], BF16, 3)
                junk2 = S.sbuf("ds_junk2", [128, 8192], U8)
                Bj2 = S.buf("ds_junk2")
                relu = Rot(S, "ds_relu", [128, 512], BF16, 9)
                dg = S.sbuf("ds_dg", [128, 8, 128], BF16)
                Bdg = S.buf("ds_dg")
                sm = {n: S.sbuf("ds_s_" + n, [128, 1], F32) for n in ("lo", "hi", "mid", "cnt", "ge", "d", "c0", "d3", "t1", "nt2", "s2", "g2")}
                Bsm = S.buf("ds_small")
                Bth = S.buf("ds_th")
                Bs2 = S.buf("ds_s2")
                for T in range(4):
                    tg = g * 4 + T
                    Qt = Q0 + T * 128
                    nk = Qt + 128
                    nb5 = (nk + 511) // 512
                    nkp = nb5 * 512
                    for h in range(8):
                        S.op("dve", lambda e, h=h, tg=tg: e.tensor_scalar(dg[:, h, :], ident[:], wi[:, tg, h:h + 1], None, op0=ALU.mult),
                             reads=[Bident, Bk], writes=[Bdg])
                    for kb5 in range(nb5):
                        ks = slice(kb5 * 512, (kb5 + 1) * 512)
                        ad, Bad = adm.next()
                        S.dma("sp", ad[:], A["adm"][tg, :, ks], writes=[Bad])
                        rl = []
                        for hp in range(4):
                            for par in range(2):
                                pb, Bpb = PB.next()
                                p0 = par * 64
                                S.op("pe", lambda e, pb=pb, hp=hp, p0=p0, T=T, ks=ks: e.matmul(
                                    pb[:], qit[p0:p0 + 64, hp, T * 128:(T + 1) * 128], kit[p0:p0 + 64, ks], start=True, stop=True),
                                    reads=[Bqit, Bk], writes=[Bpb])
                                r, Br = relu.next()
                                if par == 0 or hp % 2 == 0:
                                    S.op("act", lambda e, r=r, pb=pb: e.activation(r[:], pb[:], AF.Relu), reads=[Bpb], writes=[Br])
                                else:
                                    S.op("dve", lambda e, r=r, pb=pb: e.tensor_scalar(r[:], pb[:], 0.0, None, op0=ALU.max), reads=[Bpb], writes=[Br])
                                rl.append((r, Br))
                        ps, Bps = PB.next()
                        for h in range(8):
                            r, Br = rl[h]
                            S.op("pe", lambda e, ps=ps, h=h, r=r: e.matmul(ps[:], dg[:, h, :], r[:], start=(h == 0), stop=False),
                                 reads=[Bdg, Br], writes=[Bps])
                        S.op("pe", lambda e, ps=ps, ad=ad, ks=ks: e.matmul(ps[:], sel2[0:2, :], ad[0:2, :], start=False, stop=True),
                             reads=[Bk, Bad], writes=[Bps])
                        S.op("act", lambda e, ps=ps, ks=ks: e.activation(sc[:, ks], ps[:], AF.Copy), reads=[Bps], writes=[Bsc])
                    scv = sc[:, 0:nkp]
                    S.op("dve", lambda e, scv=scv: e.reduce_max(sm["hi"][:], scv, AX.X), reads=[Bsc], writes=[Bsm])
                    S.op("dve", lambda e: e.tensor_scalar(sm["lo"][:], sm["hi"][:], -BIS_WIN, None, op0=ALU.add), reads=[Bsm], writes=[Bsm])
                    S.op("dve", lambda e: e.tensor_scalar(sm["hi"][:], sm["hi"][:], 1e-3, None, op0=ALU.add), reads=[Bsm], writes=[Bsm])
                    S.op("dve", lambda e, scv=scv, nkp=nkp: e.tensor_scalar(mq[:, 0:nkp], scv, sm["lo"][:, 0:1], None, op0=ALU.is_ge, op1=ALU.add,
                                                                        accum_out=sm["c0"][:]), reads=[Bsc, Bsm], writes=[Bmq, Bsm])
                    for it in range(NTER):
                        S.op("dve", lambda e: e.tensor_tensor(sm["d"][:], sm["hi"][:], sm["lo"][:], ALU.subtract), reads=[Bsm], writes=[Bsm])
                        S.op("dve", lambda e: e.tensor_scalar(sm["d3"][:], sm["d"][:], 1.0 / 3.0, None, op0=ALU.mult), reads=[Bsm], writes=[Bsm])
                        S.op("dve", lambda e: e.tensor_tensor(sm["t1"][:], sm["lo"][:], sm["d3"][:], ALU.add), reads=[Bsm], writes=[Bsm])
                        S.op("dve", lambda e: e.tensor_tensor(sm["nt2"][:], sm["d3"][:], sm["hi"][:], ALU.subtract), reads=[Bsm], writes=[Bsm, Bth])
                        S.op("act", lambda e, scv=scv, nkp=nkp: e.activation(junk2[:, 0:nkp], scv, AF.Sign, bias=sm["nt2"][:, 0:1], accum_out=sm["s2"][:]),
                             reads=[Bsc, Bth], writes=[Bj2, Bs2])
                        S.op("dve", lambda e, scv=scv, nkp=nkp: e.tensor_scalar(mq[:, 0:nkp], scv, sm["t1"][:, 0:1], None, op0=ALU.is_ge, op1=ALU.add,
                                                                            accum_out=sm["cnt"][:]), reads=[Bsc, Bsm], writes=[Bmq, Bsm])
                        S.op("dve", lambda e: e.tensor_scalar(sm["ge"][:], sm["cnt"][:], TOPK - 0.5, None, op0=ALU.is_ge), reads=[Bsm], writes=[Bsm])
                        S.op("dve", lambda e, nkp=nkp: e.tensor_scalar(sm["g2"][:], sm["s2"][:], 2.0 * (TOPK - 0.5) - nkp, None, op0=ALU.is_ge), reads=[Bs2], writes=[Bsm])
                        S.op("dve", lambda e: e.tensor_tensor(sm["ge"][:], sm["ge"][:], sm["g2"][:], ALU.add), reads=[Bsm], writes=[Bsm])
                        S.op("dve", lambda e: e.scalar_tensor_tensor(sm["lo"][:], sm["d3"][:], sm["ge"][:, 0:1], sm["lo"][:], op0=ALU.mult, op1=ALU.add),
                             reads=[Bsm], writes=[Bsm])
                        S.op("dve", lambda e: e.tensor_tensor(sm["hi"][:], sm["lo"][:], sm["d3"][:], ALU.add), reads=[Bsm], writes=[Bsm, Bth])
                    S.op("dve", lambda e: e.tensor_scalar(sm["ge"][:], sm["c0"][:], TOPK - 0.5, None, op0=ALU.is_ge), reads=[Bsm], writes=[Bsm])
                    S.op("dve", lambda e: e.tensor_scalar(sm["d"][:], sm["lo"][:], 1000.0, None, op0=ALU.add), reads=[Bsm], writes=[Bsm])
                    S.op("dve", lambda e: e.tensor_scalar(sm["lo"][:], sm["d"][:], sm["ge"][:, 0:1], -1000.0, op0=ALU.mult, op1=ALU.add),
                         reads=[Bsm], writes=[Bsm])
                    S.op("dve", lambda e, scv=scv, nkp=nkp: e.tensor_scalar(mq[:, 0:nkp], scv, sm["lo"][:, 0:1], None, op0=ALU.is_ge),
                         reads=[Bsc, Bsm], writes=[Bmq])
                    S.op("dve", lambda e, nk=nk: e.scalar_tensor_tensor(sc[:, 0:nk], mq[:, 0:nk], -16384.0, dtab[:, 8192 - nk:8192], op0=ALU.mult, op1=ALU.add),
                         reads=[Bmq, Bk], writes=[Bsc])
                    S.op("dve", lambda e, nk=nk: e.tensor_reduce(dmc[:], sc[:, 0:nk], AX.X, ALU.min), reads=[Bsc], writes=[Bdmc])
                    S.op("dve", lambda e: e.tensor_scalar(dmc[:], dmc[:], 16384.0, None, op0=ALU.add), reads=[Bdmc], writes=[Bdmc])
                    pbd, Bpbd = PB.next()
                    S.op("pe", lambda e, pbd=pbd: e.matmul(pbd[0:1, 0:128], dmc[:, 0:1], G["ident_f"][:], start=True, stop=True),
                         reads=[Bdmc, G["Bidf"]], writes=[Bpbd])
                    S.op("dve", lambda e, pbd=pbd, T=T: e.tensor_copy(drow[0:1, T * 128:(T + 1) * 128], pbd[0:1, 0:128]), reads=[Bpbd], writes=[Bdrow])
                    nkb = nk // 128
                    for k4 in range(0, nkb, 4):
                        n4 = min(4, nkb - k4)
                        pb, Bpb = PB.next()
                        for j in range(n4):
                            kb = k4 + j
                            S.op("pe", lambda e, pb=pb, j=j, kb=kb: e.matmul(pb[:, j * 128:(j + 1) * 128], mq[:, kb * 128:(kb + 1) * 128], ident[:],
                                                                           start=True, stop=True), reads=[Bmq, Bident], writes=[Bpb])
                        dst = maskT[:, k4:k4 + n4, T * 128:(T + 1) * 128]
                        src = pb[:, 0:n4 * 128].rearrange("p (a b) -> p a b", a=n4)
                        if (k4 // 4) % 2 == 0:
                            S.op("act", lambda e, dst=dst, src=src: e.activation(dst, src, AF.Copy), reads=[Bpb], writes=[BmT])
                        else:
                            S.op("dve", lambda e, dst=dst, src=src: e.tensor_copy(dst, src), reads=[Bpb], writes=[BmT])
                S.op("dve", lambda e: e.tensor_tensor(drow[:], drow[:], qrel[:], ALU.subtract), reads=[Bdrow, Bk], writes=[Bdrow])
                for h in range(8):
                    S.op("dve", lambda e, h=h: e.tensor_scalar(shrow[0:1, h, :], drow[:], (2.0 ** -(h + 1)) / SM_SCALE, None, op0=ALU.mult),
                         reads=[Bdrow], writes=[Bshrow])
                S.barrier()
                S.phase_end()
            with ExitStack() as bes:
                S.es = bes
                kt = Rot(S, "ds_kt", [128, 8192], BF16, 2)
                vh = Rot(S, "ds_vh", [128, 64, 128], BF16, 2)
                qt = Rot(S, "ds_qt", [128, 512], BF16, 2)
                pT = Rot(S, "ds_pT", [128, 512], BF16, 7)
                mcr = Rot(S, "ds_mc", [128, 128], BF16, 3)
                rec = Rot(S, "ds_rec", [128, 512], F32, 1)
                ob = Rot(S, "ds_ob", [128, 512], BF16, 1)
                nkb = (Q0 + 512) // 128
                kb0 = Q0 // 128
                for h in range(8):
                    k_, Bk_ = kt.next()
                    v_, Bv_ = vh.next()
                    q_, Bq_ = qt.next()
                    S.dma("sp", k_[:, 0:nkb * 128], A["KT"][h, :, 0:nkb * 128], reads=[B["KT"]], writes=[Bk_])
                    S.dma("sp", v_[:, 0:nkb, :], A["VH"][h, :, 0:nkb, :], reads=[B["VH"]], writes=[Bv_])
                    S.dma("sp", q_[:], A["QT"][h, :, g * 512:(g + 1) * 512], reads=[B["QT"]], writes=[Bq_])
                    po, Bpo = PB.t[6], PB.b[6]
                    pd, Bpd = PB.t[7], PB.b[7]
                    pend = []

                    def stage2(kb, p_, Bp_, c0, first, last, po=po, pd=pd, Bpo=Bpo, Bpd=Bpd, v_=v_, Bv_=Bv_):
                        S.op("pe", lambda e, po=po, v_=v_, p_=p_, kb=kb, c0=c0, first=first, last=last: e.matmul(
                            po[:, c0:512], v_[:, kb, :], p_[:, c0:512], start=first, stop=last), reads=[Bv_, Bp_], writes=[Bpo])
                        S.op("pe", lambda e, pd=pd, p_=p_, c0=c0, first=first, last=last: e.matmul(
                            pd[:, c0:512], onesb[:], p_[:, c0:512], start=first, stop=last), reads=[Bk, Bp_], writes=[Bpd])

                    for kb in range(nkb):
                        r = kb - kb0
                        c0 = max(r, 0) * 128
                        first = (kb == 0)
                        last = (kb == nkb - 1)
                        pst, Bpst = PB.next(0, 6)
                        S.op("pe", lambda e, pst=pst, k_=k_, q_=q_, kb=kb, c0=c0: e.matmul(
                            pst[:, c0:512], k_[:, kb * 128:(kb + 1) * 128], q_[:, c0:512], start=True, stop=(h >= 6)),
                            reads=[Bk_, Bq_], writes=[Bpst])
                        if h < 6:
                            S.op("pe", lambda e, pst=pst, h=h, c0=c0: e.matmul(pst[:, c0:512], onesr[0:1, :], shrow[0:1, h, c0:512], start=False, stop=True),
                                 reads=[Bk, Bshrow], writes=[Bpst])
                        p_, Bp_ = pT.next()
                        bias_ap = alb[:, h, r + 60:r + 61]
                        S.op("act", lambda e, p_=p_, pst=pst, c0=c0, bias_ap=bias_ap: e.activation(
                            p_[:, c0:512], pst[:, c0:512], AF.Exp, scale=SM_SCALE, bias=bias_ap), reads=[Bpst, Bk], writes=[Bp_])
                        if r >= 0:
                            mc, Bmc = mcr.next()
                            S.op("dve", lambda e, mc=mc, kb=kb, r=r, h=h: e.tensor_tensor(mc[:], maskT[:, kb, r * 128:(r + 1) * 128], corr[:, h, :], ALU.mult),
                                 reads=[BmT, Bk], writes=[Bmc])
                            S.op("dve", lambda e, p_=p_, mc=mc, r=r: e.scalar_tensor_tensor(p_[:, r * 128:(r + 1) * 128], p_[:, r * 128:(r + 1) * 128], 3.0e38, mc[:],
                                                                                         op0=ALU.min, op1=ALU.mult), reads=[Bmc, Bp_], writes=[Bp_])
                            if c0 + 128 < 512:
                                S.op("dve", lambda e, p_=p_, kb=kb, c0=c0: e.scalar_tensor_tensor(p_[:, c0 + 128:512], p_[:, c0 + 128:512], 3.0e38, maskT[:, kb, c0 + 128:512],
                                                                                               op0=ALU.min, op1=ALU.mult), reads=[BmT, Bp_], writes=[Bp_])
                        else:
                            S.op("dve", lambda e, p_=p_, kb=kb: e.scalar_tensor_tensor(p_[:], p_[:], 3.0e38, maskT[:, kb, :], op0=ALU.min, op1=ALU.mult),
                                 reads=[BmT, Bp_], writes=[Bp_])
                        pend.append((kb, p_, Bp_, c0, first, last))
                        if len(pend) > 4:
                            stage2(*pend.pop(0))
                    while pend:
                        stage2(*pend.pop(0))
                    rc, Brc = rec.next()
                    S.op("dve", lambda e, rc=rc, pd=pd: e.reciprocal(rc[:], pd[:]), reads=[Bpd], writes=[Brc])
                    o_, Bo_ = ob.next()
                    S.op("dve", lambda e, o_=o_, po=po, rc=rc: e.tensor_tensor(o_[:], po[:], rc[:], ALU.mult), reads=[Bpo, Brc], writes=[Bo_])
                    S.dma("sp", A["BB"][h, :, g * 512:(g + 1) * 512], o_[:], reads=[Bo_], writes=[B["BB"]], nowaw=True)
                S.barrier()
                S.phase_end()
        for g in groups:
            do_group(g)
        S.es = es_outer

LN_EPS = 1e-5
DN_ALPHA = 2.0 ** 0.25
CAP = 256
NEXP = 32


def layer_norm_tile(S, G, pre, Bpre, out_t, Bout, gb, bb, Bgb, small, Bsmall, junk, Bjunk):
    s1, s2, mean, var, rstd, nmr = small
    S.op("act", lambda e: e.activation(junk[:], pre[:], AF.Identity, accum_out=s1[:]), reads=[Bpre], writes=[Bjunk, Bsmall])
    S.op("act", lambda e: e.activation(junk[:], pre[:], AF.Square, accum_out=s2[:]), reads=[Bpre], writes=[Bjunk, Bsmall])
    S.op("dve", lambda e: e.tensor_scalar(mean[:], s1[:], 1.0 / 2048.0, None, op0=ALU.mult), reads=[Bsmall], writes=[Bsmall])
    S.op("dve", lambda e: e.tensor_tensor(var[:], mean[:], mean[:], ALU.mult), reads=[Bsmall], writes=[Bsmall])
    S.op("dve", lambda e: e.scalar_tensor_tensor(var[:], s2[:], 1.0 / 2048.0, var[:], op0=ALU.mult, op1=ALU.subtract), reads=[Bsmall], writes=[Bsmall])
    S.op("act", lambda e: e.activation(rstd[:], var[:], AF.Sqrt, bias=G["cst"]["eps_ln"][:, 0:1]), reads=[Bsmall, G["Bcst"]], writes=[Bsmall])
    S.op("dve", lambda e: e.reciprocal(rstd[:], rstd[:]), reads=[Bsmall], writes=[Bsmall])
    S.op("dve", lambda e: e.scalar_tensor_tensor(nmr[:], mean[:], -1.0, rstd[:], op0=ALU.mult, op1=ALU.mult), reads=[Bsmall], writes=[Bsmall])
    S.op("act", lambda e: e.activation(pre[:], pre[:], AF.Identity, scale=rstd[:, 0:1], bias=nmr[:, 0:1]), reads=[Bpre, Bsmall], writes=[Bpre])
    S.op("dve", lambda e: e.tensor_tensor(pre[:], pre[:], gb[:], ALU.mult), reads=[Bpre, Bgb], writes=[Bpre])
    S.op("dve", lambda e: e.tensor_tensor(out_t[:], pre[:], bb[:], ALU.add), reads=[Bpre, Bgb], writes=[Bout])


def phase3a_merge(S, G):
    A = G["ap"]
    PB = G["pb"]
    B = G["B"]
    wa = S.sbuf("o_wa", [128, 8, 2048], BF16)
    wb = S.sbuf("o_wb", [128, 8, 2048], BF16)
    Bw = S.buf("o_w")
    S.dma("pool", wa[:], A["w_branch_a"].rearrange("(kc p) n -> p kc n", p=128), writes=[Bw], nowaw=True)
    S.dma("pool", wb[:], A["w_branch_b"].rearrange("(kc p) n -> p kc n", p=128), writes=[Bw], nowaw=True)
    bag = Rot(S, "o_ba", [128, 8, 512], BF16, 2)
    bbg = Rot(S, "o_bb", [128, 8, 512], BF16, 2)
    gtg = Rot(S, "o_gt", [128, 32, 512], BF16, 2)
    t1r = Rot(S, "o_t1", [128, 512], F32, 2)
    t2r = Rot(S, "o_t2", [128, 512], F32, 2)
    mgs = Rot(S, "o_mgs", [128, 512], BF16, 3)
    for g in range(4):
        ba_, Bba = bag.next()
        bb_, Bbb = bbg.next()
        gt_, Bgt = gtg.next()
        S.dma("sp", ba_[:], A["BA"][:, :, g * 512:(g + 1) * 512].rearrange("h p t -> p h t"), reads=[B["BA"]], writes=[Bba])
        S.dma("sp", bb_[:], A["BB"][:, :, g * 512:(g + 1) * 512].rearrange("h p t -> p h t"), reads=[B["BB"]], writes=[Bbb])
        S.dma("sp", gt_[:], A["GT"][:, :, g * 512:(g + 1) * 512].rearrange("c p t -> p c t"), reads=[B["GT"]], writes=[Bgt])
        for c in range(16):
            pa, Bpa = PB.next()
            for k in range(8):
                S.op("pe", lambda e, pa=pa, k=k, c=c, ba_=ba_: e.matmul(pa[:], wa[:, k, c * 128:(c + 1) * 128], ba_[:, k, :], start=(k == 0), stop=(k == 7)),
                     reads=[Bw, Bba], writes=[Bpa])
            pb2, Bpb2 = PB.next()
            for k in range(8):
                S.op("pe", lambda e, pb2=pb2, k=k, c=c, bb_=bb_: e.matmul(pb2[:], wb[:, k, c * 128:(c + 1) * 128], bb_[:, k, :], start=(k == 0), stop=(k == 7)),
                     reads=[Bw, Bbb], writes=[Bpb2])
            t1, Bt1 = t1r.next()
            t2, Bt2 = t2r.next()
            S.op("dve", lambda e, t1=t1, pa=pa, gt_=gt_, c=c: e.tensor_tensor(t1[:], pa[:], gt_[:, c, :], ALU.mult), reads=[Bpa, Bgt], writes=[Bt1])
            S.op("dve", lambda e, t2=t2, pb2=pb2, gt_=gt_, c=c: e.tensor_tensor(t2[:], pb2[:], gt_[:, 16 + c, :], ALU.mult), reads=[Bpb2, Bgt], writes=[Bt2])
            m_, Bm = mgs.next()
            S.op("dve", lambda e, t1=t1, t2=t2, m_=m_: e.tensor_tensor(m_[:], t1[:], t2[:], ALU.add), reads=[Bt1, Bt2], writes=[Bm])
            S.dma("sp", A["MG"][c, :, g * 512:(g + 1) * 512], m_[:], reads=[Bm], writes=[B["MG"]], nowaw=True)


def phase3b_out(S, G):
    A = G["ap"]
    PB = G["pb"]
    B = G["B"]
    P = G["persist"]
    ident_f = G["ident_f"]
    Bw = S.buf("o_w")
    wr = S.sbuf("o_wr", [128, 16, 36], F32)
    S.dma("sp", wr[:], A["wr"].rearrange("(kc p) n -> p kc n", p=128), writes=[Bw], nowaw=True)
    brr = S.sbuf("o_brr", [1, 36], F32)
    S.dma("sp", brr[:], A["brr"], writes=[Bw], nowaw=True)
    g1 = S.sbuf("o_g1", [128, 2048], F32)
    b1 = S.sbuf("o_b1", [128, 2048], F32)
    S.dma("sp", g1[:], A["ln1_gb"], writes=[Bw], nowaw=True)
    S.dma("sp", b1[:], A["ln1_bb"], writes=[Bw], nowaw=True)
    ltri = S.sbuf("o_ltri", [128, 128], BF16)
    S.dma("sp", ltri[:], A["ltri"], writes=[Bw], nowaw=True)
    e256 = S.sbuf("o_e256", [128, 32], F32)
    S.dma("sp", e256[:], A["e256"], writes=[Bw], nowaw=True)
    onesc = S.sbuf("o_onesc", [128, 1], BF16)
    S.op("dve", lambda e: e.memset(onesc[:], 1.0), writes=[Bw])
    onesr = S.sbuf("o_onesr", [1, 128], F32)
    S.op("dve", lambda e: e.memset(onesr[:], 1.0), writes=[Bw])
    cnt = S.sbuf("o_cnt", [1, 32], F32)
    Bcnt = S.buf("o_cnt")
    S.op("dve", lambda e: e.memset(cnt[:], 0.0), writes=[Bcnt])
    S.barrier()
    wo = Rot(S, "o_wo", [128, 16, 512], BF16, 2)
    mgr = Rot(S, "o_mg", [128, 16, 512], BF16, 2)
    pre = Rot(S, "o_pre", [128, 2048], F32, 5)
    h1t = Rot(S, "o_h1", [128, 2048], F32, 2)
    h1b = Rot(S, "o_h1b", [128, 2048], BF16, 2)
    junk = S.sbuf("o_junk", [128, 2048], BF16)
    Bjunk = S.buf("o_junk")
    hT = Rot(S, "o_hT", [128, 16, 128], F32, 1)
    smalls = [S.sbuf("o_sm%d" % i, [128, 1], F32) for i in range(6)]
    Bsmall = S.buf("o_small")
    rt = {n: S.sbuf("o_r_" + n, shp, F32) for n, shp in (
        ("L", [128, 36]), ("gmax", [128, 1]), ("ngmax", [128, 1]), ("ohg", [128, 4]), ("ge", [128, 4]), ("gsum", [128, 1]), ("gp", [128, 1]),
        ("e8", [128, 8]), ("m1", [128, 1]), ("oh1", [128, 8]), ("e8b", [128, 8]), ("m2", [128, 1]), ("oh2", [128, 8]), ("d", [128, 1]),
        ("sg", [128, 1]), ("A1", [128, 32]), ("A2", [128, 32]), ("At", [128, 32]), ("rk", [128, 32]), ("t", [128, 32]), ("i1", [128, 1]), ("i2", [128, 1]))}
    Abf = S.sbuf("o_Abf", [128, 32], BF16)
    Br = G["Br_persist"]

    def rop(fn, eng="dve"):
        S.op(eng, fn, reads=[Br, Bw], writes=[Br])

    for g in range(4):
        mg, Bmg = mgr.next()
        S.dma("sp", mg[:], A["MG"][:, :, g * 512:(g + 1) * 512].rearrange("c p t -> p c t"), reads=[B["MG"]], writes=[Bmg])
        prs = []
        for t in range(4):
            tt = g * 4 + t
            pr, Bpr = pre.next()
            S.dma("sp", pr[:], A["xs"][OWN0 + tt * 128:OWN0 + (tt + 1) * 128, :], writes=[Bpr])
            prs.append((pr, Bpr))
        for cb in range(4):
            w_, Bwo = wo.next()
            S.dma("pool", w_[:], A["w_out"][:, cb * 512:(cb + 1) * 512].rearrange("(kc p) n -> p kc n", p=128), writes=[Bwo])
            for t in range(4):
                pr, Bpr = prs[t]
                po, Bpo = PB.next()
                for c in range(16):
                    S.op("pe", lambda e, po=po, c=c, t=t, w_=w_, mg=mg: e.matmul(po[:], mg[:, c, t * 128:(t + 1) * 128], w_[:, c, :], start=(c == 0), stop=(c == 15)),
                         reads=[Bmg, Bwo], writes=[Bpo])
                S.op("dve", lambda e, pr=pr, po=po, cb=cb: e.scalar_tensor_tensor(pr[:, cb * 512:(cb + 1) * 512], pr[:, cb * 512:(cb + 1) * 512], DN_ALPHA, po[:],
                                                                                op0=ALU.mult, op1=ALU.add), reads=[Bpo, Bpr], writes=[Bpr])
        for t in range(4):
            tt = g * 4 + t
            pr, Bpr = prs[t]
            h_, Bh = h1t.next()
            layer_norm_tile(S, G, pr, Bpr, h_, Bh, g1, b1, Bw, smalls, Bsmall, junk, Bjunk)
            S.dma("sp", A["H1"][tt * 128:(tt + 1) * 128, :], h_[:], reads=[Bh], writes=[B["H1"]], nowaw=True)
            hb, Bhb = h1b.next()
            S.op("act", lambda e, hb=hb, h_=h_: e.activation(hb[:], h_[:], AF.Copy), reads=[Bh], writes=[Bhb])
            S.dma("sp", A["H1b"][tt * 128:(tt + 1) * 128, :], hb[:], reads=[Bhb], writes=[B["H1b"]], nowaw=True)
            hT_, BhT = hT.next()
            for q4 in range(4):
                pb, Bpb = PB.next()
                for j in range(4):
                    c = q4 * 4 + j
                    S.op("pe", lambda e, pb=pb, h_=h_, c=c, j=j: e.matmul(pb[:, j * 128:(j + 1) * 128], h_[:, c * 128:(c + 1) * 128], ident_f[:], start=True, stop=True),
                         reads=[Bh, G["Bidf"]], writes=[Bpb])
                S.op("act", lambda e, pb=pb, hT_=hT_, q4=q4: e.activation(hT_[:, q4 * 4:(q4 + 1) * 4, :], pb[:].rearrange("p (a b) -> p a b", a=4), AF.Copy),
                     reads=[Bpb], writes=[BhT])
            pl, Bpl = PB.next()
            for c in range(16):
                S.op("pe", lambda e, pl=pl, hT_=hT_, c=c: e.matmul(pl[:, 0:36], hT_[:, c, :], wr[:, c, :], start=(c == 0), stop=False), reads=[BhT, Bw], writes=[Bpl])
            S.op("pe", lambda e, pl=pl: e.matmul(pl[:, 0:36], onesr[0:1, :], brr[0:1, :], start=False, stop=True), reads=[Bw], writes=[Bpl])
            L = rt["L"]
            S.op("dve", lambda e, pl=pl: e.tensor_copy(L[:], pl[:, 0:36]), reads=[Bpl], writes=[Br])
            rop(lambda e: e.reduce_max(rt["gmax"][:], L[:, 0:4], AX.X))
            rop(lambda e: e.tensor_scalar(rt["ohg"][:], L[:, 0:4], rt["gmax"][:, 0:1], None, op0=ALU.is_equal))
            rop(lambda e: e.tensor_scalar(rt["ngmax"][:], rt["gmax"][:], -1.0, None, op0=ALU.mult))
            rop(lambda e: e.activation(rt["ge"][:], L[:, 0:4], AF.Exp, bias=rt["ngmax"][:, 0:1], accum_out=rt["gsum"][:]), "act")
            rop(lambda e: e.reciprocal(rt["gp"][:], rt["gsum"][:]))
            rop(lambda e: e.tensor_scalar(rt["e8"][:], L[:, 4:12], rt["ohg"][:, 0:1], None, op0=ALU.mult))
            for gg in range(1, 4):
                rop(lambda e, gg=gg: e.scalar_tensor_tensor(rt["e8"][:], L[:, 4 + 8 * gg:12 + 8 * gg], rt["ohg"][:, gg:gg + 1], rt["e8"][:], op0=ALU.mult, op1=ALU.add))
            rop(lambda e: e.reduce_max(rt["m1"][:], rt["e8"][:], AX.X))
            rop(lambda e: e.tensor_scalar(rt["oh1"][:], rt["e8"][:], rt["m1"][:, 0:1], None, op0=ALU.is_equal))
            rop(lambda e: e.scalar_tensor_tensor(rt["e8b"][:], rt["oh1"][:], -1.0e30, rt["e8"][:], op0=ALU.mult, op1=ALU.add))
            rop(lambda e: e.reduce_max(rt["m2"][:], rt["e8b"][:], AX.X))
            rop(lambda e: e.tensor_scalar(rt["oh2"][:], rt["e8b"][:], rt["m2"][:, 0:1], None, op0=ALU.is_equal))
            rop(lambda e: e.tensor_tensor(rt["d"][:], rt["m1"][:], rt["m2"][:], ALU.subtract))
            rop(lambda e: e.activation(rt["sg"][:], rt["d"][:], AF.Sigmoid), "act")
            rop(lambda e, tt=tt: e.tensor_tensor(P["wts"][:, tt, 0:1], rt["sg"][:], rt["gp"][:], ALU.mult))
            rop(lambda e, tt=tt: e.tensor_tensor(P["wts"][:, tt, 1:2], rt["gp"][:], P["wts"][:, tt, 0:1], ALU.subtract))
            for gg in range(4):
                rop(lambda e, gg=gg: e.tensor_scalar(rt["A1"][:, gg * 8:(gg + 1) * 8], rt["oh1"][:], rt["ohg"][:, gg:gg + 1], None, op0=ALU.mult))
                rop(lambda e, gg=gg: e.tensor_scalar(rt["A2"][:, gg * 8:(gg + 1) * 8], rt["oh2"][:], rt["ohg"][:, gg:gg + 1], None, op0=ALU.mult))
            rop(lambda e: e.tensor_tensor(rt["At"][:], rt["A1"][:], rt["A2"][:], ALU.add))
            rop(lambda e: e.tensor_copy(Abf[:], rt["At"][:]))
            pk, Bpk = PB.next()
            S.op("pe", lambda e, pk=pk: e.matmul(pk[:, 0:32], ltri[:], Abf[:], start=True, stop=False), reads=[Bw, Br], writes=[Bpk])
            S.op("pe", lambda e, pk=pk: e.matmul(pk[:, 0:32], onesr[0:1, :], cnt[0:1, :], start=False, stop=True), reads=[Bw, Bcnt], writes=[Bpk])
            pc, Bpc = PB.next()
            S.op("pe", lambda e, pc=pc: e.matmul(pc[0:1, 0:32], onesc[:], Abf[:], start=True, stop=True), reads=[Bw, Br], writes=[Bpc])
            S.op("dve", lambda e, pk=pk: e.tensor_copy(rt["rk"][:], pk[:, 0:32]), reads=[Bpk, Br], writes=[Br])
            S.op("dve", lambda e, pc=pc: e.tensor_tensor(cnt[:], cnt[:], pc[0:1, 0:32], ALU.add), reads=[Bpc, Bcnt], writes=[Bcnt])
            rop(lambda e: e.scalar_tensor_tensor(rt["t"][:], rt["rk"][:], 1.0, rt["At"][:], op0=ALU.add, op1=ALU.mult))
            rop(lambda e, tt=tt: e.tensor_scalar(P["RK"][:, tt, :], rt["t"][:], -1.0, None, op0=ALU.add))
            rop(lambda e: e.tensor_tensor(rt["rk"][:], rt["rk"][:], e256[:], ALU.add))
            rop(lambda e: e.tensor_tensor(rt["t"][:], rt["rk"][:], rt["A1"][:], ALU.mult))
            rop(lambda e: e.reduce_sum(rt["i1"][:], rt["t"][:], AX.X))
            rop(lambda e: e.tensor_tensor(rt["t"][:], rt["rk"][:], rt["A2"][:], ALU.mult))
            rop(lambda e: e.reduce_sum(rt["i2"][:], rt["t"][:], AX.X))
            rop(lambda e, tt=tt: e.tensor_copy(P["idx"][:, tt, 0:1], rt["i1"][:]))
            rop(lambda e, tt=tt: e.tensor_copy(P["idx"][:, tt, 1:2], rt["i2"][:]))


def phase4_moe(S, G, experts=range(NEXP)):
    A = G["ap"]
    PB = G["pb"]
    B = G["B"]
    P = G["persist"]
    h1b = S.sbuf("m_h1b", [128, 16, 2048], BF16)
    Bh1b = S.buf("m_h1b")
    S.dma("sp", h1b[:], A["H1b"].rearrange("(t p) d -> p t d", p=128), reads=[B["H1b"]], writes=[Bh1b])
    iot = S.sbuf("m_iota", [128, CAP], F32)
    Biot = S.buf("m_iota")
    S.dma("sp", iot[:], A["iota256"], writes=[Biot])
    wu = Rot(S, "m_w", [128, 16, 512], BF16, 4)
    wd = Rot(S, "m_wd", [128, 4, 2048], BF16, 2)
    sel = Rot(S, "m_sel", [128, 16, CAP], BF16, 1)
    xs_ = Rot(S, "m_xs", [128, 16, CAP], BF16, 1)
    hT = Rot(S, "m_hT", [128, 8, CAP], BF16, 2)
    sg = Rot(S, "m_sg", [128, CAP], F32, 3)
    yst = Rot(S, "m_y", [128, 1024], F32, 1)
    for e_ in experts:
        s_, Bs = sel.next()
        for tt in range(16):
            S.op("dve", lambda e, s_=s_, tt=tt, e_=e_: e.tensor_scalar(s_[:, tt, :], iot[:], P["RK"][:, tt, e_:e_ + 1], None, op0=ALU.is_equal),
                 reads=[Biot, G["Br_persist"]], writes=[Bs])
        x_, Bx = xs_.next()
        for kc in range(16):
            pb, Bpb = PB.next()
            for tt in range(16):
                S.op("pe", lambda e, pb=pb, tt=tt, kc=kc, s_=s_: e.matmul(pb[:, 0:CAP], h1b[:, tt, kc * 128:(kc + 1) * 128], s_[:, tt, :],
                                                                         start=(tt == 0), stop=(tt == 15)), reads=[Bh1b, Bs], writes=[Bpb])
            if kc % 2 == 0:
                S.op("act", lambda e, pb=pb, x_=x_, kc=kc: e.activation(x_[:, kc, :], pb[:, 0:CAP], AF.Copy), reads=[Bpb], writes=[Bx])
            else:
                S.op("dve", lambda e, pb=pb, x_=x_, kc=kc: e.tensor_copy(x_[:, kc, :], pb[:, 0:CAP]), reads=[Bpb], writes=[Bx])
        h_, Bh = hT.next()
        for half in range(2):
            wg_, Bwg = wu.next()
            S.dma("pool", wg_[:], A["w_gate"][e_, :, half * 512:(half + 1) * 512].rearrange("(kc p) n -> p kc n", p=128), writes=[Bwg])
            wu_, Bwu = wu.next()
            S.dma("pool", wu_[:], A["w_up"][e_, :, half * 512:(half + 1) * 512].rearrange("(kc p) n -> p kc n", p=128), writes=[Bwu])
            for f4 in range(4):
                f = half * 4 + f4
                pg, Bpg = PB.next()
                for kc in range(16):
                    S.op("pe", lambda e, pg=pg, kc=kc, f4=f4, wg_=wg_, x_=x_: e.matmul(pg[:, 0:CAP], wg_[:, kc, f4 * 128:(f4 + 1) * 128], x_[:, kc, :],
                                                                                    start=(kc == 0), stop=(kc == 15)), reads=[Bwg, Bx], writes=[Bpg])
                for kc in range(16):
                    S.op("pe", lambda e, pg=pg, kc=kc, f4=f4, wu_=wu_, x_=x_: e.matmul(pg[:, CAP:2 * CAP], wu_[:, kc, f4 * 128:(f4 + 1) * 128], x_[:, kc, :],
                                                                                    start=(kc == 0), stop=(kc == 15)), reads=[Bwu, Bx], writes=[Bpg])
                s1, Bs1 = sg.next()
                S.op("act", lambda e, s1=s1, pg=pg: e.activation(s1[:], pg[:, 0:CAP], AF.Silu), reads=[Bpg], writes=[Bs1])
                S.op("dve", lambda e, h_=h_, f=f, s1=s1, pg=pg: e.tensor_tensor(h_[:, f, :], s1[:], pg[:, CAP:2 * CAP], ALU.mult), reads=[Bs1, Bpg], writes=[Bh])
        wds = []
        for half in range(2):
            wd_, Bwd = wd.next()
            S.dma("pool", wd_[:], A["w_down"][e_, half * 512:(half + 1) * 512, :].rearrange("(fc p) n -> p fc n", p=128), writes=[Bwd])
            wds.append((wd_, Bwd))
        for rh in range(CAP // 128):
            for cbp in range(2):
                y_, By = yst.next()
                for c2 in range(2):
                    cb = cbp * 2 + c2
                    py, Bpy = PB.next()
                    for f in range(8):
                        wd_, Bwd = wds[f // 4]
                        S.op("pe", lambda e, py=py, f=f, rh=rh, cb=cb, wd_=wd_, h_=h_: e.matmul(py[:], h_[:, f, rh * 128:(rh + 1) * 128], wd_[:, f % 4, cb * 512:(cb + 1) * 512],
                                                                                             start=(f == 0), stop=(f == 7)), reads=[Bh, Bwd], writes=[Bpy])
                    if c2 == 0:
                        S.op("act", lambda e, y_=y_, py=py, c2=c2: e.activation(y_[:, c2 * 512:(c2 + 1) * 512], py[:], AF.Copy), reads=[Bpy], writes=[By])
                    else:
                        S.op("dve", lambda e, y_=y_, py=py, c2=c2: e.tensor_copy(y_[:, c2 * 512:(c2 + 1) * 512], py[:]), reads=[Bpy], writes=[By])
                S.dma("sp", A["Y"][e_ * CAP + rh * 128:e_ * CAP + (rh + 1) * 128, cbp * 1024:(cbp + 1) * 1024], y_[:], reads=[By], writes=[B["Y"]], nowaw=True)


def phase5_final(S, G):
    A = G["ap"]
    B = G["B"]
    P = G["persist"]
    g2 = S.sbuf("f_g2", [128, 2048], F32)
    b2 = S.sbuf("f_b2", [128, 2048], F32)
    Bw = S.buf("f_w")
    S.dma("sp", g2[:], A["ln2_gb"], writes=[Bw], nowaw=True)
    S.dma("sp", b2[:], A["ln2_bb"], writes=[Bw], nowaw=True)
    idxi = S.sbuf("f_idx", [128, 16, 2], U32)
    Bidx = S.buf("f_idx")
    S.op("dve", lambda e: e.tensor_copy(idxi[:], P["idx"][:]), reads=[G["Br_persist"]], writes=[Bidx])
    S.barrier()
    h1 = Rot(S, "f_h1", [128, 2048], F32, 2)
    y1 = Rot(S, "f_y1", [128, 2048], F32, 2)
    y2 = Rot(S, "f_y2", [128, 2048], F32, 2)
    ot = Rot(S, "f_ot", [128, 2048], F32, 2)
    junk = S.sbuf("f_junk", [128, 2048], BF16)
    Bjunk = S.buf("f_junk")
    smalls = [S.sbuf("f_sm%d" % i, [128, 1], F32) for i in range(6)]
    Bsmall = S.buf("f_small")
    for tt in range(16):
        h_, Bh = h1.next()
        S.dma("sp", h_[:], A["H1"][tt * 128:(tt + 1) * 128, :], reads=[B["H1"]], writes=[Bh])
        a_, Ba = y1.next()
        b_, Bb = y2.next()
        for (dst, Bd, k) in ((a_, Ba, 0), (b_, Bb, 1)):
            S.dma("pool", None, None, reads=[B["Y"], Bidx], writes=[Bd],
                  builder=lambda e, dst=dst, tt=tt, k=k: e.indirect_dma_start(
                      out=dst[:], out_offset=None, in_=A["Y"], in_offset=bass.IndirectOffsetOnAxis(ap=idxi[:, tt, k:k + 1], axis=0),
                      bounds_check=NEXP * CAP - 1, oob_is_err=False))
        S.op("dve", lambda e, a_=a_, tt=tt: e.tensor_scalar(a_[:], a_[:], P["wts"][:, tt, 0:1], None, op0=ALU.mult), reads=[Ba, G["Br_persist"]], writes=[Ba])
        S.op("dve", lambda e, a_=a_, b_=b_, tt=tt: e.scalar_tensor_tensor(a_[:], b_[:], P["wts"][:, tt, 1:2], a_[:], op0=ALU.mult, op1=ALU.add),
             reads=[Ba, Bb, G["Br_persist"]], writes=[Ba])
        S.op("dve", lambda e, a_=a_, h_=h_: e.scalar_tensor_tensor(a_[:], h_[:], DN_ALPHA, a_[:], op0=ALU.mult, op1=ALU.add), reads=[Ba, Bh], writes=[Ba])
        o_, Bo = ot.next()
        layer_norm_tile(S, G, a_, Ba, o_, Bo, g2, b2, Bw, smalls, Bsmall, junk, Bjunk)
        S.dma("sp", A["out"][tt * 128:(tt + 1) * 128, :], o_[:], reads=[Bo], writes=[B["out"]], nowaw=True)

from contextlib import ExitStack
from concourse.bass_utils import run_bass_kernel_spmd

BFM_IDX = {"af": 0, "aq": 8, "ag": 16, "bk": 24, "bq": 32, "iq": 40, "ik": 44, "g": 45}
NBFM = 77

SCRATCH = {
    "KT": ([8, 128, 8192], "bf16"), "VH": ([8, 128, 64, 128], "bf16"), "KIT": ([128, 8192], "bf16"),
    "HG_kdec": ([8, 128, 8192], "bf16"), "HG_kend": ([8192, 1024], "bf16"), "HG_v": ([8192, 1024], "bf16"),
    "HG_dec": ([128, 8, 128], "f32"), "HG_qdec": ([8, 128, 2048], "bf16"), "HG_gs": ([8, 128, 2048], "bf16"),
    "QT": ([8, 128, 2048], "bf16"), "QIT": ([4, 128, 2048], "bf16"), "WI": ([2048, 8], "f32"),
    "GT": ([32, 128, 2048], "bf16"), "BA": ([8, 128, 2048], "bf16"), "BB": ([8, 128, 2048], "bf16"),
    "MG": ([16, 128, 2048], "bf16"), "H1": ([2048, 2048], "f32"), "H1b": ([2048, 2048], "bf16"), "Y": ([NEXP * CAP, 2048], "f32"),
}

INPUTS = {
    "xs": ([8192, 2048], "f32"), "valid_tm": ([128, 64], "f32"), "w_in": ([2048, 11848], "f32"),
    "ident": ([128, 128], "f32"), "bfm": ([128, NBFM], "f32"), "brow": ([1, 2056], "f32"),
    "lbl": ([128, 2, 8], "f32"), "normg": ([128, 8], "f32"), "rmask": ([128, 512], "f32"), "bdmask": ([128, 128], "f32"),
    "alb": ([128, 8, 64], "f32"), "corr": ([128, 8, 128], "bf16"), "sel2": ([2, 128], "bf16"), "dtab": ([128, 8192], "bf16"),
    "qrel": ([1, 512], "f32"), "adm": ([16, 2, 8192], "bf16"),
    "w_branch_a": ([1024, 2048], "f32"), "w_branch_b": ([1024, 2048], "f32"), "w_out": ([2048, 2048], "f32"),
    "wr": ([2048, 36], "f32"), "brr": ([1, 36], "f32"),
    "ln1_gb": ([128, 2048], "f32"), "ln1_bb": ([128, 2048], "f32"), "ln2_gb": ([128, 2048], "f32"), "ln2_bb": ([128, 2048], "f32"),
    "ltri": ([128, 128], "bf16"), "e256": ([128, 32], "f32"), "iota256": ([128, CAP], "f32"),
    "w_gate": ([NEXP, 2048, 1024], "f32"), "w_up": ([NEXP, 2048, 1024], "f32"), "w_down": ([NEXP, 1024, 2048], "f32"),
}


def _dt(s):
    return {"f32": F32, "bf16": BF16, "u32": U32, "i32": I32}[s]


def host_consts(inputs):
    b_in = np.asarray(inputs["b_in"][0], np.float32)
    bfm = np.zeros((128, NBFM), np.float32)
    def put(idx, c0, n):
        for c in range(n):
            bfm[:, idx + c] = b_in[c0 + c * 128: c0 + (c + 1) * 128]
    put(BFM_IDX["af"], C_AF, 8); put(BFM_IDX["aq"], C_AQ, 8); put(BFM_IDX["ag"], C_AG, 8)
    put(BFM_IDX["bk"], C_BK, 8); put(BFM_IDX["bq"], C_BQ, 8); put(BFM_IDX["iq"], C_IQ, 4)
    put(BFM_IDX["g"], C_G, 32)
    bfm[0:64, BFM_IDX["ik"]] = b_in[C_IK:C_IK + 64]
    bfm[64:128, BFM_IDX["ik"]] = b_in[C_IK:C_IK + 64]
    brow = np.concatenate([b_in[C_AI:C_AI + 1024], b_in[C_BV:C_BV + 1024], b_in[C_IW:C_IW + 8]])[None, :].astype(np.float32)
    lbl = np.ascontiguousarray(np.asarray(inputs["hg_lb_logits"], np.float32).reshape(2, 8, 128).transpose(2, 0, 1))
    normg = np.ascontiguousarray(np.asarray(inputs["hg_norm_g"][0], np.float32).reshape(8, 128).T)
    rmask = np.ones((128, 512), np.float32)
    rmask[:, ::64] = 0.0
    ii = np.arange(128)
    bdmask = ((ii[:, None] // 64 == ii[None, :] // 64) & (ii[:, None] <= ii[None, :])).astype(np.float32)
    import ml_dtypes
    bf = ml_dtypes.bfloat16
    slopes = 2.0 ** -(np.arange(8) + 1.0)
    pp = np.arange(128)
    alb = (slopes[None, :, None] * (pp[:, None, None] + 128.0 * (np.arange(64)[None, None, :] - 60))).astype(np.float32)
    dsq = np.maximum(pp[:, None] - pp[None, :], 0).astype(np.float64)
    corr = np.exp(-2.0 * slopes[None, :, None] * dsq[:, None, :]).astype(bf)
    sel2 = np.zeros((2, 128), np.float32); sel2[0, :64] = 1; sel2[1, 64:] = 1
    dtab = np.abs(8064 + pp[:, None] - np.arange(8192)[None, :]).astype(bf)
    qrel = np.arange(512, dtype=np.float32)[None, :]
    extra = {}
    if "w_out" in inputs:
        extra["w_branch_a"] = np.ascontiguousarray(inputs["w_branch_a"][0]); extra["w_branch_b"] = np.ascontiguousarray(inputs["w_branch_b"][0])
        extra["w_out"] = np.ascontiguousarray(inputs["w_out"][0])
        extra["wr"] = np.ascontiguousarray(np.concatenate([inputs["w_group"][0], inputs["w_router"][0]], axis=1).astype(np.float32))
        extra["brr"] = np.concatenate([inputs["b_group"][0], inputs["b_router"][0]])[None, :].astype(np.float32)
        for nm in ("ln1_g", "ln1_b", "ln2_g", "ln2_b"):
            extra[nm + "b"] = np.ascontiguousarray(np.broadcast_to(np.asarray(inputs[nm][0], np.float32)[None, :], (128, 2048)))
        extra["ltri"] = (pp[:, None] < pp[None, :]).astype(bf)
        extra["e256"] = np.ascontiguousarray(np.broadcast_to((np.arange(32, dtype=np.float32) * CAP)[None, :], (128, 32)))
        extra["iota256"] = np.ascontiguousarray(np.broadcast_to(np.arange(CAP, dtype=np.float32)[None, :], (128, CAP)))
        extra["w_gate"] = np.ascontiguousarray(inputs["w_gate"][0]); extra["w_up"] = np.ascontiguousarray(inputs["w_up"][0])
        extra["w_down"] = np.ascontiguousarray(inputs["w_down"][0])
    return {**extra, "alb": alb, "corr": corr, "sel2": sel2.astype(bf), "dtab": dtab, "qrel": qrel, "bdmask": bdmask, "ident": np.eye(128, dtype=np.float32), "bfm": bfm, "brow": brow, "lbl": lbl, "normg": normg, "rmask": rmask,
            "w_in": np.ascontiguousarray(inputs["w_in"][0])}


def host_core_inputs(inputs, hc, core):
    b, j = core // 4, core % 4
    x = np.asarray(inputs["x"], np.float32)
    xs = np.zeros((8192, 2048), np.float32)
    npre = (3 - j) * 2048
    xs[npre:] = x[b, :(j + 1) * 2048]
    valid = np.zeros(8192, np.float32)
    valid[npre:] = 1.0
    d = dict(hc)
    d["xs"] = xs
    d["valid_tm"] = np.ascontiguousarray(valid.reshape(64, 128).T)
    import ml_dtypes
    chunk = np.arange(8192) // 64
    adm = np.full((16, 2, 8192), NEG_ADM, np.float32)
    for t in range(16):
        c_first = (OWN0 + t * 128) // 64
        adm[t, 0, (valid > 0) & (chunk <= c_first)] = 0.0
        adm[t, 1, (valid > 0) & (chunk <= c_first + 1)] = 0.0
    d["adm"] = adm.astype(ml_dtypes.bfloat16)
    return d


def build_program(phases=("p1a",), dump=(), p1_blocks=(0, 1, 2, 3), p2_groups=(0, 1, 2, 3), in_names=None):
    nc = bass.Bass("TRN2", target_bir_lowering=False)
    A = {}
    used_inputs = in_names if in_names is not None else list(INPUTS)
    for n in used_inputs:
        shp, dt = INPUTS[n]
        A[n] = nc.dram_tensor(n, shp, _dt(dt), kind="ExternalInput").ap()
    for n, (shp, dt) in SCRATCH.items():
        kind = "ExternalOutput" if n in dump else "Internal"
        A[n] = nc.dram_tensor(n, shp, _dt(dt), kind=kind).ap()
    A["out"] = nc.dram_tensor("out", [2048, 2048], F32, kind="ExternalOutput").ap()
    with ExitStack() as es:
        S = Sched(nc, es)
        G = {"ap": A, "pb": PBanks(S), "B": {n: S.buf(n, glob=True) for n in list(SCRATCH) + ["out"]}, "bfm_idx": BFM_IDX}
        G["persist"] = {"wts": S.sbuf("p_wts", [128, 16, 2], F32), "RK": S.sbuf("p_RK", [128, 16, 32], F32), "idx": S.sbuf("p_idx", [128, 16, 2], F32)}
        G["Br_persist"] = S.buf("persist", glob=True)
        cst = {}
        Bc = S.buf("cst", glob=True)
        G["cst"] = cst
        G["Bcst"] = Bc
        idf = S.sbuf("idf", [128, 128], F32)
        Bidf = S.buf("idf", glob=True)
        S.dma("sp", idf[:], A["ident"], writes=[Bidf])
        idb = S.sbuf("idb", [128, 128], BF16)
        Bident = S.buf("idb")
        S.op("dve", lambda e: e.tensor_copy(idb[:], idf[:]), reads=[Bidf], writes=[Bident])
        G["ident_bf"] = idb
        G["Bident"] = Bident
        G["ident_f"] = idf
        G["Bidf"] = Bidf
        for n in ("bfm", "brow", "valid_tm", "normg", "rmask", "bdmask"):
            shp, dt = INPUTS[n]
            cst[n] = S.sbuf("c_" + n, shp, _dt(dt))
            S.dma("sp", cst[n][:], A[n], writes=[Bc], nowaw=True)
        lbl = S.sbuf("c_lbl", [128, 2, 8], F32)
        Blbl = S.buf("lbl", glob=True)
        S.dma("sp", lbl[:], A["lbl"], writes=[Blbl])
        for n in ("lb", "oml", "noml", "lbd"):
            cst[n] = S.sbuf("c_" + n, [128, 8], F32)
        cst["ones_row"] = S.sbuf("c_ones_row", [1, 128], F32)
        S.op("dve", lambda e: e.memset(cst["ones_row"][:], 1.0), writes=[Bc])
        S.op("dve", lambda e: e.tensor_tensor(cst["lbd"][:], lbl[:, 0, :], lbl[:, 1, :], ALU.subtract), reads=[Blbl], writes=[Bc])
        S.op("act", lambda e: e.activation(cst["lb"][:], cst["lbd"][:], AF.Sigmoid), reads=[Bc], writes=[Bc])
        S.op("dve", lambda e: e.tensor_scalar(cst["oml"][:], cst["lb"][:], -1.0, 1.0, op0=ALU.mult, op1=ALU.add), reads=[Bc], writes=[Bc])
        S.op("dve", lambda e: e.tensor_scalar(cst["noml"][:], cst["oml"][:], -1.0, None, op0=ALU.mult), reads=[Bc], writes=[Bc])

        cst["eps_ln"] = S.sbuf("c_eps_ln", [128, 1], F32)
        S.op("dve", lambda e: e.memset(cst["eps_ln"][:], LN_EPS), writes=[Bc])
        cst["eps_rms"] = S.sbuf("c_eps_rms", [128, 1], F32)
        S.op("dve", lambda e: e.memset(cst["eps_rms"][:], RMS_EPS), writes=[Bc])
        S.barrier()
        if "p1a" in phases:
            with ExitStack() as pes:
                S.es = pes
                phase1a(S, G, blocks=p1_blocks)
                S.es = es
            S.barrier()
            S.phase_end()
        if "p1b" in phases:
            with ExitStack() as pes:
                S.es = pes
                phase1b(S, G)
                S.es = es
            S.barrier()
            S.phase_end()
        if "p2" in phases:
            phase2_dsa(S, G, groups=p2_groups)
            S.barrier()
            S.phase_end()
        for nm, fn in (("p3a", phase3a_merge), ("p3b", phase3b_out), ("p4", phase4_moe), ("p5", phase5_final)):
            if nm in phases:
                with ExitStack() as pes:
                    S.es = pes
                    fn(S, G)
                    S.es = es
                S.barrier()
                S.phase_end()
        outs = [G["B"][n] for n in dump] + ([G["B"]["out"]] if "p5" in phases else [])
        S.wait_all("sp", outs)
        print("instructions:", S.ninst, {k: len(v) for k, v in S.ops.items()})
        S.run()
    return nc


ALL_PHASES = ("p1a", "p1b", "p2", "p3a", "p3b", "p4", "p5")
_CACHE = {}


def kernel(**inputs):
    if "nc" not in _CACHE:
        _CACHE["nc"] = build_program(phases=ALL_PHASES)
    nc = _CACHE["nc"]
    hc = host_consts(inputs)
    in_maps = [host_core_inputs(inputs, hc, c) for c in range(8)]
    res = run_bass_kernel_spmd(nc, in_maps, core_ids=list(range(8)))
    out = np.zeros((2, 8192, 2048), np.float32)
    for c in range(8):
        b, j = c // 4, c % 4
        out[b, j * 2048:(j + 1) * 2048] = np.asarray(res.results[c]["out"])
    return out
```

```python
import numpy as np
import concourse.bass as bass
import concourse.mybir as mybir

F32 = mybir.dt.float32
BF16 = mybir.dt.bfloat16
U32 = mybir.dt.uint32
I32 = mybir.dt.int32
U8 = mybir.dt.uint8
AF = mybir.ActivationFunctionType
ALU = mybir.AluOpType
AX = mybir.AxisListType


class Buf:
    __slots__ = ("name", "lastw", "readers", "dsem", "dcount", "glob", "dkey")

    def __init__(self, name, glob=False):
        self.name = name
        self.glob = glob
        self.dkey = None
        self.lastw = None
        self.readers = {}
        self.dsem = None
        self.dcount = 0


class Sched:
    ENGS = ("pe", "act", "dve", "pool", "sp")
    SEM_LIMIT = 30000

    def __init__(self, nc, es):
        self.nc = nc
        self.es = es
        self.es_sem = es
        self.dbufs = []
        self.dstate = {}
        self.free_dsems = []
        self.local_dbufs = []
        self.sem = {}
        self.count = {}
        self.known = {}
        self.ops = {}
        self.epoch = {}
        for n in self.ENGS:
            self.sem[n] = es.enter_context(nc.semaphore("se_" + n))
            self.count[n] = 0
            self.epoch[n] = 0
            self.known[n] = {}
            self.ops[n] = []
        self.nbuf = 0
        self.ninst = 0

    def sbuf(self, name, shape, dtype):
        self.nbuf += 1
        name = "%s_u%d" % (name, self.nbuf)
        return self.es.enter_context(self.nc.sbuf_tensor(name, list(shape), dtype))

    def psum(self, name, shape, dtype):
        return self.es.enter_context(self.nc.psum_tensor(name, list(shape), dtype))

    def buf(self, name=None, glob=False):
        self.nbuf += 1
        return Buf("%s_b%d" % (name or "b", self.nbuf), glob)

    def bufs(self, n, name="b"):
        return [self.buf("%s%d" % (name, i)) for i in range(n)]

    def _waits(self, eng, reads, writes):
        need = {}

        def add(ev, skip_same):
            if ev is None:
                return
            key, sem, val, prod = ev
            if skip_same and prod == eng:
                return
            if self.known[eng].get(key, 0) >= val:
                return
            if key not in need or need[key][1] < val:
                need[key] = (sem, val)

        for b in reads:
            add(b.lastw, False)
        for b in writes:
            add(b.lastw, True)
            for ev in b.readers.values():
                add(ev, True)
        for key, (sem, val) in need.items():
            self.known[eng][key] = val
        return list(need.values())

    def op(self, eng, fn, reads=(), writes=()):
        waits = self._waits(eng, reads, writes)
        if self.count[eng] >= self.SEM_LIMIT:
            self.epoch[eng] += 1
            self.count[eng] = 0
            self.sem[eng] = self.es_sem.enter_context(self.nc.semaphore("se_%s_%d" % (eng, self.epoch[eng])))
        self.count[eng] += 1
        seq = self.count[eng]
        sem = self.sem[eng]
        key = "e_%s_%d" % (eng, self.epoch[eng])
        ev = (key, sem, seq, eng)
        for b in writes:
            b.lastw = ev
            b.readers = {}
        for b in reads:
            b.readers[key] = ev
        self.ninst += 1 + len(waits)

        def emit(e, fn=fn, waits=waits, sem=sem):
            for (s, v) in waits:
                e.wait_ge(s, v)
            fn(e).then_inc(sem, 1)

        self.ops[eng].append(emit)
        return ev

    def dma(self, q, out_ap, in_ap, reads=(), writes=(), nowaw=False, builder=None, **kw):
        waits = self._waits(q, reads, [] if nowaw else writes)
        tb = writes[0]
        if tb.dsem is None:
            if (not tb.glob) and self.free_dsems:
                tb.dsem, tb.dcount, tb.dkey = self.free_dsems.pop()
            else:
                tb.dsem = self.es_sem.enter_context(self.nc.semaphore("sd_" + tb.name))
                tb.dkey = "d_" + tb.name
            if not tb.glob:
                self.local_dbufs.append(tb)
        tb.dcount += 16
        self.dstate[tb.dkey] = (tb.dsem, tb.dcount)
        ev = (tb.dkey, tb.dsem, tb.dcount, None)
        for b in writes:
            b.lastw = ev
            if not nowaw:
                b.readers = {}
        for b in reads:
            b.readers[ev[0]] = ev
        self.ninst += 1 + len(waits)

        def emit(e, waits=waits, sem=tb.dsem, out_ap=out_ap, in_ap=in_ap, kw=kw, builder=builder):
            for (s, v) in waits:
                e.wait_ge(s, v)
            if builder is not None:
                builder(e).then_inc(sem, 16)
            else:
                e.dma_start(out=out_ap, in_=in_ap, **kw).then_inc(sem, 16)

        self.ops[q].append(emit)
        return ev

    def phase_end(self):
        for b in self.local_dbufs:
            self.free_dsems.append((b.dsem, b.dcount, b.dkey))
            b.dsem = None
        self.local_dbufs = []

    def raw(self, eng, fn):
        self.ops[eng].append(lambda e, fn=fn: fn(e))

    def wait_all(self, eng, bufs):
        waits = self._waits(eng, list(bufs), [])

        def emit(e, waits=waits):
            for (s, v) in waits:
                e.wait_ge(s, v)

        self.ops[eng].append(emit)

    def barrier(self):
        evs = []
        for n in self.ENGS:
            if self.count[n] > 0:
                evs.append(("e_%s_%d" % (n, self.epoch[n]), self.sem[n], self.count[n], n))
        for key, (sem, cnt) in self.dstate.items():
            evs.append((key, sem, cnt, None))
        for eng in self.ENGS:
            waits = []
            for (key, sem, val, prod) in evs:
                if prod == eng:
                    continue
                if self.known[eng].get(key, 0) >= val:
                    continue
                self.known[eng][key] = val
                waits.append((sem, val))

            def emit(e, waits=waits):
                for (s, v) in waits:
                    e.wait_ge(s, v)

            self.ops[eng].append(emit)

    def run(self):
        nc = self.nc
        ops = self.ops
        with nc.Block() as block:
            @block.tensor
            def _(e):
                for f in ops["pe"]:
                    f(e)

            @block.scalar
            def _(e):
                for f in ops["act"]:
                    f(e)

            @block.vector
            def _(e):
                for f in ops["dve"]:
                    f(e)

            @block.gpsimd
            def _(e):
                for f in ops["pool"]:
                    f(e)

            @block.sync
            def _(e):
                for f in ops["sp"]:
                    f(e)

NSLOT = 8192
NOWN = 2048
OWN0 = NSLOT - NOWN
D = 2048
KC = 16
C_AQ, C_AF, C_AI, C_AG, C_BQ, C_BK, C_BV, C_IQ, C_IK, C_IW, C_G = 0, 1024, 2048, 3072, 4096, 5120, 6144, 7168, 7680, 7744, 7752
W_SCALE = (8 ** -0.5) * (64 ** -0.5)


class Rot:
    def __init__(self, S, name, shape, dtype, n):
        self.t = [S.sbuf("%s%d" % (name, i), shape, dtype) for i in range(n)]
        self.b = [S.buf("%s%d" % (name, i)) for i in range(n)]
        self.i = 0
        self.n = n

    def next(self):
        k = self.i % self.n
        self.i += 1
        return self.t[k], self.b[k]


class PBanks:
    def __init__(self, S):
        self.t = [S.psum("pb%d" % i, [128, 512], F32) for i in range(8)]
        self.b = [S.buf("pb%d" % i) for i in range(8)]
        self.i = 0

    def next(self, lo=0, hi=8):
        n = hi - lo
        k = lo + (self.i % n)
        self.i += 1
        return self.t[k], self.b[k]


def phase1a(S, G, blocks=(0, 1, 2, 3)):
    A = G["ap"]
    PB = G["pb"]
    ident = G["ident_bf"]
    Bident = G["Bident"]
    w_in = A["w_in"]
    xT = S.sbuf("xT", [128, KC, 2048], BF16)
    BxT = S.bufs(16, "xT")
    xin = Rot(S, "xin", [128, 2048], BF16, 2)
    wt = Rot(S, "wt", [128, KC, 512], BF16, 2)
    f32t = Rot(S, "hgf", [128, 512], F32, 12)
    st16 = Rot(S, "st16", [128, 512], BF16, 8)
    stkT = Rot(S, "stkT", [128, 4, 128], BF16, 2)
    decst = S.sbuf("decst", [128, 8, 32], F32)
    Bdec = S.buf("decst")
    wist = Rot(S, "wist", [128, 8], F32, 2)
    cst = G["cst"]
    Bc = G["Bcst"]
    evq = [0]

    def evac_engine():
        evq[0] += 1
        return "act" if evq[0] % 2 else "dve"

    def copy_op(eng, out_ap, in_ap, reads, writes):
        if eng == "act":
            S.op("act", lambda e, out_ap=out_ap, in_ap=in_ap: e.activation(out_ap, in_ap, AF.Copy), reads=reads, writes=writes)
        else:
            S.op("dve", lambda e, out_ap=out_ap, in_ap=in_ap: e.tensor_copy(out_ap, in_ap), reads=reads, writes=writes)

    for blk in blocks:
        own = (blk == 3)
        s0 = blk * 2048
        for t in range(16):
            xi, Bxi = xin.next()
            S.dma("pool", xi[:], A["xs"][s0 + t * 128: s0 + (t + 1) * 128, :], writes=[Bxi])
            for q4 in range(4):
                pb, Bpb = PB.next()
                for j in range(4):
                    kc = q4 * 4 + j
                    S.op("pe", lambda e, pb=pb, xi=xi, kc=kc, j=j: e.matmul(
                        pb[:, j * 128:(j + 1) * 128], xi[:, kc * 128:(kc + 1) * 128], ident[:], start=True, stop=True),
                        reads=[Bxi, Bident], writes=[Bpb])
                copy_op(evac_engine(), xT[:, q4 * 4:(q4 + 1) * 4, t * 128:(t + 1) * 128],
                        pb[:].rearrange("p (a b) -> p a b", a=4), [Bpb], [BxT[t]])

        def load_w(cols):
            w, Bw = wt.next()
            off = 0
            for (c0, n) in cols:
                S.dma("pool", w[:, :, off:off + n], w_in[:, c0:c0 + n].rearrange("(kc p) n -> p kc n", p=128), writes=[Bw])
                off += n
            return w, Bw

        def fm_mm(w, Bw, m, g):
            pb, Bpb = PB.next()
            for kc in range(KC):
                S.op("pe", lambda e, pb=pb, w=w, kc=kc, m=m, g=g: e.matmul(
                    pb[:], w[:, kc, m * 128:(m + 1) * 128], xT[:, kc, g * 512:(g + 1) * 512], start=(kc == 0), stop=(kc == KC - 1)),
                    reads=[Bw] + BxT[g * 4:(g + 1) * 4], writes=[Bpb])
            return pb, Bpb

        def simple_fm(w, Bw, m, g, bias_ap, func, dst_ap, Bd, rows=128):
            pb, Bpb = fm_mm(w, Bw, m, g)
            st, Bst = st16.next()
            S.op("act", lambda e, st=st, pb=pb, func=func, bias_ap=bias_ap: e.activation(st[:], pb[:], func, bias=bias_ap), reads=[Bpb, Bc], writes=[Bst])
            S.dma("sp", dst_ap, st[0:rows, :], reads=[Bst], writes=[Bd], nowaw=True)

        tm_units = [("ai", C_AI), ("ai", C_AI + 512), ("bv", C_BV), ("bv", C_BV + 512)]
        for (kind, c0) in tm_units:
            w, Bw = load_w([(c0, 512)])
            for t in range(16):
                pb, Bpb = PB.next()
                for kc in range(KC):
                    S.op("pe", lambda e, pb=pb, w=w, kc=kc, t=t: e.matmul(
                        pb[:], xT[:, kc, t * 128:(t + 1) * 128], w[:, kc, :], start=(kc == 0), stop=False),
                        reads=[Bw, BxT[t]], writes=[Bpb])
                boff = (c0 - C_AI) if kind == "ai" else (1024 + c0 - C_BV)
                S.op("pe", lambda e, pb=pb, boff=boff: e.matmul(pb[:], cst["ones_row"][0:1, :], cst["brow"][0:1, boff:boff + 512],
                                                            start=False, stop=True), reads=[Bc], writes=[Bpb])
                st, Bst = st16.next()
                tt = blk * 16 + t
                if kind == "ai":
                    S.op("act", lambda e, st=st, pb=pb, tt=tt: e.activation(st[:], pb[:], AF.Identity, scale=cst["valid_tm"][:, tt:tt + 1]),
                         reads=[Bpb, Bc], writes=[Bst])
                    dst = A["HG_v"][s0 + t * 128:s0 + (t + 1) * 128, c0 - C_AI:c0 - C_AI + 512]
                else:
                    S.op("dve", lambda e, st=st, pb=pb: e.tensor_copy(st[:], pb[:]), reads=[Bpb], writes=[Bst])
                    h0 = (c0 - C_BV) // 128
                    dst = A["VH"][h0:h0 + 4, :, tt, :].rearrange("h p d -> p h d")
                if kind == "ai":
                    S.dma("sp", dst, st[:], reads=[Bst], writes=[G["B"]["HG_v"]], nowaw=True)
                else:
                    S.dma("sp", dst, st[:].rearrange("p (h d) -> p h d", h=4), reads=[Bst], writes=[G["B"]["VH"]], nowaw=True)
        if own:
            w, Bw = load_w([(C_IW, 8)])
            for t in range(16):
                pb, Bpb = PB.next()
                for kc in range(KC):
                    S.op("pe", lambda e, pb=pb, w=w, kc=kc, t=t: e.matmul(
                        pb[:, 0:8], xT[:, kc, t * 128:(t + 1) * 128], w[:, kc, 0:8], start=(kc == 0), stop=False),
                        reads=[Bw, BxT[t]], writes=[Bpb])
                S.op("pe", lambda e, pb=pb: e.matmul(pb[:, 0:8], cst["ones_row"][0:1, :], cst["brow"][0:1, 2048:2056],
                                                     start=False, stop=True), reads=[Bc], writes=[Bpb])
                st, Bst = wist.next()
                S.op("dve", lambda e, st=st, pb=pb: e.tensor_scalar(st[:], pb[:, 0:8], W_SCALE, None, op0=ALU.mult), reads=[Bpb], writes=[Bst])
                S.dma("sp", A["WI"][t * 128:(t + 1) * 128, :], st[:], reads=[Bst], writes=[G["B"]["WI"]], nowaw=True)

        for u in range(2):
            w, Bw = load_w([(C_BK + u * 512, 512)])
            for m in range(4):
                h = u * 4 + m
                for g in range(4):
                    simple_fm(w, Bw, m, g, cst["bfm"][:, G["bfm_idx"]["bk"] + h: G["bfm_idx"]["bk"] + h + 1], AF.Identity,
                              A["KT"][h, :, s0 + g * 512:s0 + (g + 1) * 512], G["B"]["KT"])
        w, Bw = load_w([(C_IK, 64), (C_IK, 64)])
        for g in range(4):
            simple_fm(w, Bw, 0, g, cst["bfm"][:, G["bfm_idx"]["ik"]: G["bfm_idx"]["ik"] + 1], AF.Identity,
                      A["KIT"][:, s0 + g * 512:s0 + (g + 1) * 512], G["B"]["KIT"])
        if own:
            for u in range(2):
                w, Bw = load_w([(C_BQ + u * 512, 512)])
                for m in range(4):
                    h = u * 4 + m
                    for g in range(4):
                        simple_fm(w, Bw, m, g, cst["bfm"][:, G["bfm_idx"]["bq"] + h: G["bfm_idx"]["bq"] + h + 1], AF.Identity,
                                  A["QT"][h, :, g * 512:(g + 1) * 512], G["B"]["QT"])
            w, Bw = load_w([(C_IQ, 512)])
            for m in range(4):
                for g in range(4):
                    simple_fm(w, Bw, m, g, cst["bfm"][:, G["bfm_idx"]["iq"] + m: G["bfm_idx"]["iq"] + m + 1], AF.Identity,
                              A["QIT"][m, :, g * 512:(g + 1) * 512], G["B"]["QIT"])
            for u in range(8):
                w, Bw = load_w([(C_G + u * 512, 512)])
                for m in range(4):
                    c = u * 4 + m
                    for g in range(4):
                        simple_fm(w, Bw, m, g, cst["bfm"][:, G["bfm_idx"]["g"] + c: G["bfm_idx"]["g"] + c + 1], AF.Sigmoid,
                                  A["GT"][c, :, g * 512:(g + 1) * 512], G["B"]["GT"])

        deferred = []
        for h in range(8):
            if own:
                w, Bw = load_w([(C_AF + h * 128, 128), (C_AQ + h * 128, 128), (C_AG + h * 128, 128)])
            else:
                if h % 4 == 0:
                    w4, Bw4 = load_w([(C_AF + h * 128, 512)])
                w, Bw = w4, Bw4
            mf = 0 if own else (h % 4)
            bi = G["bfm_idx"]
            oml_h = cst["oml"][:, h:h + 1]
            noml_h = cst["noml"][:, h:h + 1]
            lb_h = cst["lb"][:, h:h + 1]
            b_af = cst["bfm"][:, bi["af"] + h: bi["af"] + h + 1]
            b_aq = cst["bfm"][:, bi["aq"] + h: bi["aq"] + h + 1]
            b_ag = cst["bfm"][:, bi["ag"] + h: bi["ag"] + h + 1]
            for g in range(4):
                pb, Bpb = fm_mm(w, Bw, mf, g)
                while deferred:
                    deferred.pop(0)()
                sg, Bsg = f32t.next()
                S.op("act", lambda e, sg=sg, pb=pb, b_af=b_af: e.activation(sg[:], pb[:], AF.Sigmoid, bias=b_af),
                     reads=[Bpb, Bc], writes=[Bsg])
                lf, Blf = f32t.next()
                S.op("act", lambda e, lf=lf, sg=sg, oml_h=oml_h, lb_h=lb_h: e.activation(lf[:], sg[:], AF.Ln, scale=oml_h, bias=lb_h),
                     reads=[Bsg, Bc], writes=[Blf])
                cum, Bcum = f32t.next()
                S.op("dve", lambda e, cum=cum, lf=lf: e.tensor_tensor_scan(cum[:], cst["rmask"][:], lf[:], 0.0, ALU.mult, ALU.add),
                     reads=[Blf, Bc], writes=[Bcum])
                en, Ben = f32t.next()
                S.op("act", lambda e, en=en, cum=cum: e.activation(en[:], cum[:], AF.Exp, scale=-1.0), reads=[Bcum], writes=[Ben])
                kk, Bkk = f32t.next()
                S.op("dve", lambda e, kk=kk, sg=sg, noml_h=noml_h, oml_h=oml_h: e.tensor_scalar(kk[:], sg[:], noml_h, oml_h, op0=ALU.mult, op1=ALU.add),
                     reads=[Bsg, Bc], writes=[Bkk])
                kd, Bkd = st16.next()
                S.op("dve", lambda e, kd=kd, kk=kk, en=en: e.tensor_tensor(kd[:], kk[:], en[:], ALU.mult), reads=[Bkk, Ben], writes=[Bkd])
                S.dma("sp", A["HG_kdec"][h, :, s0 + g * 512:s0 + (g + 1) * 512], kd[:], reads=[Bkd], writes=[G["B"]["HG_kdec"]], nowaw=True)
                ex2, Bex2 = f32t.next()
                for c in range(8):
                    S.op("act", lambda e, ex2=ex2, cum=cum, c=c: e.activation(ex2[:, c * 64:(c + 1) * 64], cum[:, c * 64:(c + 1) * 64], AF.Exp,
                                                                           scale=-1.0, bias=cum[:, c * 64 + 63:c * 64 + 64]),
                         reads=[Bcum], writes=[Bex2])
                ke, Bke = st16.next()
                S.op("dve", lambda e, ke=ke, kk=kk, ex2=ex2: e.tensor_tensor(ke[:], kk[:], ex2[:], ALU.mult), reads=[Bkk, Bex2], writes=[Bke])
                dec_ap = decst[:, h, g * 8:(g + 1) * 8]
                S.op("act", lambda e, cum=cum, dec_ap=dec_ap: e.activation(dec_ap, cum[:].rearrange("p (c s) -> p c s", s=64)[:, :, 63], AF.Exp),
                     reads=[Bcum], writes=[Bdec])
                def kend_T(ke=ke, Bke=Bke, g=g, h=h, s0=s0):
                    pbt, Bpbt = PB.next()
                    for j in range(4):
                        S.op("pe", lambda e, pbt=pbt, ke=ke, j=j: e.matmul(pbt[:, j * 128:(j + 1) * 128], ke[:, j * 128:(j + 1) * 128], ident[:],
                                                                           start=True, stop=True), reads=[Bke, Bident], writes=[Bpbt])
                    kT, BkT = stkT.next()
                    S.op("dve", lambda e, kT=kT, pbt=pbt: e.tensor_copy(kT[:], pbt[:].rearrange("p (a b) -> p a b", a=4)), reads=[Bpbt], writes=[BkT])
                    S.dma("sp", A["HG_kend"][s0 + g * 512:s0 + (g + 1) * 512, h * 128:(h + 1) * 128].rearrange("(t p) d -> p t d", p=128),
                          kT[:], reads=[BkT], writes=[G["B"]["HG_kend"]], nowaw=True)
                deferred.append(kend_T)
                if own:
                    ec, Bec = f32t.next()
                    S.op("act", lambda e, ec=ec, cum=cum: e.activation(ec[:], cum[:], AF.Exp), reads=[Bcum], writes=[Bec])
                    pbq, Bpbq = fm_mm(w, Bw, 1, g)
                    qs, Bqs = f32t.next()
                    S.op("act", lambda e, qs=qs, pbq=pbq, b_aq=b_aq: e.activation(qs[:], pbq[:], AF.Silu, bias=b_aq),
                         reads=[Bpbq, Bc], writes=[Bqs])
                    qd, Bqd = st16.next()
                    S.op("dve", lambda e, qd=qd, qs=qs, ec=ec: e.tensor_tensor(qd[:], qs[:], ec[:], ALU.mult), reads=[Bqs, Bec], writes=[Bqd])
                    S.dma("sp", A["HG_qdec"][h, :, g * 512:(g + 1) * 512], qd[:], reads=[Bqd], writes=[G["B"]["HG_qdec"]], nowaw=True)
                    simple_fm(w, Bw, 2, g, b_ag, AF.Silu, A["HG_gs"][h, :, g * 512:(g + 1) * 512], G["B"]["HG_gs"])
        while deferred:
            deferred.pop(0)()
        S.dma("sp", A["HG_dec"][:, :, blk * 32:(blk + 1) * 32], decst[:], reads=[Bdec], writes=[G["B"]["HG_dec"]], nowaw=True)

RMS_EPS = 1e-6


def phase1b(S, G):
    A = G["ap"]
    PB = G["pb"]
    cst = G["cst"]
    Bc = G["Bcst"]
    B = G["B"]
    kend = S.sbuf("hs_kend", [128, 16, 1024], BF16)
    Bkend = S.buf("hs_kend")
    vv = S.sbuf("hs_v", [128, 16, 1024], BF16)
    Bvv = S.buf("hs_v")
    dec = S.sbuf("hs_dec", [128, 8, 128], F32)
    Bdec = S.buf("hs_dec")
    Sf = S.sbuf("hs_S", [128, 8, 128], F32)
    Sb = S.sbuf("hs_Sb", [128, 8, 128], BF16)
    BS = S.bufs(8, "hs_S")
    BSb = S.bufs(8, "hs_Sb")
    S.dma("sp", dec[:], A["HG_dec"], reads=[B["HG_dec"]], writes=[Bdec])
    S.op("dve", lambda e: e.memset(Sf[:], 0.0), writes=BS)
    S.op("dve", lambda e: e.memset(Sb[:], 0.0), writes=BSb)
    onesb = S.sbuf("hs_ones", [128, 128], BF16)
    Bones = S.buf("hs_ones")
    S.op("dve", lambda e: e.memset(onesb[:], 1.0 / 128.0), writes=[Bones])
    kdec = Rot(S, "hs_kdec", [128, 2048], BF16, 2)
    qdec = Rot(S, "hs_qdec", [128, 2048], BF16, 2)
    gs = Rot(S, "hs_gs", [128, 2048], BF16, 2)
    attm = Rot(S, "hs_attm", [128, 128], BF16, 3)
    sq = Rot(S, "hs_sq", [128, 128], BF16, 3)
    rs = Rot(S, "hs_rs", [128, 128], F32, 3)
    yy = Rot(S, "hs_y", [128, 128], F32, 3)
    bast = S.sbuf("hs_bast", [128, 2048], BF16)
    Bbast = S.buf("hs_bast")

    def state_update(t, h, half, ):
        p0 = half * 64
        chunk = None
        pk, Bpk = PB.next()
        S.op("pe", lambda e, pk=pk, t=t, h=h, p0=p0: e.matmul(pk[:, 0:128], kend[p0:p0 + 64, t, h * 128:(h + 1) * 128],
                                                            vv[p0:p0 + 64, t, h * 128:(h + 1) * 128], start=True, stop=True),
             reads=[Bkend, Bvv], writes=[Bpk])
        return pk, Bpk

    for blk in range(4):
        own = (blk == 3)
        s0 = blk * 2048
        S.dma("sp", kend[:], A["HG_kend"][s0:s0 + 2048, :].rearrange("(t p) c -> p t c", p=128), reads=[B["HG_kend"]], writes=[Bkend])
        S.dma("sp", vv[:], A["HG_v"][s0:s0 + 2048, :].rearrange("(t p) c -> p t c", p=128), reads=[B["HG_v"]], writes=[Bvv])
        if not own:
            for t in range(16):
                for half in range(2):
                    ch = blk * 32 + t * 2 + half
                    for h in range(8):
                        pk, Bpk = state_update(t, h, half)
                        S.op("dve", lambda e, pk=pk, h=h, ch=ch: e.scalar_tensor_tensor(Sf[:, h, :], Sf[:, h, :], dec[:, h, ch:ch + 1], pk[:, 0:128],
                                                                                      op0=ALU.mult, op1=ALU.add),
                             reads=[Bpk, Bdec, BS[h]], writes=[BS[h]])
            if blk == 2:
                for h in range(8):
                    S.op("act", lambda e, h=h: e.activation(Sb[:, h, :], Sf[:, h, :], AF.Copy), reads=[BS[h]], writes=[BSb[h]])
            continue
        for h in range(8):
            kd, Bkd = kdec.next()
            qd, Bqd = qdec.next()
            gg, Bgg = gs.next()
            S.dma("sp", kd[:], A["HG_kdec"][h, :, s0:s0 + 2048], reads=[B["HG_kdec"]], writes=[Bkd])
            S.dma("sp", qd[:], A["HG_qdec"][h], reads=[B["HG_qdec"]], writes=[Bqd])
            S.dma("sp", gg[:], A["HG_gs"][h], reads=[B["HG_gs"]], writes=[Bgg])
            for t in range(16):
                tc = slice(t * 128, (t + 1) * 128)
                pa, Bpa = PB.next()
                S.op("pe", lambda e, pa=pa, kd=kd, qd=qd, tc=tc: e.matmul(pa[:, 0:128], kd[:, tc], qd[:, tc], start=True, stop=True),
                     reads=[Bkd, Bqd], writes=[Bpa])
                am, Bam = attm.next()
                S.op("dve", lambda e, am=am, pa=pa: e.tensor_tensor(am[:], pa[:, 0:128], cst["bdmask"][:], ALU.mult), reads=[Bpa, Bc], writes=[Bam])
                po, Bpo = PB.next()
                S.op("pe", lambda e, po=po, am=am, t=t, h=h: e.matmul(po[:, 0:128], vv[:, t, h * 128:(h + 1) * 128], am[:], start=True, stop=False),
                     reads=[Bvv, Bam], writes=[Bpo])
                for half in range(2):
                    ch = blk * 32 + t * 2 + half
                    c0 = t * 128 + half * 64
                    S.op("pe", lambda e, po=po, qd=qd, h=h, c0=c0, half=half: e.matmul(po[:, half * 64:(half + 1) * 64], Sb[:, h, :], qd[:, c0:c0 + 64],
                                                                                  start=False, stop=(half == 1)),
                         reads=[BSb[h], Bqd], writes=[Bpo])
                    pk, Bpk = state_update(t, h, half)
                    S.op("dve", lambda e, pk=pk, h=h, ch=ch: e.scalar_tensor_tensor(Sf[:, h, :], Sf[:, h, :], dec[:, h, ch:ch + 1], pk[:, 0:128],
                                                                                  op0=ALU.mult, op1=ALU.add),
                         reads=[Bpk, Bdec, BS[h]], writes=[BS[h]])
                    S.op("act", lambda e, h=h: e.activation(Sb[:, h, :], Sf[:, h, :], AF.Copy), reads=[BS[h]], writes=[BSb[h]])
                q2, Bq2 = sq.next()
                S.op("act", lambda e, q2=q2, po=po: e.activation(q2[:], po[:, 0:128], AF.Square), reads=[Bpo], writes=[Bq2])
                pm, Bpm = PB.next()
                S.op("pe", lambda e, pm=pm, q2=q2: e.matmul(pm[:, 0:128], onesb[:], q2[:], start=True, stop=True), reads=[Bones, Bq2], writes=[Bpm])
                r1, Br1 = rs.next()
                S.op("act", lambda e, r1=r1, pm=pm: e.activation(r1[:], pm[:, 0:128], AF.Sqrt, bias=cst["eps_rms"][:, 0:1]), reads=[Bpm, Bc], writes=[Br1])
                S.op("dve", lambda e, r1=r1: e.reciprocal(r1[:], r1[:]), reads=[Br1], writes=[Br1])
                y1, By1 = yy.next()
                S.op("dve", lambda e, y1=y1, po=po, r1=r1: e.tensor_tensor(y1[:], po[:, 0:128], r1[:], ALU.mult), reads=[Bpo, Br1], writes=[By1])
                S.op("dve", lambda e, y1=y1, gg=gg, tc=tc, h=h: e.scalar_tensor_tensor(bast[:, tc], y1[:], cst["normg"][:, h:h + 1], gg[:, tc],
                                                                                    op0=ALU.mult, op1=ALU.mult),
                     reads=[By1, Bgg, Bc], writes=[Bbast])
            S.dma("sp", A["BA"][h], bast[:], reads=[Bbast], writes=[B["BA"]], nowaw=True)

SM_SCALE = 128 ** -0.5
TOPK = 256
NBIS = 18
NTER = 11
BIS_WIN = 16.0
NEG_ADM = -30000.0


def phase2_dsa(S, G, groups=(0, 1, 2, 3)):
    A = G["ap"]
    PB = G["pb"]
    cst = G["cst"]
    Bc = G["Bcst"]
    B = G["B"]
    ident = G["ident_bf"]
    Bident = G["Bident"]
    es_outer = S.es
    with ExitStack() as pes:
        S.es = pes
        alb = S.sbuf("ds_alb", [128, 8, 64], F32)
        dtab = S.sbuf("ds_dtab", [128, 8192], BF16)
        qrel = S.sbuf("ds_qrel", [1, 512], F32)
        onesr = S.sbuf("ds_onesr", [1, 128], BF16)
        drow = S.sbuf("ds_drow", [1, 512], F32)
        Bdrow = S.buf("ds_drow")
        shrow = S.sbuf("ds_shrow", [1, 8, 512], BF16)
        Bshrow = S.buf("ds_shrow")
        dmc = S.sbuf("ds_dmc", [128, 1], F32)
        Bdmc = S.buf("ds_dmc")
        corr = S.sbuf("ds_corr", [128, 8, 128], BF16)
        sel2 = S.sbuf("ds_sel2", [2, 128], BF16)
        onesb = S.sbuf("ds_ones", [128, 128], BF16)
        Bk = S.buf("ds_const")
        S.dma("sp", alb[:], A["alb"], writes=[Bk], nowaw=True)
        S.dma("sp", corr[:], A["corr"], writes=[Bk], nowaw=True)
        S.dma("sp", sel2[:], A["sel2"], writes=[Bk], nowaw=True)
        S.op("dve", lambda e: e.memset(onesb[:], 1.0), writes=[Bk])
        S.op("dve", lambda e: e.memset(onesr[:], 1.0), writes=[Bk])
        S.dma("sp", dtab[:], A["dtab"], writes=[Bk], nowaw=True)
        S.dma("sp", qrel[:], A["qrel"], writes=[Bk], nowaw=True)
        kit = S.sbuf("ds_kit", [128, 8192], BF16)
        S.dma("sp", kit[:], A["KIT"], reads=[B["KIT"]], writes=[Bk], nowaw=True)
        wi = S.sbuf("ds_wi", [128, 16, 8], F32)
        S.dma("sp", wi[:], A["WI"].rearrange("(t p) h -> p t h", p=128), reads=[B["WI"]], writes=[Bk], nowaw=True)
        maskT = S.sbuf("ds_maskT", [128, 64, 512], U8)
        BmT = S.buf("ds_maskT")
        S.barrier()

        def do_group(g):
            Q0 = OWN0 + g * 512
            with ExitStack() as aes:
                S.es = aes
                qit = S.sbuf("ds_qit", [128, 4, 512], BF16)
                Bqit = S.buf("ds_qit")
                S.dma("sp", qit[:], A["QIT"][:, :, g * 512:(g + 1) * 512].rearrange("m p q -> p m q"), reads=[B["QIT"]], writes=[Bqit])
                sc2 = [S.sbuf("ds_sc", [128, 8192], F32) for _ in range(2)]
                Bsc2 = [S.buf("ds_sc"), S.buf("ds_sc")]
                mq = S.sbuf("ds_mq", [128, 8192], BF16)
                Bmq = S.buf("ds_mq")
                adm = Rot(S, "ds_adm", [2, 512], BF16, 3)
                junk2 = S.sbuf("ds_junk2", [128, 8192], U8)
                Bj2 = S.buf("ds_junk2")
                relu = Rot(S, "ds_relu", [128, 512], BF16, 9)
                dg2 = [S.sbuf("ds_dg", [128, 8, 128], BF16) for _ in range(2)]
                Bdg2 = [S.buf("ds_dg"), S.buf("ds_dg")]
                sm = {n: S.sbuf("ds_s_" + n, [128, 1], F32) for n in ("lo", "hi", "mid", "cnt", "ge", "d", "c0", "d3", "t1", "nt2", "s2", "g2")}
                Bsm = S.buf("ds_small")
                Bth = S.buf("ds_th")
                Bs2 = S.buf("ds_s2")

                def tparams(T):
                    tg = g * 4 + T
                    Qt = Q0 + T * 128
                    nk = Qt + 128
                    nb5 = (nk + 511) // 512
                    return tg, Qt, nk, nb5, nb5 * 512

                def score_blocks(T):
                    tg, Qt, nk, nb5, nkp = tparams(T)
                    sc, Bsc = sc2[T % 2], Bsc2[T % 2]
                    dg, Bdg = dg2[T % 2], Bdg2[T % 2]
                    out = []

                    def prep():
                        for h in range(8):
                            S.op("dve", lambda e, h=h: e.tensor_scalar(dg[:, h, :], ident[:], wi[:, tg, h:h + 1], None, op0=ALU.mult),
                                 reads=[Bident, Bk], writes=[Bdg])
                    out.append(prep)

                    def blk(kb5):
                        ks = slice(kb5 * 512, (kb5 + 1) * 512)
                        ad, Bad = adm.next()
                        S.dma("sp", ad[:], A["adm"][tg, :, ks], writes=[Bad])
                        rl = []
                        for hp in range(4):
                            for par in range(2):
                                pb, Bpb = PB.next()
                                p0 = par * 64
                                S.op("pe", lambda e, pb=pb, hp=hp, p0=p0: e.matmul(
                                    pb[:], qit[p0:p0 + 64, hp, T * 128:(T + 1) * 128], kit[p0:p0 + 64, ks], start=True, stop=True),
                                    reads=[Bqit, Bk], writes=[Bpb])
                                r, Br = relu.next()
                                if par == 0 or hp % 2 == 0:
                                    S.op("act", lambda e, r=r, pb=pb: e.activation(r[:], pb[:], AF.Relu), reads=[Bpb], writes=[Br])
                                else:
                                    S.op("dve", lambda e, r=r, pb=pb: e.tensor_scalar(r[:], pb[:], 0.0, None, op0=ALU.max), reads=[Bpb], writes=[Br])
                                rl.append((r, Br))
                        ps, Bps = PB.next()
                        for h in range(8):
                            r, Br = rl[h]
                            S.op("pe", lambda e, ps=ps, h=h, r=r: e.matmul(ps[:], dg[:, h, :], r[:], start=(h == 0), stop=False),
                                 reads=[Bdg, Br], writes=[Bps])
                        S.op("pe", lambda e, ps=ps, ad=ad: e.matmul(ps[:], sel2[0:2, :], ad[0:2, :], start=False, stop=True),
                             reads=[Bk, Bad], writes=[Bps])
                        S.op("act", lambda e, ps=ps: e.activation(sc[:, ks], ps[:], AF.Copy), reads=[Bps], writes=[Bsc])
                    for kb5 in range(nb5):
                        out.append(lambda kb5=kb5: blk(kb5))
                    return out

                def search_init(T):
                    tg, Qt, nk, nb5, nkp = tparams(T)
                    sc, Bsc = sc2[T % 2], Bsc2[T % 2]
                    scv = sc[:, 0:nkp]
                    S.op("dve", lambda e: e.reduce_max(sm["hi"][:], scv, AX.X), reads=[Bsc], writes=[Bsm])
                    S.op("dve", lambda e: e.tensor_scalar(sm["lo"][:], sm["hi"][:], -BIS_WIN, None, op0=ALU.add), reads=[Bsm], writes=[Bsm])
                    S.op("dve", lambda e: e.tensor_scalar(sm["hi"][:], sm["hi"][:], 1e-3, None, op0=ALU.add), reads=[Bsm], writes=[Bsm, Bth])
                    S.op("dve", lambda e: e.tensor_scalar(mq[:, 0:nkp], scv, sm["lo"][:, 0:1], None, op0=ALU.is_ge, op1=ALU.add,
                                                          accum_out=sm["c0"][:]), reads=[Bsc, Bsm], writes=[Bmq, Bsm])

                def search_round(T):
                    tg, Qt, nk, nb5, nkp = tparams(T)
                    sc, Bsc = sc2[T % 2], Bsc2[T % 2]
                    scv = sc[:, 0:nkp]
                    S.op("dve", lambda e: e.tensor_tensor(sm["d"][:], sm["hi"][:], sm["lo"][:], ALU.subtract), reads=[Bsm], writes=[Bsm])
                    S.op("dve", lambda e: e.tensor_scalar(sm["d3"][:], sm["d"][:], 1.0 / 3.0, None, op0=ALU.mult), reads=[Bsm], writes=[Bsm])
                    S.op("dve", lambda e: e.tensor_tensor(sm["t1"][:], sm["lo"][:], sm["d3"][:], ALU.add), reads=[Bsm], writes=[Bsm])
                    S.op("dve", lambda e: e.tensor_tensor(sm["nt2"][:], sm["d3"][:], sm["hi"][:], ALU.subtract), reads=[Bsm], writes=[Bsm, Bth])
                    S.op("act", lambda e: e.activation(junk2[:, 0:nkp], scv, AF.Sign, bias=sm["nt2"][:, 0:1], accum_out=sm["s2"][:]),
                         reads=[Bsc, Bth], writes=[Bj2, Bs2])
                    S.op("dve", lambda e: e.tensor_scalar(mq[:, 0:nkp], scv, sm["t1"][:, 0:1], None, op0=ALU.is_ge, op1=ALU.add,
                                                          accum_out=sm["cnt"][:]), reads=[Bsc, Bsm], writes=[Bmq, Bsm])
                    S.op("dve", lambda e: e.tensor_scalar(sm["ge"][:], sm["cnt"][:], TOPK - 0.5, None, op0=ALU.is_ge), reads=[Bsm], writes=[Bsm])
                    S.op("dve", lambda e: e.tensor_scalar(sm["g2"][:], sm["s2"][:], 2.0 * (TOPK - 0.5) - nkp, None, op0=ALU.is_ge), reads=[Bs2], writes=[Bsm])
                    S.op("dve", lambda e: e.tensor_tensor(sm["ge"][:], sm["ge"][:], sm["g2"][:], ALU.add), reads=[Bsm], writes=[Bsm])
                    S.op("dve", lambda e: e.scalar_tensor_tensor(sm["lo"][:], sm["d3"][:], sm["ge"][:, 0:1], sm["lo"][:], op0=ALU.mult, op1=ALU.add),
                         reads=[Bsm], writes=[Bsm])
                    S.op("dve", lambda e: e.tensor_tensor(sm["hi"][:], sm["lo"][:], sm["d3"][:], ALU.add), reads=[Bsm], writes=[Bsm, Bth])

                def finalize(T):
                    tg, Qt, nk, nb5, nkp = tparams(T)
                    sc, Bsc = sc2[T % 2], Bsc2[T % 2]
                    scv = sc[:, 0:nkp]
                    S.op("dve", lambda e: e.tensor_scalar(sm["ge"][:], sm["c0"][:], TOPK - 0.5, None, op0=ALU.is_ge), reads=[Bsm], writes=[Bsm])
                    S.op("dve", lambda e: e.tensor_scalar(sm["d"][:], sm["lo"][:], 1000.0, None, op0=ALU.add), reads=[Bsm], writes=[Bsm])
                    S.op("dve", lambda e: e.tensor_scalar(sm["lo"][:], sm["d"][:], sm["ge"][:, 0:1], -1000.0, op0=ALU.mult, op1=ALU.add),
                         reads=[Bsm], writes=[Bsm])
                    S.op("dve", lambda e: e.tensor_scalar(mq[:, 0:nkp], scv, sm["lo"][:, 0:1], None, op0=ALU.is_ge), reads=[Bsc, Bsm], writes=[Bmq])
                    S.op("dve", lambda e: e.scalar_tensor_tensor(sc[:, 0:nk], mq[:, 0:nk], -16384.0, dtab[:, 8192 - nk:8192], op0=ALU.mult, op1=ALU.add),
                         reads=[Bmq, Bk], writes=[Bsc])
                    S.op("dve", lambda e: e.tensor_reduce(dmc[:], sc[:, 0:nk], AX.X, ALU.min), reads=[Bsc], writes=[Bdmc])
                    S.op("dve", lambda e: e.tensor_scalar(dmc[:], dmc[:], 16384.0, None, op0=ALU.add), reads=[Bdmc], writes=[Bdmc])
                    pbd, Bpbd = PB.next()
                    S.op("pe", lambda e: e.matmul(pbd[0:1, 0:128], dmc[:, 0:1], G["ident_f"][:], start=True, stop=True),
                         reads=[Bdmc, G["Bidf"]], writes=[Bpbd])
                    S.op("dve", lambda e: e.tensor_copy(drow[0:1, T * 128:(T + 1) * 128], pbd[0:1, 0:128]), reads=[Bpbd], writes=[Bdrow])
                    nkb = nk // 128
                    for k4 in range(0, nkb, 4):
                        n4 = min(4, nkb - k4)
                        pb, Bpb = PB.next()
                        for j in range(n4):
                            kb = k4 + j
                            S.op("pe", lambda e, pb=pb, j=j, kb=kb: e.matmul(pb[:, j * 128:(j + 1) * 128], mq[:, kb * 128:(kb + 1) * 128], ident[:],
                                                                           start=True, stop=True), reads=[Bmq, Bident], writes=[Bpb])
                        dst = maskT[:, k4:k4 + n4, T * 128:(T + 1) * 128]
                        src = pb[:, 0:n4 * 128].rearrange("p (a b) -> p a b", a=n4)
                        if (k4 // 4) % 2 == 0:
                            S.op("act", lambda e, dst=dst, src=src: e.activation(dst, src, AF.Copy), reads=[Bpb], writes=[BmT])
                        else:
                            S.op("dve", lambda e, dst=dst, src=src: e.tensor_copy(dst, src), reads=[Bpb], writes=[BmT])

                for f_ in score_blocks(0):
                    f_()
                for T in range(4):
                    nxt = score_blocks(T + 1) if T < 3 else []
                    per = -(-len(nxt) // NTER)
                    search_init(T)
                    for it in range(NTER):
                        search_round(T)
                        for _ in range(per):
                            if nxt:
                                nxt.pop(0)()
                    while nxt:
                        nxt.pop(0)()
                    finalize(T)
                S.op("dve", lambda e: e.tensor_tensor(drow[:], drow[:], qrel[:], ALU.subtract), reads=[Bdrow, Bk], writes=[Bdrow])
                for h in range(8):
                    S.op("dve", lambda e, h=h: e.tensor_scalar(shrow[0:1, h, :], drow[:], (2.0 ** -(h + 1)) / SM_SCALE, None, op0=ALU.mult),
                         reads=[Bdrow], writes=[Bshrow])
                S.barrier()
                S.phase_end()
            with ExitStack() as bes:
                S.es = bes
                kt = Rot(S, "ds_kt", [128, 8192], BF16, 2)
                vh = Rot(S, "ds_vh", [128, 64, 128], BF16, 2)
                qt = Rot(S, "ds_qt", [128, 512], BF16, 2)
                pT = Rot(S, "ds_pT", [128, 512], BF16, 7)
                mcr = Rot(S, "ds_mc", [128, 128], BF16, 3)
                rec = Rot(S, "ds_rec", [128, 512], F32, 1)
                ob = Rot(S, "ds_ob", [128, 512], BF16, 1)
                nkb = (Q0 + 512) // 128
                kb0 = Q0 // 128
                for h in range(8):
                    k_, Bk_ = kt.next()
                    v_, Bv_ = vh.next()
                    q_, Bq_ = qt.next()
                    S.dma("sp", k_[:, 0:nkb * 128], A["KT"][h, :, 0:nkb * 128], reads=[B["KT"]], writes=[Bk_])
                    S.dma("sp", v_[:, 0:nkb, :], A["VH"][h, :, 0:nkb, :], reads=[B["VH"]], writes=[Bv_])
                    S.dma("sp", q_[:], A["QT"][h, :, g * 512:(g + 1) * 512], reads=[B["QT"]], writes=[Bq_])
                    po, Bpo = PB.t[6], PB.b[6]
                    pd, Bpd = PB.t[7], PB.b[7]
                    pend = []

                    def stage2(kb, p_, Bp_, c0, first, last, po=po, pd=pd, Bpo=Bpo, Bpd=Bpd, v_=v_, Bv_=Bv_):
                        S.op("pe", lambda e, po=po, v_=v_, p_=p_, kb=kb, c0=c0, first=first, last=last: e.matmul(
                            po[:, c0:512], v_[:, kb, :], p_[:, c0:512], start=first, stop=last), reads=[Bv_, Bp_], writes=[Bpo])
                        S.op("pe", lambda e, pd=pd, p_=p_, c0=c0, first=first, last=last: e.matmul(
                            pd[:, c0:512], onesb[:], p_[:, c0:512], start=first, stop=last), reads=[Bk, Bp_], writes=[Bpd])

                    for kb in range(nkb):
                        r = kb - kb0
                        c0 = max(r, 0) * 128
                        first = (kb == 0)
                        last = (kb == nkb - 1)
                        pst, Bpst = PB.next(0, 6)
                        S.op("pe", lambda e, pst=pst, k_=k_, q_=q_, kb=kb, c0=c0: e.matmul(
                            pst[:, c0:512], k_[:, kb * 128:(kb + 1) * 128], q_[:, c0:512], start=True, stop=(h >= 6)),
                            reads=[Bk_, Bq_], writes=[Bpst])
                        if h < 6:
                            S.op("pe", lambda e, pst=pst, h=h, c0=c0: e.matmul(pst[:, c0:512], onesr[0:1, :], shrow[0:1, h, c0:512], start=False, stop=True),
                                 reads=[Bk, Bshrow], writes=[Bpst])
                        p_, Bp_ = pT.next()
                        bias_ap = alb[:, h, r + 60:r + 61]
                        S.op("act", lambda e, p_=p_, pst=pst, c0=c0, bias_ap=bias_ap: e.activation(
                            p_[:, c0:512], pst[:, c0:512], AF.Exp, scale=SM_SCALE, bias=bias_ap), reads=[Bpst, Bk], writes=[Bp_])
                        if r >= 0:
                            mc, Bmc = mcr.next()
                            S.op("dve", lambda e, mc=mc, kb=kb, r=r, h=h: e.tensor_tensor(mc[:], maskT[:, kb, r * 128:(r + 1) * 128], corr[:, h, :], ALU.mult),
                                 reads=[BmT, Bk], writes=[Bmc])
                            S.op("dve", lambda e, p_=p_, mc=mc, r=r: e.scalar_tensor_tensor(p_[:, r * 128:(r + 1) * 128], p_[:, r * 128:(r + 1) * 128], 3.0e38, mc[:],
                                                                                         op0=ALU.min, op1=ALU.mult), reads=[Bmc, Bp_], writes=[Bp_])
                            if c0 + 128 < 512:
                                S.op("dve", lambda e, p_=p_, kb=kb, c0=c0: e.scalar_tensor_tensor(p_[:, c0 + 128:512], p_[:, c0 + 128:512], 3.0e38, maskT[:, kb, c0 + 128:512],
                                                                                               op0=ALU.min, op1=ALU.mult), reads=[BmT, Bp_], writes=[Bp_])
                        else:
                            S.op("dve", lambda e, p_=p_, kb=kb: e.scalar_tensor_tensor(p_[:], p_[:], 3.0e38, maskT[:, kb, :], op0=ALU.min, op1=ALU.mult),
                                 reads=[BmT, Bp_], writes=[Bp_])
                        pend.append((kb, p_, Bp_, c0, first, last))
                        if len(pend) > 4:
                            stage2(*pend.pop(0))
                    while pend:
                        stage2(*pend.pop(0))
                    rc, Brc = rec.next()
                    S.op("dve", lambda e, rc=rc, pd=pd: e.reciprocal(rc[:], pd[:]), reads=[Bpd], writes=[Brc])
                    o_, Bo_ = ob.next()
                    S.op("dve", lambda e, o_=o_, po=po, rc=rc: e.tensor_tensor(o_[:], po[:], rc[:], ALU.mult), reads=[Bpo, Brc], writes=[Bo_])
                    S.dma("sp", A["BB"][h, :, g * 512:(g + 1) * 512], o_[:], reads=[Bo_], writes=[B["BB"]], nowaw=True)
                S.barrier()
                S.phase_end()
        for g in groups:
            do_group(g)
        S.es = es_outer

LN_EPS = 1e-5
DN_ALPHA = 2.0 ** 0.25
CAP = 256
NEXP = 32


def layer_norm_tile(S, G, pre, Bpre, out_t, Bout, gb, bb, Bgb, small, Bsmall, junk, Bjunk):
    s1, s2, mean, var, rstd, nmr = small
    S.op("act", lambda e: e.activation(junk[:], pre[:], AF.Identity, accum_out=s1[:]), reads=[Bpre], writes=[Bjunk, Bsmall])
    S.op("act", lambda e: e.activation(junk[:], pre[:], AF.Square, accum_out=s2[:]), reads=[Bpre], writes=[Bjunk, Bsmall])
    S.op("dve", lambda e: e.tensor_scalar(mean[:], s1[:], 1.0 / 2048.0, None, op0=ALU.mult), reads=[Bsmall], writes=[Bsmall])
    S.op("dve", lambda e: e.tensor_tensor(var[:], mean[:], mean[:], ALU.mult), reads=[Bsmall], writes=[Bsmall])
    S.op("dve", lambda e: e.scalar_tensor_tensor(var[:], s2[:], 1.0 / 2048.0, var[:], op0=ALU.mult, op1=ALU.subtract), reads=[Bsmall], writes=[Bsmall])
    S.op("act", lambda e: e.activation(rstd[:], var[:], AF.Sqrt, bias=G["cst"]["eps_ln"][:, 0:1]), reads=[Bsmall, G["Bcst"]], writes=[Bsmall])
    S.op("dve", lambda e: e.reciprocal(rstd[:], rstd[:]), reads=[Bsmall], writes=[Bsmall])
    S.op("dve", lambda e: e.scalar_tensor_tensor(nmr[:], mean[:], -1.0, rstd[:], op0=ALU.mult, op1=ALU.mult), reads=[Bsmall], writes=[Bsmall])
    S.op("act", lambda e: e.activation(pre[:], pre[:], AF.Identity, scale=rstd[:, 0:1], bias=nmr[:, 0:1]), reads=[Bpre, Bsmall], writes=[Bpre])
    S.op("dve", lambda e: e.tensor_tensor(pre[:], pre[:], gb[:], ALU.mult), reads=[Bpre, Bgb], writes=[Bpre])
    S.op("dve", lambda e: e.tensor_tensor(out_t[:], pre[:], bb[:], ALU.add), reads=[Bpre, Bgb], writes=[Bout])


def phase3a_merge(S, G):
    A = G["ap"]
    PB = G["pb"]
    B = G["B"]
    wa = S.sbuf("o_wa", [128, 8, 2048], BF16)
    wb = S.sbuf("o_wb", [128, 8, 2048], BF16)
    Bw = S.buf("o_w")
    S.dma("pool", wa[:], A["w_branch_a"].rearrange("(kc p) n -> p kc n", p=128), writes=[Bw], nowaw=True)
    S.dma("pool", wb[:], A["w_branch_b"].rearrange("(kc p) n -> p kc n", p=128), writes=[Bw], nowaw=True)
    bag = Rot(S, "o_ba", [128, 8, 512], BF16, 2)
    bbg = Rot(S, "o_bb", [128, 8, 512], BF16, 2)
    gtg = Rot(S, "o_gt", [128, 32, 512], BF16, 2)
    t1r = Rot(S, "o_t1", [128, 512], F32, 2)
    t2r = Rot(S, "o_t2", [128, 512], F32, 2)
    mgs = Rot(S, "o_mgs", [128, 512], BF16, 3)
    for g in range(4):
        ba_, Bba = bag.next()
        bb_, Bbb = bbg.next()
        gt_, Bgt = gtg.next()
        S.dma("sp", ba_[:], A["BA"][:, :, g * 512:(g + 1) * 512].rearrange("h p t -> p h t"), reads=[B["BA"]], writes=[Bba])
        S.dma("sp", bb_[:], A["BB"][:, :, g * 512:(g + 1) * 512].rearrange("h p t -> p h t"), reads=[B["BB"]], writes=[Bbb])
        S.dma("sp", gt_[:], A["GT"][:, :, g * 512:(g + 1) * 512].rearrange("c p t -> p c t"), reads=[B["GT"]], writes=[Bgt])
        for c in range(16):
            pa, Bpa = PB.next()
            for k in range(8):
                S.op("pe", lambda e, pa=pa, k=k, c=c, ba_=ba_: e.matmul(pa[:], wa[:, k, c * 128:(c + 1) * 128], ba_[:, k, :], start=(k == 0), stop=(k == 7)),
                     reads=[Bw, Bba], writes=[Bpa])
            pb2, Bpb2 = PB.next()
            for k in range(8):
                S.op("pe", lambda e, pb2=pb2, k=k, c=c, bb_=bb_: e.matmul(pb2[:], wb[:, k, c * 128:(c + 1) * 128], bb_[:, k, :], start=(k == 0), stop=(k == 7)),
                     reads=[Bw, Bbb], writes=[Bpb2])
            t1, Bt1 = t1r.next()
            t2, Bt2 = t2r.next()
            S.op("dve", lambda e, t1=t1, pa=pa, gt_=gt_, c=c: e.tensor_tensor(t1[:], pa[:], gt_[:, c, :], ALU.mult), reads=[Bpa, Bgt], writes=[Bt1])
            S.op("dve", lambda e, t2=t2, pb2=pb2, gt_=gt_, c=c: e.tensor_tensor(t2[:], pb2[:], gt_[:, 16 + c, :], ALU.mult), reads=[Bpb2, Bgt], writes=[Bt2])
            m_, Bm = mgs.next()
            S.op("dve", lambda e, t1=t1, t2=t2, m_=m_: e.tensor_tensor(m_[:], t1[:], t2[:], ALU.add), reads=[Bt1, Bt2], writes=[Bm])
            S.dma("sp", A["MG"][c, :, g * 512:(g + 1) * 512], m_[:], reads=[Bm], writes=[B["MG"]], nowaw=True)


def phase3b_out(S, G):
    A = G["ap"]
    PB = G["pb"]
    B = G["B"]
    P = G["persist"]
    ident_f = G["ident_f"]
    Bw = S.buf("o_w")
    wr = S.sbuf("o_wr", [128, 16, 36], F32)
    S.dma("sp", wr[:], A["wr"].rearrange("(kc p) n -> p kc n", p=128), writes=[Bw], nowaw=True)
    brr = S.sbuf("o_brr", [1, 36], F32)
    S.dma("sp", brr[:], A["brr"], writes=[Bw], nowaw=True)
    g1 = S.sbuf("o_g1", [128, 2048], F32)
    b1 = S.sbuf("o_b1", [128, 2048], F32)
    S.dma("sp", g1[:], A["ln1_gb"], writes=[Bw], nowaw=True)
    S.dma("sp", b1[:], A["ln1_bb"], writes=[Bw], nowaw=True)
    ltri = S.sbuf("o_ltri", [128, 128], BF16)
    S.dma("sp", ltri[:], A["ltri"], writes=[Bw], nowaw=True)
    e256 = S.sbuf("o_e256", [128, 32], F32)
    S.dma("sp", e256[:], A["e256"], writes=[Bw], nowaw=True)
    onesc = S.sbuf("o_onesc", [128, 1], BF16)
    S.op("dve", lambda e: e.memset(onesc[:], 1.0), writes=[Bw])
    onesr = S.sbuf("o_onesr", [1, 128], F32)
    S.op("dve", lambda e: e.memset(onesr[:], 1.0), writes=[Bw])
    cnt = S.sbuf("o_cnt", [1, 32], F32)
    Bcnt = S.buf("o_cnt")
    S.op("dve", lambda e: e.memset(cnt[:], 0.0), writes=[Bcnt])
    S.barrier()
    wo = Rot(S, "o_wo", [128, 16, 512], BF16, 2)
    mgr = Rot(S, "o_mg", [128, 16, 512], BF16, 2)
    pre = Rot(S, "o_pre", [128, 2048], F32, 5)
    h1t = Rot(S, "o_h1", [128, 2048], F32, 2)
    h1b = Rot(S, "o_h1b", [128, 2048], BF16, 2)
    junk = S.sbuf("o_junk", [128, 2048], BF16)
    Bjunk = S.buf("o_junk")
    hT = Rot(S, "o_hT", [128, 16, 128], F32, 1)
    smalls = [S.sbuf("o_sm%d" % i, [128, 1], F32) for i in range(6)]
    Bsmall = S.buf("o_small")
    rt = {n: S.sbuf("o_r_" + n, shp, F32) for n, shp in (
        ("L", [128, 36]), ("gmax", [128, 1]), ("ngmax", [128, 1]), ("ohg", [128, 4]), ("ge", [128, 4]), ("gsum", [128, 1]), ("gp", [128, 1]),
        ("e8", [128, 8]), ("m1", [128, 1]), ("oh1", [128, 8]), ("e8b", [128, 8]), ("m2", [128, 1]), ("oh2", [128, 8]), ("d", [128, 1]),
        ("sg", [128, 1]), ("A1", [128, 32]), ("A2", [128, 32]), ("At", [128, 32]), ("rk", [128, 32]), ("t", [128, 32]), ("i1", [128, 1]), ("i2", [128, 1]))}
    Abf = S.sbuf("o_Abf", [128, 32], BF16)
    Br = G["Br_persist"]

    def rop(fn, eng="dve"):
        S.op(eng, fn, reads=[Br, Bw], writes=[Br])

    for g in range(4):
        mg, Bmg = mgr.next()
        S.dma("sp", mg[:], A["MG"][:, :, g * 512:(g + 1) * 512].rearrange("c p t -> p c t"), reads=[B["MG"]], writes=[Bmg])
        prs = []
        for t in range(4):
            tt = g * 4 + t
            pr, Bpr = pre.next()
            S.dma("sp", pr[:], A["xs"][OWN0 + tt * 128:OWN0 + (tt + 1) * 128, :], writes=[Bpr])
            prs.append((pr, Bpr))
        for cb in range(4):
            w_, Bwo = wo.next()
            S.dma("pool", w_[:], A["w_out"][:, cb * 512:(cb + 1) * 512].rearrange("(kc p) n -> p kc n", p=128), writes=[Bwo])
            for t in range(4):
                pr, Bpr = prs[t]
                po, Bpo = PB.next()
                for c in range(16):
                    S.op("pe", lambda e, po=po, c=c, t=t, w_=w_, mg=mg: e.matmul(po[:], mg[:, c, t * 128:(t + 1) * 128], w_[:, c, :], start=(c == 0), stop=(c == 15)),
                         reads=[Bmg, Bwo], writes=[Bpo])
                S.op("dve", lambda e, pr=pr, po=po, cb=cb: e.scalar_tensor_tensor(pr[:, cb * 512:(cb + 1) * 512], pr[:, cb * 512:(cb + 1) * 512], DN_ALPHA, po[:],
                                                                                op0=ALU.mult, op1=ALU.add), reads=[Bpo, Bpr], writes=[Bpr])
        for t in range(4):
            tt = g * 4 + t
            pr, Bpr = prs[t]
            h_, Bh = h1t.next()
            layer_norm_tile(S, G, pr, Bpr, h_, Bh, g1, b1, Bw, smalls, Bsmall, junk, Bjunk)
            S.dma("sp", A["H1"][tt * 128:(tt + 1) * 128, :], h_[:], reads=[Bh], writes=[B["H1"]], nowaw=True)
            hb, Bhb = h1b.next()
            S.op("act", lambda e, hb=hb, h_=h_: e.activation(hb[:], h_[:], AF.Copy), reads=[Bh], writes=[Bhb])
            S.dma("sp", A["H1b"][tt * 128:(tt + 1) * 128, :], hb[:], reads=[Bhb], writes=[B["H1b"]], nowaw=True)
            hT_, BhT = hT.next()
            for q4 in range(4):
                pb, Bpb = PB.next()
                for j in range(4):
                    c = q4 * 4 + j
                    S.op("pe", lambda e, pb=pb, h_=h_, c=c, j=j: e.matmul(pb[:, j * 128:(j + 1) * 128], h_[:, c * 128:(c + 1) * 128], ident_f[:], start=True, stop=True),
                         reads=[Bh, G["Bidf"]], writes=[Bpb])
                S.op("act", lambda e, pb=pb, hT_=hT_, q4=q4: e.activation(hT_[:, q4 * 4:(q4 + 1) * 4, :], pb[:].rearrange("p (a b) -> p a b", a=4), AF.Copy),
                     reads=[Bpb], writes=[BhT])
            pl, Bpl = PB.next()
            for c in range(16):
                S.op("pe", lambda e, pl=pl, hT_=hT_, c=c: e.matmul(pl[:, 0:36], hT_[:, c, :], wr[:, c, :], start=(c == 0), stop=False), reads=[BhT, Bw], writes=[Bpl])
            S.op("pe", lambda e, pl=pl: e.matmul(pl[:, 0:36], onesr[0:1, :], brr[0:1, :], start=False, stop=True), reads=[Bw], writes=[Bpl])
            L = rt["L"]
            S.op("dve", lambda e, pl=pl: e.tensor_copy(L[:], pl[:, 0:36]), reads=[Bpl], writes=[Br])
            rop(lambda e: e.reduce_max(rt["gmax"][:], L[:, 0:4], AX.X))
            rop(lambda e: e.tensor_scalar(rt["ohg"][:], L[:, 0:4], rt["gmax"][:, 0:1], None, op0=ALU.is_equal))
            rop(lambda e: e.tensor_scalar(rt["ngmax"][:], rt["gmax"][:], -1.0, None, op0=ALU.mult))
            rop(lambda e: e.activation(rt["ge"][:], L[:, 0:4], AF.Exp, bias=rt["ngmax"][:, 0:1], accum_out=rt["gsum"][:]), "act")
            rop(lambda e: e.reciprocal(rt["gp"][:], rt["gsum"][:]))
            rop(lambda e: e.tensor_scalar(rt["e8"][:], L[:, 4:12], rt["ohg"][:, 0:1], None, op0=ALU.mult))
            for gg in range(1, 4):
                rop(lambda e, gg=gg: e.scalar_tensor_tensor(rt["e8"][:], L[:, 4 + 8 * gg:12 + 8 * gg], rt["ohg"][:, gg:gg + 1], rt["e8"][:], op0=ALU.mult, op1=ALU.add))
            rop(lambda e: e.reduce_max(rt["m1"][:], rt["e8"][:], AX.X))
            rop(lambda e: e.tensor_scalar(rt["oh1"][:], rt["e8"][:], rt["m1"][:, 0:1], None, op0=ALU.is_equal))
            rop(lambda e: e.scalar_tensor_tensor(rt["e8b"][:], rt["oh1"][:], -1.0e30, rt["e8"][:], op0=ALU.mult, op1=ALU.add))
            rop(lambda e: e.reduce_max(rt["m2"][:], rt["e8b"][:], AX.X))
            rop(lambda e: e.tensor_scalar(rt["oh2"][:], rt["e8b"][:], rt["m2"][:, 0:1], None, op0=ALU.is_equal))
            rop(lambda e: e.tensor_tensor(rt["d"][:], rt["m1"][:], rt["m2"][:], ALU.subtract))
            rop(lambda e: e.activation(rt["sg"][:], rt["d"][:], AF.Sigmoid), "act")
            rop(lambda e, tt=tt: e.tensor_tensor(P["wts"][:, tt, 0:1], rt["sg"][:], rt["gp"][:], ALU.mult))
            rop(lambda e, tt=tt: e.tensor_tensor(P["wts"][:, tt, 1:2], rt["gp"][:], P["wts"][:, tt, 0:1], ALU.subtract))
            for gg in range(4):
                rop(lambda e, gg=gg: e.tensor_scalar(rt["A1"][:, gg * 8:(gg + 1) * 8], rt["oh1"][:], rt["ohg"][:, gg:gg + 1], None, op0=ALU.mult))
                rop(lambda e, gg=gg: e.tensor_scalar(rt["A2"][:, gg * 8:(gg + 1) * 8], rt["oh2"][:], rt["ohg"][:, gg:gg + 1], None, op0=ALU.mult))
            rop(lambda e: e.tensor_tensor(rt["At"][:], rt["A1"][:], rt["A2"][:], ALU.add))
            rop(lambda e: e.tensor_copy(Abf[:], rt["At"][:]))
            pk, Bpk = PB.next()
            S.op("pe", lambda e, pk=pk: e.matmul(pk[:, 0:32], ltri[:], Abf[:], start=True, stop=False), reads=[Bw, Br], writes=[Bpk])
            S.op("pe", lambda e, pk=pk: e.matmul(pk[:, 0:32], onesr[0:1, :], cnt[0:1, :], start=False, stop=True), reads=[Bw, Bcnt], writes=[Bpk])
            pc, Bpc = PB.next()
            S.op("pe", lambda e, pc=pc: e.matmul(pc[0:1, 0:32], onesc[:], Abf[:], start=True, stop=True), reads=[Bw, Br], writes=[Bpc])
            S.op("dve", lambda e, pk=pk: e.tensor_copy(rt["rk"][:], pk[:, 0:32]), reads=[Bpk, Br], writes=[Br])
            S.op("dve", lambda e, pc=pc: e.tensor_tensor(cnt[:], cnt[:], pc[0:1, 0:32], ALU.add), reads=[Bpc, Bcnt], writes=[Bcnt])
            rop(lambda e: e.scalar_tensor_tensor(rt["t"][:], rt["rk"][:], 1.0, rt["At"][:], op0=ALU.add, op1=ALU.mult))
            rop(lambda e, tt=tt: e.tensor_scalar(P["RK"][:, tt, :], rt["t"][:], -1.0, None, op0=ALU.add))
            rop(lambda e: e.tensor_tensor(rt["rk"][:], rt["rk"][:], e256[:], ALU.add))
            rop(lambda e: e.tensor_tensor(rt["t"][:], rt["rk"][:], rt["A1"][:], ALU.mult))
            rop(lambda e: e.reduce_sum(rt["i1"][:], rt["t"][:], AX.X))
            rop(lambda e: e.tensor_tensor(rt["t"][:], rt["rk"][:], rt["A2"][:], ALU.mult))
            rop(lambda e: e.reduce_sum(rt["i2"][:], rt["t"][:], AX.X))
            rop(lambda e, tt=tt: e.tensor_copy(P["idx"][:, tt, 0:1], rt["i1"][:]))
            rop(lambda e, tt=tt: e.tensor_copy(P["idx"][:, tt, 1:2], rt["i2"][:]))


def phase4_moe(S, G, experts=range(NEXP)):
    A = G["ap"]
    PB = G["pb"]
    B = G["B"]
    P = G["persist"]
    h1b = S.sbuf("m_h1b", [128, 16, 2048], BF16)
    Bh1b = S.buf("m_h1b")
    S.dma("sp", h1b[:], A["H1b"].rearrange("(t p) d -> p t d", p=128), reads=[B["H1b"]], writes=[Bh1b])
    iot = S.sbuf("m_iota", [128, CAP], F32)
    Biot = S.buf("m_iota")
    S.dma("sp", iot[:], A["iota256"], writes=[Biot])
    wu = Rot(S, "m_w", [128, 16, 512], BF16, 4)
    wd = Rot(S, "m_wd", [128, 4, 2048], BF16, 2)
    sel = Rot(S, "m_sel", [128, 16, CAP], BF16, 1)
    xs_ = Rot(S, "m_xs", [128, 16, CAP], BF16, 1)
    hT = Rot(S, "m_hT", [128, 8, CAP], BF16, 2)
    sg = Rot(S, "m_sg", [128, CAP], F32, 3)
    yst = Rot(S, "m_y", [128, 1024], F32, 1)
    for e_ in experts:
        s_, Bs = sel.next()
        for tt in range(16):
            S.op("dve", lambda e, s_=s_, tt=tt, e_=e_: e.tensor_scalar(s_[:, tt, :], iot[:], P["RK"][:, tt, e_:e_ + 1], None, op0=ALU.is_equal),
                 reads=[Biot, G["Br_persist"]], writes=[Bs])
        x_, Bx = xs_.next()
        for kc in range(16):
            pb, Bpb = PB.next()
            for tt in range(16):
                S.op("pe", lambda e, pb=pb, tt=tt, kc=kc, s_=s_: e.matmul(pb[:, 0:CAP], h1b[:, tt, kc * 128:(kc + 1) * 128], s_[:, tt, :],
                                                                         start=(tt == 0), stop=(tt == 15)), reads=[Bh1b, Bs], writes=[Bpb])
            if kc % 2 == 0:
                S.op("act", lambda e, pb=pb, x_=x_, kc=kc: e.activation(x_[:, kc, :], pb[:, 0:CAP], AF.Copy), reads=[Bpb], writes=[Bx])
            else:
                S.op("dve", lambda e, pb=pb, x_=x_, kc=kc: e.tensor_copy(x_[:, kc, :], pb[:, 0:CAP]), reads=[Bpb], writes=[Bx])
        h_, Bh = hT.next()
        for half in range(2):
            wg_, Bwg = wu.next()
            S.dma("pool", wg_[:], A["w_gate"][e_, :, half * 512:(half + 1) * 512].rearrange("(kc p) n -> p kc n", p=128), writes=[Bwg])
            wu_, Bwu = wu.next()
            S.dma("pool", wu_[:], A["w_up"][e_, :, half * 512:(half + 1) * 512].rearrange("(kc p) n -> p kc n", p=128), writes=[Bwu])
            for f4 in range(4):
                f = half * 4 + f4
                pg, Bpg = PB.next()
                for kc in range(16):
                    S.op("pe", lambda e, pg=pg, kc=kc, f4=f4, wg_=wg_, x_=x_: e.matmul(pg[:, 0:CAP], wg_[:, kc, f4 * 128:(f4 + 1) * 128], x_[:, kc, :],
                                                                                    start=(kc == 0), stop=(kc == 15)), reads=[Bwg, Bx], writes=[Bpg])
                for kc in range(16):
                    S.op("pe", lambda e, pg=pg, kc=kc, f4=f4, wu_=wu_, x_=x_: e.matmul(pg[:, CAP:2 * CAP], wu_[:, kc, f4 * 128:(f4 + 1) * 128], x_[:, kc, :],
                                                                                    start=(kc == 0), stop=(kc == 15)), reads=[Bwu, Bx], writes=[Bpg])
                s1, Bs1 = sg.next()
                S.op("act", lambda e, s1=s1, pg=pg: e.activation(s1[:], pg[:, 0:CAP], AF.Silu), reads=[Bpg], writes=[Bs1])
                S.op("dve", lambda e, h_=h_, f=f, s1=s1, pg=pg: e.tensor_tensor(h_[:, f, :], s1[:], pg[:, CAP:2 * CAP], ALU.mult), reads=[Bs1, Bpg], writes=[Bh])
        wds = []
        for half in range(2):
            wd_, Bwd = wd.next()
            S.dma("pool", wd_[:], A["w_down"][e_, half * 512:(half + 1) * 512, :].rearrange("(fc p) n -> p fc n", p=128), writes=[Bwd])
            wds.append((wd_, Bwd))
        for rh in range(CAP // 128):
            for cbp in range(2):
                y_, By = yst.next()
                for c2 in range(2):
                    cb = cbp * 2 + c2
                    py, Bpy = PB.next()
                    for f in range(8):
                        wd_, Bwd = wds[f // 4]
                        S.op("pe", lambda e, py=py, f=f, rh=rh, cb=cb, wd_=wd_, h_=h_: e.matmul(py[:], h_[:, f, rh * 128:(rh + 1) * 128], wd_[:, f % 4, cb * 512:(cb + 1) * 512],
                                                                                             start=(f == 0), stop=(f == 7)), reads=[Bh, Bwd], writes=[Bpy])
                    if c2 == 0:
                        S.op("act", lambda e, y_=y_, py=py, c2=c2: e.activation(y_[:, c2 * 512:(c2 + 1) * 512], py[:], AF.Copy), reads=[Bpy], writes=[By])
                    else:
                        S.op("dve", lambda e, y_=y_, py=py, c2=c2: e.tensor_copy(y_[:, c2 * 512:(c2 + 1) * 512], py[:]), reads=[Bpy], writes=[By])
                S.dma("sp", A["Y"][e_ * CAP + rh * 128:e_ * CAP + (rh + 1) * 128, cbp * 1024:(cbp + 1) * 1024], y_[:], reads=[By], writes=[B["Y"]], nowaw=True)


def phase5_final(S, G):
    A = G["ap"]
    B = G["B"]
    P = G["persist"]
    g2 = S.sbuf("f_g2", [128, 2048], F32)
    b2 = S.sbuf("f_b2", [128, 2048], F32)
    Bw = S.buf("f_w")
    S.dma("sp", g2[:], A["ln2_gb"], writes=[Bw], nowaw=True)
    S.dma("sp", b2[:], A["ln2_bb"], writes=[Bw], nowaw=True)
    idxi = S.sbuf("f_idx", [128, 16, 2], U32)
    Bidx = S.buf("f_idx")
    S.op("dve", lambda e: e.tensor_copy(idxi[:], P["idx"][:]), reads=[G["Br_persist"]], writes=[Bidx])
    S.barrier()
    h1 = Rot(S, "f_h1", [128, 2048], F32, 2)
    y1 = Rot(S, "f_y1", [128, 2048], F32, 2)
    y2 = Rot(S, "f_y2", [128, 2048], F32, 2)
    ot = Rot(S, "f_ot", [128, 2048], F32, 2)
    junk = S.sbuf("f_junk", [128, 2048], BF16)
    Bjunk = S.buf("f_junk")
    smalls = [S.sbuf("f_sm%d" % i, [128, 1], F32) for i in range(6)]
    Bsmall = S.buf("f_small")
    for tt in range(16):
        h_, Bh = h1.next()
        S.dma("sp", h_[:], A["H1"][tt * 128:(tt + 1) * 128, :], reads=[B["H1"]], writes=[Bh])
        a_, Ba = y1.next()
        b_, Bb = y2.next()
        for (dst, Bd, k) in ((a_, Ba, 0), (b_, Bb, 1)):
            S.dma("pool", None, None, reads=[B["Y"], Bidx], writes=[Bd],
                  builder=lambda e, dst=dst, tt=tt, k=k: e.indirect_dma_start(
                      out=dst[:], out_offset=None, in_=A["Y"], in_offset=bass.IndirectOffsetOnAxis(ap=idxi[:, tt, k:k + 1], axis=0),
                      bounds_check=NEXP * CAP - 1, oob_is_err=False))
        S.op("dve", lambda e, a_=a_, tt=tt: e.tensor_scalar(a_[:], a_[:], P["wts"][:, tt, 0:1], None, op0=ALU.mult), reads=[Ba, G["Br_persist"]], writes=[Ba])
        S.op("dve", lambda e, a_=a_, b_=b_, tt=tt: e.scalar_tensor_tensor(a_[:], b_[:], P["wts"][:, tt, 1:2], a_[:], op0=ALU.mult, op1=ALU.add),
             reads=[Ba, Bb, G["Br_persist"]], writes=[Ba])
        S.op("dve", lambda e, a_=a_, h_=h_: e.scalar_tensor_tensor(a_[:], h_[:], DN_ALPHA, a_[:], op0=ALU.mult, op1=ALU.add), reads=[Ba, Bh], writes=[Ba])
        o_, Bo = ot.next()
        layer_norm_tile(S, G, a_, Ba, o_, Bo, g2, b2, Bw, smalls, Bsmall, junk, Bjunk)
        S.dma("sp", A["out"][tt * 128:(tt + 1) * 128, :], o_[:], reads=[Bo], writes=[B["out"]], nowaw=True)

from contextlib import ExitStack
from concourse.bass_utils import run_bass_kernel_spmd

BFM_IDX = {"af": 0, "aq": 8, "ag": 16, "bk": 24, "bq": 32, "iq": 40, "ik": 44, "g": 45}
NBFM = 77

SCRATCH = {
    "KT": ([8, 128, 8192], "bf16"), "VH": ([8, 128, 64, 128], "bf16"), "KIT": ([128, 8192], "bf16"),
    "HG_kdec": ([8, 128, 8192], "bf16"), "HG_kend": ([8192, 1024], "bf16"), "HG_v": ([8192, 1024], "bf16"),
    "HG_dec": ([128, 8, 128], "f32"), "HG_qdec": ([8, 128, 2048], "bf16"), "HG_gs": ([8, 128, 2048], "bf16"),
    "QT": ([8, 128, 2048], "bf16"), "QIT": ([4, 128, 2048], "bf16"), "WI": ([2048, 8], "f32"),
    "GT": ([32, 128, 2048], "bf16"), "BA": ([8, 128, 2048], "bf16"), "BB": ([8, 128, 2048], "bf16"),
    "MG": ([16, 128, 2048], "bf16"), "H1": ([2048, 2048], "f32"), "H1b": ([2048, 2048], "bf16"), "Y": ([NEXP * CAP, 2048], "f32"),
}

INPUTS = {
    "xs": ([8192, 2048], "f32"), "valid_tm": ([128, 64], "f32"), "w_in": ([2048, 11848], "f32"),
    "ident": ([128, 128], "f32"), "bfm": ([128, NBFM], "f32"), "brow": ([1, 2056], "f32"),
    "lbl": ([128, 2, 8], "f32"), "normg": ([128, 8], "f32"), "rmask": ([128, 512], "f32"), "bdmask": ([128, 128], "f32"),
    "alb": ([128, 8, 64], "f32"), "corr": ([128, 8, 128], "bf16"), "sel2": ([2, 128], "bf16"), "dtab": ([128, 8192], "bf16"),
    "qrel": ([1, 512], "f32"), "adm": ([16, 2, 8192], "bf16"),
    "w_branch_a": ([1024, 2048], "f32"), "w_branch_b": ([1024, 2048], "f32"), "w_out": ([2048, 2048], "f32"),
    "wr": ([2048, 36], "f32"), "brr": ([1, 36], "f32"),
    "ln1_gb": ([128, 2048], "f32"), "ln1_bb": ([128, 2048], "f32"), "ln2_gb": ([128, 2048], "f32"), "ln2_bb": ([128, 2048], "f32"),
    "ltri": ([128, 128], "bf16"), "e256": ([128, 32], "f32"), "iota256": ([128, CAP], "f32"),
    "w_gate": ([NEXP, 2048, 1024], "f32"), "w_up": ([NEXP, 2048, 1024], "f32"), "w_down": ([NEXP, 1024, 2048], "f32"),
}


def _dt(s):
    return {"f32": F32, "bf16": BF16, "u32": U32, "i32": I32}[s]


def host_consts(inputs):
    b_in = np.asarray(inputs["b_in"][0], np.float32)
    bfm = np.zeros((128, NBFM), np.float32)
    def put(idx, c0, n):
        for c in range(n):
            bfm[:, idx + c] = b_in[c0 + c * 128: c0 + (c + 1) * 128]
    put(BFM_IDX["af"], C_AF, 8); put(BFM_IDX["aq"], C_AQ, 8); put(BFM_IDX["ag"], C_AG, 8)
    put(BFM_IDX["bk"], C_BK, 8); put(BFM_IDX["bq"], C_BQ, 8); put(BFM_IDX["iq"], C_IQ, 4)
    put(BFM_IDX["g"], C_G, 32)
    bfm[0:64, BFM_IDX["ik"]] = b_in[C_IK:C_IK + 64]
    bfm[64:128, BFM_IDX["ik"]] = b_in[C_IK:C_IK + 64]
    brow = np.concatenate([b_in[C_AI:C_AI + 1024], b_in[C_BV:C_BV + 1024], b_in[C_IW:C_IW + 8]])[None, :].astype(np.float32)
    lbl = np.ascontiguousarray(np.asarray(inputs["hg_lb_logits"], np.float32).reshape(2, 8, 128).transpose(2, 0, 1))
    normg = np.ascontiguousarray(np.asarray(inputs["hg_norm_g"][0], np.float32).reshape(8, 128).T)
    rmask = np.ones((128, 512), np.float32)
    rmask[:, ::64] = 0.0
    ii = np.arange(128)
    bdmask = ((ii[:, None] // 64 == ii[None, :] // 64) & (ii[:, None] <= ii[None, :])).astype(np.float32)
    import ml_dtypes
    bf = ml_dtypes.bfloat16
    slopes = 2.0 ** -(np.arange(8) + 1.0)
    pp = np.arange(128)
    alb = (slopes[None, :, None] * (pp[:, None, None] + 128.0 * (np.arange(64)[None, None, :] - 60))).astype(np.float32)
    dsq = np.maximum(pp[:, None] - pp[None, :], 0).astype(np.float64)
    corr = np.exp(-2.0 * slopes[None, :, None] * dsq[:, None, :]).astype(bf)
    sel2 = np.zeros((2, 128), np.float32); sel2[0, :64] = 1; sel2[1, 64:] = 1
    dtab = np.abs(8064 + pp[:, None] - np.arange(8192)[None, :]).astype(bf)
    qrel = np.arange(512, dtype=np.float32)[None, :]
    extra = {}
    if "w_out" in inputs:
        extra["w_branch_a"] = np.ascontiguousarray(inputs["w_branch_a"][0]); extra["w_branch_b"] = np.ascontiguousarray(inputs["w_branch_b"][0])
        extra["w_out"] = np.ascontiguousarray(inputs["w_out"][0])
        extra["wr"] = np.ascontiguousarray(np.concatenate([inputs["w_group"][0], inputs["w_router"][0]], axis=1).astype(np.float32))
        extra["brr"] = np.concatenate([inputs["b_group"][0], inputs["b_router"][0]])[None, :].astype(np.float32)
        for nm in ("ln1_g", "ln1_b", "ln2_g", "ln2_b"):
            extra[nm + "b"] = np.ascontiguousarray(np.broadcast_to(np.asarray(inputs[nm][0], np.float32)[None, :], (128, 2048)))
        extra["ltri"] = (pp[:, None] < pp[None, :]).astype(bf)
        extra["e256"] = np.ascontiguousarray(np.broadcast_to((np.arange(32, dtype=np.float32) * CAP)[None, :], (128, 32)))
        extra["iota256"] = np.ascontiguousarray(np.broadcast_to(np.arange(CAP, dtype=np.float32)[None, :], (128, CAP)))
        extra["w_gate"] = np.ascontiguousarray(inputs["w_gate"][0]); extra["w_up"] = np.ascontiguousarray(inputs["w_up"][0])
        extra["w_down"] = np.ascontiguousarray(inputs["w_down"][0])
    return {**extra, "alb": alb, "corr": corr, "sel2": sel2.astype(bf), "dtab": dtab, "qrel": qrel, "bdmask": bdmask, "ident": np.eye(128, dtype=np.float32), "bfm": bfm, "brow": brow, "lbl": lbl, "normg": normg, "rmask": rmask,
            "w_in": np.ascontiguousarray(inputs["w_in"][0])}


def host_core_inputs(inputs, hc, core):
    b, j = core // 4, core % 4
    x = np.asarray(inputs["x"], np.float32)
    xs = np.zeros((8192, 2048), np.float32)
    npre = (3 - j) * 2048
    xs[npre:] = x[b, :(j + 1) * 2048]
    valid = np.zeros(8192, np.float32)
    valid[npre:] = 1.0
    d = dict(hc)
    d["xs"] = xs
    d["valid_tm"] = np.ascontiguousarray(valid.reshape(64, 128).T)
    import ml_dtypes
    chunk = np.arange(8192) // 64
    adm = np.full((16, 2, 8192), NEG_ADM, np.float32)
    for t in range(16):
        c_first = (OWN0 + t * 128) // 64
        adm[t, 0, (valid > 0) & (chunk <= c_first)] = 0.0
        adm[t, 1, (valid > 0) & (chunk <= c_first + 1)] = 0.0
    d["adm"] = adm.astype(ml_dtypes.bfloat16)
    return d


def build_program(phases=("p1a",), dump=(), p1_blocks=(0, 1, 2, 3), p2_groups=(0, 1, 2, 3), in_names=None):
    nc = bass.Bass("TRN2", target_bir_lowering=False)
    A = {}
    used_inputs = in_names if in_names is not None else list(INPUTS)
    for n in used_inputs:
        shp, dt = INPUTS[n]
        A[n] = nc.dram_tensor(n, shp, _dt(dt), kind="ExternalInput").ap()
    for n, (shp, dt) in SCRATCH.items():
        kind = "ExternalOutput" if n in dump else "Internal"
        A[n] = nc.dram_tensor(n, shp, _dt(dt), kind=kind).ap()
    A["out"] = nc.dram_tensor("out", [2048, 2048], F32, kind="ExternalOutput").ap()
    with ExitStack() as es:
        S = Sched(nc, es)
        G = {"ap": A, "pb": PBanks(S), "B": {n: S.buf(n, glob=True) for n in list(SCRATCH) + ["out"]}, "bfm_idx": BFM_IDX}
        G["persist"] = {"wts": S.sbuf("p_wts", [128, 16, 2], F32), "RK": S.sbuf("p_RK", [128, 16, 32], F32), "idx": S.sbuf("p_idx", [128, 16, 2], F32)}
        G["Br_persist"] = S.buf("persist", glob=True)
        cst = {}
        Bc = S.buf("cst", glob=True)
        G["cst"] = cst
        G["Bcst"] = Bc
        idf = S.sbuf("idf", [128, 128], F32)
        Bidf = S.buf("idf", glob=True)
        S.dma("sp", idf[:], A["ident"], writes=[Bidf])
        idb = S.sbuf("idb", [128, 128], BF16)
        Bident = S.buf("idb")
        S.op("dve", lambda e: e.tensor_copy(idb[:], idf[:]), reads=[Bidf], writes=[Bident])
        G["ident_bf"] = idb
        G["Bident"] = Bident
        G["ident_f"] = idf
        G["Bidf"] = Bidf
        for n in ("bfm", "brow", "valid_tm", "normg", "rmask", "bdmask"):
            shp, dt = INPUTS[n]
            cst[n] = S.sbuf("c_" + n, shp, _dt(dt))
            S.dma("sp", cst[n][:], A[n], writes=[Bc], nowaw=True)
        lbl = S.sbuf("c_lbl", [128, 2, 8], F32)
        Blbl = S.buf("lbl", glob=True)
        S.dma("sp", lbl[:], A["lbl"], writes=[Blbl])
        for n in ("lb", "oml", "noml", "lbd"):
            cst[n] = S.sbuf("c_" + n, [128, 8], F32)
        cst["ones_row"] = S.sbuf("c_ones_row", [1, 128], F32)
        S.op("dve", lambda e: e.memset(cst["ones_row"][:], 1.0), writes=[Bc])
        S.op("dve", lambda e: e.tensor_tensor(cst["lbd"][:], lbl[:, 0, :], lbl[:, 1, :], ALU.subtract), reads=[Blbl], writes=[Bc])
        S.op("act", lambda e: e.activation(cst["lb"][:], cst["lbd"][:], AF.Sigmoid), reads=[Bc], writes=[Bc])
        S.op("dve", lambda e: e.tensor_scalar(cst["oml"][:], cst["lb"][:], -1.0, 1.0, op0=ALU.mult, op1=ALU.add), reads=[Bc], writes=[Bc])
        S.op("dve", lambda e: e.tensor_scalar(cst["noml"][:], cst["oml"][:], -1.0, None, op0=ALU.mult), reads=[Bc], writes=[Bc])

        cst["eps_ln"] = S.sbuf("c_eps_ln", [128, 1], F32)
        S.op("dve", lambda e: e.memset(cst["eps_ln"][:], LN_EPS), writes=[Bc])
        cst["eps_rms"] = S.sbuf("c_eps_rms", [128, 1], F32)
        S.op("dve", lambda e: e.memset(cst["eps_rms"][:], RMS_EPS), writes=[Bc])
        S.barrier()
        if "p1a" in phases:
            with ExitStack() as pes:
                S.es = pes
                phase1a(S, G, blocks=p1_blocks)
                S.es = es
            S.barrier()
            S.phase_end()
        if "p1b" in phases:
            with ExitStack() as pes:
                S.es = pes
                phase1b(S, G)
                S.es = es
            S.barrier()
            S.phase_end()
        if "p2" in phases:
            phase2_dsa(S, G, groups=p2_groups)
            S.barrier()
            S.phase_end()
        for nm, fn in (("p3a", phase3a_merge), ("p3b", phase3b_out), ("p4", phase4_moe), ("p5", phase5_final)):
            if nm in phases:
                with ExitStack() as pes:
                    S.es = pes
                    fn(S, G)
                    S.es = es
                S.barrier()
                S.phase_end()
        outs = [G["B"][n] for n in dump] + ([G["B"]["out"]] if "p5" in phases else [])
        S.wait_all("sp", outs)
        print("instructions:", S.ninst, {k: len(v) for k, v in S.ops.items()})
        S.run()
    return nc


ALL_PHASES = ("p1a", "p1b", "p2", "p3a", "p3b", "p4", "p5")
_CACHE = {}


def kernel(**inputs):
    if "nc" not in _CACHE:
        _CACHE["nc"] = build_program(phases=ALL_PHASES)
    nc = _CACHE["nc"]
    hc = host_consts(inputs)
    in_maps = [host_core_inputs(inputs, hc, c) for c in range(8)]
    res = run_bass_kernel_spmd(nc, in_maps, core_ids=list(range(8)))
    out = np.zeros((2, 8192, 2048), np.float32)
    for c in range(8):
        b, j = c // 4, c % 4
        out[b, j * 2048:(j + 1) * 2048] = np.asarray(res.results[c]["out"])
    return out
```

```python
import numpy as np
import concourse.bass as bass
import concourse.mybir as mybir

F32 = mybir.dt.float32
BF16 = mybir.dt.bfloat16
U32 = mybir.dt.uint32
I32 = mybir.dt.int32
U8 = mybir.dt.uint8
AF = mybir.ActivationFunctionType
ALU = mybir.AluOpType
AX = mybir.AxisListType


class Buf:
    __slots__ = ("name", "lastw", "readers", "dsem", "dcount", "glob", "dkey")

    def __init__(self, name, glob=False):
        self.name = name
        self.glob = glob
        self.dkey = None
        self.lastw = None
        self.readers = {}
        self.dsem = None
        self.dcount = 0


class Sched:
    ENGS = ("pe", "act", "dve", "pool", "sp")
    SEM_LIMIT = 30000

    def __init__(self, nc, es):
        self.nc = nc
        self.es = es
        self.es_sem = es
        self.dbufs = []
        self.dstate = {}
        self.free_dsems = []
        self.local_dbufs = []
        self.sem = {}
        self.count = {}
        self.known = {}
        self.ops = {}
        self.epoch = {}
        for n in self.ENGS:
            self.sem[n] = es.enter_context(nc.semaphore("se_" + n))
            self.count[n] = 0
            self.epoch[n] = 0
            self.known[n] = {}
            self.ops[n] = []
        self.nbuf = 0
        self.ninst = 0

    def sbuf(self, name, shape, dtype):
        self.nbuf += 1
        name = "%s_u%d" % (name, self.nbuf)
        return self.es.enter_context(self.nc.sbuf_tensor(name, list(shape), dtype))

    def psum(self, name, shape, dtype):
        return self.es.enter_context(self.nc.psum_tensor(name, list(shape), dtype))

    def buf(self, name=None, glob=False):
        self.nbuf += 1
        return Buf("%s_b%d" % (name or "b", self.nbuf), glob)

    def bufs(self, n, name="b"):
        return [self.buf("%s%d" % (name, i)) for i in range(n)]

    def _waits(self, eng, reads, writes):
        need = {}

        def add(ev, skip_same):
            if ev is None:
                return
            key, sem, val, prod = ev
            if skip_same and prod == eng:
                return
            if self.known[eng].get(key, 0) >= val:
                return
            if key not in need or need[key][1] < val:
                need[key] = (sem, val)

        for b in reads:
            add(b.lastw, False)
        for b in writes:
            add(b.lastw, True)
            for ev in b.readers.values():
                add(ev, True)
        for key, (sem, val) in need.items():
            self.known[eng][key] = val
        return list(need.values())

    def op(self, eng, fn, reads=(), writes=()):
        waits = self._waits(eng, reads, writes)
        if self.count[eng] >= self.SEM_LIMIT:
            self.epoch[eng] += 1
            self.count[eng] = 0
            self.sem[eng] = self.es_sem.enter_context(self.nc.semaphore("se_%s_%d" % (eng, self.epoch[eng])))
        self.count[eng] += 1
        seq = self.count[eng]
        sem = self.sem[eng]
        key = "e_%s_%d" % (eng, self.epoch[eng])
        ev = (key, sem, seq, eng)
        for b in writes:
            b.lastw = ev
            b.readers = {}
        for b in reads:
            b.readers[key] = ev
        self.ninst += 1 + len(waits)

        def emit(e, fn=fn, waits=waits, sem=sem):
            for (s, v) in waits:
                e.wait_ge(s, v)
            fn(e).then_inc(sem, 1)

        self.ops[eng].append(emit)
        return ev

    def dma(self, q, out_ap, in_ap, reads=(), writes=(), nowaw=False, builder=None, **kw):
        waits = self._waits(q, reads, [] if nowaw else writes)
        tb = writes[0]
        if tb.dsem is None:
            if (not tb.glob) and self.free_dsems:
                tb.dsem, tb.dcount, tb.dkey = self.free_dsems.pop()
            else:
                tb.dsem = self.es_sem.enter_context(self.nc.semaphore("sd_" + tb.name))
                tb.dkey = "d_" + tb.name
            if not tb.glob:
                self.local_dbufs.append(tb)
        tb.dcount += 16
        self.dstate[tb.dkey] = (tb.dsem, tb.dcount)
        ev = (tb.dkey, tb.dsem, tb.dcount, None)
        for b in writes:
            b.lastw = ev
            if not nowaw:
                b.readers = {}
        for b in reads:
            b.readers[ev[0]] = ev
        self.ninst += 1 + len(waits)

        def emit(e, waits=waits, sem=tb.dsem, out_ap=out_ap, in_ap=in_ap, kw=kw, builder=builder):
            for (s, v) in waits:
                e.wait_ge(s, v)
            if builder is not None:
                builder(e).then_inc(sem, 16)
            else:
                e.dma_start(out=out_ap, in_=in_ap, **kw).then_inc(sem, 16)

        self.ops[q].append(emit)
        return ev

    def phase_end(self):
        for b in self.local_dbufs:
            self.free_dsems.append((b.dsem, b.dcount, b.dkey))
            b.dsem = None
        self.local_dbufs = []

    def raw(self, eng, fn):
        self.ops[eng].append(lambda e, fn=fn: fn(e))

    def wait_all(self, eng, bufs):
        waits = self._waits(eng, list(bufs), [])

        def emit(e, waits=waits):
            for (s, v) in waits:
                e.wait_ge(s, v)

        self.ops[eng].append(emit)

    def barrier(self):
        evs = []
        for n in self.ENGS:
            if self.count[n] > 0:
                evs.append(("e_%s_%d" % (n, self.epoch[n]), self.sem[n], self.count[n], n))
        for key, (sem, cnt) in self.dstate.items():
            evs.append((key, sem, cnt, None))
        for eng in self.ENGS:
            waits = []
            for (key, sem, val, prod) in evs:
                if prod == eng:
                    continue
                if self.known[eng].get(key, 0) >= val:
                    continue
                self.known[eng][key] = val
                waits.append((sem, val))

            def emit(e, waits=waits):
                for (s, v) in waits:
                    e.wait_ge(s, v)

            self.ops[eng].append(emit)

    def run(self):
        nc = self.nc
        ops = self.ops
        with nc.Block() as block:
            @block.tensor
            def _(e):
                for f in ops["pe"]:
                    f(e)

            @block.scalar
            def _(e):
                for f in ops["act"]:
                    f(e)

            @block.vector
            def _(e):
                for f in ops["dve"]:
                    f(e)

            @block.gpsimd
            def _(e):
                for f in ops["pool"]:
                    f(e)

            @block.sync
            def _(e):
                for f in ops["sp"]:
                    f(e)

NSLOT = 8192
NOWN = 2048
OWN0 = NSLOT - NOWN
D = 2048
KC = 16
C_AQ, C_AF, C_AI, C_AG, C_BQ, C_BK, C_BV, C_IQ, C_IK, C_IW, C_G = 0, 1024, 2048, 3072, 4096, 5120, 6144, 7168, 7680, 7744, 7752
W_SCALE = (8 ** -0.5) * (64 ** -0.5)


class Rot:
    def __init__(self, S, name, shape, dtype, n):
        self.t = [S.sbuf("%s%d" % (name, i), shape, dtype) for i in range(n)]
        self.b = [S.buf("%s%d" % (name, i)) for i in range(n)]
        self.i = 0
        self.n = n

    def next(self):
        k = self.i % self.n
        self.i += 1
        return self.t[k], self.b[k]


class PBanks:
    def __init__(self, S):
        self.t = [S.psum("pb%d" % i, [128, 512], F32) for i in range(8)]
        self.b = [S.buf("pb%d" % i) for i in range(8)]
        self.i = 0

    def next(self, lo=0, hi=8):
        n = hi - lo
        k = lo + (self.i % n)
        self.i += 1
        return self.t[k], self.b[k]


def phase1a(S, G, blocks=(0, 1, 2, 3)):
    A = G["ap"]
    PB = G["pb"]
    ident = G["ident_bf"]
    Bident = G["Bident"]
    w_in = A["w_in"]
    xT = S.sbuf("xT", [128, KC, 2048], BF16)
    BxT = S.bufs(16, "xT")
    xin = Rot(S, "xin", [128, 2048], BF16, 2)
    wt = Rot(S, "wt", [128, KC, 512], BF16, 2)
    f32t = Rot(S, "hgf", [128, 512], F32, 12)
    st16 = Rot(S, "st16", [128, 512], BF16, 8)
    stkT = Rot(S, "stkT", [128, 4, 128], BF16, 2)
    decst = S.sbuf("decst", [128, 8, 32], F32)
    Bdec = S.buf("decst")
    wist = Rot(S, "wist", [128, 8], F32, 2)
    cst = G["cst"]
    Bc = G["Bcst"]
    evq = [0]

    def evac_engine():
        evq[0] += 1
        return "act" if evq[0] % 2 else "dve"

    def copy_op(eng, out_ap, in_ap, reads, writes):
        if eng == "act":
            S.op("act", lambda e, out_ap=out_ap, in_ap=in_ap: e.activation(out_ap, in_ap, AF.Copy), reads=reads, writes=writes)
        else:
            S.op("dve", lambda e, out_ap=out_ap, in_ap=in_ap: e.tensor_copy(out_ap, in_ap), reads=reads, writes=writes)

    for blk in blocks:
        own = (blk == 3)
        s0 = blk * 2048
        for t in range(16):
            xi, Bxi = xin.next()
            S.dma("pool", xi[:], A["xs"][s0 + t * 128: s0 + (t + 1) * 128, :], writes=[Bxi])
            for q4 in range(4):
                pb, Bpb = PB.next()
                for j in range(4):
                    kc = q4 * 4 + j
                    S.op("pe", lambda e, pb=pb, xi=xi, kc=kc, j=j: e.matmul(
                        pb[:, j * 128:(j + 1) * 128], xi[:, kc * 128:(kc + 1) * 128], ident[:], start=True, stop=True),
                        reads=[Bxi, Bident], writes=[Bpb])
                copy_op(evac_engine(), xT[:, q4 * 4:(q4 + 1) * 4, t * 128:(t + 1) * 128],
                        pb[:].rearrange("p (a b) -> p a b", a=4), [Bpb], [BxT[t]])

        def load_w(cols):
            w, Bw = wt.next()
            off = 0
            for (c0, n) in cols:
                S.dma("pool", w[:, :, off:off + n], w_in[:, c0:c0 + n].rearrange("(kc p) n -> p kc n", p=128), writes=[Bw])
                off += n
            return w, Bw

        def fm_mm(w, Bw, m, g):
            pb, Bpb = PB.next()
            for kc in range(KC):
                S.op("pe", lambda e, pb=pb, w=w, kc=kc, m=m, g=g: e.matmul(
                    pb[:], w[:, kc, m * 128:(m + 1) * 128], xT[:, kc, g * 512:(g + 1) * 512], start=(kc == 0), stop=(kc == KC - 1)),
                    reads=[Bw] + BxT[g * 4:(g + 1) * 4], writes=[Bpb])
            return pb, Bpb

        def simple_fm(w, Bw, m, g, bias_ap, func, dst_ap, Bd, rows=128):
            pb, Bpb = fm_mm(w, Bw, m, g)
            st, Bst = st16.next()
            S.op("act", lambda e, st=st, pb=pb, func=func, bias_ap=bias_ap: e.activation(st[:], pb[:], func, bias=bias_ap), reads=[Bpb, Bc], writes=[Bst])
            S.dma("sp", dst_ap, st[0:rows, :], reads=[Bst], writes=[Bd], nowaw=True)

        tm_units = [("ai", C_AI), ("ai", C_AI + 512), ("bv", C_BV), ("bv", C_BV + 512)]
        for (kind, c0) in tm_units:
            w, Bw = load_w([(c0, 512)])
            for t in range(16):
                pb, Bpb = PB.next()
                for kc in range(KC):
                    S.op("pe", lambda e, pb=pb, w=w, kc=kc, t=t: e.matmul(
                        pb[:], xT[:, kc, t * 128:(t + 1) * 128], w[:, kc, :], start=(kc == 0), stop=False),
                        reads=[Bw, BxT[t]], writes=[Bpb])
                boff = (c0 - C_AI) if kind == "ai" else (1024 + c0 - C_BV)
                S.op("pe", lambda e, pb=pb, boff=boff: e.matmul(pb[:], cst["ones_row"][0:1, :], cst["brow"][0:1, boff:boff + 512],
                                                            start=False, stop=True), reads=[Bc], writes=[Bpb])
                st, Bst = st16.next()
                tt = blk * 16 + t
                if kind == "ai":
                    S.op("act", lambda e, st=st, pb=pb, tt=tt: e.activation(st[:], pb[:], AF.Identity, scale=cst["valid_tm"][:, tt:tt + 1]),
                         reads=[Bpb, Bc], writes=[Bst])
                    dst = A["HG_v"][s0 + t * 128:s0 + (t + 1) * 128, c0 - C_AI:c0 - C_AI + 512]
                else:
                    S.op("dve", lambda e, st=st, pb=pb: e.tensor_copy(st[:], pb[:]), reads=[Bpb], writes=[Bst])
                    h0 = (c0 - C_BV) // 128
                    dst = A["VH"][h0:h0 + 4, :, tt, :].rearrange("h p d -> p h d")
                if kind == "ai":
                    S.dma("sp", dst, st[:], reads=[Bst], writes=[G["B"]["HG_v"]], nowaw=True)
                else:
                    S.dma("sp", dst, st[:].rearrange("p (h d) -> p h d", h=4), reads=[Bst], writes=[G["B"]["VH"]], nowaw=True)
        if own:
            w, Bw = load_w([(C_IW, 8)])
            for t in range(16):
                pb, Bpb = PB.next()
                for kc in range(KC):
                    S.op("pe", lambda e, pb=pb, w=w, kc=kc, t=t: e.matmul(
                        pb[:, 0:8], xT[:, kc, t * 128:(t + 1) * 128], w[:, kc, 0:8], start=(kc == 0), stop=False),
                        reads=[Bw, BxT[t]], writes=[Bpb])
                S.op("pe", lambda e, pb=pb: e.matmul(pb[:, 0:8], cst["ones_row"][0:1, :], cst["brow"][0:1, 2048:2056],
                                                     start=False, stop=True), reads=[Bc], writes=[Bpb])
                st, Bst = wist.next()
                S.op("dve", lambda e, st=st, pb=pb: e.tensor_scalar(st[:], pb[:, 0:8], W_SCALE, None, op0=ALU.mult), reads=[Bpb], writes=[Bst])
                S.dma("sp", A["WI"][t * 128:(t + 1) * 128, :], st[:], reads=[Bst], writes=[G["B"]["WI"]], nowaw=True)

        for u in range(2):
            w, Bw = load_w([(C_BK + u * 512, 512)])
            for m in range(4):
                h = u * 4 + m
                for g in range(4):
                    simple_fm(w, Bw, m, g, cst["bfm"][:, G["bfm_idx"]["bk"] + h: G["bfm_idx"]["bk"] + h + 1], AF.Identity,
                              A["KT"][h, :, s0 + g * 512:s0 + (g + 1) * 512], G["B"]["KT"])
        w, Bw = load_w([(C_IK, 64), (C_IK, 64)])
        for g in range(4):
            simple_fm(w, Bw, 0, g, cst["bfm"][:, G["bfm_idx"]["ik"]: G["bfm_idx"]["ik"] + 1], AF.Identity,
                      A["KIT"][:, s0 + g * 512:s0 + (g + 1) * 512], G["B"]["KIT"])
        if own:
            for u in range(2):
                w, Bw = load_w([(C_BQ + u * 512, 512)])
                for m in range(4):
                    h = u * 4 + m
                    for g in range(4):
                        simple_fm(w, Bw, m, g, cst["bfm"][:, G["bfm_idx"]["bq"] + h: G["bfm_idx"]["bq"] + h + 1], AF.Identity,
                                  A["QT"][h, :, g * 512:(g + 1) * 512], G["B"]["QT"])
            w, Bw = load_w([(C_IQ, 512)])
            for m in range(4):
                for g in range(4):
                    simple_fm(w, Bw, m, g, cst["bfm"][:, G["bfm_idx"]["iq"] + m: G["bfm_idx"]["iq"] + m + 1], AF.Identity,
                              A["QIT"][m, :, g * 512:(g + 1) * 512], G["B"]["QIT"])
            for u in range(8):
                w, Bw = load_w([(C_G + u * 512, 512)])
                for m in range(4):
                    c = u * 4 + m
                    for g in range(4):
                        simple_fm(w, Bw, m, g, cst["bfm"][:, G["bfm_idx"]["g"] + c: G["bfm_idx"]["g"] + c + 1], AF.Sigmoid,
                                  A["GT"][c, :, g * 512:(g + 1) * 512], G["B"]["GT"])

        deferred = []
        for h in range(8):
            if own:
                w, Bw = load_w([(C_AF + h * 128, 128), (C_AQ + h * 128, 128), (C_AG + h * 128, 128)])
            else:
                if h % 4 == 0:
                    w4, Bw4 = load_w([(C_AF + h * 128, 512)])
                w, Bw = w4, Bw4
            mf = 0 if own else (h % 4)
            bi = G["bfm_idx"]
            oml_h = cst["oml"][:, h:h + 1]
            noml_h = cst["noml"][:, h:h + 1]
            lb_h = cst["lb"][:, h:h + 1]
            b_af = cst["bfm"][:, bi["af"] + h: bi["af"] + h + 1]
            b_aq = cst["bfm"][:, bi["aq"] + h: bi["aq"] + h + 1]
            b_ag = cst["bfm"][:, bi["ag"] + h: bi["ag"] + h + 1]
            for g in range(4):
                pb, Bpb = fm_mm(w, Bw, mf, g)
                while deferred:
                    deferred.pop(0)()
                sg, Bsg = f32t.next()
                S.op("act", lambda e, sg=sg, pb=pb, b_af=b_af: e.activation(sg[:], pb[:], AF.Sigmoid, bias=b_af),
                     reads=[Bpb, Bc], writes=[Bsg])
                lf, Blf = f32t.next()
                S.op("act", lambda e, lf=lf, sg=sg, oml_h=oml_h, lb_h=lb_h: e.activation(lf[:], sg[:], AF.Ln, scale=oml_h, bias=lb_h),
                     reads=[Bsg, Bc], writes=[Blf])
                cum, Bcum = f32t.next()
                S.op("dve", lambda e, cum=cum, lf=lf: e.tensor_tensor_scan(cum[:], cst["rmask"][:], lf[:], 0.0, ALU.mult, ALU.add),
                     reads=[Blf, Bc], writes=[Bcum])
                en, Ben = f32t.next()
                S.op("act", lambda e, en=en, cum=cum: e.activation(en[:], cum[:], AF.Exp, scale=-1.0), reads=[Bcum], writes=[Ben])
                kk, Bkk = f32t.next()
                S.op("dve", lambda e, kk=kk, sg=sg, noml_h=noml_h, oml_h=oml_h: e.tensor_scalar(kk[:], sg[:], noml_h, oml_h, op0=ALU.mult, op1=ALU.add),
                     reads=[Bsg, Bc], writes=[Bkk])
                kd, Bkd = st16.next()
                S.op("dve", lambda e, kd=kd, kk=kk, en=en: e.tensor_tensor(kd[:], kk[:], en[:], ALU.mult), reads=[Bkk, Ben], writes=[Bkd])
                S.dma("sp", A["HG_kdec"][h, :, s0 + g * 512:s0 + (g + 1) * 512], kd[:], reads=[Bkd], writes=[G["B"]["HG_kdec"]], nowaw=True)
                ex2, Bex2 = f32t.next()
                for c in range(8):
                    S.op("act", lambda e, ex2=ex2, cum=cum, c=c: e.activation(ex2[:, c * 64:(c + 1) * 64], cum[:, c * 64:(c + 1) * 64], AF.Exp,
                                                                           scale=-1.0, bias=cum[:, c * 64 + 63:c * 64 + 64]),
                         reads=[Bcum], writes=[Bex2])
                ke, Bke = st16.next()
                S.op("dve", lambda e, ke=ke, kk=kk, ex2=ex2: e.tensor_tensor(ke[:], kk[:], ex2[:], ALU.mult), reads=[Bkk, Bex2], writes=[Bke])
                dec_ap = decst[:, h, g * 8:(g + 1) * 8]
                S.op("act", lambda e, cum=cum, dec_ap=dec_ap: e.activation(dec_ap, cum[:].rearrange("p (c s) -> p c s", s=64)[:, :, 63], AF.Exp),
                     reads=[Bcum], writes=[Bdec])
                def kend_T(ke=ke, Bke=Bke, g=g, h=h, s0=s0):
                    pbt, Bpbt = PB.next()
                    for j in range(4):
                        S.op("pe", lambda e, pbt=pbt, ke=ke, j=j: e.matmul(pbt[:, j * 128:(j + 1) * 128], ke[:, j * 128:(j + 1) * 128], ident[:],
                                                                           start=True, stop=True), reads=[Bke, Bident], writes=[Bpbt])
                    kT, BkT = stkT.next()
                    S.op("dve", lambda e, kT=kT, pbt=pbt: e.tensor_copy(kT[:], pbt[:].rearrange("p (a b) -> p a b", a=4)), reads=[Bpbt], writes=[BkT])
                    S.dma("sp", A["HG_kend"][s0 + g * 512:s0 + (g + 1) * 512, h * 128:(h + 1) * 128].rearrange("(t p) d -> p t d", p=128),
                          kT[:], reads=[BkT], writes=[G["B"]["HG_kend"]], nowaw=True)
                deferred.append(kend_T)
                if own:
                    ec, Bec = f32t.next()
                    S.op("act", lambda e, ec=ec, cum=cum: e.activation(ec[:], cum[:], AF.Exp), reads=[Bcum], writes=[Bec])
                    pbq, Bpbq = fm_mm(w, Bw, 1, g)
                    qs, Bqs = f32t.next()
                    S.op("act", lambda e, qs=qs, pbq=pbq, b_aq=b_aq: e.activation(qs[:], pbq[:], AF.Silu, bias=b_aq),
                         reads=[Bpbq, Bc], writes=[Bqs])
                    qd, Bqd = st16.next()
                    S.op("dve", lambda e, qd=qd, qs=qs, ec=ec: e.tensor_tensor(qd[:], qs[:], ec[:], ALU.mult), reads=[Bqs, Bec], writes=[Bqd])
                    S.dma("sp", A["HG_qdec"][h, :, g * 512:(g + 1) * 512], qd[:], reads=[Bqd], writes=[G["B"]["HG_qdec"]], nowaw=True)
                    simple_fm(w, Bw, 2, g, b_ag, AF.Silu, A["HG_gs"][h, :, g * 512:(g + 1) * 512], G["B"]["HG_gs"])
        while deferred:
            deferred.pop(0)()
        S.dma("sp", A["HG_dec"][:, :, blk * 32:(blk + 1) * 32], decst[:], reads=[Bdec], writes=[G["B"]["HG_dec"]], nowaw=True)

RMS_EPS = 1e-6


def phase1b(S, G):
    A = G["ap"]
    PB = G["pb"]
    cst = G["cst"]
    Bc = G["Bcst"]
    B = G["B"]
    kend = S.sbuf("hs_kend", [128, 16, 1024], BF16)
    Bkend = S.buf("hs_kend")
    vv = S.sbuf("hs_v", [128, 16, 1024], BF16)
    Bvv = S.buf("hs_v")
    dec = S.sbuf("hs_dec", [128, 8, 128], F32)
    Bdec = S.buf("hs_dec")
    Sf = S.sbuf("hs_S", [128, 8, 128], F32)
    Sb = S.sbuf("hs_Sb", [128, 8, 128], BF16)
    BS = S.bufs(8, "hs_S")
    BSb = S.bufs(8, "hs_Sb")
    S.dma("sp", dec[:], A["HG_dec"], reads=[B["HG_dec"]], writes=[Bdec])
    S.op("dve", lambda e: e.memset(Sf[:], 0.0), writes=BS)
    S.op("dve", lambda e: e.memset(Sb[:], 0.0), writes=BSb)
    onesb = S.sbuf("hs_ones", [128, 128], BF16)
    Bones = S.buf("hs_ones")
    S.op("dve", lambda e: e.memset(onesb[:], 1.0 / 128.0), writes=[Bones])
    kdec = Rot(S, "hs_kdec", [128, 2048], BF16, 8)
    qdec = Rot(S, "hs_qdec", [128, 2048], BF16, 8)
    gs = Rot(S, "hs_gs", [128, 2048], BF16, 8)
    attm = Rot(S, "hs_attm", [128, 128], BF16, 8)
    sq = Rot(S, "hs_sq", [128, 128], BF16, 6)
    rs = Rot(S, "hs_rs", [128, 128], F32, 6)
    yy = Rot(S, "hs_y", [128, 128], F32, 6)
    bastr = Rot(S, "hs_bast", [128, 8, 128], BF16, 2)

    def state_update(t, h, half, ):
        p0 = half * 64
        chunk = None
        pk, Bpk = PB.next()
        S.op("pe", lambda e, pk=pk, t=t, h=h, p0=p0: e.matmul(pk[:, 0:128], kend[p0:p0 + 64, t, h * 128:(h + 1) * 128],
                                                            vv[p0:p0 + 64, t, h * 128:(h + 1) * 128], start=True, stop=True),
             reads=[Bkend, Bvv], writes=[Bpk])
        return pk, Bpk

    for blk in range(4):
        own = (blk == 3)
        s0 = blk * 2048
        S.dma("sp", kend[:], A["HG_kend"][s0:s0 + 2048, :].rearrange("(t p) c -> p t c", p=128), reads=[B["HG_kend"]], writes=[Bkend])
        S.dma("sp", vv[:], A["HG_v"][s0:s0 + 2048, :].rearrange("(t p) c -> p t c", p=128), reads=[B["HG_v"]], writes=[Bvv])
        if not own:
            for t in range(16):
                for half in range(2):
                    ch = blk * 32 + t * 2 + half
                    for h in range(8):
                        pk, Bpk = state_update(t, h, half)
                        S.op("dve", lambda e, pk=pk, h=h, ch=ch: e.scalar_tensor_tensor(Sf[:, h, :], Sf[:, h, :], dec[:, h, ch:ch + 1], pk[:, 0:128],
                                                                                      op0=ALU.mult, op1=ALU.add),
                             reads=[Bpk, Bdec, BS[h]], writes=[BS[h]])
            if blk == 2:
                for h in range(8):
                    S.op("act", lambda e, h=h: e.activation(Sb[:, h, :], Sf[:, h, :], AF.Copy), reads=[BS[h]], writes=[BSb[h]])
            continue
        kds, qds, ggs = [], [], []
        for h in range(8):
            kd, Bkd = kdec.next()
            qd, Bqd = qdec.next()
            gg, Bgg = gs.next()
            S.dma("sp", kd[:], A["HG_kdec"][h, :, s0:s0 + 2048], reads=[B["HG_kdec"]], writes=[Bkd])
            S.dma("sp", qd[:], A["HG_qdec"][h], reads=[B["HG_qdec"]], writes=[Bqd])
            S.dma("sp", gg[:], A["HG_gs"][h], reads=[B["HG_gs"]], writes=[Bgg])
            kds.append((kd, Bkd)); qds.append((qd, Bqd)); ggs.append((gg, Bgg))
        for t in range(16):
            tc = slice(t * 128, (t + 1) * 128)
            bt, Bbt = bastr.next()
            for h in range(8):
                kd, Bkd = kds[h]
                qd, Bqd = qds[h]
                gg, Bgg = ggs[h]
                pa, Bpa = PB.next()
                S.op("pe", lambda e, pa=pa, kd=kd, qd=qd, tc=tc: e.matmul(pa[:, 0:128], kd[:, tc], qd[:, tc], start=True, stop=True),
                     reads=[Bkd, Bqd], writes=[Bpa])
                am, Bam = attm.next()
                S.op("dve", lambda e, am=am, pa=pa: e.tensor_tensor(am[:], pa[:, 0:128], cst["bdmask"][:], ALU.mult), reads=[Bpa, Bc], writes=[Bam])
                po, Bpo = PB.next()
                S.op("pe", lambda e, po=po, am=am, t=t, h=h: e.matmul(po[:, 0:128], vv[:, t, h * 128:(h + 1) * 128], am[:], start=True, stop=False),
                     reads=[Bvv, Bam], writes=[Bpo])
                for half in range(2):
                    ch = blk * 32 + t * 2 + half
                    c0 = t * 128 + half * 64
                    S.op("pe", lambda e, po=po, qd=qd, h=h, c0=c0, half=half: e.matmul(po[:, half * 64:(half + 1) * 64], Sb[:, h, :], qd[:, c0:c0 + 64],
                                                                                  start=False, stop=(half == 1)),
                         reads=[BSb[h], Bqd], writes=[Bpo])
                    pk, Bpk = state_update(t, h, half)
                    S.op("dve", lambda e, pk=pk, h=h, ch=ch: e.scalar_tensor_tensor(Sf[:, h, :], Sf[:, h, :], dec[:, h, ch:ch + 1], pk[:, 0:128],
                                                                                  op0=ALU.mult, op1=ALU.add),
                         reads=[Bpk, Bdec, BS[h]], writes=[BS[h]])
                    S.op("act", lambda e, h=h: e.activation(Sb[:, h, :], Sf[:, h, :], AF.Copy), reads=[BS[h]], writes=[BSb[h]])
                q2, Bq2 = sq.next()
                S.op("act", lambda e, q2=q2, po=po: e.activation(q2[:], po[:, 0:128], AF.Square), reads=[Bpo], writes=[Bq2])
                pm, Bpm = PB.next()
                S.op("pe", lambda e, pm=pm, q2=q2: e.matmul(pm[:, 0:128], onesb[:], q2[:], start=True, stop=True), reads=[Bones, Bq2], writes=[Bpm])
                r1, Br1 = rs.next()
                S.op("act", lambda e, r1=r1, pm=pm: e.activation(r1[:], pm[:, 0:128], AF.Sqrt, bias=cst["eps_rms"][:, 0:1]), reads=[Bpm, Bc], writes=[Br1])
                S.op("dve", lambda e, r1=r1: e.reciprocal(r1[:], r1[:]), reads=[Br1], writes=[Br1])
                y1, By1 = yy.next()
                S.op("dve", lambda e, y1=y1, po=po, r1=r1: e.tensor_tensor(y1[:], po[:, 0:128], r1[:], ALU.mult), reads=[Bpo, Br1], writes=[By1])
                S.op("dve", lambda e, y1=y1, gg=gg, tc=tc, h=h, bt=bt: e.scalar_tensor_tensor(bt[:, h, :], y1[:], cst["normg"][:, h:h + 1], gg[:, tc],
                                                                                           op0=ALU.mult, op1=ALU.mult),
                     reads=[By1, Bgg, Bc], writes=[Bbt])
            S.dma("sp", A["BA"][:, :, tc].rearrange("h p t -> p h t"), bt[:], reads=[Bbt], writes=[B["BA"]], nowaw=True)

SM_SCALE = 128 ** -0.5
TOPK = 256
NBIS = 18
NTER = 10
BIS_WIN = 16.0
NEG_ADM = -30000.0


def phase2_dsa(S, G, groups=(0, 1, 2, 3)):
    A = G["ap"]
    PB = G["pb"]
    cst = G["cst"]
    Bc = G["Bcst"]
    B = G["B"]
    ident = G["ident_bf"]
    Bident = G["Bident"]
    es_outer = S.es
    with ExitStack() as pes:
        S.es = pes
        alb = S.sbuf("ds_alb", [128, 8, 64], F32)
        dtab = S.sbuf("ds_dtab", [128, 8192], BF16)
        qrel = S.sbuf("ds_qrel", [1, 512], F32)
        onesr = S.sbuf("ds_onesr", [1, 128], BF16)
        drow = S.sbuf("ds_drow", [1, 512], F32)
        Bdrow = S.buf("ds_drow")
        shrow = S.sbuf("ds_shrow", [1, 8, 512], BF16)
        Bshrow = S.buf("ds_shrow")
        dmc = S.sbuf("ds_dmc", [128, 1], F32)
        Bdmc = S.buf("ds_dmc")
        corr = S.sbuf("ds_corr", [128, 8, 128], BF16)
        sel2 = S.sbuf("ds_sel2", [2, 128], BF16)
        onesb = S.sbuf("ds_ones", [128, 128], BF16)
        Bk = S.buf("ds_const")
        S.dma("sp", alb[:], A["alb"], writes=[Bk], nowaw=True)
        S.dma("sp", corr[:], A["corr"], writes=[Bk], nowaw=True)
        S.dma("sp", sel2[:], A["sel2"], writes=[Bk], nowaw=True)
        S.op("dve", lambda e: e.memset(onesb[:], 1.0), writes=[Bk])
        S.op("dve", lambda e: e.memset(onesr[:], 1.0), writes=[Bk])
        S.dma("sp", dtab[:], A["dtab"], writes=[Bk], nowaw=True)
        S.dma("sp", qrel[:], A["qrel"], writes=[Bk], nowaw=True)
        kit = S.sbuf("ds_kit", [128, 8192], BF16)
        S.dma("sp", kit[:], A["KIT"], reads=[B["KIT"]], writes=[Bk], nowaw=True)
        wi = S.sbuf("ds_wi", [128, 16, 8], F32)
        S.dma("sp", wi[:], A["WI"].rearrange("(t p) h -> p t h", p=128), reads=[B["WI"]], writes=[Bk], nowaw=True)
        maskT = S.sbuf("ds_maskT", [128, 64, 512], U8)
        BmT = S.buf("ds_maskT")
        S.barrier()

        def do_group(g):
            Q0 = OWN0 + g * 512
            with ExitStack() as aes:
                S.es = aes
                qit = S.sbuf("ds_qit", [128, 4, 512], BF16)
                Bqit = S.buf("ds_qit")
                S.dma("sp", qit[:], A["QIT"][:, :, g * 512:(g + 1) * 512].rearrange("m p q -> p m q"), reads=[B["QIT"]], writes=[Bqit])
                sc2 = [S.sbuf("ds_sc", [128, 8192], F32) for _ in range(2)]
                Bsc2 = [S.buf("ds_sc"), S.buf("ds_sc")]
                mq = S.sbuf("ds_mq", [128, 8192], BF16)
                Bmq = S.buf("ds_mq")
                adm = Rot(S, "ds_adm", [2, 512], BF16, 3)
                junk2 = S.sbuf("ds_junk2", [128, 8192], U8)
                Bj2 = S.buf("ds_junk2")
                relu = Rot(S, "ds_relu", [128, 512], BF16, 9)
                dg2 = [S.sbuf("ds_dg", [128, 8, 128], BF16) for _ in range(2)]
                Bdg2 = [S.buf("ds_dg"), S.buf("ds_dg")]
                sm = {n: S.sbuf("ds_s_" + n, [128, 1], F32) for n in ("lo", "hi", "mid", "cnt", "ge", "d", "c0", "d3", "t1", "nt2", "s2", "g2")}
                Bsm = S.buf("ds_small")
                Bth = S.buf("ds_th")
                Bs2 = S.buf("ds_s2")

                def tparams(T):
                    tg = g * 4 + T
                    Qt = Q0 + T * 128
                    nk = Qt + 128
                    nb5 = (nk + 511) // 512
                    return tg, Qt, nk, nb5, nb5 * 512

                def score_blocks(T):
                    tg, Qt, nk, nb5, nkp = tparams(T)
                    sc, Bsc = sc2[T % 2], Bsc2[T % 2]
                    dg, Bdg = dg2[T % 2], Bdg2[T % 2]
                    out = []

                    def prep():
                        for h in range(8):
                            S.op("dve", lambda e, h=h: e.tensor_scalar(dg[:, h, :], ident[:], wi[:, tg, h:h + 1], None, op0=ALU.mult),
                                 reads=[Bident, Bk], writes=[Bdg])
                    out.append(prep)

                    def blk(kb5):
                        ks = slice(kb5 * 512, (kb5 + 1) * 512)
                        ad, Bad = adm.next()
                        S.dma("sp", ad[:], A["adm"][tg, :, ks], writes=[Bad])
                        rl = []
                        for hp in range(4):
                            for par in range(2):
                                pb, Bpb = PB.next()
                                p0 = par * 64
                                S.op("pe", lambda e, pb=pb, hp=hp, p0=p0: e.matmul(
                                    pb[:], qit[p0:p0 + 64, hp, T * 128:(T + 1) * 128], kit[p0:p0 + 64, ks], start=True, stop=True),
                                    reads=[Bqit, Bk], writes=[Bpb])
                                r, Br = relu.next()
                                if par == 0 or hp % 2 == 0:
                                    S.op("act", lambda e, r=r, pb=pb: e.activation(r[:], pb[:], AF.Relu), reads=[Bpb], writes=[Br])
                                else:
                                    S.op("dve", lambda e, r=r, pb=pb: e.tensor_scalar(r[:], pb[:], 0.0, None, op0=ALU.max), reads=[Bpb], writes=[Br])
                                rl.append((r, Br))
                        ps, Bps = PB.next()
                        for h in range(8):
                            r, Br = rl[h]
                            S.op("pe", lambda e, ps=ps, h=h, r=r: e.matmul(ps[:], dg[:, h, :], r[:], start=(h == 0), stop=False),
                                 reads=[Bdg, Br], writes=[Bps])
                        S.op("pe", lambda e, ps=ps, ad=ad: e.matmul(ps[:], sel2[0:2, :], ad[0:2, :], start=False, stop=True),
                             reads=[Bk, Bad], writes=[Bps])
                        S.op("act", lambda e, ps=ps: e.activation(sc[:, ks], ps[:], AF.Copy), reads=[Bps], writes=[Bsc])
                    for kb5 in range(nb5):
                        out.append(lambda kb5=kb5: blk(kb5))
                    return out

                def search_init(T):
                    tg, Qt, nk, nb5, nkp = tparams(T)
                    sc, Bsc = sc2[T % 2], Bsc2[T % 2]
                    scv = sc[:, 0:nkp]
                    S.op("dve", lambda e: e.reduce_max(sm["hi"][:], scv, AX.X), reads=[Bsc], writes=[Bsm])
                    S.op("dve", lambda e: e.tensor_scalar(sm["lo"][:], sm["hi"][:], -BIS_WIN, None, op0=ALU.add), reads=[Bsm], writes=[Bsm])
                    S.op("dve", lambda e: e.tensor_scalar(sm["hi"][:], sm["hi"][:], 1e-3, None, op0=ALU.add), reads=[Bsm], writes=[Bsm, Bth])
                    S.op("dve", lambda e: e.tensor_scalar(mq[:, 0:nkp], scv, sm["lo"][:, 0:1], None, op0=ALU.is_ge, op1=ALU.add,
                                                          accum_out=sm["c0"][:]), reads=[Bsc, Bsm], writes=[Bmq, Bsm])

                def search_round(T):
                    tg, Qt, nk, nb5, nkp = tparams(T)
                    sc, Bsc = sc2[T % 2], Bsc2[T % 2]
                    scv = sc[:, 0:nkp]
                    S.op("dve", lambda e: e.tensor_tensor(sm["d"][:], sm["hi"][:], sm["lo"][:], ALU.subtract), reads=[Bsm], writes=[Bsm])
                    S.op("dve", lambda e: e.tensor_scalar(sm["d3"][:], sm["d"][:], 1.0 / 3.0, None, op0=ALU.mult), reads=[Bsm], writes=[Bsm])
                    S.op("dve", lambda e: e.tensor_tensor(sm["t1"][:], sm["lo"][:], sm["d3"][:], ALU.add), reads=[Bsm], writes=[Bsm])
                    S.op("dve", lambda e: e.tensor_tensor(sm["nt2"][:], sm["d3"][:], sm["hi"][:], ALU.subtract), reads=[Bsm], writes=[Bsm, Bth])
                    S.op("act", lambda e: e.activation(junk2[:, 0:nkp], scv, AF.Sign, bias=sm["nt2"][:, 0:1], accum_out=sm["s2"][:]),
                         reads=[Bsc, Bth], writes=[Bj2, Bs2])
                    S.op("dve", lambda e: e.tensor_scalar(mq[:, 0:nkp], scv, sm["t1"][:, 0:1], None, op0=ALU.is_ge, op1=ALU.add,
                                                          accum_out=sm["cnt"][:]), reads=[Bsc, Bsm], writes=[Bmq, Bsm])
                    S.op("dve", lambda e: e.tensor_scalar(sm["ge"][:], sm["cnt"][:], TOPK - 0.5, None, op0=ALU.is_ge), reads=[Bsm], writes=[Bsm])
                    S.op("dve", lambda e: e.tensor_scalar(sm["g2"][:], sm["s2"][:], 2.0 * (TOPK - 0.5) - nkp, None, op0=ALU.is_ge), reads=[Bs2], writes=[Bsm])
                    S.op("dve", lambda e: e.tensor_tensor(sm["ge"][:], sm["ge"][:], sm["g2"][:], ALU.add), reads=[Bsm], writes=[Bsm])
                    S.op("dve", lambda e: e.scalar_tensor_tensor(sm["lo"][:], sm["d3"][:], sm["ge"][:, 0:1], sm["lo"][:], op0=ALU.mult, op1=ALU.add),
                         reads=[Bsm], writes=[Bsm])
                    S.op("dve", lambda e: e.tensor_tensor(sm["hi"][:], sm["lo"][:], sm["d3"][:], ALU.add), reads=[Bsm], writes=[Bsm, Bth])

                def finalize(T):
                    tg, Qt, nk, nb5, nkp = tparams(T)
                    sc, Bsc = sc2[T % 2], Bsc2[T % 2]
                    scv = sc[:, 0:nkp]
                    S.op("dve", lambda e: e.tensor_scalar(sm["ge"][:], sm["c0"][:], TOPK - 0.5, None, op0=ALU.is_ge), reads=[Bsm], writes=[Bsm])
                    S.op("dve", lambda e: e.tensor_scalar(sm["d"][:], sm["lo"][:], 1000.0, None, op0=ALU.add), reads=[Bsm], writes=[Bsm])
                    S.op("dve", lambda e: e.tensor_scalar(sm["lo"][:], sm["d"][:], sm["ge"][:, 0:1], -1000.0, op0=ALU.mult, op1=ALU.add),
                         reads=[Bsm], writes=[Bsm])
                    S.op("dve", lambda e: e.tensor_scalar(mq[:, 0:nkp], scv, sm["lo"][:, 0:1], None, op0=ALU.is_ge), reads=[Bsc, Bsm], writes=[Bmq])
                    S.op("dve", lambda e: e.scalar_tensor_tensor(sc[:, 0:nk], mq[:, 0:nk], -16384.0, dtab[:, 8192 - nk:8192], op0=ALU.mult, op1=ALU.add),
                         reads=[Bmq, Bk], writes=[Bsc])
                    S.op("dve", lambda e: e.tensor_reduce(dmc[:], sc[:, 0:nk], AX.X, ALU.min), reads=[Bsc], writes=[Bdmc])
                    S.op("dve", lambda e: e.tensor_scalar(dmc[:], dmc[:], 16384.0, None, op0=ALU.add), reads=[Bdmc], writes=[Bdmc])
                    pbd, Bpbd = PB.next()
                    S.op("pe", lambda e: e.matmul(pbd[0:1, 0:128], dmc[:, 0:1], G["ident_f"][:], start=True, stop=True),
                         reads=[Bdmc, G["Bidf"]], writes=[Bpbd])
                    S.op("dve", lambda e: e.tensor_copy(drow[0:1, T * 128:(T + 1) * 128], pbd[0:1, 0:128]), reads=[Bpbd], writes=[Bdrow])
                    nkb = nk // 128
                    for k4 in range(0, nkb, 4):
                        n4 = min(4, nkb - k4)
                        pb, Bpb = PB.next()
                        for j in range(n4):
                            kb = k4 + j
                            S.op("pe", lambda e, pb=pb, j=j, kb=kb: e.matmul(pb[:, j * 128:(j + 1) * 128], mq[:, kb * 128:(kb + 1) * 128], ident[:],
                                                                           start=True, stop=True), reads=[Bmq, Bident], writes=[Bpb])
                        dst = maskT[:, k4:k4 + n4, T * 128:(T + 1) * 128]
                        src = pb[:, 0:n4 * 128].rearrange("p (a b) -> p a b", a=n4)
                        if (k4 // 4) % 2 == 0:
                            S.op("act", lambda e, dst=dst, src=src: e.activation(dst, src, AF.Copy), reads=[Bpb], writes=[BmT])
                        else:
                            S.op("dve", lambda e, dst=dst, src=src: e.tensor_copy(dst, src), reads=[Bpb], writes=[BmT])

                for f_ in score_blocks(0):
                    f_()
                for T in range(4):
                    nxt = score_blocks(T + 1) if T < 3 else []
                    per = -(-len(nxt) // NTER)
                    search_init(T)
                    for it in range(NTER):
                        search_round(T)
                        for _ in range(per):
                            if nxt:
                                nxt.pop(0)()
                    while nxt:
                        nxt.pop(0)()
                    finalize(T)
                S.op("dve", lambda e: e.tensor_tensor(drow[:], drow[:], qrel[:], ALU.subtract), reads=[Bdrow, Bk], writes=[Bdrow])
                for h in range(8):
                    S.op("dve", lambda e, h=h: e.tensor_scalar(shrow[0:1, h, :], drow[:], (2.0 ** -(h + 1)) / SM_SCALE, None, op0=ALU.mult),
                         reads=[Bdrow], writes=[Bshrow])
                S.barrier()
                S.phase_end()
            with ExitStack() as bes:
                S.es = bes
                kt = Rot(S, "ds_kt", [128, 8192], BF16, 2)
                vh = Rot(S, "ds_vh", [128, 64, 128], BF16, 2)
                qt = Rot(S, "ds_qt", [128, 512], BF16, 2)
                pT = Rot(S, "ds_pT", [128, 512], BF16, 9)
                mcr = Rot(S, "ds_mc", [128, 128], BF16, 3)
                rec = Rot(S, "ds_rec", [128, 512], F32, 1)
                ob = Rot(S, "ds_ob", [128, 512], BF16, 1)
                nkb = (Q0 + 512) // 128
                kb0 = Q0 // 128
                for h in range(8):
                    k_, Bk_ = kt.next()
                    v_, Bv_ = vh.next()
                    q_, Bq_ = qt.next()
                    S.dma("sp", k_[:, 0:nkb * 128], A["KT"][h, :, 0:nkb * 128], reads=[B["KT"]], writes=[Bk_])
                    S.dma("sp", v_[:, 0:nkb, :], A["VH"][h, :, 0:nkb, :], reads=[B["VH"]], writes=[Bv_])
                    S.dma("sp", q_[:], A["QT"][h, :, g * 512:(g + 1) * 512], reads=[B["QT"]], writes=[Bq_])
                    po, Bpo = PB.t[6], PB.b[6]
                    pd, Bpd = PB.t[7], PB.b[7]
                    pend = []

                    def stage2(kb, p_, Bp_, c0, first, last, po=po, pd=pd, Bpo=Bpo, Bpd=Bpd, v_=v_, Bv_=Bv_):
                        S.op("pe", lambda e, po=po, v_=v_, p_=p_, kb=kb, c0=c0, first=first, last=last: e.matmul(
                            po[:, c0:512], v_[:, kb, :], p_[:, c0:512], start=first, stop=last), reads=[Bv_, Bp_], writes=[Bpo])
                        S.op("pe", lambda e, pd=pd, p_=p_, c0=c0, first=first, last=last: e.matmul(
                            pd[:, c0:512], onesb[:], p_[:, c0:512], start=first, stop=last), reads=[Bk, Bp_], writes=[Bpd])

                    for kb in range(nkb):
                        r = kb - kb0
                        c0 = max(r, 0) * 128
                        first = (kb == 0)
                        last = (kb == nkb - 1)
                        pst, Bpst = PB.next(0, 6)
                        S.op("pe", lambda e, pst=pst, k_=k_, q_=q_, kb=kb, c0=c0: e.matmul(
                            pst[:, c0:512], k_[:, kb * 128:(kb + 1) * 128], q_[:, c0:512], start=True, stop=(h >= 6)),
                            reads=[Bk_, Bq_], writes=[Bpst])
                        if h < 6:
                            S.op("pe", lambda e, pst=pst, h=h, c0=c0: e.matmul(pst[:, c0:512], onesr[0:1, :], shrow[0:1, h, c0:512], start=False, stop=True),
                                 reads=[Bk, Bshrow], writes=[Bpst])
                        p_, Bp_ = pT.next()
                        bias_ap = alb[:, h, r + 60:r + 61]
                        S.op("act", lambda e, p_=p_, pst=pst, c0=c0, bias_ap=bias_ap: e.activation(
                            p_[:, c0:512], pst[:, c0:512], AF.Exp, scale=SM_SCALE, bias=bias_ap), reads=[Bpst, Bk], writes=[Bp_])
                        if r >= 0:
                            mc, Bmc = mcr.next()
                            S.op("dve", lambda e, mc=mc, kb=kb, r=r, h=h: e.tensor_tensor(mc[:], maskT[:, kb, r * 128:(r + 1) * 128], corr[:, h, :], ALU.mult),
                                 reads=[BmT, Bk], writes=[Bmc])
                            S.op("dve", lambda e, p_=p_, mc=mc, r=r: e.scalar_tensor_tensor(p_[:, r * 128:(r + 1) * 128], p_[:, r * 128:(r + 1) * 128], 3.0e38, mc[:],
                                                                                         op0=ALU.min, op1=ALU.mult), reads=[Bmc, Bp_], writes=[Bp_])
                            if c0 + 128 < 512:
                                S.op("dve", lambda e, p_=p_, kb=kb, c0=c0: e.scalar_tensor_tensor(p_[:, c0 + 128:512], p_[:, c0 + 128:512], 3.0e38, maskT[:, kb, c0 + 128:512],
                                                                                               op0=ALU.min, op1=ALU.mult), reads=[BmT, Bp_], writes=[Bp_])
                        else:
                            S.op("dve", lambda e, p_=p_, kb=kb: e.scalar_tensor_tensor(p_[:], p_[:], 3.0e38, maskT[:, kb, :], op0=ALU.min, op1=ALU.mult),
                                 reads=[BmT, Bp_], writes=[Bp_])
                        pend.append((kb, p_, Bp_, c0, first, last))
                        if len(pend) > 6:
                            stage2(*pend.pop(0))
                    while pend:
                        stage2(*pend.pop(0))
                    rc, Brc = rec.next()
                    S.op("dve", lambda e, rc=rc, pd=pd: e.reciprocal(rc[:], pd[:]), reads=[Bpd], writes=[Brc])
                    o_, Bo_ = ob.next()
                    S.op("dve", lambda e, o_=o_, po=po, rc=rc: e.tensor_tensor(o_[:], po[:], rc[:], ALU.mult), reads=[Bpo, Brc], writes=[Bo_])
                    S.dma("sp", A["BB"][h, :, g * 512:(g + 1) * 512], o_[:], reads=[Bo_], writes=[B["BB"]], nowaw=True)
                S.barrier()
                S.phase_end()
        for g in groups:
            do_group(g)
        S.es = es_outer

LN_EPS = 1e-5
DN_ALPHA = 2.0 ** 0.25
CAP = 256
NEXP = 32


def layer_norm_tile(S, G, pre, Bpre, out_t, Bout, gb, bb, Bgb, small, Bsmall, junk, Bjunk):
    s1, s2, mean, var, rstd, nmr = small
    S.op("act", lambda e: e.activation(junk[:], pre[:], AF.Identity, accum_out=s1[:]), reads=[Bpre], writes=[Bjunk, Bsmall])
    S.op("act", lambda e: e.activation(junk[:], pre[:], AF.Square, accum_out=s2[:]), reads=[Bpre], writes=[Bjunk, Bsmall])
    S.op("dve", lambda e: e.tensor_scalar(mean[:], s1[:], 1.0 / 2048.0, None, op0=ALU.mult), reads=[Bsmall], writes=[Bsmall])
    S.op("dve", lambda e: e.tensor_tensor(var[:], mean[:], mean[:], ALU.mult), reads=[Bsmall], writes=[Bsmall])
    S.op("dve", lambda e: e.scalar_tensor_tensor(var[:], s2[:], 1.0 / 2048.0, var[:], op0=ALU.mult, op1=ALU.subtract), reads=[Bsmall], writes=[Bsmall])
    S.op("act", lambda e: e.activation(rstd[:], var[:], AF.Sqrt, bias=G["cst"]["eps_ln"][:, 0:1]), reads=[Bsmall, G["Bcst"]], writes=[Bsmall])
    S.op("dve", lambda e: e.reciprocal(rstd[:], rstd[:]), reads=[Bsmall], writes=[Bsmall])
    S.op("dve", lambda e: e.scalar_tensor_tensor(nmr[:], mean[:], -1.0, rstd[:], op0=ALU.mult, op1=ALU.mult), reads=[Bsmall], writes=[Bsmall])
    S.op("act", lambda e: e.activation(pre[:], pre[:], AF.Identity, scale=rstd[:, 0:1], bias=nmr[:, 0:1]), reads=[Bpre, Bsmall], writes=[Bpre])
    S.op("dve", lambda e: e.tensor_tensor(pre[:], pre[:], gb[:], ALU.mult), reads=[Bpre, Bgb], writes=[Bpre])
    S.op("dve", lambda e: e.tensor_tensor(out_t[:], pre[:], bb[:], ALU.add), reads=[Bpre, Bgb], writes=[Bout])


def phase3a_merge(S, G):
    A = G["ap"]
    PB = G["pb"]
    B = G["B"]
    wa = S.sbuf("o_wa", [128, 8, 2048], BF16)
    wb = S.sbuf("o_wb", [128, 8, 2048], BF16)
    Bw = S.buf("o_w")
    S.dma("pool", wa[:], A["w_branch_a"].rearrange("(kc p) n -> p kc n", p=128), writes=[Bw], nowaw=True)
    S.dma("pool", wb[:], A["w_branch_b"].rearrange("(kc p) n -> p kc n", p=128), writes=[Bw], nowaw=True)
    bag = Rot(S, "o_ba", [128, 8, 512], BF16, 2)
    bbg = Rot(S, "o_bb", [128, 8, 512], BF16, 2)
    gtg = Rot(S, "o_gt", [128, 32, 512], BF16, 2)
    t1r = Rot(S, "o_t1", [128, 512], F32, 2)
    t2r = Rot(S, "o_t2", [128, 512], F32, 2)
    mgs = Rot(S, "o_mgs", [128, 512], BF16, 3)
    for g in range(4):
        ba_, Bba = bag.next()
        bb_, Bbb = bbg.next()
        gt_, Bgt = gtg.next()
        S.dma("sp", ba_[:], A["BA"][:, :, g * 512:(g + 1) * 512].rearrange("h p t -> p h t"), reads=[B["BA"]], writes=[Bba])
        S.dma("sp", bb_[:], A["BB"][:, :, g * 512:(g + 1) * 512].rearrange("h p t -> p h t"), reads=[B["BB"]], writes=[Bbb])
        S.dma("sp", gt_[:], A["GT"][:, :, g * 512:(g + 1) * 512].rearrange("c p t -> p c t"), reads=[B["GT"]], writes=[Bgt])
        for c in range(16):
            pa, Bpa = PB.next()
            for k in range(8):
                S.op("pe", lambda e, pa=pa, k=k, c=c, ba_=ba_: e.matmul(pa[:], wa[:, k, c * 128:(c + 1) * 128], ba_[:, k, :], start=(k == 0), stop=(k == 7)),
                     reads=[Bw, Bba], writes=[Bpa])
            pb2, Bpb2 = PB.next()
            for k in range(8):
                S.op("pe", lambda e, pb2=pb2, k=k, c=c, bb_=bb_: e.matmul(pb2[:], wb[:, k, c * 128:(c + 1) * 128], bb_[:, k, :], start=(k == 0), stop=(k == 7)),
                     reads=[Bw, Bbb], writes=[Bpb2])
            t1, Bt1 = t1r.next()
            t2, Bt2 = t2r.next()
            S.op("dve", lambda e, t1=t1, pa=pa, gt_=gt_, c=c: e.tensor_tensor(t1[:], pa[:], gt_[:, c, :], ALU.mult), reads=[Bpa, Bgt], writes=[Bt1])
            S.op("dve", lambda e, t2=t2, pb2=pb2, gt_=gt_, c=c: e.tensor_tensor(t2[:], pb2[:], gt_[:, 16 + c, :], ALU.mult), reads=[Bpb2, Bgt], writes=[Bt2])
            m_, Bm = mgs.next()
            S.op("dve", lambda e, t1=t1, t2=t2, m_=m_: e.tensor_tensor(m_[:], t1[:], t2[:], ALU.add), reads=[Bt1, Bt2], writes=[Bm])
            S.dma("sp", A["MG"][c, :, g * 512:(g + 1) * 512], m_[:], reads=[Bm], writes=[B["MG"]], nowaw=True)


def phase3b_out(S, G):
    A = G["ap"]
    PB = G["pb"]
    B = G["B"]
    P = G["persist"]
    ident_f = G["ident_f"]
    Bw = S.buf("o_w")
    wr = S.sbuf("o_wr", [128, 16, 36], F32)
    S.dma("sp", wr[:], A["wr"].rearrange("(kc p) n -> p kc n", p=128), writes=[Bw], nowaw=True)
    brr = S.sbuf("o_brr", [1, 36], F32)
    S.dma("sp", brr[:], A["brr"], writes=[Bw], nowaw=True)
    g1 = S.sbuf("o_g1", [128, 2048], F32)
    b1 = S.sbuf("o_b1", [128, 2048], F32)
    S.dma("sp", g1[:], A["ln1_gb"], writes=[Bw], nowaw=True)
    S.dma("sp", b1[:], A["ln1_bb"], writes=[Bw], nowaw=True)
    ltri = S.sbuf("o_ltri", [128, 128], BF16)
    S.dma("sp", ltri[:], A["ltri"], writes=[Bw], nowaw=True)
    e256 = S.sbuf("o_e256", [128, 32], F32)
    S.dma("sp", e256[:], A["e256"], writes=[Bw], nowaw=True)
    onesc = S.sbuf("o_onesc", [128, 1], BF16)
    S.op("dve", lambda e: e.memset(onesc[:], 1.0), writes=[Bw])
    onesr = S.sbuf("o_onesr", [1, 128], F32)
    S.op("dve", lambda e: e.memset(onesr[:], 1.0), writes=[Bw])
    cnt = S.sbuf("o_cnt", [1, 32], F32)
    Bcnt = S.buf("o_cnt")
    S.op("dve", lambda e: e.memset(cnt[:], 0.0), writes=[Bcnt])
    S.barrier()
    wo = Rot(S, "o_wo", [128, 16, 512], BF16, 2)
    mgr = Rot(S, "o_mg", [128, 16, 512], BF16, 2)
    pre = Rot(S, "o_pre", [128, 2048], F32, 5)
    h1t = Rot(S, "o_h1", [128, 2048], F32, 2)
    h1b = Rot(S, "o_h1b", [128, 2048], BF16, 2)
    junk = S.sbuf("o_junk", [128, 2048], BF16)
    Bjunk = S.buf("o_junk")
    hT = Rot(S, "o_hT", [128, 16, 128], F32, 1)
    smalls = [S.sbuf("o_sm%d" % i, [128, 1], F32) for i in range(6)]
    Bsmall = S.buf("o_small")
    rt = {n: S.sbuf("o_r_" + n, shp, F32) for n, shp in (
        ("L", [128, 36]), ("gmax", [128, 1]), ("ngmax", [128, 1]), ("ohg", [128, 4]), ("ge", [128, 4]), ("gsum", [128, 1]), ("gp", [128, 1]),
        ("e8", [128, 8]), ("m1", [128, 1]), ("oh1", [128, 8]), ("e8b", [128, 8]), ("m2", [128, 1]), ("oh2", [128, 8]), ("d", [128, 1]),
        ("sg", [128, 1]), ("A1", [128, 32]), ("A2", [128, 32]), ("At", [128, 32]), ("rk", [128, 32]), ("t", [128, 32]), ("i1", [128, 1]), ("i2", [128, 1]))}
    Abf = S.sbuf("o_Abf", [128, 32], BF16)
    Br = G["Br_persist"]

    def rop(fn, eng="dve"):
        S.op(eng, fn, reads=[Br, Bw], writes=[Br])

    for g in range(4):
        mg, Bmg = mgr.next()
        S.dma("sp", mg[:], A["MG"][:, :, g * 512:(g + 1) * 512].rearrange("c p t -> p c t"), reads=[B["MG"]], writes=[Bmg])
        prs = []
        for t in range(4):
            tt = g * 4 + t
            pr, Bpr = pre.next()
            S.dma("sp", pr[:], A["xs"][OWN0 + tt * 128:OWN0 + (tt + 1) * 128, :], writes=[Bpr])
            prs.append((pr, Bpr))
        for cb in range(4):
            w_, Bwo = wo.next()
            S.dma("pool", w_[:], A["w_out"][:, cb * 512:(cb + 1) * 512].rearrange("(kc p) n -> p kc n", p=128), writes=[Bwo])
            for t in range(4):
                pr, Bpr = prs[t]
                po, Bpo = PB.next()
                for c in range(16):
                    S.op("pe", lambda e, po=po, c=c, t=t, w_=w_, mg=mg: e.matmul(po[:], mg[:, c, t * 128:(t + 1) * 128], w_[:, c, :], start=(c == 0), stop=(c == 15)),
                         reads=[Bmg, Bwo], writes=[Bpo])
                S.op("dve", lambda e, pr=pr, po=po, cb=cb: e.scalar_tensor_tensor(pr[:, cb * 512:(cb + 1) * 512], pr[:, cb * 512:(cb + 1) * 512], DN_ALPHA, po[:],
                                                                                op0=ALU.mult, op1=ALU.add), reads=[Bpo, Bpr], writes=[Bpr])
        for t in range(4):
            tt = g * 4 + t
            pr, Bpr = prs[t]
            h_, Bh = h1t.next()
            layer_norm_tile(S, G, pr, Bpr, h_, Bh, g1, b1, Bw, smalls, Bsmall, junk, Bjunk)
            S.dma("sp", A["H1"][tt * 128:(tt + 1) * 128, :], h_[:], reads=[Bh], writes=[B["H1"]], nowaw=True)
            hb, Bhb = h1b.next()
            S.op("act", lambda e, hb=hb, h_=h_: e.activation(hb[:], h_[:], AF.Copy), reads=[Bh], writes=[Bhb])
            S.dma("sp", A["H1b"][tt * 128:(tt + 1) * 128, :], hb[:], reads=[Bhb], writes=[B["H1b"]], nowaw=True)
            hT_, BhT = hT.next()
            for q4 in range(4):
                pb, Bpb = PB.next()
                for j in range(4):
                    c = q4 * 4 + j
                    S.op("pe", lambda e, pb=pb, h_=h_, c=c, j=j: e.matmul(pb[:, j * 128:(j + 1) * 128], h_[:, c * 128:(c + 1) * 128], ident_f[:], start=True, stop=True),
                         reads=[Bh, G["Bidf"]], writes=[Bpb])
                S.op("act", lambda e, pb=pb, hT_=hT_, q4=q4: e.activation(hT_[:, q4 * 4:(q4 + 1) * 4, :], pb[:].rearrange("p (a b) -> p a b", a=4), AF.Copy),
                     reads=[Bpb], writes=[BhT])
            pl, Bpl = PB.next()
            for c in range(16):
                S.op("pe", lambda e, pl=pl, hT_=hT_, c=c: e.matmul(pl[:, 0:36], hT_[:, c, :], wr[:, c, :], start=(c == 0), stop=False), reads=[BhT, Bw], writes=[Bpl])
            S.op("pe", lambda e, pl=pl: e.matmul(pl[:, 0:36], onesr[0:1, :], brr[0:1, :], start=False, stop=True), reads=[Bw], writes=[Bpl])
            L = rt["L"]
            S.op("dve", lambda e, pl=pl: e.tensor_copy(L[:], pl[:, 0:36]), reads=[Bpl], writes=[Br])
            rop(lambda e: e.reduce_max(rt["gmax"][:], L[:, 0:4], AX.X))
            rop(lambda e: e.tensor_scalar(rt["ohg"][:], L[:, 0:4], rt["gmax"][:, 0:1], None, op0=ALU.is_equal))
            rop(lambda e: e.tensor_scalar(rt["ngmax"][:], rt["gmax"][:], -1.0, None, op0=ALU.mult))
            rop(lambda e: e.activation(rt["ge"][:], L[:, 0:4], AF.Exp, bias=rt["ngmax"][:, 0:1], accum_out=rt["gsum"][:]), "act")
            rop(lambda e: e.reciprocal(rt["gp"][:], rt["gsum"][:]))
            rop(lambda e: e.tensor_scalar(rt["e8"][:], L[:, 4:12], rt["ohg"][:, 0:1], None, op0=ALU.mult))
            for gg in range(1, 4):
                rop(lambda e, gg=gg: e.scalar_tensor_tensor(rt["e8"][:], L[:, 4 + 8 * gg:12 + 8 * gg], rt["ohg"][:, gg:gg + 1], rt["e8"][:], op0=ALU.mult, op1=ALU.add))
            rop(lambda e: e.reduce_max(rt["m1"][:], rt["e8"][:], AX.X))
            rop(lambda e: e.tensor_scalar(rt["oh1"][:], rt["e8"][:], rt["m1"][:, 0:1], None, op0=ALU.is_equal))
            rop(lambda e: e.scalar_tensor_tensor(rt["e8b"][:], rt["oh1"][:], -1.0e30, rt["e8"][:], op0=ALU.mult, op1=ALU.add))
            rop(lambda e: e.reduce_max(rt["m2"][:], rt["e8b"][:], AX.X))
            rop(lambda e: e.tensor_scalar(rt["oh2"][:], rt["e8b"][:], rt["m2"][:, 0:1], None, op0=ALU.is_equal))
            rop(lambda e: e.tensor_tensor(rt["d"][:], rt["m1"][:], rt["m2"][:], ALU.subtract))
            rop(lambda e: e.activation(rt["sg"][:], rt["d"][:], AF.Sigmoid), "act")
            rop(lambda e, tt=tt: e.tensor_tensor(P["wts"][:, tt, 0:1], rt["sg"][:], rt["gp"][:], ALU.mult))
            rop(lambda e, tt=tt: e.tensor_tensor(P["wts"][:, tt, 1:2], rt["gp"][:], P["wts"][:, tt, 0:1], ALU.subtract))
            for gg in range(4):
                rop(lambda e, gg=gg: e.tensor_scalar(rt["A1"][:, gg * 8:(gg + 1) * 8], rt["oh1"][:], rt["ohg"][:, gg:gg + 1], None, op0=ALU.mult))
                rop(lambda e, gg=gg: e.tensor_scalar(rt["A2"][:, gg * 8:(gg + 1) * 8], rt["oh2"][:], rt["ohg"][:, gg:gg + 1], None, op0=ALU.mult))
            rop(lambda e: e.tensor_tensor(rt["At"][:], rt["A1"][:], rt["A2"][:], ALU.add))
            rop(lambda e: e.tensor_copy(Abf[:], rt["At"][:]))
            pk, Bpk = PB.next()
            S.op("pe", lambda e, pk=pk: e.matmul(pk[:, 0:32], ltri[:], Abf[:], start=True, stop=False), reads=[Bw, Br], writes=[Bpk])
            S.op("pe", lambda e, pk=pk: e.matmul(pk[:, 0:32], onesr[0:1, :], cnt[0:1, :], start=False, stop=True), reads=[Bw, Bcnt], writes=[Bpk])
            pc, Bpc = PB.next()
            S.op("pe", lambda e, pc=pc: e.matmul(pc[0:1, 0:32], onesc[:], Abf[:], start=True, stop=True), reads=[Bw, Br], writes=[Bpc])
            S.op("dve", lambda e, pk=pk: e.tensor_copy(rt["rk"][:], pk[:, 0:32]), reads=[Bpk, Br], writes=[Br])
            S.op("dve", lambda e, pc=pc: e.tensor_tensor(cnt[:], cnt[:], pc[0:1, 0:32], ALU.add), reads=[Bpc, Bcnt], writes=[Bcnt])
            rop(lambda e: e.scalar_tensor_tensor(rt["t"][:], rt["rk"][:], 1.0, rt["At"][:], op0=ALU.add, op1=ALU.mult))
            rop(lambda e, tt=tt: e.tensor_scalar(P["RK"][:, tt, :], rt["t"][:], -1.0, None, op0=ALU.add))
            rop(lambda e: e.tensor_tensor(rt["rk"][:], rt["rk"][:], e256[:], ALU.add))
            rop(lambda e: e.tensor_tensor(rt["t"][:], rt["rk"][:], rt["A1"][:], ALU.mult))
            rop(lambda e: e.reduce_sum(rt["i1"][:], rt["t"][:], AX.X))
            rop(lambda e: e.tensor_tensor(rt["t"][:], rt["rk"][:], rt["A2"][:], ALU.mult))
            rop(lambda e: e.reduce_sum(rt["i2"][:], rt["t"][:], AX.X))
            rop(lambda e, tt=tt: e.tensor_copy(P["idx"][:, tt, 0:1], rt["i1"][:]))
            rop(lambda e, tt=tt: e.tensor_copy(P["idx"][:, tt, 1:2], rt["i2"][:]))


def phase4_moe(S, G, experts=range(NEXP)):
    A = G["ap"]
    PB = G["pb"]
    B = G["B"]
    P = G["persist"]
    h1b = S.sbuf("m_h1b", [128, 16, 2048], BF16)
    Bh1b = S.buf("m_h1b")
    S.dma("sp", h1b[:], A["H1b"].rearrange("(t p) d -> p t d", p=128), reads=[B["H1b"]], writes=[Bh1b])
    iot = S.sbuf("m_iota", [128, CAP], F32)
    Biot = S.buf("m_iota")
    S.dma("sp", iot[:], A["iota256"], writes=[Biot])
    wu = Rot(S, "m_w", [128, 16, 512], BF16, 4)
    wd = Rot(S, "m_wd", [128, 4, 2048], BF16, 2)
    sel = Rot(S, "m_sel", [128, 16, CAP], BF16, 1)
    xs_ = Rot(S, "m_xs", [128, 16, CAP], BF16, 1)
    hT = Rot(S, "m_hT", [128, 8, CAP], BF16, 2)
    sg = Rot(S, "m_sg", [128, CAP], F32, 3)
    yst = Rot(S, "m_y", [128, 1024], F32, 1)
    for e_ in experts:
        s_, Bs = sel.next()
        for tt in range(16):
            S.op("dve", lambda e, s_=s_, tt=tt, e_=e_: e.tensor_scalar(s_[:, tt, :], iot[:], P["RK"][:, tt, e_:e_ + 1], None, op0=ALU.is_equal),
                 reads=[Biot, G["Br_persist"]], writes=[Bs])
        x_, Bx = xs_.next()
        for kc in range(16):
            pb, Bpb = PB.next()
            for tt in range(16):
                S.op("pe", lambda e, pb=pb, tt=tt, kc=kc, s_=s_: e.matmul(pb[:, 0:CAP], h1b[:, tt, kc * 128:(kc + 1) * 128], s_[:, tt, :],
                                                                         start=(tt == 0), stop=(tt == 15)), reads=[Bh1b, Bs], writes=[Bpb])
            if kc % 2 == 0:
                S.op("act", lambda e, pb=pb, x_=x_, kc=kc: e.activation(x_[:, kc, :], pb[:, 0:CAP], AF.Copy), reads=[Bpb], writes=[Bx])
            else:
                S.op("dve", lambda e, pb=pb, x_=x_, kc=kc: e.tensor_copy(x_[:, kc, :], pb[:, 0:CAP]), reads=[Bpb], writes=[Bx])
        h_, Bh = hT.next()
        for half in range(2):
            wg_, Bwg = wu.next()
            S.dma("pool", wg_[:], A["w_gate"][e_, :, half * 512:(half + 1) * 512].rearrange("(kc p) n -> p kc n", p=128), writes=[Bwg])
            wu_, Bwu = wu.next()
            S.dma("pool", wu_[:], A["w_up"][e_, :, half * 512:(half + 1) * 512].rearrange("(kc p) n -> p kc n", p=128), writes=[Bwu])
            for f4 in range(4):
                f = half * 4 + f4
                pg, Bpg = PB.next()
                for kc in range(16):
                    S.op("pe", lambda e, pg=pg, kc=kc, f4=f4, wg_=wg_, x_=x_: e.matmul(pg[:, 0:CAP], wg_[:, kc, f4 * 128:(f4 + 1) * 128], x_[:, kc, :],
                                                                                    start=(kc == 0), stop=(kc == 15)), reads=[Bwg, Bx], writes=[Bpg])
                for kc in range(16):
                    S.op("pe", lambda e, pg=pg, kc=kc, f4=f4, wu_=wu_, x_=x_: e.matmul(pg[:, CAP:2 * CAP], wu_[:, kc, f4 * 128:(f4 + 1) * 128], x_[:, kc, :],
                                                                                    start=(kc == 0), stop=(kc == 15)), reads=[Bwu, Bx], writes=[Bpg])
                s1, Bs1 = sg.next()
                S.op("act", lambda e, s1=s1, pg=pg: e.activation(s1[:], pg[:, 0:CAP], AF.Silu), reads=[Bpg], writes=[Bs1])
                S.op("dve", lambda e, h_=h_, f=f, s1=s1, pg=pg: e.tensor_tensor(h_[:, f, :], s1[:], pg[:, CAP:2 * CAP], ALU.mult), reads=[Bs1, Bpg], writes=[Bh])
        wds = []
        for half in range(2):
            wd_, Bwd = wd.next()
            S.dma("pool", wd_[:], A["w_down"][e_, half * 512:(half + 1) * 512, :].rearrange("(fc p) n -> p fc n", p=128), writes=[Bwd])
            wds.append((wd_, Bwd))
        for rh in range(CAP // 128):
            for cbp in range(2):
                y_, By = yst.next()
                for c2 in range(2):
                    cb = cbp * 2 + c2
                    py, Bpy = PB.next()
                    for f in range(8):
                        wd_, Bwd = wds[f // 4]
                        S.op("pe", lambda e, py=py, f=f, rh=rh, cb=cb, wd_=wd_, h_=h_: e.matmul(py[:], h_[:, f, rh * 128:(rh + 1) * 128], wd_[:, f % 4, cb * 512:(cb + 1) * 512],
                                                                                             start=(f == 0), stop=(f == 7)), reads=[Bh, Bwd], writes=[Bpy])
                    if c2 == 0:
                        S.op("act", lambda e, y_=y_, py=py, c2=c2: e.activation(y_[:, c2 * 512:(c2 + 1) * 512], py[:], AF.Copy), reads=[Bpy], writes=[By])
                    else:
                        S.op("dve", lambda e, y_=y_, py=py, c2=c2: e.tensor_copy(y_[:, c2 * 512:(c2 + 1) * 512], py[:]), reads=[Bpy], writes=[By])
                S.dma("sp", A["Y"][e_ * CAP + rh * 128:e_ * CAP + (rh + 1) * 128, cbp * 1024:(cbp + 1) * 1024], y_[:], reads=[By], writes=[B["Y"]], nowaw=True)


def phase5_final(S, G):
    A = G["ap"]
    B = G["B"]
    P = G["persist"]
    g2 = S.sbuf("f_g2", [128, 2048], F32)
    b2 = S.sbuf("f_b2", [128, 2048], F32)
    Bw = S.buf("f_w")
    S.dma("sp", g2[:], A["ln2_gb"], writes=[Bw], nowaw=True)
    S.dma("sp", b2[:], A["ln2_bb"], writes=[Bw], nowaw=True)
    idxi = S.sbuf("f_idx", [128, 16, 2], U32)
    Bidx = S.buf("f_idx")
    S.op("dve", lambda e: e.tensor_copy(idxi[:], P["idx"][:]), reads=[G["Br_persist"]], writes=[Bidx])
    S.barrier()
    h1 = Rot(S, "f_h1", [128, 2048], F32, 2)
    y1 = Rot(S, "f_y1", [128, 2048], F32, 2)
    y2 = Rot(S, "f_y2", [128, 2048], F32, 2)
    ot = Rot(S, "f_ot", [128, 2048], F32, 2)
    junk = S.sbuf("f_junk", [128, 2048], BF16)
    Bjunk = S.buf("f_junk")
    smalls = [S.sbuf("f_sm%d" % i, [128, 1], F32) for i in range(6)]
    Bsmall = S.buf("f_small")
    for tt in range(16):
        h_, Bh = h1.next()
        S.dma("sp", h_[:], A["H1"][tt * 128:(tt + 1) * 128, :], reads=[B["H1"]], writes=[Bh])
        a_, Ba = y1.next()
        b_, Bb = y2.next()
        for (dst, Bd, k) in ((a_, Ba, 0), (b_, Bb, 1)):
            S.dma("pool", None, None, reads=[B["Y"], Bidx], writes=[Bd],
                  builder=lambda e, dst=dst, tt=tt, k=k: e.indirect_dma_start(
                      out=dst[:], out_offset=None, in_=A["Y"], in_offset=bass.IndirectOffsetOnAxis(ap=idxi[:, tt, k:k + 1], axis=0),
                      bounds_check=NEXP * CAP - 1, oob_is_err=False))
        S.op("dve", lambda e, a_=a_, tt=tt: e.tensor_scalar(a_[:], a_[:], P["wts"][:, tt, 0:1], None, op0=ALU.mult), reads=[Ba, G["Br_persist"]], writes=[Ba])
        S.op("dve", lambda e, a_=a_, b_=b_, tt=tt: e.scalar_tensor_tensor(a_[:], b_[:], P["wts"][:, tt, 1:2], a_[:], op0=ALU.mult, op1=ALU.add),
             reads=[Ba, Bb, G["Br_persist"]], writes=[Ba])
        S.op("dve", lambda e, a_=a_, h_=h_: e.scalar_tensor_tensor(a_[:], h_[:], DN_ALPHA, a_[:], op0=ALU.mult, op1=ALU.add), reads=[Ba, Bh], writes=[Ba])
        o_, Bo = ot.next()
        layer_norm_tile(S, G, a_, Ba, o_, Bo, g2, b2, Bw, smalls, Bsmall, junk, Bjunk)
        S.dma("sp", A["out"][tt * 128:(tt + 1) * 128, :], o_[:], reads=[Bo], writes=[B["out"]], nowaw=True)

from contextlib import ExitStack
from concourse.bass_utils import run_bass_kernel_spmd

BFM_IDX = {"af": 0, "aq": 8, "ag": 16, "bk": 24, "bq": 32, "iq": 40, "ik": 44, "g": 45}
NBFM = 77

SCRATCH = {
    "KT": ([8, 128, 8192], "bf16"), "VH": ([8, 128, 64, 128], "bf16"), "KIT": ([128, 8192], "bf16"),
    "HG_kdec": ([8, 128, 8192], "bf16"), "HG_kend": ([8192, 1024], "bf16"), "HG_v": ([8192, 1024], "bf16"),
    "HG_dec": ([128, 8, 128], "f32"), "HG_qdec": ([8, 128, 2048], "bf16"), "HG_gs": ([8, 128, 2048], "bf16"),
    "QT": ([8, 128, 2048], "bf16"), "QIT": ([4, 128, 2048], "bf16"), "WI": ([2048, 8], "f32"),
    "GT": ([32, 128, 2048], "bf16"), "BA": ([8, 128, 2048], "bf16"), "BB": ([8, 128, 2048], "bf16"),
    "MG": ([16, 128, 2048], "bf16"), "H1": ([2048, 2048], "f32"), "H1b": ([2048, 2048], "bf16"), "Y": ([NEXP * CAP, 2048], "f32"),
}

INPUTS = {
    "xs": ([8192, 2048], "f32"), "valid_tm": ([128, 64], "f32"), "w_in": ([2048, 11848], "f32"),
    "ident": ([128, 128], "f32"), "bfm": ([128, NBFM], "f32"), "brow": ([1, 2056], "f32"),
    "lbl": ([128, 2, 8], "f32"), "normg": ([128, 8], "f32"), "rmask": ([128, 512], "f32"), "bdmask": ([128, 128], "f32"),
    "alb": ([128, 8, 64], "f32"), "corr": ([128, 8, 128], "bf16"), "sel2": ([2, 128], "bf16"), "dtab": ([128, 8192], "bf16"),
    "qrel": ([1, 512], "f32"), "adm": ([16, 2, 8192], "bf16"),
    "w_branch_a": ([1024, 2048], "f32"), "w_branch_b": ([1024, 2048], "f32"), "w_out": ([2048, 2048], "f32"),
    "wr": ([2048, 36], "f32"), "brr": ([1, 36], "f32"),
    "ln1_gb": ([128, 2048], "f32"), "ln1_bb": ([128, 2048], "f32"), "ln2_gb": ([128, 2048], "f32"), "ln2_bb": ([128, 2048], "f32"),
    "ltri": ([128, 128], "bf16"), "e256": ([128, 32], "f32"), "iota256": ([128, CAP], "f32"),
    "w_gate": ([NEXP, 2048, 1024], "f32"), "w_up": ([NEXP, 2048, 1024], "f32"), "w_down": ([NEXP, 1024, 2048], "f32"),
}


def _dt(s):
    return {"f32": F32, "bf16": BF16, "u32": U32, "i32": I32}[s]


def host_consts(inputs):
    b_in = np.asarray(inputs["b_in"][0], np.float32)
    bfm = np.zeros((128, NBFM), np.float32)
    def put(idx, c0, n):
        for c in range(n):
            bfm[:, idx + c] = b_in[c0 + c * 128: c0 + (c + 1) * 128]
    put(BFM_IDX["af"], C_AF, 8); put(BFM_IDX["aq"], C_AQ, 8); put(BFM_IDX["ag"], C_AG, 8)
    put(BFM_IDX["bk"], C_BK, 8); put(BFM_IDX["bq"], C_BQ, 8); put(BFM_IDX["iq"], C_IQ, 4)
    put(BFM_IDX["g"], C_G, 32)
    bfm[0:64, BFM_IDX["ik"]] = b_in[C_IK:C_IK + 64]
    bfm[64:128, BFM_IDX["ik"]] = b_in[C_IK:C_IK + 64]
    brow = np.concatenate([b_in[C_AI:C_AI + 1024], b_in[C_BV:C_BV + 1024], b_in[C_IW:C_IW + 8]])[None, :].astype(np.float32)
    lbl = np.ascontiguousarray(np.asarray(inputs["hg_lb_logits"], np.float32).reshape(2, 8, 128).transpose(2, 0, 1))
    normg = np.ascontiguousarray(np.asarray(inputs["hg_norm_g"][0], np.float32).reshape(8, 128).T)
    rmask = np.ones((128, 512), np.float32)
    rmask[:, ::64] = 0.0
    ii = np.arange(128)
    bdmask = ((ii[:, None] // 64 == ii[None, :] // 64) & (ii[:, None] <= ii[None, :])).astype(np.float32)
    import ml_dtypes
    bf = ml_dtypes.bfloat16
    slopes = 2.0 ** -(np.arange(8) + 1.0)
    pp = np.arange(128)
    alb = (slopes[None, :, None] * (pp[:, None, None] + 128.0 * (np.arange(64)[None, None, :] - 60))).astype(np.float32)
    dsq = np.maximum(pp[:, None] - pp[None, :], 0).astype(np.float64)
    corr = np.exp(-2.0 * slopes[None, :, None] * dsq[:, None, :]).astype(bf)
    sel2 = np.zeros((2, 128), np.float32); sel2[0, :64] = 1; sel2[1, 64:] = 1
    dtab = np.abs(8064 + pp[:, None] - np.arange(8192)[None, :]).astype(bf)
    qrel = np.arange(512, dtype=np.float32)[None, :]
    extra = {}
    if "w_out" in inputs:
        extra["w_branch_a"] = np.ascontiguousarray(inputs["w_branch_a"][0]); extra["w_branch_b"] = np.ascontiguousarray(inputs["w_branch_b"][0])
        extra["w_out"] = np.ascontiguousarray(inputs["w_out"][0])
        extra["wr"] = np.ascontiguousarray(np.concatenate([inputs["w_group"][0], inputs["w_router"][0]], axis=1).astype(np.float32))
        extra["brr"] = np.concatenate([inputs["b_group"][0], inputs["b_router"][0]])[None, :].astype(np.float32)
        for nm in ("ln1_g", "ln1_b", "ln2_g", "ln2_b"):
            extra[nm + "b"] = np.ascontiguousarray(np.broadcast_to(np.asarray(inputs[nm][0], np.float32)[None, :], (128, 2048)))
        extra["ltri"] = (pp[:, None] < pp[None, :]).astype(bf)
        extra["e256"] = np.ascontiguousarray(np.broadcast_to((np.arange(32, dtype=np.float32) * CAP)[None, :], (128, 32)))
        extra["iota256"] = np.ascontiguousarray(np.broadcast_to(np.arange(CAP, dtype=np.float32)[None, :], (128, CAP)))
        extra["w_gate"] = np.ascontiguousarray(inputs["w_gate"][0]); extra["w_up"] = np.ascontiguousarray(inputs["w_up"][0])
        extra["w_down"] = np.ascontiguousarray(inputs["w_down"][0])
    return {**extra, "alb": alb, "corr": corr, "sel2": sel2.astype(bf), "dtab": dtab, "qrel": qrel, "bdmask": bdmask, "ident": np.eye(128, dtype=np.float32), "bfm": bfm, "brow": brow, "lbl": lbl, "normg": normg, "rmask": rmask,
            "w_in": np.ascontiguousarray(inputs["w_in"][0])}


def host_core_inputs(inputs, hc, core):
    b, j = core // 4, core % 4
    x = np.asarray(inputs["x"], np.float32)
    xs = np.zeros((8192, 2048), np.float32)
    npre = (3 - j) * 2048
    xs[npre:] = x[b, :(j + 1) * 2048]
    valid = np.zeros(8192, np.float32)
    valid[npre:] = 1.0
    d = dict(hc)
    d["xs"] = xs
    d["valid_tm"] = np.ascontiguousarray(valid.reshape(64, 128).T)
    import ml_dtypes
    chunk = np.arange(8192) // 64
    adm = np.full((16, 2, 8192), NEG_ADM, np.float32)
    for t in range(16):
        c_first = (OWN0 + t * 128) // 64
        adm[t, 0, (valid > 0) & (chunk <= c_first)] = 0.0
        adm[t, 1, (valid > 0) & (chunk <= c_first + 1)] = 0.0
    d["adm"] = adm.astype(ml_dtypes.bfloat16)
    return d


def build_program(phases=("p1a",), dump=(), p1_blocks=(0, 1, 2, 3), p2_groups=(0, 1, 2, 3), in_names=None):
    nc = bass.Bass("TRN2", target_bir_lowering=False)
    A = {}
    used_inputs = in_names if in_names is not None else list(INPUTS)
    for n in used_inputs:
        shp, dt = INPUTS[n]
        A[n] = nc.dram_tensor(n, shp, _dt(dt), kind="ExternalInput").ap()
    for n, (shp, dt) in SCRATCH.items():
        kind = "ExternalOutput" if n in dump else "Internal"
        A[n] = nc.dram_tensor(n, shp, _dt(dt), kind=kind).ap()
    A["out"] = nc.dram_tensor("out", [2048, 2048], F32, kind="ExternalOutput").ap()
    with ExitStack() as es:
        S = Sched(nc, es)
        G = {"ap": A, "pb": PBanks(S), "B": {n: S.buf(n, glob=True) for n in list(SCRATCH) + ["out"]}, "bfm_idx": BFM_IDX}
        G["persist"] = {"wts": S.sbuf("p_wts", [128, 16, 2], F32), "RK": S.sbuf("p_RK", [128, 16, 32], F32), "idx": S.sbuf("p_idx", [128, 16, 2], F32)}
        G["Br_persist"] = S.buf("persist", glob=True)
        cst = {}
        Bc = S.buf("cst", glob=True)
        G["cst"] = cst
        G["Bcst"] = Bc
        idf = S.sbuf("idf", [128, 128], F32)
        Bidf = S.buf("idf", glob=True)
        S.dma("sp", idf[:], A["ident"], writes=[Bidf])
        idb = S.sbuf("idb", [128, 128], BF16)
        Bident = S.buf("idb")
        S.op("dve", lambda e: e.tensor_copy(idb[:], idf[:]), reads=[Bidf], writes=[Bident])
        G["ident_bf"] = idb
        G["Bident"] = Bident
        G["ident_f"] = idf
        G["Bidf"] = Bidf
        for n in ("bfm", "brow", "valid_tm", "normg", "rmask", "bdmask"):
            shp, dt = INPUTS[n]
            cst[n] = S.sbuf("c_" + n, shp, _dt(dt))
            S.dma("sp", cst[n][:], A[n], writes=[Bc], nowaw=True)
        lbl = S.sbuf("c_lbl", [128, 2, 8], F32)
        Blbl = S.buf("lbl", glob=True)
        S.dma("sp", lbl[:], A["lbl"], writes=[Blbl])
        for n in ("lb", "oml", "noml", "lbd"):
            cst[n] = S.sbuf("c_" + n, [128, 8], F32)
        cst["ones_row"] = S.sbuf("c_ones_row", [1, 128], F32)
        S.op("dve", lambda e: e.memset(cst["ones_row"][:], 1.0), writes=[Bc])
        S.op("dve", lambda e: e.tensor_tensor(cst["lbd"][:], lbl[:, 0, :], lbl[:, 1, :], ALU.subtract), reads=[Blbl], writes=[Bc])
        S.op("act", lambda e: e.activation(cst["lb"][:], cst["lbd"][:], AF.Sigmoid), reads=[Bc], writes=[Bc])
        S.op("dve", lambda e: e.tensor_scalar(cst["oml"][:], cst["lb"][:], -1.0, 1.0, op0=ALU.mult, op1=ALU.add), reads=[Bc], writes=[Bc])
        S.op("dve", lambda e: e.tensor_scalar(cst["noml"][:], cst["oml"][:], -1.0, None, op0=ALU.mult), reads=[Bc], writes=[Bc])

        cst["eps_ln"] = S.sbuf("c_eps_ln", [128, 1], F32)
        S.op("dve", lambda e: e.memset(cst["eps_ln"][:], LN_EPS), writes=[Bc])
        cst["eps_rms"] = S.sbuf("c_eps_rms", [128, 1], F32)
        S.op("dve", lambda e: e.memset(cst["eps_rms"][:], RMS_EPS), writes=[Bc])
        S.barrier()
        if "p1a" in phases:
            with ExitStack() as pes:
                S.es = pes
                phase1a(S, G, blocks=p1_blocks)
                S.es = es
            S.barrier()
            S.phase_end()
        if "p1b" in phases:
            with ExitStack() as pes:
                S.es = pes
                phase1b(S, G)
                S.es = es
            S.barrier()
            S.phase_end()
        if "p2" in phases:
            phase2_dsa(S, G, groups=p2_groups)
            S.barrier()
            S.phase_end()
        for nm, fn in (("p3a", phase3a_merge), ("p3b", phase3b_out), ("p4", phase4_moe), ("p5", phase5_final)):
            if nm in phases:
                with ExitStack() as pes:
                    S.es = pes
                    fn(S, G)
                    S.es = es
                S.barrier()
                S.phase_end()
        outs = [G["B"][n] for n in dump] + ([G["B"]["out"]] if "p5" in phases else [])
        S.wait_all("sp", outs)
        print("instructions:", S.ninst, {k: len(v) for k, v in S.ops.items()})
        S.run()
    return nc


ALL_PHASES = ("p1a", "p1b", "p2", "p3a", "p3b", "p4", "p5")
_CACHE = {}


def kernel(**inputs):
    if "nc" not in _CACHE:
        _CACHE["nc"] = build_program(phases=ALL_PHASES)
    nc = _CACHE["nc"]
    hc = host_consts(inputs)
    in_maps = [host_core_inputs(inputs, hc, c) for c in range(8)]
    res = run_bass_kernel_spmd(nc, in_maps, core_ids=list(range(8)))
    out = np.zeros((2, 8192, 2048), np.float32)
    for c in range(8):
        b, j = c // 4, c % 4
        out[b, j * 2048:(j + 1) * 2048] = np.asarray(res.results[c]["out"])
    return out
```

```python
import numpy as np
import concourse.bass as bass
import concourse.mybir as mybir

F32 = mybir.dt.float32
BF16 = mybir.dt.bfloat16
U32 = mybir.dt.uint32
I32 = mybir.dt.int32
U8 = mybir.dt.uint8
AF = mybir.ActivationFunctionType
ALU = mybir.AluOpType
AX = mybir.AxisListType


class Buf:
    __slots__ = ("name", "lastw", "readers", "dsem", "dcount", "glob", "dkey")

    def __init__(self, name, glob=False):
        self.name = name
        self.glob = glob
        self.dkey = None
        self.lastw = None
        self.readers = {}
        self.dsem = None
        self.dcount = 0


class Sched:
    ENGS = ("pe", "act", "dve", "pool", "sp")
    SEM_LIMIT = 30000

    def __init__(self, nc, es):
        self.nc = nc
        self.es = es
        self.es_sem = es
        self.dbufs = []
        self.dstate = {}
        self.free_dsems = []
        self.local_dbufs = []
        self.sem = {}
        self.count = {}
        self.known = {}
        self.ops = {}
        self.epoch = {}
        for n in self.ENGS:
            self.sem[n] = es.enter_context(nc.semaphore("se_" + n))
            self.count[n] = 0
            self.epoch[n] = 0
            self.known[n] = {}
            self.ops[n] = []
        self.nbuf = 0
        self.ninst = 0

    def sbuf(self, name, shape, dtype):
        self.nbuf += 1
        name = "%s_u%d" % (name, self.nbuf)
        return self.es.enter_context(self.nc.sbuf_tensor(name, list(shape), dtype))

    def psum(self, name, shape, dtype):
        return self.es.enter_context(self.nc.psum_tensor(name, list(shape), dtype))

    def buf(self, name=None, glob=False):
        self.nbuf += 1
        return Buf("%s_b%d" % (name or "b", self.nbuf), glob)

    def bufs(self, n, name="b"):
        return [self.buf("%s%d" % (name, i)) for i in range(n)]

    def _waits(self, eng, reads, writes):
        need = {}

        def add(ev, skip_same):
            if ev is None:
                return
            key, sem, val, prod = ev
            if skip_same and prod == eng:
                return
            if self.known[eng].get(key, 0) >= val:
                return
            if key not in need or need[key][1] < val:
                need[key] = (sem, val)

        for b in reads:
            add(b.lastw, False)
        for b in writes:
            add(b.lastw, True)
            for ev in b.readers.values():
                add(ev, True)
        for key, (sem, val) in need.items():
            self.known[eng][key] = val
        return list(need.values())

    def op(self, eng, fn, reads=(), writes=()):
        waits = self._waits(eng, reads, writes)
        if self.count[eng] >= self.SEM_LIMIT:
            self.epoch[eng] += 1
            self.count[eng] = 0
            self.sem[eng] = self.es_sem.enter_context(self.nc.semaphore("se_%s_%d" % (eng, self.epoch[eng])))
        self.count[eng] += 1
        seq = self.count[eng]
        sem = self.sem[eng]
        key = "e_%s_%d" % (eng, self.epoch[eng])
        ev = (key, sem, seq, eng)
        for b in writes:
            b.lastw = ev
            b.readers = {}
        for b in reads:
            b.readers[key] = ev
        self.ninst += 1 + len(waits)

        def emit(e, fn=fn, waits=waits, sem=sem):
            for (s, v) in waits:
                e.wait_ge(s, v)
            fn(e).then_inc(sem, 1)

        self.ops[eng].append(emit)
        return ev

    def dma(self, q, out_ap, in_ap, reads=(), writes=(), nowaw=False, builder=None, **kw):
        waits = self._waits(q, reads, [] if nowaw else writes)
        tb = writes[0]
        if tb.dsem is None:
            if (not tb.glob) and self.free_dsems:
                tb.dsem, tb.dcount, tb.dkey = self.free_dsems.pop()
            else:
                tb.dsem = self.es_sem.enter_context(self.nc.semaphore("sd_" + tb.name))
                tb.dkey = "d_" + tb.name
            if not tb.glob:
                self.local_dbufs.append(tb)
        tb.dcount += 16
        self.dstate[tb.dkey] = (tb.dsem, tb.dcount)
        ev = (tb.dkey, tb.dsem, tb.dcount, None)
        for b in writes:
            b.lastw = ev
            if not nowaw:
                b.readers = {}
        for b in reads:
            b.readers[ev[0]] = ev
        self.ninst += 1 + len(waits)

        def emit(e, waits=waits, sem=tb.dsem, out_ap=out_ap, in_ap=in_ap, kw=kw, builder=builder):
            for (s, v) in waits:
                e.wait_ge(s, v)
            if builder is not None:
                builder(e).then_inc(sem, 16)
            else:
                e.dma_start(out=out_ap, in_=in_ap, **kw).then_inc(sem, 16)

        self.ops[q].append(emit)
        return ev

    def phase_end(self):
        for b in self.local_dbufs:
            self.free_dsems.append((b.dsem, b.dcount, b.dkey))
            b.dsem = None
        self.local_dbufs = []

    def raw(self, eng, fn):
        self.ops[eng].append(lambda e, fn=fn: fn(e))

    def wait_all(self, eng, bufs):
        waits = self._waits(eng, list(bufs), [])

        def emit(e, waits=waits):
            for (s, v) in waits:
                e.wait_ge(s, v)

        self.ops[eng].append(emit)

    def barrier(self):
        evs = []
        for n in self.ENGS:
            if self.count[n] > 0:
                evs.append(("e_%s_%d" % (n, self.epoch[n]), self.sem[n], self.count[n], n))
        for key, (sem, cnt) in self.dstate.items():
            evs.append((key, sem, cnt, None))
        for eng in self.ENGS:
            waits = []
            for (key, sem, val, prod) in evs:
                if prod == eng:
                    continue
                if self.known[eng].get(key, 0) >= val:
                    continue
                self.known[eng][key] = val
                waits.append((sem, val))

            def emit(e, waits=waits):
                for (s, v) in waits:
                    e.wait_ge(s, v)

            self.ops[eng].append(emit)

    def run(self):
        nc = self.nc
        ops = self.ops
        with nc.Block() as block:
            @block.tensor
            def _(e):
                for f in ops["pe"]:
                    f(e)

            @block.scalar
            def _(e):
                for f in ops["act"]:
                    f(e)

            @block.vector
            def _(e):
                for f in ops["dve"]:
                    f(e)

            @block.gpsimd
            def _(e):
                for f in ops["pool"]:
                    f(e)

            @block.sync
            def _(e):
                for f in ops["sp"]:
                    f(e)

NSLOT = 8192
NOWN = 2048
OWN0 = NSLOT - NOWN
D = 2048
KC = 16
C_AQ, C_AF, C_AI, C_AG, C_BQ, C_BK, C_BV, C_IQ, C_IK, C_IW, C_G = 0, 1024, 2048, 3072, 4096, 5120, 6144, 7168, 7680, 7744, 7752
W_SCALE = (8 ** -0.5) * (64 ** -0.5)


class Rot:
    def __init__(self, S, name, shape, dtype, n):
        self.t = [S.sbuf("%s%d" % (name, i), shape, dtype) for i in range(n)]
        self.b = [S.buf("%s%d" % (name, i)) for i in range(n)]
        self.i = 0
        self.n = n

    def next(self):
        k = self.i % self.n
        self.i += 1
        return self.t[k], self.b[k]


class PBanks:
    def __init__(self, S):
        self.t = [S.psum("pb%d" % i, [128, 512], F32) for i in range(8)]
        self.b = [S.buf("pb%d" % i) for i in range(8)]
        self.i = 0

    def next(self, lo=0, hi=8):
        n = hi - lo
        k = lo + (self.i % n)
        self.i += 1
        return self.t[k], self.b[k]


def phase1a(S, G, blocks=(0, 1, 2, 3)):
    A = G["ap"]
    PB = G["pb"]
    ident = G["ident_bf"]
    Bident = G["Bident"]
    w_in = A["w_in"]
    xT = S.sbuf("xT", [128, KC, 2048], BF16)
    BxT = S.bufs(16, "xT")
    xin = Rot(S, "xin", [128, 2048], BF16, 2)
    wt = Rot(S, "wt", [128, KC, 512], BF16, 2)
    f32t = Rot(S, "hgf", [128, 512], F32, 12)
    st16 = Rot(S, "st16", [128, 512], BF16, 8)
    stkT = Rot(S, "stkT", [128, 4, 128], BF16, 2)
    decst = S.sbuf("decst", [128, 8, 32], F32)
    Bdec = S.buf("decst")
    wist = Rot(S, "wist", [128, 8], F32, 2)
    cst = G["cst"]
    Bc = G["Bcst"]
    evq = [0]

    def evac_engine():
        evq[0] += 1
        return "act" if evq[0] % 2 else "dve"

    def copy_op(eng, out_ap, in_ap, reads, writes):
        if eng == "act":
            S.op("act", lambda e, out_ap=out_ap, in_ap=in_ap: e.activation(out_ap, in_ap, AF.Copy), reads=reads, writes=writes)
        else:
            S.op("dve", lambda e, out_ap=out_ap, in_ap=in_ap: e.tensor_copy(out_ap, in_ap), reads=reads, writes=writes)

    for blk in blocks:
        own = (blk == 3)
        s0 = blk * 2048
        for t in range(16):
            xi, Bxi = xin.next()
            S.dma("pool", xi[:], A["xs"][s0 + t * 128: s0 + (t + 1) * 128, :], writes=[Bxi])
            for q4 in range(4):
                pb, Bpb = PB.next()
                for j in range(4):
                    kc = q4 * 4 + j
                    S.op("pe", lambda e, pb=pb, xi=xi, kc=kc, j=j: e.matmul(
                        pb[:, j * 128:(j + 1) * 128], xi[:, kc * 128:(kc + 1) * 128], ident[:], start=True, stop=True),
                        reads=[Bxi, Bident], writes=[Bpb])
                copy_op(evac_engine(), xT[:, q4 * 4:(q4 + 1) * 4, t * 128:(t + 1) * 128],
                        pb[:].rearrange("p (a b) -> p a b", a=4), [Bpb], [BxT[t]])

        def load_w(cols):
            w, Bw = wt.next()
            off = 0
            for (c0, n) in cols:
                S.dma("pool", w[:, :, off:off + n], w_in[:, c0:c0 + n].rearrange("(kc p) n -> p kc n", p=128), writes=[Bw])
                off += n
            return w, Bw

        def fm_mm(w, Bw, m, g):
            pb, Bpb = PB.next()
            for kc in range(KC):
                S.op("pe", lambda e, pb=pb, w=w, kc=kc, m=m, g=g: e.matmul(
                    pb[:], w[:, kc, m * 128:(m + 1) * 128], xT[:, kc, g * 512:(g + 1) * 512], start=(kc == 0), stop=(kc == KC - 1)),
                    reads=[Bw] + BxT[g * 4:(g + 1) * 4], writes=[Bpb])
            return pb, Bpb

        def simple_fm(w, Bw, m, g, bias_ap, func, dst_ap, Bd, rows=128):
            pb, Bpb = fm_mm(w, Bw, m, g)
            st, Bst = st16.next()
            S.op("act", lambda e, st=st, pb=pb, func=func, bias_ap=bias_ap: e.activation(st[:], pb[:], func, bias=bias_ap), reads=[Bpb, Bc], writes=[Bst])
            S.dma("sp", dst_ap, st[0:rows, :], reads=[Bst], writes=[Bd], nowaw=True)

        tm_units = [("ai", C_AI), ("ai", C_AI + 512), ("bv", C_BV), ("bv", C_BV + 512)]
        for (kind, c0) in tm_units:
            w, Bw = load_w([(c0, 512)])
            for t in range(16):
                pb, Bpb = PB.next()
                for kc in range(KC):
                    S.op("pe", lambda e, pb=pb, w=w, kc=kc, t=t: e.matmul(
                        pb[:], xT[:, kc, t * 128:(t + 1) * 128], w[:, kc, :], start=(kc == 0), stop=False),
                        reads=[Bw, BxT[t]], writes=[Bpb])
                boff = (c0 - C_AI) if kind == "ai" else (1024 + c0 - C_BV)
                S.op("pe", lambda e, pb=pb, boff=boff: e.matmul(pb[:], cst["ones_row"][0:1, :], cst["brow"][0:1, boff:boff + 512],
                                                            start=False, stop=True), reads=[Bc], writes=[Bpb])
                st, Bst = st16.next()
                tt = blk * 16 + t
                if kind == "ai":
                    S.op("act", lambda e, st=st, pb=pb, tt=tt: e.activation(st[:], pb[:], AF.Identity, scale=cst["valid_tm"][:, tt:tt + 1]),
                         reads=[Bpb, Bc], writes=[Bst])
                    dst = A["HG_v"][s0 + t * 128:s0 + (t + 1) * 128, c0 - C_AI:c0 - C_AI + 512]
                else:
                    S.op("dve", lambda e, st=st, pb=pb: e.tensor_copy(st[:], pb[:]), reads=[Bpb], writes=[Bst])
                    h0 = (c0 - C_BV) // 128
                    dst = A["VH"][h0:h0 + 4, :, tt, :].rearrange("h p d -> p h d")
                if kind == "ai":
                    S.dma("sp", dst, st[:], reads=[Bst], writes=[G["B"]["HG_v"]], nowaw=True)
                else:
                    S.dma("sp", dst, st[:].rearrange("p (h d) -> p h d", h=4), reads=[Bst], writes=[G["B"]["VH"]], nowaw=True)
        if own:
            w, Bw = load_w([(C_IW, 8)])
            for t in range(16):
                pb, Bpb = PB.next()
                for kc in range(KC):
                    S.op("pe", lambda e, pb=pb, w=w, kc=kc, t=t: e.matmul(
                        pb[:, 0:8], xT[:, kc, t * 128:(t + 1) * 128], w[:, kc, 0:8], start=(kc == 0), stop=False),
                        reads=[Bw, BxT[t]], writes=[Bpb])
                S.op("pe", lambda e, pb=pb: e.matmul(pb[:, 0:8], cst["ones_row"][0:1, :], cst["brow"][0:1, 2048:2056],
                                                     start=False, stop=True), reads=[Bc], writes=[Bpb])
                st, Bst = wist.next()
                S.op("dve", lambda e, st=st, pb=pb: e.tensor_scalar(st[:], pb[:, 0:8], W_SCALE, None, op0=ALU.mult), reads=[Bpb], writes=[Bst])
                S.dma("sp", A["WI"][t * 128:(t + 1) * 128, :], st[:], reads=[Bst], writes=[G["B"]["WI"]], nowaw=True)

        for u in range(2):
            w, Bw = load_w([(C_BK + u * 512, 512)])
            for m in range(4):
                h = u * 4 + m
                for g in range(4):
                    simple_fm(w, Bw, m, g, cst["bfm"][:, G["bfm_idx"]["bk"] + h: G["bfm_idx"]["bk"] + h + 1], AF.Identity,
                              A["KT"][h, :, s0 + g * 512:s0 + (g + 1) * 512], G["B"]["KT"])
        w, Bw = load_w([(C_IK, 64), (C_IK, 64)])
        for g in range(4):
            simple_fm(w, Bw, 0, g, cst["bfm"][:, G["bfm_idx"]["ik"]: G["bfm_idx"]["ik"] + 1], AF.Identity,
                      A["KIT"][:, s0 + g * 512:s0 + (g + 1) * 512], G["B"]["KIT"])
        if own:
            for u in range(2):
                w, Bw = load_w([(C_BQ + u * 512, 512)])
                for m in range(4):
                    h = u * 4 + m
                    for g in range(4):
                        simple_fm(w, Bw, m, g, cst["bfm"][:, G["bfm_idx"]["bq"] + h: G["bfm_idx"]["bq"] + h + 1], AF.Identity,
                                  A["QT"][h, :, g * 512:(g + 1) * 512], G["B"]["QT"])
            w, Bw = load_w([(C_IQ, 512)])
            for m in range(4):
                for g in range(4):
                    simple_fm(w, Bw, m, g, cst["bfm"][:, G["bfm_idx"]["iq"] + m: G["bfm_idx"]["iq"] + m + 1], AF.Identity,
                              A["QIT"][m, :, g * 512:(g + 1) * 512], G["B"]["QIT"])
            for u in range(8):
                w, Bw = load_w([(C_G + u * 512, 512)])
                for m in range(4):
                    c = u * 4 + m
                    for g in range(4):
                        simple_fm(w, Bw, m, g, cst["bfm"][:, G["bfm_idx"]["g"] + c: G["bfm_idx"]["g"] + c + 1], AF.Sigmoid,
                                  A["GT"][c, :, g * 512:(g + 1) * 512], G["B"]["GT"])

        deferred = []
        for h in range(8):
            if own:
                w, Bw = load_w([(C_AF + h * 128, 128), (C_AQ + h * 128, 128), (C_AG + h * 128, 128)])
            else:
                if h % 4 == 0:
                    w4, Bw4 = load_w([(C_AF + h * 128, 512)])
                w, Bw = w4, Bw4
            mf = 0 if own else (h % 4)
            bi = G["bfm_idx"]
            oml_h = cst["oml"][:, h:h + 1]
            noml_h = cst["noml"][:, h:h + 1]
            lb_h = cst["lb"][:, h:h + 1]
            b_af = cst["bfm"][:, bi["af"] + h: bi["af"] + h + 1]
            b_aq = cst["bfm"][:, bi["aq"] + h: bi["aq"] + h + 1]
            b_ag = cst["bfm"][:, bi["ag"] + h: bi["ag"] + h + 1]
            for g in range(4):
                pb, Bpb = fm_mm(w, Bw, mf, g)
                while deferred:
                    deferred.pop(0)()
                sg, Bsg = f32t.next()
                S.op("act", lambda e, sg=sg, pb=pb, b_af=b_af: e.activation(sg[:], pb[:], AF.Sigmoid, bias=b_af),
                     reads=[Bpb, Bc], writes=[Bsg])
                lf, Blf = f32t.next()
                S.op("act", lambda e, lf=lf, sg=sg, oml_h=oml_h, lb_h=lb_h: e.activation(lf[:], sg[:], AF.Ln, scale=oml_h, bias=lb_h),
                     reads=[Bsg, Bc], writes=[Blf])
                cum, Bcum = f32t.next()
                S.op("dve", lambda e, cum=cum, lf=lf: e.tensor_tensor_scan(cum[:], cst["rmask"][:], lf[:], 0.0, ALU.mult, ALU.add),
                     reads=[Blf, Bc], writes=[Bcum])
                en, Ben = f32t.next()
                S.op("act", lambda e, en=en, cum=cum: e.activation(en[:], cum[:], AF.Exp, scale=-1.0), reads=[Bcum], writes=[Ben])
                kk, Bkk = f32t.next()
                S.op("dve", lambda e, kk=kk, sg=sg, noml_h=noml_h, oml_h=oml_h: e.tensor_scalar(kk[:], sg[:], noml_h, oml_h, op0=ALU.mult, op1=ALU.add),
                     reads=[Bsg, Bc], writes=[Bkk])
                kd, Bkd = st16.next()
                S.op("dve", lambda e, kd=kd, kk=kk, en=en: e.tensor_tensor(kd[:], kk[:], en[:], ALU.mult), reads=[Bkk, Ben], writes=[Bkd])
                S.dma("sp", A["HG_kdec"][h, :, s0 + g * 512:s0 + (g + 1) * 512], kd[:], reads=[Bkd], writes=[G["B"]["HG_kdec"]], nowaw=True)
                ex2, Bex2 = f32t.next()
                for c in range(8):
                    S.op("act", lambda e, ex2=ex2, cum=cum, c=c: e.activation(ex2[:, c * 64:(c + 1) * 64], cum[:, c * 64:(c + 1) * 64], AF.Exp,
                                                                           scale=-1.0, bias=cum[:, c * 64 + 63:c * 64 + 64]),
                         reads=[Bcum], writes=[Bex2])
                ke, Bke = st16.next()
                S.op("dve", lambda e, ke=ke, kk=kk, ex2=ex2: e.tensor_tensor(ke[:], kk[:], ex2[:], ALU.mult), reads=[Bkk, Bex2], writes=[Bke])
                dec_ap = decst[:, h, g * 8:(g + 1) * 8]
                S.op("act", lambda e, cum=cum, dec_ap=dec_ap: e.activation(dec_ap, cum[:].rearrange("p (c s) -> p c s", s=64)[:, :, 63], AF.Exp),
                     reads=[Bcum], writes=[Bdec])
                def kend_T(ke=ke, Bke=Bke, g=g, h=h, s0=s0):
                    pbt, Bpbt = PB.next()
                    for j in range(4):
                        S.op("pe", lambda e, pbt=pbt, ke=ke, j=j: e.matmul(pbt[:, j * 128:(j + 1) * 128], ke[:, j * 128:(j + 1) * 128], ident[:],
                                                                           start=True, stop=True), reads=[Bke, Bident], writes=[Bpbt])
                    kT, BkT = stkT.next()
                    S.op("dve", lambda e, kT=kT, pbt=pbt: e.tensor_copy(kT[:], pbt[:].rearrange("p (a b) -> p a b", a=4)), reads=[Bpbt], writes=[BkT])
                    S.dma("sp", A["HG_kend"][s0 + g * 512:s0 + (g + 1) * 512, h * 128:(h + 1) * 128].rearrange("(t p) d -> p t d", p=128),
                          kT[:], reads=[BkT], writes=[G["B"]["HG_kend"]], nowaw=True)
                deferred.append(kend_T)
                if own:
                    ec, Bec = f32t.next()
                    S.op("act", lambda e, ec=ec, cum=cum: e.activation(ec[:], cum[:], AF.Exp), reads=[Bcum], writes=[Bec])
                    pbq, Bpbq = fm_mm(w, Bw, 1, g)
                    qs, Bqs = f32t.next()
                    S.op("act", lambda e, qs=qs, pbq=pbq, b_aq=b_aq: e.activation(qs[:], pbq[:], AF.Silu, bias=b_aq),
                         reads=[Bpbq, Bc], writes=[Bqs])
                    qd, Bqd = st16.next()
                    S.op("dve", lambda e, qd=qd, qs=qs, ec=ec: e.tensor_tensor(qd[:], qs[:], ec[:], ALU.mult), reads=[Bqs, Bec], writes=[Bqd])
                    S.dma("sp", A["HG_qdec"][h, :, g * 512:(g + 1) * 512], qd[:], reads=[Bqd], writes=[G["B"]["HG_qdec"]], nowaw=True)
                    simple_fm(w, Bw, 2, g, b_ag, AF.Silu, A["HG_gs"][h, :, g * 512:(g + 1) * 512], G["B"]["HG_gs"])
        while deferred:
            deferred.pop(0)()
        S.dma("sp", A["HG_dec"][:, :, blk * 32:(blk + 1) * 32], decst[:], reads=[Bdec], writes=[G["B"]["HG_dec"]], nowaw=True)

RMS_EPS = 1e-6


def phase1b(S, G):
    A = G["ap"]
    PB = G["pb"]
    cst = G["cst"]
    Bc = G["Bcst"]
    B = G["B"]
    kend = S.sbuf("hs_kend", [128, 16, 1024], BF16)
    Bkend = S.buf("hs_kend")
    vv = S.sbuf("hs_v", [128, 16, 1024], BF16)
    Bvv = S.buf("hs_v")
    dec = S.sbuf("hs_dec", [128, 8, 128], F32)
    Bdec = S.buf("hs_dec")
    Sf = S.sbuf("hs_S", [128, 8, 128], F32)
    Sb = S.sbuf("hs_Sb", [128, 8, 128], BF16)
    BS = S.bufs(8, "hs_S")
    BSb = S.bufs(8, "hs_Sb")
    S.dma("sp", dec[:], A["HG_dec"], reads=[B["HG_dec"]], writes=[Bdec])
    S.op("dve", lambda e: e.memset(Sf[:], 0.0), writes=BS)
    S.op("dve", lambda e: e.memset(Sb[:], 0.0), writes=BSb)
    onesb = S.sbuf("hs_ones", [128, 128], BF16)
    Bones = S.buf("hs_ones")
    S.op("dve", lambda e: e.memset(onesb[:], 1.0 / 128.0), writes=[Bones])
    kdec = Rot(S, "hs_kdec", [128, 2048], BF16, 8)
    qdec = Rot(S, "hs_qdec", [128, 2048], BF16, 8)
    gs = Rot(S, "hs_gs", [128, 2048], BF16, 8)
    attm = Rot(S, "hs_attm", [128, 128], BF16, 8)
    sq = Rot(S, "hs_sq", [128, 128], BF16, 6)
    rs = Rot(S, "hs_rs", [128, 128], F32, 6)
    yy = Rot(S, "hs_y", [128, 128], F32, 6)
    bastr = Rot(S, "hs_bast", [128, 8, 128], BF16, 2)

    def state_update(t, h, half, ):
        p0 = half * 64
        chunk = None
        pk, Bpk = PB.next()
        S.op("pe", lambda e, pk=pk, t=t, h=h, p0=p0: e.matmul(pk[:, 0:128], kend[p0:p0 + 64, t, h * 128:(h + 1) * 128],
                                                            vv[p0:p0 + 64, t, h * 128:(h + 1) * 128], start=True, stop=True),
             reads=[Bkend, Bvv], writes=[Bpk])
        return pk, Bpk

    for blk in range(4):
        own = (blk == 3)
        s0 = blk * 2048
        S.dma("sp", kend[:], A["HG_kend"][s0:s0 + 2048, :].rearrange("(t p) c -> p t c", p=128), reads=[B["HG_kend"]], writes=[Bkend])
        S.dma("sp", vv[:], A["HG_v"][s0:s0 + 2048, :].rearrange("(t p) c -> p t c", p=128), reads=[B["HG_v"]], writes=[Bvv])
        if not own:
            for t in range(16):
                for half in range(2):
                    ch = blk * 32 + t * 2 + half
                    for h in range(8):
                        pk, Bpk = state_update(t, h, half)
                        S.op("dve", lambda e, pk=pk, h=h, ch=ch: e.scalar_tensor_tensor(Sf[:, h, :], Sf[:, h, :], dec[:, h, ch:ch + 1], pk[:, 0:128],
                                                                                      op0=ALU.mult, op1=ALU.add),
                             reads=[Bpk, Bdec, BS[h]], writes=[BS[h]])
            if blk == 2:
                for h in range(8):
                    S.op("act", lambda e, h=h: e.activation(Sb[:, h, :], Sf[:, h, :], AF.Copy), reads=[BS[h]], writes=[BSb[h]])
            continue
        kds, qds, ggs = [], [], []
        for h in range(8):
            kd, Bkd = kdec.next()
            qd, Bqd = qdec.next()
            gg, Bgg = gs.next()
            S.dma("sp", kd[:], A["HG_kdec"][h, :, s0:s0 + 2048], reads=[B["HG_kdec"]], writes=[Bkd])
            S.dma("sp", qd[:], A["HG_qdec"][h], reads=[B["HG_qdec"]], writes=[Bqd])
            S.dma("sp", gg[:], A["HG_gs"][h], reads=[B["HG_gs"]], writes=[Bgg])
            kds.append((kd, Bkd)); qds.append((qd, Bqd)); ggs.append((gg, Bgg))
        for t in range(16):
            tc = slice(t * 128, (t + 1) * 128)
            bt, Bbt = bastr.next()
            for h in range(8):
                kd, Bkd = kds[h]
                qd, Bqd = qds[h]
                gg, Bgg = ggs[h]
                pa, Bpa = PB.next()
                S.op("pe", lambda e, pa=pa, kd=kd, qd=qd, tc=tc: e.matmul(pa[:, 0:128], kd[:, tc], qd[:, tc], start=True, stop=True),
                     reads=[Bkd, Bqd], writes=[Bpa])
                am, Bam = attm.next()
                S.op("dve", lambda e, am=am, pa=pa: e.tensor_tensor(am[:], pa[:, 0:128], cst["bdmask"][:], ALU.mult), reads=[Bpa, Bc], writes=[Bam])
                po, Bpo = PB.next()
                S.op("pe", lambda e, po=po, am=am, t=t, h=h: e.matmul(po[:, 0:128], vv[:, t, h * 128:(h + 1) * 128], am[:], start=True, stop=False),
                     reads=[Bvv, Bam], writes=[Bpo])
                for half in range(2):
                    ch = blk * 32 + t * 2 + half
                    c0 = t * 128 + half * 64
                    S.op("pe", lambda e, po=po, qd=qd, h=h, c0=c0, half=half: e.matmul(po[:, half * 64:(half + 1) * 64], Sb[:, h, :], qd[:, c0:c0 + 64],
                                                                                  start=False, stop=(half == 1)),
                         reads=[BSb[h], Bqd], writes=[Bpo])
                    pk, Bpk = state_update(t, h, half)
                    S.op("dve", lambda e, pk=pk, h=h, ch=ch: e.scalar_tensor_tensor(Sf[:, h, :], Sf[:, h, :], dec[:, h, ch:ch + 1], pk[:, 0:128],
                                                                                  op0=ALU.mult, op1=ALU.add),
                         reads=[Bpk, Bdec, BS[h]], writes=[BS[h]])
                    S.op("act", lambda e, h=h: e.activation(Sb[:, h, :], Sf[:, h, :], AF.Copy), reads=[BS[h]], writes=[BSb[h]])
                q2, Bq2 = sq.next()
                S.op("act", lambda e, q2=q2, po=po: e.activation(q2[:], po[:, 0:128], AF.Square), reads=[Bpo], writes=[Bq2])
                pm, Bpm = PB.next()
                S.op("pe", lambda e, pm=pm, q2=q2: e.matmul(pm[:, 0:128], onesb[:], q2[:], start=True, stop=True), reads=[Bones, Bq2], writes=[Bpm])
                r1, Br1 = rs.next()
                S.op("act", lambda e, r1=r1, pm=pm: e.activation(r1[:], pm[:, 0:128], AF.Sqrt, bias=cst["eps_rms"][:, 0:1]), reads=[Bpm, Bc], writes=[Br1])
                S.op("dve", lambda e, r1=r1: e.reciprocal(r1[:], r1[:]), reads=[Br1], writes=[Br1])
                y1, By1 = yy.next()
                S.op("dve", lambda e, y1=y1, po=po, r1=r1: e.tensor_tensor(y1[:], po[:, 0:128], r1[:], ALU.mult), reads=[Bpo, Br1], writes=[By1])
                S.op("dve", lambda e, y1=y1, gg=gg, tc=tc, h=h, bt=bt: e.scalar_tensor_tensor(bt[:, h, :], y1[:], cst["normg"][:, h:h + 1], gg[:, tc],
                                                                                           op0=ALU.mult, op1=ALU.mult),
                     reads=[By1, Bgg, Bc], writes=[Bbt])
            S.dma("sp", A["BA"][:, :, tc].rearrange("h p t -> p h t"), bt[:], reads=[Bbt], writes=[B["BA"]], nowaw=True)

SM_SCALE = 128 ** -0.5
TOPK = 256
NBIS = 18
NTER = 10
BIS_WIN = 16.0
NEG_ADM = -30000.0


def phase2_dsa(S, G, groups=(0, 1, 2, 3)):
    A = G["ap"]
    PB = G["pb"]
    cst = G["cst"]
    Bc = G["Bcst"]
    B = G["B"]
    ident = G["ident_bf"]
    Bident = G["Bident"]
    es_outer = S.es
    with ExitStack() as pes:
        S.es = pes
        alb = S.sbuf("ds_alb", [128, 8, 64], F32)
        dtab = S.sbuf("ds_dtab", [128, 8192], BF16)
        qrel = S.sbuf("ds_qrel", [1, 512], F32)
        onesr = S.sbuf("ds_onesr", [1, 128], BF16)
        drow = S.sbuf("ds_drow", [1, 512], F32)
        Bdrow = S.buf("ds_drow")
        shrow = S.sbuf("ds_shrow", [1, 8, 512], BF16)
        Bshrow = S.buf("ds_shrow")
        dmc = S.sbuf("ds_dmc", [128, 1], F32)
        Bdmc = S.buf("ds_dmc")
        corr = S.sbuf("ds_corr", [128, 8, 128], BF16)
        sel2 = S.sbuf("ds_sel2", [2, 128], BF16)
        onesb = S.sbuf("ds_ones", [128, 128], BF16)
        Bk = S.buf("ds_const")
        S.dma("sp", alb[:], A["alb"], writes=[Bk], nowaw=True)
        S.dma("sp", corr[:], A["corr"], writes=[Bk], nowaw=True)
        S.dma("sp", sel2[:], A["sel2"], writes=[Bk], nowaw=True)
        S.op("dve", lambda e: e.memset(onesb[:], 1.0), writes=[Bk])
        S.op("dve", lambda e: e.memset(onesr[:], 1.0), writes=[Bk])
        S.dma("sp", dtab[:], A["dtab"], writes=[Bk], nowaw=True)
        S.dma("sp", qrel[:], A["qrel"], writes=[Bk], nowaw=True)
        kit = S.sbuf("ds_kit", [128, 8192], BF16)
        S.dma("sp", kit[:], A["KIT"], reads=[B["KIT"]], writes=[Bk], nowaw=True)
        wi = S.sbuf("ds_wi", [128, 16, 8], F32)
        S.dma("sp", wi[:], A["WI"].rearrange("(t p) h -> p t h", p=128), reads=[B["WI"]], writes=[Bk], nowaw=True)
        maskT = S.sbuf("ds_maskT", [128, 64, 512], U8)
        BmT = S.buf("ds_maskT")
        S.barrier()

        def do_group(g):
            Q0 = OWN0 + g * 512
            with ExitStack() as aes:
                S.es = aes
                qit = S.sbuf("ds_qit", [128, 4, 512], BF16)
                Bqit = S.buf("ds_qit")
                S.dma("sp", qit[:], A["QIT"][:, :, g * 512:(g + 1) * 512].rearrange("m p q -> p m q"), reads=[B["QIT"]], writes=[Bqit])
                sc2 = [S.sbuf("ds_sc", [128, 8192], F32) for _ in range(2)]
                Bsc2 = [S.buf("ds_sc"), S.buf("ds_sc")]
                mq = S.sbuf("ds_mq", [128, 8192], BF16)
                Bmq = S.buf("ds_mq")
                adm = Rot(S, "ds_adm", [2, 512], BF16, 3)
                junk2 = S.sbuf("ds_junk2", [128, 8192], U8)
                Bj2 = S.buf("ds_junk2")
                relu = Rot(S, "ds_relu", [128, 512], BF16, 9)
                dg2 = [S.sbuf("ds_dg", [128, 8, 128], BF16) for _ in range(2)]
                Bdg2 = [S.buf("ds_dg"), S.buf("ds_dg")]
                sm = {n: S.sbuf("ds_s_" + n, [128, 1], F32) for n in ("lo", "hi", "mid", "cnt", "ge", "d", "c0", "d3", "t1", "nt2", "s2", "g2")}
                Bsm = S.buf("ds_small")
                Bth = S.buf("ds_th")
                Bs2 = S.buf("ds_s2")

                def tparams(T):
                    tg = g * 4 + T
                    Qt = Q0 + T * 128
                    nk = Qt + 128
                    nb5 = (nk + 511) // 512
                    return tg, Qt, nk, nb5, nb5 * 512

                def score_blocks(T):
                    tg, Qt, nk, nb5, nkp = tparams(T)
                    sc, Bsc = sc2[T % 2], Bsc2[T % 2]
                    dg, Bdg = dg2[T % 2], Bdg2[T % 2]
                    out = []

                    def prep():
                        for h in range(8):
                            S.op("dve", lambda e, h=h: e.tensor_scalar(dg[:, h, :], ident[:], wi[:, tg, h:h + 1], None, op0=ALU.mult),
                                 reads=[Bident, Bk], writes=[Bdg])
                    out.append(prep)

                    def blk(kb5):
                        ks = slice(kb5 * 512, (kb5 + 1) * 512)
                        ad, Bad = adm.next()
                        S.dma("sp", ad[:], A["adm"][tg, :, ks], writes=[Bad])
                        rl = []
                        for hp in range(4):
                            for par in range(2):
                                pb, Bpb = PB.next()
                                p0 = par * 64
                                S.op("pe", lambda e, pb=pb, hp=hp, p0=p0: e.matmul(
                                    pb[:], qit[p0:p0 + 64, hp, T * 128:(T + 1) * 128], kit[p0:p0 + 64, ks], start=True, stop=True),
                                    reads=[Bqit, Bk], writes=[Bpb])
                                r, Br = relu.next()
                                if par == 0 or hp % 2 == 0:
                                    S.op("act", lambda e, r=r, pb=pb: e.activation(r[:], pb[:], AF.Relu), reads=[Bpb], writes=[Br])
                                else:
                                    S.op("dve", lambda e, r=r, pb=pb: e.tensor_scalar(r[:], pb[:], 0.0, None, op0=ALU.max), reads=[Bpb], writes=[Br])
                                rl.append((r, Br))
                        ps, Bps = PB.next()
                        for h in range(8):
                            r, Br = rl[h]
                            S.op("pe", lambda e, ps=ps, h=h, r=r: e.matmul(ps[:], dg[:, h, :], r[:], start=(h == 0), stop=False),
                                 reads=[Bdg, Br], writes=[Bps])
                        S.op("pe", lambda e, ps=ps, ad=ad: e.matmul(ps[:], sel2[0:2, :], ad[0:2, :], start=False, stop=True),
                             reads=[Bk, Bad], writes=[Bps])
                        S.op("act", lambda e, ps=ps: e.activation(sc[:, ks], ps[:], AF.Copy), reads=[Bps], writes=[Bsc])
                    for kb5 in range(nb5):
                        out.append(lambda kb5=kb5: blk(kb5))
                    return out

                def search_init(T):
                    tg, Qt, nk, nb5, nkp = tparams(T)
                    sc, Bsc = sc2[T % 2], Bsc2[T % 2]
                    scv = sc[:, 0:nkp]
                    S.op("dve", lambda e: e.reduce_max(sm["hi"][:], scv, AX.X), reads=[Bsc], writes=[Bsm])
                    S.op("dve", lambda e: e.tensor_scalar(sm["lo"][:], sm["hi"][:], -BIS_WIN, None, op0=ALU.add), reads=[Bsm], writes=[Bsm])
                    S.op("dve", lambda e: e.tensor_scalar(sm["hi"][:], sm["hi"][:], 1e-3, None, op0=ALU.add), reads=[Bsm], writes=[Bsm, Bth])
                    S.op("dve", lambda e: e.tensor_scalar(mq[:, 0:nkp], scv, sm["lo"][:, 0:1], None, op0=ALU.is_ge, op1=ALU.add,
                                                          accum_out=sm["c0"][:]), reads=[Bsc, Bsm], writes=[Bmq, Bsm])

                def search_round(T, it):
                    tg, Qt, nk, nb5, nkp = tparams(T)
                    sc, Bsc = sc2[T % 2], Bsc2[T % 2]
                    scv = sc[:, 0:nkp]
                    d3 = (BIS_WIN + 1e-3) / (3.0 ** (it + 1))
                    S.op("dve", lambda e: e.tensor_scalar(sm["t1"][:], sm["lo"][:], d3, None, op0=ALU.add), reads=[Bsm], writes=[Bsm])
                    S.op("dve", lambda e: e.tensor_scalar(sm["nt2"][:], sm["lo"][:], -1.0, -2.0 * d3, op0=ALU.mult, op1=ALU.add), reads=[Bsm], writes=[Bsm, Bth])
                    S.op("act", lambda e: e.activation(junk2[:, 0:nkp], scv, AF.Sign, bias=sm["nt2"][:, 0:1], accum_out=sm["s2"][:]),
                         reads=[Bsc, Bth], writes=[Bj2, Bs2])
                    S.op("dve", lambda e: e.tensor_scalar(mq[:, 0:nkp], scv, sm["t1"][:, 0:1], None, op0=ALU.is_ge, op1=ALU.add,
                                                          accum_out=sm["cnt"][:]), reads=[Bsc, Bsm], writes=[Bmq, Bsm])
                    S.op("dve", lambda e: e.tensor_scalar(sm["ge"][:], sm["cnt"][:], TOPK - 0.5, None, op0=ALU.is_ge), reads=[Bsm], writes=[Bsm])
                    S.op("dve", lambda e: e.scalar_tensor_tensor(sm["g2"][:], sm["s2"][:], 2.0 * (TOPK - 0.5) - nkp, sm["ge"][:], op0=ALU.is_ge, op1=ALU.add),
                         reads=[Bs2, Bsm], writes=[Bsm])
                    S.op("dve", lambda e: e.scalar_tensor_tensor(sm["lo"][:], sm["g2"][:], d3, sm["lo"][:], op0=ALU.mult, op1=ALU.add),
                         reads=[Bsm], writes=[Bsm])

                def finalize(T):
                    tg, Qt, nk, nb5, nkp = tparams(T)
                    sc, Bsc = sc2[T % 2], Bsc2[T % 2]
                    scv = sc[:, 0:nkp]
                    S.op("dve", lambda e: e.tensor_scalar(sm["ge"][:], sm["c0"][:], TOPK - 0.5, None, op0=ALU.is_ge), reads=[Bsm], writes=[Bsm])
                    S.op("dve", lambda e: e.tensor_scalar(sm["d"][:], sm["lo"][:], 1000.0, None, op0=ALU.add), reads=[Bsm], writes=[Bsm])
                    S.op("dve", lambda e: e.tensor_scalar(sm["lo"][:], sm["d"][:], sm["ge"][:, 0:1], -1000.0, op0=ALU.mult, op1=ALU.add),
                         reads=[Bsm], writes=[Bsm])
                    S.op("dve", lambda e: e.tensor_scalar(mq[:, 0:nkp], scv, sm["lo"][:, 0:1], None, op0=ALU.is_ge), reads=[Bsc, Bsm], writes=[Bmq])
                    S.op("dve", lambda e: e.scalar_tensor_tensor(sc[:, 0:nk], mq[:, 0:nk], -16384.0, dtab[:, 8192 - nk:8192], op0=ALU.mult, op1=ALU.add),
                         reads=[Bmq, Bk], writes=[Bsc])
                    S.op("dve", lambda e: e.tensor_reduce(dmc[:], sc[:, 0:nk], AX.X, ALU.min), reads=[Bsc], writes=[Bdmc])
                    S.op("dve", lambda e: e.tensor_scalar(dmc[:], dmc[:], 16384.0, None, op0=ALU.add), reads=[Bdmc], writes=[Bdmc])
                    pbd, Bpbd = PB.next()
                    S.op("pe", lambda e: e.matmul(pbd[0:1, 0:128], dmc[:, 0:1], G["ident_f"][:], start=True, stop=True),
                         reads=[Bdmc, G["Bidf"]], writes=[Bpbd])
                    S.op("dve", lambda e: e.tensor_copy(drow[0:1, T * 128:(T + 1) * 128], pbd[0:1, 0:128]), reads=[Bpbd], writes=[Bdrow])
                    nkb = nk // 128
                    for k4 in range(0, nkb, 4):
                        n4 = min(4, nkb - k4)
                        pb, Bpb = PB.next()
                        for j in range(n4):
                            kb = k4 + j
                            S.op("pe", lambda e, pb=pb, j=j, kb=kb: e.matmul(pb[:, j * 128:(j + 1) * 128], mq[:, kb * 128:(kb + 1) * 128], ident[:],
                                                                           start=True, stop=True), reads=[Bmq, Bident], writes=[Bpb])
                        dst = maskT[:, k4:k4 + n4, T * 128:(T + 1) * 128]
                        src = pb[:, 0:n4 * 128].rearrange("p (a b) -> p a b", a=n4)
                        if (k4 // 4) % 2 == 0:
                            S.op("act", lambda e, dst=dst, src=src: e.activation(dst, src, AF.Copy), reads=[Bpb], writes=[BmT])
                        else:
                            S.op("dve", lambda e, dst=dst, src=src: e.tensor_copy(dst, src), reads=[Bpb], writes=[BmT])

                for f_ in score_blocks(0):
                    f_()
                for T in range(4):
                    nxt = score_blocks(T + 1) if T < 3 else []
                    per = -(-len(nxt) // NTER)
                    search_init(T)
                    for it in range(NTER):
                        search_round(T, it)
                        for _ in range(per):
                            if nxt:
                                nxt.pop(0)()
                    while nxt:
                        nxt.pop(0)()
                    finalize(T)
                S.op("dve", lambda e: e.tensor_tensor(drow[:], drow[:], qrel[:], ALU.subtract), reads=[Bdrow, Bk], writes=[Bdrow])
                for h in range(8):
                    S.op("dve", lambda e, h=h: e.tensor_scalar(shrow[0:1, h, :], drow[:], (2.0 ** -(h + 1)) / SM_SCALE, None, op0=ALU.mult),
                         reads=[Bdrow], writes=[Bshrow])
                S.barrier()
                S.phase_end()
            with ExitStack() as bes:
                S.es = bes
                kt = Rot(S, "ds_kt", [128, 8192], BF16, 2)
                vh = Rot(S, "ds_vh", [128, 64, 128], BF16, 2)
                qt = Rot(S, "ds_qt", [128, 512], BF16, 2)
                pT = Rot(S, "ds_pT", [128, 512], BF16, 9)
                mcr = Rot(S, "ds_mc", [128, 128], BF16, 3)
                rec = Rot(S, "ds_rec", [128, 512], F32, 1)
                ob = Rot(S, "ds_ob", [128, 512], BF16, 1)
                nkb = (Q0 + 512) // 128
                kb0 = Q0 // 128
                for h in range(8):
                    k_, Bk_ = kt.next()
                    v_, Bv_ = vh.next()
                    q_, Bq_ = qt.next()
                    S.dma("sp", k_[:, 0:nkb * 128], A["KT"][h, :, 0:nkb * 128], reads=[B["KT"]], writes=[Bk_])
                    S.dma("sp", v_[:, 0:nkb, :], A["VH"][h, :, 0:nkb, :], reads=[B["VH"]], writes=[Bv_])
                    S.dma("sp", q_[:], A["QT"][h, :, g * 512:(g + 1) * 512], reads=[B["QT"]], writes=[Bq_])
                    po, Bpo = PB.t[6], PB.b[6]
                    pd, Bpd = PB.t[7], PB.b[7]
                    pend = []

                    def stage2(kb, p_, Bp_, c0, first, last, po=po, pd=pd, Bpo=Bpo, Bpd=Bpd, v_=v_, Bv_=Bv_):
                        S.op("pe", lambda e, po=po, v_=v_, p_=p_, kb=kb, c0=c0, first=first, last=last: e.matmul(
                            po[:, c0:512], v_[:, kb, :], p_[:, c0:512], start=first, stop=last), reads=[Bv_, Bp_], writes=[Bpo])
                        S.op("pe", lambda e, pd=pd, p_=p_, c0=c0, first=first, last=last: e.matmul(
                            pd[:, c0:512], onesb[:], p_[:, c0:512], start=first, stop=last), reads=[Bk, Bp_], writes=[Bpd])

                    for kb in range(nkb):
                        r = kb - kb0
                        c0 = max(r, 0) * 128
                        first = (kb == 0)
                        last = (kb == nkb - 1)
                        pst, Bpst = PB.next(0, 6)
                        S.op("pe", lambda e, pst=pst, k_=k_, q_=q_, kb=kb, c0=c0: e.matmul(
                            pst[:, c0:512], k_[:, kb * 128:(kb + 1) * 128], q_[:, c0:512], start=True, stop=(h >= 6)),
                            reads=[Bk_, Bq_], writes=[Bpst])
                        if h < 6:
                            S.op("pe", lambda e, pst=pst, h=h, c0=c0: e.matmul(pst[:, c0:512], onesr[0:1, :], shrow[0:1, h, c0:512], start=False, stop=True),
                                 reads=[Bk, Bshrow], writes=[Bpst])
                        p_, Bp_ = pT.next()
                        bias_ap = alb[:, h, r + 60:r + 61]
                        S.op("act", lambda e, p_=p_, pst=pst, c0=c0, bias_ap=bias_ap: e.activation(
                            p_[:, c0:512], pst[:, c0:512], AF.Exp, scale=SM_SCALE, bias=bias_ap), reads=[Bpst, Bk], writes=[Bp_])
                        if r >= 0:
                            mc, Bmc = mcr.next()
                            S.op("dve", lambda e, mc=mc, kb=kb, r=r, h=h: e.tensor_tensor(mc[:], maskT[:, kb, r * 128:(r + 1) * 128], corr[:, h, :], ALU.mult),
                                 reads=[BmT, Bk], writes=[Bmc])
                            S.op("dve", lambda e, p_=p_, mc=mc, r=r: e.scalar_tensor_tensor(p_[:, r * 128:(r + 1) * 128], p_[:, r * 128:(r + 1) * 128], 3.0e38, mc[:],
                                                                                         op0=ALU.min, op1=ALU.mult), reads=[Bmc, Bp_], writes=[Bp_])
                            if c0 + 128 < 512:
                                S.op("dve", lambda e, p_=p_, kb=kb, c0=c0: e.scalar_tensor_tensor(p_[:, c0 + 128:512], p_[:, c0 + 128:512], 3.0e38, maskT[:, kb, c0 + 128:512],
                                                                                               op0=ALU.min, op1=ALU.mult), reads=[BmT, Bp_], writes=[Bp_])
                        else:
                            S.op("dve", lambda e, p_=p_, kb=kb: e.scalar_tensor_tensor(p_[:], p_[:], 3.0e38, maskT[:, kb, :], op0=ALU.min, op1=ALU.mult),
                                 reads=[BmT, Bp_], writes=[Bp_])
                        pend.append((kb, p_, Bp_, c0, first, last))
                        if len(pend) > 6:
                            stage2(*pend.pop(0))
                    while pend:
                        stage2(*pend.pop(0))
                    rc, Brc = rec.next()
                    S.op("dve", lambda e, rc=rc, pd=pd: e.reciprocal(rc[:], pd[:]), reads=[Bpd], writes=[Brc])
                    o_, Bo_ = ob.next()
                    S.op("dve", lambda e, o_=o_, po=po, rc=rc: e.tensor_tensor(o_[:], po[:], rc[:], ALU.mult), reads=[Bpo, Brc], writes=[Bo_])
                    S.dma("sp", A["BB"][h, :, g * 512:(g + 1) * 512], o_[:], reads=[Bo_], writes=[B["BB"]], nowaw=True)
                S.barrier()
                S.phase_end()
        for g in groups:
            do_group(g)
        S.es = es_outer

LN_EPS = 1e-5
DN_ALPHA = 2.0 ** 0.25
CAP = 256
NEXP = 32


def layer_norm_tile(S, G, pre, Bpre, out_t, Bout, gb, bb, Bgb, small, Bsmall, junk, Bjunk):
    s1, s2, mean, var, rstd, nmr = small
    S.op("act", lambda e: e.activation(junk[:], pre[:], AF.Identity, accum_out=s1[:]), reads=[Bpre], writes=[Bjunk, Bsmall])
    S.op("act", lambda e: e.activation(junk[:], pre[:], AF.Square, accum_out=s2[:]), reads=[Bpre], writes=[Bjunk, Bsmall])
    S.op("dve", lambda e: e.tensor_scalar(mean[:], s1[:], 1.0 / 2048.0, None, op0=ALU.mult), reads=[Bsmall], writes=[Bsmall])
    S.op("dve", lambda e: e.tensor_tensor(var[:], mean[:], mean[:], ALU.mult), reads=[Bsmall], writes=[Bsmall])
    S.op("dve", lambda e: e.scalar_tensor_tensor(var[:], s2[:], 1.0 / 2048.0, var[:], op0=ALU.mult, op1=ALU.subtract), reads=[Bsmall], writes=[Bsmall])
    S.op("act", lambda e: e.activation(rstd[:], var[:], AF.Sqrt, bias=G["cst"]["eps_ln"][:, 0:1]), reads=[Bsmall, G["Bcst"]], writes=[Bsmall])
    S.op("dve", lambda e: e.reciprocal(rstd[:], rstd[:]), reads=[Bsmall], writes=[Bsmall])
    S.op("dve", lambda e: e.scalar_tensor_tensor(nmr[:], mean[:], -1.0, rstd[:], op0=ALU.mult, op1=ALU.mult), reads=[Bsmall], writes=[Bsmall])
    S.op("act", lambda e: e.activation(pre[:], pre[:], AF.Identity, scale=rstd[:, 0:1], bias=nmr[:, 0:1]), reads=[Bpre, Bsmall], writes=[Bpre])
    S.op("dve", lambda e: e.tensor_tensor(pre[:], pre[:], gb[:], ALU.mult), reads=[Bpre, Bgb], writes=[Bpre])
    S.op("dve", lambda e: e.tensor_tensor(out_t[:], pre[:], bb[:], ALU.add), reads=[Bpre, Bgb], writes=[Bout])


def phase3a_merge(S, G):
    A = G["ap"]
    PB = G["pb"]
    B = G["B"]
    wa = S.sbuf("o_wa", [128, 8, 2048], BF16)
    wb = S.sbuf("o_wb", [128, 8, 2048], BF16)
    Bw = S.buf("o_w")
    S.dma("pool", wa[:], A["w_branch_a"].rearrange("(kc p) n -> p kc n", p=128), writes=[Bw], nowaw=True)
    S.dma("pool", wb[:], A["w_branch_b"].rearrange("(kc p) n -> p kc n", p=128), writes=[Bw], nowaw=True)
    bag = Rot(S, "o_ba", [128, 8, 512], BF16, 2)
    bbg = Rot(S, "o_bb", [128, 8, 512], BF16, 2)
    gtg = Rot(S, "o_gt", [128, 32, 512], BF16, 2)
    t1r = Rot(S, "o_t1", [128, 512], F32, 2)
    t2r = Rot(S, "o_t2", [128, 512], F32, 2)
    mgs = Rot(S, "o_mgs", [128, 512], BF16, 3)
    for g in range(4):
        ba_, Bba = bag.next()
        bb_, Bbb = bbg.next()
        gt_, Bgt = gtg.next()
        S.dma("sp", ba_[:], A["BA"][:, :, g * 512:(g + 1) * 512].rearrange("h p t -> p h t"), reads=[B["BA"]], writes=[Bba])
        S.dma("sp", bb_[:], A["BB"][:, :, g * 512:(g + 1) * 512].rearrange("h p t -> p h t"), reads=[B["BB"]], writes=[Bbb])
        S.dma("sp", gt_[:], A["GT"][:, :, g * 512:(g + 1) * 512].rearrange("c p t -> p c t"), reads=[B["GT"]], writes=[Bgt])
        for c in range(16):
            pa, Bpa = PB.next()
            for k in range(8):
                S.op("pe", lambda e, pa=pa, k=k, c=c, ba_=ba_: e.matmul(pa[:], wa[:, k, c * 128:(c + 1) * 128], ba_[:, k, :], start=(k == 0), stop=(k == 7)),
                     reads=[Bw, Bba], writes=[Bpa])
            pb2, Bpb2 = PB.next()
            for k in range(8):
                S.op("pe", lambda e, pb2=pb2, k=k, c=c, bb_=bb_: e.matmul(pb2[:], wb[:, k, c * 128:(c + 1) * 128], bb_[:, k, :], start=(k == 0), stop=(k == 7)),
                     reads=[Bw, Bbb], writes=[Bpb2])
            t1, Bt1 = t1r.next()
            t2, Bt2 = t2r.next()
            S.op("dve", lambda e, t1=t1, pa=pa, gt_=gt_, c=c: e.tensor_tensor(t1[:], pa[:], gt_[:, c, :], ALU.mult), reads=[Bpa, Bgt], writes=[Bt1])
            S.op("dve", lambda e, t2=t2, pb2=pb2, gt_=gt_, c=c: e.tensor_tensor(t2[:], pb2[:], gt_[:, 16 + c, :], ALU.mult), reads=[Bpb2, Bgt], writes=[Bt2])
            m_, Bm = mgs.next()
            S.op("dve", lambda e, t1=t1, t2=t2, m_=m_: e.tensor_tensor(m_[:], t1[:], t2[:], ALU.add), reads=[Bt1, Bt2], writes=[Bm])
            S.dma("sp", A["MG"][c, :, g * 512:(g + 1) * 512], m_[:], reads=[Bm], writes=[B["MG"]], nowaw=True)


def phase3b_out(S, G):
    A = G["ap"]
    PB = G["pb"]
    B = G["B"]
    P = G["persist"]
    ident_f = G["ident_f"]
    Bw = S.buf("o_w")
    wr = S.sbuf("o_wr", [128, 16, 36], F32)
    S.dma("sp", wr[:], A["wr"].rearrange("(kc p) n -> p kc n", p=128), writes=[Bw], nowaw=True)
    brr = S.sbuf("o_brr", [1, 36], F32)
    S.dma("sp", brr[:], A["brr"], writes=[Bw], nowaw=True)
    g1 = S.sbuf("o_g1", [128, 2048], F32)
    b1 = S.sbuf("o_b1", [128, 2048], F32)
    S.dma("sp", g1[:], A["ln1_gb"], writes=[Bw], nowaw=True)
    S.dma("sp", b1[:], A["ln1_bb"], writes=[Bw], nowaw=True)
    ltri = S.sbuf("o_ltri", [128, 128], BF16)
    S.dma("sp", ltri[:], A["ltri"], writes=[Bw], nowaw=True)
    e256 = S.sbuf("o_e256", [128, 32], F32)
    S.dma("sp", e256[:], A["e256"], writes=[Bw], nowaw=True)
    onesc = S.sbuf("o_onesc", [128, 1], BF16)
    S.op("dve", lambda e: e.memset(onesc[:], 1.0), writes=[Bw])
    onesr = S.sbuf("o_onesr", [1, 128], F32)
    S.op("dve", lambda e: e.memset(onesr[:], 1.0), writes=[Bw])
    cnt = S.sbuf("o_cnt", [1, 32], F32)
    Bcnt = S.buf("o_cnt")
    S.op("dve", lambda e: e.memset(cnt[:], 0.0), writes=[Bcnt])
    S.barrier()
    wo = Rot(S, "o_wo", [128, 16, 512], BF16, 2)
    mgr = Rot(S, "o_mg", [128, 16, 512], BF16, 2)
    pre = Rot(S, "o_pre", [128, 2048], F32, 5)
    h1t = Rot(S, "o_h1", [128, 2048], F32, 2)
    h1b = Rot(S, "o_h1b", [128, 2048], BF16, 2)
    junk = S.sbuf("o_junk", [128, 2048], BF16)
    Bjunk = S.buf("o_junk")
    hT = Rot(S, "o_hT", [128, 16, 128], F32, 1)
    smalls = [S.sbuf("o_sm%d" % i, [128, 1], F32) for i in range(6)]
    Bsmall = S.buf("o_small")
    rt = {n: S.sbuf("o_r_" + n, shp, F32) for n, shp in (
        ("L", [128, 36]), ("gmax", [128, 1]), ("ngmax", [128, 1]), ("ohg", [128, 4]), ("ge", [128, 4]), ("gsum", [128, 1]), ("gp", [128, 1]),
        ("e8", [128, 8]), ("m1", [128, 1]), ("oh1", [128, 8]), ("e8b", [128, 8]), ("m2", [128, 1]), ("oh2", [128, 8]), ("d", [128, 1]),
        ("sg", [128, 1]), ("A1", [128, 32]), ("A2", [128, 32]), ("At", [128, 32]), ("rk", [128, 32]), ("t", [128, 32]), ("i1", [128, 1]), ("i2", [128, 1]))}
    Abf = S.sbuf("o_Abf", [128, 32], BF16)
    Br = G["Br_persist"]

    def rop(fn, eng="dve"):
        S.op(eng, fn, reads=[Br, Bw], writes=[Br])

    for g in range(4):
        mg, Bmg = mgr.next()
        S.dma("sp", mg[:], A["MG"][:, :, g * 512:(g + 1) * 512].rearrange("c p t -> p c t"), reads=[B["MG"]], writes=[Bmg])
        prs = []
        for t in range(4):
            tt = g * 4 + t
            pr, Bpr = pre.next()
            S.dma("sp", pr[:], A["xs"][OWN0 + tt * 128:OWN0 + (tt + 1) * 128, :], writes=[Bpr])
            prs.append((pr, Bpr))
        for cb in range(4):
            w_, Bwo = wo.next()
            S.dma("pool", w_[:], A["w_out"][:, cb * 512:(cb + 1) * 512].rearrange("(kc p) n -> p kc n", p=128), writes=[Bwo])
            for t in range(4):
                pr, Bpr = prs[t]
                po, Bpo = PB.next()
                for c in range(16):
                    S.op("pe", lambda e, po=po, c=c, t=t, w_=w_, mg=mg: e.matmul(po[:], mg[:, c, t * 128:(t + 1) * 128], w_[:, c, :], start=(c == 0), stop=(c == 15)),
                         reads=[Bmg, Bwo], writes=[Bpo])
                S.op("dve", lambda e, pr=pr, po=po, cb=cb: e.scalar_tensor_tensor(pr[:, cb * 512:(cb + 1) * 512], pr[:, cb * 512:(cb + 1) * 512], DN_ALPHA, po[:],
                                                                                op0=ALU.mult, op1=ALU.add), reads=[Bpo, Bpr], writes=[Bpr])
        for t in range(4):
            tt = g * 4 + t
            pr, Bpr = prs[t]
            h_, Bh = h1t.next()
            layer_norm_tile(S, G, pr, Bpr, h_, Bh, g1, b1, Bw, smalls, Bsmall, junk, Bjunk)
            S.dma("sp", A["H1"][tt * 128:(tt + 1) * 128, :], h_[:], reads=[Bh], writes=[B["H1"]], nowaw=True)
            hb, Bhb = h1b.next()
            S.op("act", lambda e, hb=hb, h_=h_: e.activation(hb[:], h_[:], AF.Copy), reads=[Bh], writes=[Bhb])
            S.dma("sp", A["H1b"][tt * 128:(tt + 1) * 128, :], hb[:], reads=[Bhb], writes=[B["H1b"]], nowaw=True)
            hT_, BhT = hT.next()
            for q4 in range(4):
                pb, Bpb = PB.next()
                for j in range(4):
                    c = q4 * 4 + j
                    S.op("pe", lambda e, pb=pb, h_=h_, c=c, j=j: e.matmul(pb[:, j * 128:(j + 1) * 128], h_[:, c * 128:(c + 1) * 128], ident_f[:], start=True, stop=True),
                         reads=[Bh, G["Bidf"]], writes=[Bpb])
                S.op("act", lambda e, pb=pb, hT_=hT_, q4=q4: e.activation(hT_[:, q4 * 4:(q4 + 1) * 4, :], pb[:].rearrange("p (a b) -> p a b", a=4), AF.Copy),
                     reads=[Bpb], writes=[BhT])
            pl, Bpl = PB.next()
            for c in range(16):
                S.op("pe", lambda e, pl=pl, hT_=hT_, c=c: e.matmul(pl[:, 0:36], hT_[:, c, :], wr[:, c, :], start=(c == 0), stop=False), reads=[BhT, Bw], writes=[Bpl])
            S.op("pe", lambda e, pl=pl: e.matmul(pl[:, 0:36], onesr[0:1, :], brr[0:1, :], start=False, stop=True), reads=[Bw], writes=[Bpl])
            L = rt["L"]
            S.op("dve", lambda e, pl=pl: e.tensor_copy(L[:], pl[:, 0:36]), reads=[Bpl], writes=[Br])
            rop(lambda e: e.reduce_max(rt["gmax"][:], L[:, 0:4], AX.X))
            rop(lambda e: e.tensor_scalar(rt["ohg"][:], L[:, 0:4], rt["gmax"][:, 0:1], None, op0=ALU.is_equal))
            rop(lambda e: e.tensor_scalar(rt["ngmax"][:], rt["gmax"][:], -1.0, None, op0=ALU.mult))
            rop(lambda e: e.activation(rt["ge"][:], L[:, 0:4], AF.Exp, bias=rt["ngmax"][:, 0:1], accum_out=rt["gsum"][:]), "act")
            rop(lambda e: e.reciprocal(rt["gp"][:], rt["gsum"][:]))
            rop(lambda e: e.tensor_scalar(rt["e8"][:], L[:, 4:12], rt["ohg"][:, 0:1], None, op0=ALU.mult))
            for gg in range(1, 4):
                rop(lambda e, gg=gg: e.scalar_tensor_tensor(rt["e8"][:], L[:, 4 + 8 * gg:12 + 8 * gg], rt["ohg"][:, gg:gg + 1], rt["e8"][:], op0=ALU.mult, op1=ALU.add))
            rop(lambda e: e.reduce_max(rt["m1"][:], rt["e8"][:], AX.X))
            rop(lambda e: e.tensor_scalar(rt["oh1"][:], rt["e8"][:], rt["m1"][:, 0:1], None, op0=ALU.is_equal))
            rop(lambda e: e.scalar_tensor_tensor(rt["e8b"][:], rt["oh1"][:], -1.0e30, rt["e8"][:], op0=ALU.mult, op1=ALU.add))
            rop(lambda e: e.reduce_max(rt["m2"][:], rt["e8b"][:], AX.X))
            rop(lambda e: e.tensor_scalar(rt["oh2"][:], rt["e8b"][:], rt["m2"][:, 0:1], None, op0=ALU.is_equal))
            rop(lambda e: e.tensor_tensor(rt["d"][:], rt["m1"][:], rt["m2"][:], ALU.subtract))
            rop(lambda e: e.activation(rt["sg"][:], rt["d"][:], AF.Sigmoid), "act")
            rop(lambda e, tt=tt: e.tensor_tensor(P["wts"][:, tt, 0:1], rt["sg"][:], rt["gp"][:], ALU.mult))
            rop(lambda e, tt=tt: e.tensor_tensor(P["wts"][:, tt, 1:2], rt["gp"][:], P["wts"][:, tt, 0:1], ALU.subtract))
            for gg in range(4):
                rop(lambda e, gg=gg: e.tensor_scalar(rt["A1"][:, gg * 8:(gg + 1) * 8], rt["oh1"][:], rt["ohg"][:, gg:gg + 1], None, op0=ALU.mult))
                rop(lambda e, gg=gg: e.tensor_scalar(rt["A2"][:, gg * 8:(gg + 1) * 8], rt["oh2"][:], rt["ohg"][:, gg:gg + 1], None, op0=ALU.mult))
            rop(lambda e: e.tensor_tensor(rt["At"][:], rt["A1"][:], rt["A2"][:], ALU.add))
            rop(lambda e: e.tensor_copy(Abf[:], rt["At"][:]))
            pk, Bpk = PB.next()
            S.op("pe", lambda e, pk=pk: e.matmul(pk[:, 0:32], ltri[:], Abf[:], start=True, stop=False), reads=[Bw, Br], writes=[Bpk])
            S.op("pe", lambda e, pk=pk: e.matmul(pk[:, 0:32], onesr[0:1, :], cnt[0:1, :], start=False, stop=True), reads=[Bw, Bcnt], writes=[Bpk])
            pc, Bpc = PB.next()
            S.op("pe", lambda e, pc=pc: e.matmul(pc[0:1, 0:32], onesc[:], Abf[:], start=True, stop=True), reads=[Bw, Br], writes=[Bpc])
            S.op("dve", lambda e, pk=pk: e.tensor_copy(rt["rk"][:], pk[:, 0:32]), reads=[Bpk, Br], writes=[Br])
            S.op("dve", lambda e, pc=pc: e.tensor_tensor(cnt[:], cnt[:], pc[0:1, 0:32], ALU.add), reads=[Bpc, Bcnt], writes=[Bcnt])
            rop(lambda e: e.scalar_tensor_tensor(rt["t"][:], rt["rk"][:], 1.0, rt["At"][:], op0=ALU.add, op1=ALU.mult))
            rop(lambda e, tt=tt: e.tensor_scalar(P["RK"][:, tt, :], rt["t"][:], -1.0, None, op0=ALU.add))
            rop(lambda e: e.tensor_tensor(rt["rk"][:], rt["rk"][:], e256[:], ALU.add))
            rop(lambda e: e.tensor_tensor(rt["t"][:], rt["rk"][:], rt["A1"][:], ALU.mult))
            rop(lambda e: e.reduce_sum(rt["i1"][:], rt["t"][:], AX.X))
            rop(lambda e: e.tensor_tensor(rt["t"][:], rt["rk"][:], rt["A2"][:], ALU.mult))
            rop(lambda e: e.reduce_sum(rt["i2"][:], rt["t"][:], AX.X))
            rop(lambda e, tt=tt: e.tensor_copy(P["idx"][:, tt, 0:1], rt["i1"][:]))
            rop(lambda e, tt=tt: e.tensor_copy(P["idx"][:, tt, 1:2], rt["i2"][:]))


def phase4_moe(S, G, experts=range(NEXP)):
    A = G["ap"]
    PB = G["pb"]
    B = G["B"]
    P = G["persist"]
    h1b = S.sbuf("m_h1b", [128, 16, 2048], BF16)
    Bh1b = S.buf("m_h1b")
    S.dma("sp", h1b[:], A["H1b"].rearrange("(t p) d -> p t d", p=128), reads=[B["H1b"]], writes=[Bh1b])
    iot = S.sbuf("m_iota", [128, CAP], F32)
    Biot = S.buf("m_iota")
    S.dma("sp", iot[:], A["iota256"], writes=[Biot])
    wu = Rot(S, "m_w", [128, 16, 512], BF16, 4)
    wd = Rot(S, "m_wd", [128, 4, 2048], BF16, 2)
    sel = Rot(S, "m_sel", [128, 16, CAP], BF16, 1)
    xs_ = Rot(S, "m_xs", [128, 16, CAP], BF16, 1)
    hT = Rot(S, "m_hT", [128, 8, CAP], BF16, 2)
    sg = Rot(S, "m_sg", [128, CAP], F32, 3)
    yst = Rot(S, "m_y", [128, 1024], F32, 1)
    for e_ in experts:
        s_, Bs = sel.next()
        for tt in range(16):
            S.op("dve", lambda e, s_=s_, tt=tt, e_=e_: e.tensor_scalar(s_[:, tt, :], iot[:], P["RK"][:, tt, e_:e_ + 1], None, op0=ALU.is_equal),
                 reads=[Biot, G["Br_persist"]], writes=[Bs])
        x_, Bx = xs_.next()
        for kc in range(16):
            pb, Bpb = PB.next()
            for tt in range(16):
                S.op("pe", lambda e, pb=pb, tt=tt, kc=kc, s_=s_: e.matmul(pb[:, 0:CAP], h1b[:, tt, kc * 128:(kc + 1) * 128], s_[:, tt, :],
                                                                         start=(tt == 0), stop=(tt == 15)), reads=[Bh1b, Bs], writes=[Bpb])
            if kc % 2 == 0:
                S.op("act", lambda e, pb=pb, x_=x_, kc=kc: e.activation(x_[:, kc, :], pb[:, 0:CAP], AF.Copy), reads=[Bpb], writes=[Bx])
            else:
                S.op("dve", lambda e, pb=pb, x_=x_, kc=kc: e.tensor_copy(x_[:, kc, :], pb[:, 0:CAP]), reads=[Bpb], writes=[Bx])
        h_, Bh = hT.next()
        for half in range(2):
            wg_, Bwg = wu.next()
            S.dma("pool", wg_[:], A["w_gate"][e_, :, half * 512:(half + 1) * 512].rearrange("(kc p) n -> p kc n", p=128), writes=[Bwg])
            wu_, Bwu = wu.next()
            S.dma("pool", wu_[:], A["w_up"][e_, :, half * 512:(half + 1) * 512].rearrange("(kc p) n -> p kc n", p=128), writes=[Bwu])
            for f4 in range(4):
                f = half * 4 + f4
                pg, Bpg = PB.next()
                for kc in range(16):
                    S.op("pe", lambda e, pg=pg, kc=kc, f4=f4, wg_=wg_, x_=x_: e.matmul(pg[:, 0:CAP], wg_[:, kc, f4 * 128:(f4 + 1) * 128], x_[:, kc, :],
                                                                                    start=(kc == 0), stop=(kc == 15)), reads=[Bwg, Bx], writes=[Bpg])
                for kc in range(16):
                    S.op("pe", lambda e, pg=pg, kc=kc, f4=f4, wu_=wu_, x_=x_: e.matmul(pg[:, CAP:2 * CAP], wu_[:, kc, f4 * 128:(f4 + 1) * 128], x_[:, kc, :],
                                                                                    start=(kc == 0), stop=(kc == 15)), reads=[Bwu, Bx], writes=[Bpg])
                s1, Bs1 = sg.next()
                S.op("act", lambda e, s1=s1, pg=pg: e.activation(s1[:], pg[:, 0:CAP], AF.Silu), reads=[Bpg], writes=[Bs1])
                S.op("dve", lambda e, h_=h_, f=f, s1=s1, pg=pg: e.tensor_tensor(h_[:, f, :], s1[:], pg[:, CAP:2 * CAP], ALU.mult), reads=[Bs1, Bpg], writes=[Bh])
        wds = []
        for half in range(2):
            wd_, Bwd = wd.next()
            S.dma("pool", wd_[:], A["w_down"][e_, half * 512:(half + 1) * 512, :].rearrange("(fc p) n -> p fc n", p=128), writes=[Bwd])
            wds.append((wd_, Bwd))
        for rh in range(CAP // 128):
            for cbp in range(2):
                y_, By = yst.next()
                for c2 in range(2):
                    cb = cbp * 2 + c2
                    py, Bpy = PB.next()
                    for f in range(8):
                        wd_, Bwd = wds[f // 4]
                        S.op("pe", lambda e, py=py, f=f, rh=rh, cb=cb, wd_=wd_, h_=h_: e.matmul(py[:], h_[:, f, rh * 128:(rh + 1) * 128], wd_[:, f % 4, cb * 512:(cb + 1) * 512],
                                                                                             start=(f == 0), stop=(f == 7)), reads=[Bh, Bwd], writes=[Bpy])
                    if c2 == 0:
                        S.op("act", lambda e, y_=y_, py=py, c2=c2: e.activation(y_[:, c2 * 512:(c2 + 1) * 512], py[:], AF.Copy), reads=[Bpy], writes=[By])
                    else:
                        S.op("dve", lambda e, y_=y_, py=py, c2=c2: e.tensor_copy(y_[:, c2 * 512:(c2 + 1) * 512], py[:]), reads=[Bpy], writes=[By])
                S.dma("sp", A["Y"][e_ * CAP + rh * 128:e_ * CAP + (rh + 1) * 128, cbp * 1024:(cbp + 1) * 1024], y_[:], reads=[By], writes=[B["Y"]], nowaw=True)


def phase5_final(S, G):
    A = G["ap"]
    B = G["B"]
    P = G["persist"]
    g2 = S.sbuf("f_g2", [128, 2048], F32)
    b2 = S.sbuf("f_b2", [128, 2048], F32)
    Bw = S.buf("f_w")
    S.dma("sp", g2[:], A["ln2_gb"], writes=[Bw], nowaw=True)
    S.dma("sp", b2[:], A["ln2_bb"], writes=[Bw], nowaw=True)
    idxi = S.sbuf("f_idx", [128, 16, 2], U32)
    Bidx = S.buf("f_idx")
    S.op("dve", lambda e: e.tensor_copy(idxi[:], P["idx"][:]), reads=[G["Br_persist"]], writes=[Bidx])
    S.barrier()
    h1 = Rot(S, "f_h1", [128, 2048], F32, 2)
    y1 = Rot(S, "f_y1", [128, 2048], F32, 2)
    y2 = Rot(S, "f_y2", [128, 2048], F32, 2)
    ot = Rot(S, "f_ot", [128, 2048], F32, 2)
    junk = S.sbuf("f_junk", [128, 2048], BF16)
    Bjunk = S.buf("f_junk")
    smalls = [S.sbuf("f_sm%d" % i, [128, 1], F32) for i in range(6)]
    Bsmall = S.buf("f_small")
    for tt in range(16):
        h_, Bh = h1.next()
        S.dma("sp", h_[:], A["H1"][tt * 128:(tt + 1) * 128, :], reads=[B["H1"]], writes=[Bh])
        a_, Ba = y1.next()
        b_, Bb = y2.next()
        for (dst, Bd, k) in ((a_, Ba, 0), (b_, Bb, 1)):
            S.dma("pool", None, None, reads=[B["Y"], Bidx], writes=[Bd],
                  builder=lambda e, dst=dst, tt=tt, k=k: e.indirect_dma_start(
                      out=dst[:], out_offset=None, in_=A["Y"], in_offset=bass.IndirectOffsetOnAxis(ap=idxi[:, tt, k:k + 1], axis=0),
                      bounds_check=NEXP * CAP - 1, oob_is_err=False))
        S.op("dve", lambda e, a_=a_, tt=tt: e.tensor_scalar(a_[:], a_[:], P["wts"][:, tt, 0:1], None, op0=ALU.mult), reads=[Ba, G["Br_persist"]], writes=[Ba])
        S.op("dve", lambda e, a_=a_, b_=b_, tt=tt: e.scalar_tensor_tensor(a_[:], b_[:], P["wts"][:, tt, 1:2], a_[:], op0=ALU.mult, op1=ALU.add),
             reads=[Ba, Bb, G["Br_persist"]], writes=[Ba])
        S.op("dve", lambda e, a_=a_, h_=h_: e.scalar_tensor_tensor(a_[:], h_[:], DN_ALPHA, a_[:], op0=ALU.mult, op1=ALU.add), reads=[Ba, Bh], writes=[Ba])
        o_, Bo = ot.next()
        layer_norm_tile(S, G, a_, Ba, o_, Bo, g2, b2, Bw, smalls, Bsmall, junk, Bjunk)
        S.dma("sp", A["out"][tt * 128:(tt + 1) * 128, :], o_[:], reads=[Bo], writes=[B["out"]], nowaw=True)

from contextlib import ExitStack
from concourse.bass_utils import run_bass_kernel_spmd

BFM_IDX = {"af": 0, "aq": 8, "ag": 16, "bk": 24, "bq": 32, "iq": 40, "ik": 44, "g": 45}
NBFM = 77

SCRATCH = {
    "KT": ([8, 128, 8192], "bf16"), "VH": ([8, 128, 64, 128], "bf16"), "KIT": ([128, 8192], "bf16"),
    "HG_kdec": ([8, 128, 8192], "bf16"), "HG_kend": ([8192, 1024], "bf16"), "HG_v": ([8192, 1024], "bf16"),
    "HG_dec": ([128, 8, 128], "f32"), "HG_qdec": ([8, 128, 2048], "bf16"), "HG_gs": ([8, 128, 2048], "bf16"),
    "QT": ([8, 128, 2048], "bf16"), "QIT": ([4, 128, 2048], "bf16"), "WI": ([2048, 8], "f32"),
    "GT": ([32, 128, 2048], "bf16"), "BA": ([8, 128, 2048], "bf16"), "BB": ([8, 128, 2048], "bf16"),
    "MG": ([16, 128, 2048], "bf16"), "H1": ([2048, 2048], "f32"), "H1b": ([2048, 2048], "bf16"), "Y": ([NEXP * CAP, 2048], "f32"),
}

INPUTS = {
    "xs": ([8192, 2048], "f32"), "valid_tm": ([128, 64], "f32"), "w_in": ([2048, 11848], "f32"),
    "ident": ([128, 128], "f32"), "bfm": ([128, NBFM], "f32"), "brow": ([1, 2056], "f32"),
    "lbl": ([128, 2, 8], "f32"), "normg": ([128, 8], "f32"), "rmask": ([128, 512], "f32"), "bdmask": ([128, 128], "f32"),
    "alb": ([128, 8, 64], "f32"), "corr": ([128, 8, 128], "bf16"), "sel2": ([2, 128], "bf16"), "dtab": ([128, 8192], "bf16"),
    "qrel": ([1, 512], "f32"), "adm": ([16, 2, 8192], "bf16"),
    "w_branch_a": ([1024, 2048], "f32"), "w_branch_b": ([1024, 2048], "f32"), "w_out": ([2048, 2048], "f32"),
    "wr": ([2048, 36], "f32"), "brr": ([1, 36], "f32"),
    "ln1_gb": ([128, 2048], "f32"), "ln1_bb": ([128, 2048], "f32"), "ln2_gb": ([128, 2048], "f32"), "ln2_bb": ([128, 2048], "f32"),
    "ltri": ([128, 128], "bf16"), "e256": ([128, 32], "f32"), "iota256": ([128, CAP], "f32"),
    "w_gate": ([NEXP, 2048, 1024], "f32"), "w_up": ([NEXP, 2048, 1024], "f32"), "w_down": ([NEXP, 1024, 2048], "f32"),
}


def _dt(s):
    return {"f32": F32, "bf16": BF16, "u32": U32, "i32": I32}[s]


def host_consts(inputs):
    b_in = np.asarray(inputs["b_in"][0], np.float32)
    bfm = np.zeros((128, NBFM), np.float32)
    def put(idx, c0, n):
        for c in range(n):
            bfm[:, idx + c] = b_in[c0 + c * 128: c0 + (c + 1) * 128]
    put(BFM_IDX["af"], C_AF, 8); put(BFM_IDX["aq"], C_AQ, 8); put(BFM_IDX["ag"], C_AG, 8)
    put(BFM_IDX["bk"], C_BK, 8); put(BFM_IDX["bq"], C_BQ, 8); put(BFM_IDX["iq"], C_IQ, 4)
    put(BFM_IDX["g"], C_G, 32)
    bfm[0:64, BFM_IDX["ik"]] = b_in[C_IK:C_IK + 64]
    bfm[64:128, BFM_IDX["ik"]] = b_in[C_IK:C_IK + 64]
    brow = np.concatenate([b_in[C_AI:C_AI + 1024], b_in[C_BV:C_BV + 1024], b_in[C_IW:C_IW + 8]])[None, :].astype(np.float32)
    lbl = np.ascontiguousarray(np.asarray(inputs["hg_lb_logits"], np.float32).reshape(2, 8, 128).transpose(2, 0, 1))
    normg = np.ascontiguousarray(np.asarray(inputs["hg_norm_g"][0], np.float32).reshape(8, 128).T)
    rmask = np.ones((128, 512), np.float32)
    rmask[:, ::64] = 0.0
    ii = np.arange(128)
    bdmask = ((ii[:, None] // 64 == ii[None, :] // 64) & (ii[:, None] <= ii[None, :])).astype(np.float32)
    import ml_dtypes
    bf = ml_dtypes.bfloat16
    slopes = 2.0 ** -(np.arange(8) + 1.0)
    pp = np.arange(128)
    alb = (slopes[None, :, None] * (pp[:, None, None] + 128.0 * (np.arange(64)[None, None, :] - 60))).astype(np.float32)
    dsq = np.maximum(pp[:, None] - pp[None, :], 0).astype(np.float64)
    corr = np.exp(-2.0 * slopes[None, :, None] * dsq[:, None, :]).astype(bf)
    sel2 = np.zeros((2, 128), np.float32); sel2[0, :64] = 1; sel2[1, 64:] = 1
    dtab = np.abs(8064 + pp[:, None] - np.arange(8192)[None, :]).astype(bf)
    qrel = np.arange(512, dtype=np.float32)[None, :]
    extra = {}
    if "w_out" in inputs:
        extra["w_branch_a"] = np.ascontiguousarray(inputs["w_branch_a"][0]); extra["w_branch_b"] = np.ascontiguousarray(inputs["w_branch_b"][0])
        extra["w_out"] = np.ascontiguousarray(inputs["w_out"][0])
        extra["wr"] = np.ascontiguousarray(np.concatenate([inputs["w_group"][0], inputs["w_router"][0]], axis=1).astype(np.float32))
        extra["brr"] = np.concatenate([inputs["b_group"][0], inputs["b_router"][0]])[None, :].astype(np.float32)
        for nm in ("ln1_g", "ln1_b", "ln2_g", "ln2_b"):
            extra[nm + "b"] = np.ascontiguousarray(np.broadcast_to(np.asarray(inputs[nm][0], np.float32)[None, :], (128, 2048)))
        extra["ltri"] = (pp[:, None] < pp[None, :]).astype(bf)
        extra["e256"] = np.ascontiguousarray(np.broadcast_to((np.arange(32, dtype=np.float32) * CAP)[None, :], (128, 32)))
        extra["iota256"] = np.ascontiguousarray(np.broadcast_to(np.arange(CAP, dtype=np.float32)[None, :], (128, CAP)))
        extra["w_gate"] = np.ascontiguousarray(inputs["w_gate"][0]); extra["w_up"] = np.ascontiguousarray(inputs["w_up"][0])
        extra["w_down"] = np.ascontiguousarray(inputs["w_down"][0])
    return {**extra, "alb": alb, "corr": corr, "sel2": sel2.astype(bf), "dtab": dtab, "qrel": qrel, "bdmask": bdmask, "ident": np.eye(128, dtype=np.float32), "bfm": bfm, "brow": brow, "lbl": lbl, "normg": normg, "rmask": rmask,
            "w_in": np.ascontiguousarray(inputs["w_in"][0])}


def host_core_inputs(inputs, hc, core):
    b, j = core // 4, core % 4
    x = np.asarray(inputs["x"], np.float32)
    xs = np.zeros((8192, 2048), np.float32)
    npre = (3 - j) * 2048
    xs[npre:] = x[b, :(j + 1) * 2048]
    valid = np.zeros(8192, np.float32)
    valid[npre:] = 1.0
    d = dict(hc)
    d["xs"] = xs
    d["valid_tm"] = np.ascontiguousarray(valid.reshape(64, 128).T)
    import ml_dtypes
    chunk = np.arange(8192) // 64
    adm = np.full((16, 2, 8192), NEG_ADM, np.float32)
    for t in range(16):
        c_first = (OWN0 + t * 128) // 64
        adm[t, 0, (valid > 0) & (chunk <= c_first)] = 0.0
        adm[t, 1, (valid > 0) & (chunk <= c_first + 1)] = 0.0
    d["adm"] = adm.astype(ml_dtypes.bfloat16)
    return d


def build_program(phases=("p1a",), dump=(), p1_blocks=(0, 1, 2, 3), p2_groups=(0, 1, 2, 3), in_names=None):
    nc = bass.Bass("TRN2", target_bir_lowering=False)
    A = {}
    used_inputs = in_names if in_names is not None else list(INPUTS)
    for n in used_inputs:
        shp, dt = INPUTS[n]
        A[n] = nc.dram_tensor(n, shp, _dt(dt), kind="ExternalInput").ap()
    for n, (shp, dt) in SCRATCH.items():
        kind = "ExternalOutput" if n in dump else "Internal"
        A[n] = nc.dram_tensor(n, shp, _dt(dt), kind=kind).ap()
    A["out"] = nc.dram_tensor("out", [2048, 2048], F32, kind="ExternalOutput").ap()
    with ExitStack() as es:
        S = Sched(nc, es)
        G = {"ap": A, "pb": PBanks(S), "B": {n: S.buf(n, glob=True) for n in list(SCRATCH) + ["out"]}, "bfm_idx": BFM_IDX}
        G["persist"] = {"wts": S.sbuf("p_wts", [128, 16, 2], F32), "RK": S.sbuf("p_RK", [128, 16, 32], F32), "idx": S.sbuf("p_idx", [128, 16, 2], F32)}
        G["Br_persist"] = S.buf("persist", glob=True)
        cst = {}
        Bc = S.buf("cst", glob=True)
        G["cst"] = cst
        G["Bcst"] = Bc
        idf = S.sbuf("idf", [128, 128], F32)
        Bidf = S.buf("idf", glob=True)
        S.dma("sp", idf[:], A["ident"], writes=[Bidf])
        idb = S.sbuf("idb", [128, 128], BF16)
        Bident = S.buf("idb")
        S.op("dve", lambda e: e.tensor_copy(idb[:], idf[:]), reads=[Bidf], writes=[Bident])
        G["ident_bf"] = idb
        G["Bident"] = Bident
        G["ident_f"] = idf
        G["Bidf"] = Bidf
        for n in ("bfm", "brow", "valid_tm", "normg", "rmask", "bdmask"):
            shp, dt = INPUTS[n]
            cst[n] = S.sbuf("c_" + n, shp, _dt(dt))
            S.dma("sp", cst[n][:], A[n], writes=[Bc], nowaw=True)
        lbl = S.sbuf("c_lbl", [128, 2, 8], F32)
        Blbl = S.buf("lbl", glob=True)
        S.dma("sp", lbl[:], A["lbl"], writes=[Blbl])
        for n in ("lb", "oml", "noml", "lbd"):
            cst[n] = S.sbuf("c_" + n, [128, 8], F32)
        cst["ones_row"] = S.sbuf("c_ones_row", [1, 128], F32)
        S.op("dve", lambda e: e.memset(cst["ones_row"][:], 1.0), writes=[Bc])
        S.op("dve", lambda e: e.tensor_tensor(cst["lbd"][:], lbl[:, 0, :], lbl[:, 1, :], ALU.subtract), reads=[Blbl], writes=[Bc])
        S.op("act", lambda e: e.activation(cst["lb"][:], cst["lbd"][:], AF.Sigmoid), reads=[Bc], writes=[Bc])
        S.op("dve", lambda e: e.tensor_scalar(cst["oml"][:], cst["lb"][:], -1.0, 1.0, op0=ALU.mult, op1=ALU.add), reads=[Bc], writes=[Bc])
        S.op("dve", lambda e: e.tensor_scalar(cst["noml"][:], cst["oml"][:], -1.0, None, op0=ALU.mult), reads=[Bc], writes=[Bc])

        cst["eps_ln"] = S.sbuf("c_eps_ln", [128, 1], F32)
        S.op("dve", lambda e: e.memset(cst["eps_ln"][:], LN_EPS), writes=[Bc])
        cst["eps_rms"] = S.sbuf("c_eps_rms", [128, 1], F32)
        S.op("dve", lambda e: e.memset(cst["eps_rms"][:], RMS_EPS), writes=[Bc])
        S.barrier()
        if "p1a" in phases:
            with ExitStack() as pes:
                S.es = pes
                phase1a(S, G, blocks=p1_blocks)
                S.es = es
            S.barrier()
            S.phase_end()
        if "p1b" in phases:
            with ExitStack() as pes:
                S.es = pes
                phase1b(S, G)
                S.es = es
            S.barrier()
            S.phase_end()
        if "p2" in phases:
            phase2_dsa(S, G, groups=p2_groups)
            S.barrier()
            S.phase_end()
        for nm, fn in (("p3a", phase3a_merge), ("p3b", phase3b_out), ("p4", phase4_moe), ("p5", phase5_final)):
            if nm in phases:
                with ExitStack() as pes:
                    S.es = pes
                    fn(S, G)
                    S.es = es
                S.barrier()
                S.phase_end()
        outs = [G["B"][n] for n in dump] + ([G["B"]["out"]] if "p5" in phases else [])
        S.wait_all("sp", outs)
        print("instructions:", S.ninst, {k: len(v) for k, v in S.ops.items()})
        S.run()
    return nc


ALL_PHASES = ("p1a", "p1b", "p2", "p3a", "p3b", "p4", "p5")
_CACHE = {}


def kernel(**inputs):
    if "nc" not in _CACHE:
        _CACHE["nc"] = build_program(phases=ALL_PHASES)
    nc = _CACHE["nc"]
    hc = host_consts(inputs)
    in_maps = [host_core_inputs(inputs, hc, c) for c in range(8)]
    res = run_bass_kernel_spmd(nc, in_maps, core_ids=list(range(8)))
    out = np.zeros((2, 8192, 2048), np.float32)
    for c in range(8):
        b, j = c // 4, c % 4
        out[b, j * 2048:(j + 1) * 2048] = np.asarray(res.results[c]["out"])
    return out
```

```python
import numpy as np
import concourse.bass as bass
import concourse.mybir as mybir

F32 = mybir.dt.float32
BF16 = mybir.dt.bfloat16
U32 = mybir.dt.uint32
I32 = mybir.dt.int32
U8 = mybir.dt.uint8
AF = mybir.ActivationFunctionType
ALU = mybir.AluOpType
AX = mybir.AxisListType


class Buf:
    __slots__ = ("name", "lastw", "readers", "dsem", "dcount", "glob", "dkey")

    def __init__(self, name, glob=False):
        self.name = name
        self.glob = glob
        self.dkey = None
        self.lastw = None
        self.readers = {}
        self.dsem = None
        self.dcount = 0


class Sched:
    ENGS = ("pe", "act", "dve", "pool", "sp")
    SEM_LIMIT = 30000

    def __init__(self, nc, es):
        self.nc = nc
        self.es = es
        self.es_sem = es
        self.dbufs = []
        self.dstate = {}
        self.free_dsems = []
        self.local_dbufs = []
        self.sem = {}
        self.count = {}
        self.known = {}
        self.ops = {}
        self.epoch = {}
        for n in self.ENGS:
            self.sem[n] = es.enter_context(nc.semaphore("se_" + n))
            self.count[n] = 0
            self.epoch[n] = 0
            self.known[n] = {}
            self.ops[n] = []
        self.nbuf = 0
        self.ninst = 0

    def sbuf(self, name, shape, dtype):
        self.nbuf += 1
        name = "%s_u%d" % (name, self.nbuf)
        return self.es.enter_context(self.nc.sbuf_tensor(name, list(shape), dtype))

    def psum(self, name, shape, dtype):
        return self.es.enter_context(self.nc.psum_tensor(name, list(shape), dtype))

    def buf(self, name=None, glob=False):
        self.nbuf += 1
        return Buf("%s_b%d" % (name or "b", self.nbuf), glob)

    def bufs(self, n, name="b"):
        return [self.buf("%s%d" % (name, i)) for i in range(n)]

    def _waits(self, eng, reads, writes):
        need = {}

        def add(ev, skip_same):
            if ev is None:
                return
            key, sem, val, prod = ev
            if skip_same and prod == eng:
                return
            if self.known[eng].get(key, 0) >= val:
                return
            if key not in need or need[key][1] < val:
                need[key] = (sem, val)

        for b in reads:
            add(b.lastw, False)
        for b in writes:
            add(b.lastw, True)
            for ev in b.readers.values():
                add(ev, True)
        for key, (sem, val) in need.items():
            self.known[eng][key] = val
        return list(need.values())

    def op(self, eng, fn, reads=(), writes=()):
        waits = self._waits(eng, reads, writes)
        if self.count[eng] >= self.SEM_LIMIT:
            self.epoch[eng] += 1
            self.count[eng] = 0
            self.sem[eng] = self.es_sem.enter_context(self.nc.semaphore("se_%s_%d" % (eng, self.epoch[eng])))
        self.count[eng] += 1
        seq = self.count[eng]
        sem = self.sem[eng]
        key = "e_%s_%d" % (eng, self.epoch[eng])
        ev = (key, sem, seq, eng)
        for b in writes:
            b.lastw = ev
            b.readers = {}
        for b in reads:
            b.readers[key] = ev
        self.ninst += 1 + len(waits)

        def emit(e, fn=fn, waits=waits, sem=sem):
            for (s, v) in waits:
                e.wait_ge(s, v)
            fn(e).then_inc(sem, 1)

        self.ops[eng].append(emit)
        return ev

    def dma(self, q, out_ap, in_ap, reads=(), writes=(), nowaw=False, builder=None, **kw):
        waits = self._waits(q, reads, [] if nowaw else writes)
        tb = writes[0]
        if tb.dsem is None:
            if (not tb.glob) and self.free_dsems:
                tb.dsem, tb.dcount, tb.dkey = self.free_dsems.pop()
            else:
                tb.dsem = self.es_sem.enter_context(self.nc.semaphore("sd_" + tb.name))
                tb.dkey = "d_" + tb.name
            if not tb.glob:
                self.local_dbufs.append(tb)
        tb.dcount += 16
        self.dstate[tb.dkey] = (tb.dsem, tb.dcount)
        ev = (tb.dkey, tb.dsem, tb.dcount, None)
        for b in writes:
            b.lastw = ev
            if not nowaw:
                b.readers = {}
        for b in reads:
            b.readers[ev[0]] = ev
        self.ninst += 1 + len(waits)

        def emit(e, waits=waits, sem=tb.dsem, out_ap=out_ap, in_ap=in_ap, kw=kw, builder=builder):
            for (s, v) in waits:
                e.wait_ge(s, v)
            if builder is not None:
                builder(e).then_inc(sem, 16)
            else:
                e.dma_start(out=out_ap, in_=in_ap, **kw).then_inc(sem, 16)

        self.ops[q].append(emit)
        return ev

    def phase_end(self):
        for b in self.local_dbufs:
            self.free_dsems.append((b.dsem, b.dcount, b.dkey))
            b.dsem = None
        self.local_dbufs = []

    def raw(self, eng, fn):
        self.ops[eng].append(lambda e, fn=fn: fn(e))

    def wait_all(self, eng, bufs):
        waits = self._waits(eng, list(bufs), [])

        def emit(e, waits=waits):
            for (s, v) in waits:
                e.wait_ge(s, v)

        self.ops[eng].append(emit)

    def barrier(self):
        evs = []
        for n in self.ENGS:
            if self.count[n] > 0:
                evs.append(("e_%s_%d" % (n, self.epoch[n]), self.sem[n], self.count[n], n))
        for key, (sem, cnt) in self.dstate.items():
            evs.append((key, sem, cnt, None))
        for eng in self.ENGS:
            waits = []
            for (key, sem, val, prod) in evs:
                if prod == eng:
                    continue
                if self.known[eng].get(key, 0) >= val:
                    continue
                self.known[eng][key] = val
                waits.append((sem, val))

            def emit(e, waits=waits):
                for (s, v) in waits:
                    e.wait_ge(s, v)

            self.ops[eng].append(emit)

    def run(self):
        nc = self.nc
        ops = self.ops
        with nc.Block() as block:
            @block.tensor
            def _(e):
                for f in ops["pe"]:
                    f(e)

            @block.scalar
            def _(e):
                for f in ops["act"]:
                    f(e)

            @block.vector
            def _(e):
                for f in ops["dve"]:
                    f(e)

            @block.gpsimd
            def _(e):
                for f in ops["pool"]:
                    f(e)

            @block.sync
            def _(e):
                for f in ops["sp"]:
                    f(e)

NSLOT = 8192
NOWN = 2048
OWN0 = NSLOT - NOWN
D = 2048
KC = 16
C_AQ, C_AF, C_AI, C_AG, C_BQ, C_BK, C_BV, C_IQ, C_IK, C_IW, C_G = 0, 1024, 2048, 3072, 4096, 5120, 6144, 7168, 7680, 7744, 7752
W_SCALE = (8 ** -0.5) * (64 ** -0.5)


class Rot:
    def __init__(self, S, name, shape, dtype, n):
        self.t = [S.sbuf("%s%d" % (name, i), shape, dtype) for i in range(n)]
        self.b = [S.buf("%s%d" % (name, i)) for i in range(n)]
        self.i = 0
        self.n = n

    def next(self):
        k = self.i % self.n
        self.i += 1
        return self.t[k], self.b[k]


class PBanks:
    def __init__(self, S):
        self.t = [S.psum("pb%d" % i, [128, 512], F32) for i in range(8)]
        self.b = [S.buf("pb%d" % i) for i in range(8)]
        self.i = 0

    def next(self, lo=0, hi=8):
        n = hi - lo
        k = lo + (self.i % n)
        self.i += 1
        return self.t[k], self.b[k]


def phase1a(S, G, blocks=(0, 1, 2, 3)):
    A = G["ap"]
    PB = G["pb"]
    ident = G["ident_bf"]
    Bident = G["Bident"]
    w_in = A["w_in"]
    xT = S.sbuf("xT", [128, KC, 2048], BF16)
    BxT = S.bufs(16, "xT")
    xin = Rot(S, "xin", [128, 2048], BF16, 2)
    wt = Rot(S, "wt", [128, KC, 512], BF16, 2)
    f32t = Rot(S, "hgf", [128, 512], F32, 12)
    st16 = Rot(S, "st16", [128, 512], BF16, 8)
    stkT = Rot(S, "stkT", [128, 4, 128], BF16, 2)
    decst = S.sbuf("decst", [128, 8, 32], F32)
    Bdec = S.buf("decst")
    wist = Rot(S, "wist", [128, 8], F32, 2)
    cst = G["cst"]
    Bc = G["Bcst"]
    evq = [0]

    def evac_engine():
        evq[0] += 1
        return "act" if evq[0] % 2 else "dve"

    def copy_op(eng, out_ap, in_ap, reads, writes):
        if eng == "act":
            S.op("act", lambda e, out_ap=out_ap, in_ap=in_ap: e.activation(out_ap, in_ap, AF.Copy), reads=reads, writes=writes)
        else:
            S.op("dve", lambda e, out_ap=out_ap, in_ap=in_ap: e.tensor_copy(out_ap, in_ap), reads=reads, writes=writes)

    for blk in blocks:
        own = (blk == 3)
        s0 = blk * 2048
        for t in range(16):
            xi, Bxi = xin.next()
            S.dma("pool", xi[:], A["xs"][s0 + t * 128: s0 + (t + 1) * 128, :], writes=[Bxi])
            for q4 in range(4):
                pb, Bpb = PB.next()
                for j in range(4):
                    kc = q4 * 4 + j
                    S.op("pe", lambda e, pb=pb, xi=xi, kc=kc, j=j: e.matmul(
                        pb[:, j * 128:(j + 1) * 128], xi[:, kc * 128:(kc + 1) * 128], ident[:], start=True, stop=True),
                        reads=[Bxi, Bident], writes=[Bpb])
                copy_op(evac_engine(), xT[:, q4 * 4:(q4 + 1) * 4, t * 128:(t + 1) * 128],
                        pb[:].rearrange("p (a b) -> p a b", a=4), [Bpb], [BxT[t]])

        def load_w(cols):
            w, Bw = wt.next()
            off = 0
            for (c0, n) in cols:
                S.dma("pool", w[:, :, off:off + n], w_in[:, c0:c0 + n].rearrange("(kc p) n -> p kc n", p=128), writes=[Bw])
                off += n
            return w, Bw

        def fm_mm(w, Bw, m, g):
            pb, Bpb = PB.next()
            for kc in range(KC):
                S.op("pe", lambda e, pb=pb, w=w, kc=kc, m=m, g=g: e.matmul(
                    pb[:], w[:, kc, m * 128:(m + 1) * 128], xT[:, kc, g * 512:(g + 1) * 512], start=(kc == 0), stop=(kc == KC - 1)),
                    reads=[Bw] + BxT[g * 4:(g + 1) * 4], writes=[Bpb])
            return pb, Bpb

        def simple_fm(w, Bw, m, g, bias_ap, func, dst_ap, Bd, rows=128):
            pb, Bpb = fm_mm(w, Bw, m, g)
            st, Bst = st16.next()
            S.op("act", lambda e, st=st, pb=pb, func=func, bias_ap=bias_ap: e.activation(st[:], pb[:], func, bias=bias_ap), reads=[Bpb, Bc], writes=[Bst])
            S.dma("sp", dst_ap, st[0:rows, :], reads=[Bst], writes=[Bd], nowaw=True)

        tm_units = [("ai", C_AI), ("ai", C_AI + 512), ("bv", C_BV), ("bv", C_BV + 512)]
        for (kind, c0) in tm_units:
            w, Bw = load_w([(c0, 512)])
            for t in range(16):
                pb, Bpb = PB.next()
                for kc in range(KC):
                    S.op("pe", lambda e, pb=pb, w=w, kc=kc, t=t: e.matmul(
                        pb[:], xT[:, kc, t * 128:(t + 1) * 128], w[:, kc, :], start=(kc == 0), stop=False),
                        reads=[Bw, BxT[t]], writes=[Bpb])
                boff = (c0 - C_AI) if kind == "ai" else (1024 + c0 - C_BV)
                S.op("pe", lambda e, pb=pb, boff=boff: e.matmul(pb[:], cst["ones_row"][0:1, :], cst["brow"][0:1, boff:boff + 512],
                                                            start=False, stop=True), reads=[Bc], writes=[Bpb])
                st, Bst = st16.next()
                tt = blk * 16 + t
                if kind == "ai":
                    S.op("act", lambda e, st=st, pb=pb, tt=tt: e.activation(st[:], pb[:], AF.Identity, scale=cst["valid_tm"][:, tt:tt + 1]),
                         reads=[Bpb, Bc], writes=[Bst])
                    dst = A["HG_v"][s0 + t * 128:s0 + (t + 1) * 128, c0 - C_AI:c0 - C_AI + 512]
                else:
                    S.op("dve", lambda e, st=st, pb=pb: e.tensor_copy(st[:], pb[:]), reads=[Bpb], writes=[Bst])
                    h0 = (c0 - C_BV) // 128
                    dst = A["VH"][h0:h0 + 4, :, tt, :].rearrange("h p d -> p h d")
                if kind == "ai":
                    S.dma("sp", dst, st[:], reads=[Bst], writes=[G["B"]["HG_v"]], nowaw=True)
                else:
                    S.dma("sp", dst, st[:].rearrange("p (h d) -> p h d", h=4), reads=[Bst], writes=[G["B"]["VH"]], nowaw=True)
        if own:
            w, Bw = load_w([(C_IW, 8)])
            for t in range(16):
                pb, Bpb = PB.next()
                for kc in range(KC):
                    S.op("pe", lambda e, pb=pb, w=w, kc=kc, t=t: e.matmul(
                        pb[:, 0:8], xT[:, kc, t * 128:(t + 1) * 128], w[:, kc, 0:8], start=(kc == 0), stop=False),
                        reads=[Bw, BxT[t]], writes=[Bpb])
                S.op("pe", lambda e, pb=pb: e.matmul(pb[:, 0:8], cst["ones_row"][0:1, :], cst["brow"][0:1, 2048:2056],
                                                     start=False, stop=True), reads=[Bc], writes=[Bpb])
                st, Bst = wist.next()
                S.op("dve", lambda e, st=st, pb=pb: e.tensor_scalar(st[:], pb[:, 0:8], W_SCALE, None, op0=ALU.mult), reads=[Bpb], writes=[Bst])
                S.dma("sp", A["WI"][t * 128:(t + 1) * 128, :], st[:], reads=[Bst], writes=[G["B"]["WI"]], nowaw=True)

        for u in range(2):
            w, Bw = load_w([(C_BK + u * 512, 512)])
            for m in range(4):
                h = u * 4 + m
                for g in range(4):
                    simple_fm(w, Bw, m, g, cst["bfm"][:, G["bfm_idx"]["bk"] + h: G["bfm_idx"]["bk"] + h + 1], AF.Identity,
                              A["KT"][h, :, s0 + g * 512:s0 + (g + 1) * 512], G["B"]["KT"])
        w, Bw = load_w([(C_IK, 64), (C_IK, 64)])
        for g in range(4):
            simple_fm(w, Bw, 0, g, cst["bfm"][:, G["bfm_idx"]["ik"]: G["bfm_idx"]["ik"] + 1], AF.Identity,
                      A["KIT"][:, s0 + g * 512:s0 + (g + 1) * 512], G["B"]["KIT"])
        if own:
            for u in range(2):
                w, Bw = load_w([(C_BQ + u * 512, 512)])
                for m in range(4):
                    h = u * 4 + m
                    for g in range(4):
                        simple_fm(w, Bw, m, g, cst["bfm"][:, G["bfm_idx"]["bq"] + h: G["bfm_idx"]["bq"] + h + 1], AF.Identity,
                                  A["QT"][h, :, g * 512:(g + 1) * 512], G["B"]["QT"])
            w, Bw = load_w([(C_IQ, 512)])
            for m in range(4):
                for g in range(4):
                    simple_fm(w, Bw, m, g, cst["bfm"][:, G["bfm_idx"]["iq"] + m: G["bfm_idx"]["iq"] + m + 1], AF.Identity,
                              A["QIT"][m, :, g * 512:(g + 1) * 512], G["B"]["QIT"])
            for u in range(8):
                w, Bw = load_w([(C_G + u * 512, 512)])
                for m in range(4):
                    c = u * 4 + m
                    for g in range(4):
                        simple_fm(w, Bw, m, g, cst["bfm"][:, G["bfm_idx"]["g"] + c: G["bfm_idx"]["g"] + c + 1], AF.Sigmoid,
                                  A["GT"][c, :, g * 512:(g + 1) * 512], G["B"]["GT"])

        deferred = []
        for h in range(8):
            if own:
                w, Bw = load_w([(C_AF + h * 128, 128), (C_AQ + h * 128, 128), (C_AG + h * 128, 128)])
            else:
                if h % 4 == 0:
                    w4, Bw4 = load_w([(C_AF + h * 128, 512)])
                w, Bw = w4, Bw4
            mf = 0 if own else (h % 4)
            bi = G["bfm_idx"]
            oml_h = cst["oml"][:, h:h + 1]
            noml_h = cst["noml"][:, h:h + 1]
            lb_h = cst["lb"][:, h:h + 1]
            b_af = cst["bfm"][:, bi["af"] + h: bi["af"] + h + 1]
            b_aq = cst["bfm"][:, bi["aq"] + h: bi["aq"] + h + 1]
            b_ag = cst["bfm"][:, bi["ag"] + h: bi["ag"] + h + 1]
            for g in range(4):
                pb, Bpb = fm_mm(w, Bw, mf, g)
                while deferred:
                    deferred.pop(0)()
                sg, Bsg = f32t.next()
                S.op("act", lambda e, sg=sg, pb=pb, b_af=b_af: e.activation(sg[:], pb[:], AF.Sigmoid, bias=b_af),
                     reads=[Bpb, Bc], writes=[Bsg])
                lf, Blf = f32t.next()
                S.op("act", lambda e, lf=lf, sg=sg, oml_h=oml_h, lb_h=lb_h: e.activation(lf[:], sg[:], AF.Ln, scale=oml_h, bias=lb_h),
                     reads=[Bsg, Bc], writes=[Blf])
                cum, Bcum = f32t.next()
                S.op("dve", lambda e, cum=cum, lf=lf: e.tensor_tensor_scan(cum[:], cst["rmask"][:], lf[:], 0.0, ALU.mult, ALU.add),
                     reads=[Blf, Bc], writes=[Bcum])
                en, Ben = f32t.next()
                S.op("act", lambda e, en=en, cum=cum: e.activation(en[:], cum[:], AF.Exp, scale=-1.0), reads=[Bcum], writes=[Ben])
                kk, Bkk = f32t.next()
                S.op("dve", lambda e, kk=kk, sg=sg, noml_h=noml_h, oml_h=oml_h: e.tensor_scalar(kk[:], sg[:], noml_h, oml_h, op0=ALU.mult, op1=ALU.add),
                     reads=[Bsg, Bc], writes=[Bkk])
                kd, Bkd = st16.next()
                S.op("dve", lambda e, kd=kd, kk=kk, en=en: e.tensor_tensor(kd[:], kk[:], en[:], ALU.mult), reads=[Bkk, Ben], writes=[Bkd])
                S.dma("sp", A["HG_kdec"][h, :, s0 + g * 512:s0 + (g + 1) * 512], kd[:], reads=[Bkd], writes=[G["B"]["HG_kdec"]], nowaw=True)
                dec_ap = decst[:, h, g * 8:(g + 1) * 8]
                S.op("act", lambda e, cum=cum, dec_ap=dec_ap: e.activation(dec_ap, cum[:].rearrange("p (c s) -> p c s", s=64)[:, :, 63], AF.Exp),
                     reads=[Bcum], writes=[Bdec])
                ke, Bke = st16.next()
                for c in range(8):
                    S.op("dve", lambda e, ke=ke, kk=kk, en=en, c=c, h=h, g=g: e.scalar_tensor_tensor(
                        ke[:, c * 64:(c + 1) * 64], kk[:, c * 64:(c + 1) * 64], decst[:, h, g * 8 + c:g * 8 + c + 1], en[:, c * 64:(c + 1) * 64],
                        op0=ALU.mult, op1=ALU.mult), reads=[Bkk, Ben, Bdec], writes=[Bke])
                def kend_T(ke=ke, Bke=Bke, g=g, h=h, s0=s0):
                    pbt, Bpbt = PB.next()
                    for j in range(4):
                        S.op("pe", lambda e, pbt=pbt, ke=ke, j=j: e.matmul(pbt[:, j * 128:(j + 1) * 128], ke[:, j * 128:(j + 1) * 128], ident[:],
                                                                           start=True, stop=True), reads=[Bke, Bident], writes=[Bpbt])
                    kT, BkT = stkT.next()
                    S.op("dve", lambda e, kT=kT, pbt=pbt: e.tensor_copy(kT[:], pbt[:].rearrange("p (a b) -> p a b", a=4)), reads=[Bpbt], writes=[BkT])
                    S.dma("sp", A["HG_kend"][s0 + g * 512:s0 + (g + 1) * 512, h * 128:(h + 1) * 128].rearrange("(t p) d -> p t d", p=128),
                          kT[:], reads=[BkT], writes=[G["B"]["HG_kend"]], nowaw=True)
                deferred.append(kend_T)
                if own:
                    ec, Bec = f32t.next()
                    S.op("act", lambda e, ec=ec, cum=cum: e.activation(ec[:], cum[:], AF.Exp), reads=[Bcum], writes=[Bec])
                    pbq, Bpbq = fm_mm(w, Bw, 1, g)
                    qs, Bqs = f32t.next()
                    S.op("act", lambda e, qs=qs, pbq=pbq, b_aq=b_aq: e.activation(qs[:], pbq[:], AF.Silu, bias=b_aq),
                         reads=[Bpbq, Bc], writes=[Bqs])
                    qd, Bqd = st16.next()
                    S.op("dve", lambda e, qd=qd, qs=qs, ec=ec: e.tensor_tensor(qd[:], qs[:], ec[:], ALU.mult), reads=[Bqs, Bec], writes=[Bqd])
                    S.dma("sp", A["HG_qdec"][h, :, g * 512:(g + 1) * 512], qd[:], reads=[Bqd], writes=[G["B"]["HG_qdec"]], nowaw=True)
                    simple_fm(w, Bw, 2, g, b_ag, AF.Silu, A["HG_gs"][h, :, g * 512:(g + 1) * 512], G["B"]["HG_gs"])
        while deferred:
            deferred.pop(0)()
        S.dma("sp", A["HG_dec"][:, :, blk * 32:(blk + 1) * 32], decst[:], reads=[Bdec], writes=[G["B"]["HG_dec"]], nowaw=True)

RMS_EPS = 1e-6


def phase1b(S, G):
    A = G["ap"]
    PB = G["pb"]
    cst = G["cst"]
    Bc = G["Bcst"]
    B = G["B"]
    kend = S.sbuf("hs_kend", [128, 16, 1024], BF16)
    Bkend = S.buf("hs_kend")
    vv = S.sbuf("hs_v", [128, 16, 1024], BF16)
    Bvv = S.buf("hs_v")
    dec = S.sbuf("hs_dec", [128, 8, 128], F32)
    Bdec = S.buf("hs_dec")
    Sf = S.sbuf("hs_S", [128, 8, 128], F32)
    Sb = S.sbuf("hs_Sb", [128, 8, 128], BF16)
    BS = S.bufs(8, "hs_S")
    BSb = S.bufs(8, "hs_Sb")
    S.dma("sp", dec[:], A["HG_dec"], reads=[B["HG_dec"]], writes=[Bdec])
    S.op("dve", lambda e: e.memset(Sf[:], 0.0), writes=BS)
    S.op("dve", lambda e: e.memset(Sb[:], 0.0), writes=BSb)
    onesb = S.sbuf("hs_ones", [128, 128], BF16)
    Bones = S.buf("hs_ones")
    S.op("dve", lambda e: e.memset(onesb[:], 1.0 / 128.0), writes=[Bones])
    kdec = Rot(S, "hs_kdec", [128, 2048], BF16, 8)
    qdec = Rot(S, "hs_qdec", [128, 2048], BF16, 8)
    gs = Rot(S, "hs_gs", [128, 2048], BF16, 8)
    attm = Rot(S, "hs_attm", [128, 128], BF16, 8)
    sq = Rot(S, "hs_sq", [128, 128], BF16, 6)
    rs = Rot(S, "hs_rs", [128, 128], F32, 6)
    yy = Rot(S, "hs_y", [128, 128], F32, 6)
    bastr = Rot(S, "hs_bast", [128, 8, 128], BF16, 2)

    def state_update(t, h, half, ):
        p0 = half * 64
        chunk = None
        pk, Bpk = PB.next()
        S.op("pe", lambda e, pk=pk, t=t, h=h, p0=p0: e.matmul(pk[:, 0:128], kend[p0:p0 + 64, t, h * 128:(h + 1) * 128],
                                                            vv[p0:p0 + 64, t, h * 128:(h + 1) * 128], start=True, stop=True),
             reads=[Bkend, Bvv], writes=[Bpk])
        return pk, Bpk

    for blk in range(4):
        own = (blk == 3)
        s0 = blk * 2048
        S.dma("sp", kend[:], A["HG_kend"][s0:s0 + 2048, :].rearrange("(t p) c -> p t c", p=128), reads=[B["HG_kend"]], writes=[Bkend])
        S.dma("sp", vv[:], A["HG_v"][s0:s0 + 2048, :].rearrange("(t p) c -> p t c", p=128), reads=[B["HG_v"]], writes=[Bvv])
        if not own:
            for t in range(16):
                for half in range(2):
                    ch = blk * 32 + t * 2 + half
                    for h in range(8):
                        pk, Bpk = state_update(t, h, half)
                        S.op("dve", lambda e, pk=pk, h=h, ch=ch: e.scalar_tensor_tensor(Sf[:, h, :], Sf[:, h, :], dec[:, h, ch:ch + 1], pk[:, 0:128],
                                                                                      op0=ALU.mult, op1=ALU.add),
                             reads=[Bpk, Bdec, BS[h]], writes=[BS[h]])
            if blk == 2:
                for h in range(8):
                    S.op("act", lambda e, h=h: e.activation(Sb[:, h, :], Sf[:, h, :], AF.Copy), reads=[BS[h]], writes=[BSb[h]])
            continue
        kds, qds, ggs = [], [], []
        for h in range(8):
            kd, Bkd = kdec.next()
            qd, Bqd = qdec.next()
            gg, Bgg = gs.next()
            S.dma("sp", kd[:], A["HG_kdec"][h, :, s0:s0 + 2048], reads=[B["HG_kdec"]], writes=[Bkd])
            S.dma("sp", qd[:], A["HG_qdec"][h], reads=[B["HG_qdec"]], writes=[Bqd])
            S.dma("sp", gg[:], A["HG_gs"][h], reads=[B["HG_gs"]], writes=[Bgg])
            kds.append((kd, Bkd)); qds.append((qd, Bqd)); ggs.append((gg, Bgg))
        for t in range(16):
            tc = slice(t * 128, (t + 1) * 128)
            bt, Bbt = bastr.next()
            for h in range(8):
                kd, Bkd = kds[h]
                qd, Bqd = qds[h]
                gg, Bgg = ggs[h]
                pa, Bpa = PB.next()
                S.op("pe", lambda e, pa=pa, kd=kd, qd=qd, tc=tc: e.matmul(pa[:, 0:128], kd[:, tc], qd[:, tc], start=True, stop=True),
                     reads=[Bkd, Bqd], writes=[Bpa])
                am, Bam = attm.next()
                S.op("dve", lambda e, am=am, pa=pa: e.tensor_tensor(am[:], pa[:, 0:128], cst["bdmask"][:], ALU.mult), reads=[Bpa, Bc], writes=[Bam])
                po, Bpo = PB.next()
                S.op("pe", lambda e, po=po, am=am, t=t, h=h: e.matmul(po[:, 0:128], vv[:, t, h * 128:(h + 1) * 128], am[:], start=True, stop=False),
                     reads=[Bvv, Bam], writes=[Bpo])
                for half in range(2):
                    ch = blk * 32 + t * 2 + half
                    c0 = t * 128 + half * 64
                    S.op("pe", lambda e, po=po, qd=qd, h=h, c0=c0, half=half: e.matmul(po[:, half * 64:(half + 1) * 64], Sb[:, h, :], qd[:, c0:c0 + 64],
                                                                                  start=False, stop=(half == 1)),
                         reads=[BSb[h], Bqd], writes=[Bpo])
                    pk, Bpk = state_update(t, h, half)
                    S.op("dve", lambda e, pk=pk, h=h, ch=ch: e.scalar_tensor_tensor(Sf[:, h, :], Sf[:, h, :], dec[:, h, ch:ch + 1], pk[:, 0:128],
                                                                                  op0=ALU.mult, op1=ALU.add),
                         reads=[Bpk, Bdec, BS[h]], writes=[BS[h]])
                    S.op("act", lambda e, h=h: e.activation(Sb[:, h, :], Sf[:, h, :], AF.Copy), reads=[BS[h]], writes=[BSb[h]])
                q2, Bq2 = sq.next()
                S.op("act", lambda e, q2=q2, po=po: e.activation(q2[:], po[:, 0:128], AF.Square), reads=[Bpo], writes=[Bq2])
                pm, Bpm = PB.next()
                S.op("pe", lambda e, pm=pm, q2=q2: e.matmul(pm[:, 0:128], onesb[:], q2[:], start=True, stop=True), reads=[Bones, Bq2], writes=[Bpm])
                r1, Br1 = rs.next()
                S.op("act", lambda e, r1=r1, pm=pm: e.activation(r1[:], pm[:, 0:128], AF.Sqrt, bias=cst["eps_rms"][:, 0:1]), reads=[Bpm, Bc], writes=[Br1])
                S.op("dve", lambda e, r1=r1: e.reciprocal(r1[:], r1[:]), reads=[Br1], writes=[Br1])
                y1, By1 = yy.next()
                S.op("dve", lambda e, y1=y1, po=po, r1=r1: e.tensor_tensor(y1[:], po[:, 0:128], r1[:], ALU.mult), reads=[Bpo, Br1], writes=[By1])
                S.op("dve", lambda e, y1=y1, gg=gg, tc=tc, h=h, bt=bt: e.scalar_tensor_tensor(bt[:, h, :], y1[:], cst["normg"][:, h:h + 1], gg[:, tc],
                                                                                           op0=ALU.mult, op1=ALU.mult),
                     reads=[By1, Bgg, Bc], writes=[Bbt])
            S.dma("sp", A["BA"][:, :, tc].rearrange("h p t -> p h t"), bt[:], reads=[Bbt], writes=[B["BA"]], nowaw=True)

SM_SCALE = 128 ** -0.5
TOPK = 256
NBIS = 18
NTER = 10
BIS_WIN = 16.0
NEG_ADM = -30000.0


def phase2_dsa(S, G, groups=(0, 1, 2, 3)):
    A = G["ap"]
    PB = G["pb"]
    cst = G["cst"]
    Bc = G["Bcst"]
    B = G["B"]
    ident = G["ident_bf"]
    Bident = G["Bident"]
    es_outer = S.es
    with ExitStack() as pes:
        S.es = pes
        alb = S.sbuf("ds_alb", [128, 8, 64], F32)
        dtab = S.sbuf("ds_dtab", [128, 8192], BF16)
        qrel = S.sbuf("ds_qrel", [1, 512], F32)
        onesr = S.sbuf("ds_onesr", [1, 128], BF16)
        drow = S.sbuf("ds_drow", [1, 512], F32)
        Bdrow = S.buf("ds_drow")
        shrow = S.sbuf("ds_shrow", [1, 8, 512], BF16)
        Bshrow = S.buf("ds_shrow")
        dmc = S.sbuf("ds_dmc", [128, 1], F32)
        Bdmc = S.buf("ds_dmc")
        corr = S.sbuf("ds_corr", [128, 8, 128], BF16)
        sel2 = S.sbuf("ds_sel2", [2, 128], BF16)
        onesb = S.sbuf("ds_ones", [128, 128], BF16)
        Bk = S.buf("ds_const")
        S.dma("sp", alb[:], A["alb"], writes=[Bk], nowaw=True)
        S.dma("sp", corr[:], A["corr"], writes=[Bk], nowaw=True)
        S.dma("sp", sel2[:], A["sel2"], writes=[Bk], nowaw=True)
        S.op("dve", lambda e: e.memset(onesb[:], 1.0), writes=[Bk])
        S.op("dve", lambda e: e.memset(onesr[:], 1.0), writes=[Bk])
        S.dma("sp", dtab[:], A["dtab"], writes=[Bk], nowaw=True)
        S.dma("sp", qrel[:], A["qrel"], writes=[Bk], nowaw=True)
        kit = S.sbuf("ds_kit", [128, 8192], BF16)
        S.dma("sp", kit[:], A["KIT"], reads=[B["KIT"]], writes=[Bk], nowaw=True)
        wi = S.sbuf("ds_wi", [128, 16, 8], F32)
        S.dma("sp", wi[:], A["WI"].rearrange("(t p) h -> p t h", p=128), reads=[B["WI"]], writes=[Bk], nowaw=True)
        maskT = S.sbuf("ds_maskT", [128, 64, 512], U8)
        BmT = S.buf("ds_maskT")
        S.barrier()

        def do_group(g):
            Q0 = OWN0 + g * 512
            with ExitStack() as aes:
                S.es = aes
                qit = S.sbuf("ds_qit", [128, 4, 512], BF16)
                Bqit = S.buf("ds_qit")
                S.dma("sp", qit[:], A["QIT"][:, :, g * 512:(g + 1) * 512].rearrange("m p q -> p m q"), reads=[B["QIT"]], writes=[Bqit])
                sc2 = [S.sbuf("ds_sc", [128, 8192], F32) for _ in range(2)]
                Bsc2 = [S.buf("ds_sc"), S.buf("ds_sc")]
                mq = S.sbuf("ds_mq", [128, 8192], BF16)
                Bmq = S.buf("ds_mq")
                adm = Rot(S, "ds_adm", [2, 512], BF16, 3)
                junk2 = S.sbuf("ds_junk2", [128, 8192], U8)
                Bj2 = S.buf("ds_junk2")
                relu = Rot(S, "ds_relu", [128, 512], BF16, 9)
                dg2 = [S.sbuf("ds_dg", [128, 8, 128], BF16) for _ in range(2)]
                Bdg2 = [S.buf("ds_dg"), S.buf("ds_dg")]
                sm = {n: S.sbuf("ds_s_" + n, [128, 1], F32) for n in ("lo", "hi", "mid", "cnt", "ge", "d", "c0", "d3", "t1", "nt2", "s2", "g2")}
                Bsm = S.buf("ds_small")
                Bth = S.buf("ds_th")
                Bs2 = S.buf("ds_s2")

                def tparams(T):
                    tg = g * 4 + T
                    Qt = Q0 + T * 128
                    nk = Qt + 128
                    nb5 = (nk + 511) // 512
                    return tg, Qt, nk, nb5, nb5 * 512

                def score_blocks(T):
                    tg, Qt, nk, nb5, nkp = tparams(T)
                    sc, Bsc = sc2[T % 2], Bsc2[T % 2]
                    dg, Bdg = dg2[T % 2], Bdg2[T % 2]
                    out = []

                    def prep():
                        for h in range(8):
                            S.op("dve", lambda e, h=h: e.tensor_scalar(dg[:, h, :], ident[:], wi[:, tg, h:h + 1], None, op0=ALU.mult),
                                 reads=[Bident, Bk], writes=[Bdg])
                    out.append(prep)

                    def blk(kb5):
                        ks = slice(kb5 * 512, (kb5 + 1) * 512)
                        ad, Bad = adm.next()
                        S.dma("sp", ad[:], A["adm"][tg, :, ks], writes=[Bad])
                        rl = []
                        for hp in range(4):
                            for par in range(2):
                                pb, Bpb = PB.next()
                                p0 = par * 64
                                S.op("pe", lambda e, pb=pb, hp=hp, p0=p0: e.matmul(
                                    pb[:], qit[p0:p0 + 64, hp, T * 128:(T + 1) * 128], kit[p0:p0 + 64, ks], start=True, stop=True),
                                    reads=[Bqit, Bk], writes=[Bpb])
                                r, Br = relu.next()
                                if par == 0 or hp % 2 == 0:
                                    S.op("act", lambda e, r=r, pb=pb: e.activation(r[:], pb[:], AF.Relu), reads=[Bpb], writes=[Br])
                                else:
                                    S.op("dve", lambda e, r=r, pb=pb: e.tensor_scalar(r[:], pb[:], 0.0, None, op0=ALU.max), reads=[Bpb], writes=[Br])
                                rl.append((r, Br))
                        ps, Bps = PB.next()
                        for h in range(8):
                            r, Br = rl[h]
                            S.op("pe", lambda e, ps=ps, h=h, r=r: e.matmul(ps[:], dg[:, h, :], r[:], start=(h == 0), stop=False),
                                 reads=[Bdg, Br], writes=[Bps])
                        S.op("pe", lambda e, ps=ps, ad=ad: e.matmul(ps[:], sel2[0:2, :], ad[0:2, :], start=False, stop=True),
                             reads=[Bk, Bad], writes=[Bps])
                        S.op("act", lambda e, ps=ps: e.activation(sc[:, ks], ps[:], AF.Copy), reads=[Bps], writes=[Bsc])
                    for kb5 in range(nb5):
                        out.append(lambda kb5=kb5: blk(kb5))
                    return out

                def search_init(T):
                    tg, Qt, nk, nb5, nkp = tparams(T)
                    sc, Bsc = sc2[T % 2], Bsc2[T % 2]
                    scv = sc[:, 0:nkp]
                    S.op("dve", lambda e: e.reduce_max(sm["hi"][:], scv, AX.X), reads=[Bsc], writes=[Bsm])
                    S.op("dve", lambda e: e.tensor_scalar(sm["lo"][:], sm["hi"][:], -BIS_WIN, None, op0=ALU.add), reads=[Bsm], writes=[Bsm])
                    S.op("dve", lambda e: e.tensor_scalar(sm["hi"][:], sm["hi"][:], 1e-3, None, op0=ALU.add), reads=[Bsm], writes=[Bsm, Bth])
                    S.op("dve", lambda e: e.tensor_scalar(mq[:, 0:nkp], scv, sm["lo"][:, 0:1], None, op0=ALU.is_ge, op1=ALU.add,
                                                          accum_out=sm["c0"][:]), reads=[Bsc, Bsm], writes=[Bmq, Bsm])

                def search_round(T, it):
                    tg, Qt, nk, nb5, nkp = tparams(T)
                    sc, Bsc = sc2[T % 2], Bsc2[T % 2]
                    scv = sc[:, 0:nkp]
                    d3 = (BIS_WIN + 1e-3) / (3.0 ** (it + 1))
                    S.op("dve", lambda e: e.tensor_scalar(sm["t1"][:], sm["lo"][:], d3, None, op0=ALU.add), reads=[Bsm], writes=[Bsm])
                    S.op("dve", lambda e: e.tensor_scalar(sm["nt2"][:], sm["lo"][:], -1.0, -2.0 * d3, op0=ALU.mult, op1=ALU.add), reads=[Bsm], writes=[Bsm, Bth])
                    S.op("act", lambda e: e.activation(junk2[:, 0:nkp], scv, AF.Sign, bias=sm["nt2"][:, 0:1], accum_out=sm["s2"][:]),
                         reads=[Bsc, Bth], writes=[Bj2, Bs2])
                    S.op("dve", lambda e: e.tensor_scalar(mq[:, 0:nkp], scv, sm["t1"][:, 0:1], None, op0=ALU.is_ge, op1=ALU.add,
                                                          accum_out=sm["cnt"][:]), reads=[Bsc, Bsm], writes=[Bmq, Bsm])
                    S.op("dve", lambda e: e.tensor_scalar(sm["ge"][:], sm["cnt"][:], TOPK - 0.5, None, op0=ALU.is_ge), reads=[Bsm], writes=[Bsm])
                    S.op("dve", lambda e: e.scalar_tensor_tensor(sm["g2"][:], sm["s2"][:], 2.0 * (TOPK - 0.5) - nkp, sm["ge"][:], op0=ALU.is_ge, op1=ALU.add),
                         reads=[Bs2, Bsm], writes=[Bsm])
                    S.op("dve", lambda e: e.scalar_tensor_tensor(sm["lo"][:], sm["g2"][:], d3, sm["lo"][:], op0=ALU.mult, op1=ALU.add),
                         reads=[Bsm], writes=[Bsm])

                def finalize(T):
                    tg, Qt, nk, nb5, nkp = tparams(T)
                    sc, Bsc = sc2[T % 2], Bsc2[T % 2]
                    scv = sc[:, 0:nkp]
                    S.op("dve", lambda e: e.tensor_scalar(sm["ge"][:], sm["c0"][:], TOPK - 0.5, None, op0=ALU.is_ge), reads=[Bsm], writes=[Bsm])
                    S.op("dve", lambda e: e.tensor_scalar(sm["d"][:], sm["lo"][:], 1000.0, None, op0=ALU.add), reads=[Bsm], writes=[Bsm])
                    S.op("dve", lambda e: e.tensor_scalar(sm["lo"][:], sm["d"][:], sm["ge"][:, 0:1], -1000.0, op0=ALU.mult, op1=ALU.add),
                         reads=[Bsm], writes=[Bsm])
                    S.op("dve", lambda e: e.tensor_scalar(mq[:, 0:nkp], scv, sm["lo"][:, 0:1], None, op0=ALU.is_ge), reads=[Bsc, Bsm], writes=[Bmq])
                    S.op("dve", lambda e: e.scalar_tensor_tensor(sc[:, 0:nk], mq[:, 0:nk], -16384.0, dtab[:, 8192 - nk:8192], op0=ALU.mult, op1=ALU.add),
                         reads=[Bmq, Bk], writes=[Bsc])
                    S.op("dve", lambda e: e.tensor_reduce(dmc[:], sc[:, 0:nk], AX.X, ALU.min), reads=[Bsc], writes=[Bdmc])
                    S.op("dve", lambda e: e.tensor_scalar(dmc[:], dmc[:], 16384.0, None, op0=ALU.add), reads=[Bdmc], writes=[Bdmc])
                    pbd, Bpbd = PB.next()
                    S.op("pe", lambda e: e.matmul(pbd[0:1, 0:128], dmc[:, 0:1], G["ident_f"][:], start=True, stop=True),
                         reads=[Bdmc, G["Bidf"]], writes=[Bpbd])
                    S.op("dve", lambda e: e.tensor_copy(drow[0:1, T * 128:(T + 1) * 128], pbd[0:1, 0:128]), reads=[Bpbd], writes=[Bdrow])
                    nkb = nk // 128
                    for k4 in range(0, nkb, 4):
                        n4 = min(4, nkb - k4)
                        pb, Bpb = PB.next()
                        for j in range(n4):
                            kb = k4 + j
                            S.op("pe", lambda e, pb=pb, j=j, kb=kb: e.matmul(pb[:, j * 128:(j + 1) * 128], mq[:, kb * 128:(kb + 1) * 128], ident[:],
                                                                           start=True, stop=True), reads=[Bmq, Bident], writes=[Bpb])
                        dst = maskT[:, k4:k4 + n4, T * 128:(T + 1) * 128]
                        src = pb[:, 0:n4 * 128].rearrange("p (a b) -> p a b", a=n4)
                        if (k4 // 4) % 2 == 0:
                            S.op("act", lambda e, dst=dst, src=src: e.activation(dst, src, AF.Copy), reads=[Bpb], writes=[BmT])
                        else:
                            S.op("dve", lambda e, dst=dst, src=src: e.tensor_copy(dst, src), reads=[Bpb], writes=[BmT])

                for f_ in score_blocks(0):
                    f_()
                for T in range(4):
                    nxt = score_blocks(T + 1) if T < 3 else []
                    per = -(-len(nxt) // NTER)
                    search_init(T)
                    for it in range(NTER):
                        search_round(T, it)
                        for _ in range(per):
                            if nxt:
                                nxt.pop(0)()
                    while nxt:
                        nxt.pop(0)()
                    finalize(T)
                S.op("dve", lambda e: e.tensor_tensor(drow[:], drow[:], qrel[:], ALU.subtract), reads=[Bdrow, Bk], writes=[Bdrow])
                for h in range(8):
                    S.op("dve", lambda e, h=h: e.tensor_scalar(shrow[0:1, h, :], drow[:], (2.0 ** -(h + 1)) / SM_SCALE, None, op0=ALU.mult),
                         reads=[Bdrow], writes=[Bshrow])
                S.barrier()
                S.phase_end()
            with ExitStack() as bes:
                S.es = bes
                kt = Rot(S, "ds_kt", [128, 8192], BF16, 2)
                vh = Rot(S, "ds_vh", [128, 64, 128], BF16, 2)
                qt = Rot(S, "ds_qt", [128, 512], BF16, 2)
                pT = Rot(S, "ds_pT", [128, 512], BF16, 9)
                mcr = Rot(S, "ds_mc", [128, 128], BF16, 3)
                rec = Rot(S, "ds_rec", [128, 512], F32, 1)
                ob = Rot(S, "ds_ob", [128, 512], BF16, 1)
                nkb = (Q0 + 512) // 128
                kb0 = Q0 // 128
                for h in range(8):
                    k_, Bk_ = kt.next()
                    v_, Bv_ = vh.next()
                    q_, Bq_ = qt.next()
                    S.dma("sp", k_[:, 0:nkb * 128], A["KT"][h, :, 0:nkb * 128], reads=[B["KT"]], writes=[Bk_])
                    S.dma("sp", v_[:, 0:nkb, :], A["VH"][h, :, 0:nkb, :], reads=[B["VH"]], writes=[Bv_])
                    S.dma("sp", q_[:], A["QT"][h, :, g * 512:(g + 1) * 512], reads=[B["QT"]], writes=[Bq_])
                    po, Bpo = PB.t[6], PB.b[6]
                    pd, Bpd = PB.t[7], PB.b[7]
                    pend = []

                    def stage2(kb, p_, Bp_, c0, first, last, po=po, pd=pd, Bpo=Bpo, Bpd=Bpd, v_=v_, Bv_=Bv_):
                        S.op("pe", lambda e, po=po, v_=v_, p_=p_, kb=kb, c0=c0, first=first, last=last: e.matmul(
                            po[:, c0:512], v_[:, kb, :], p_[:, c0:512], start=first, stop=last), reads=[Bv_, Bp_], writes=[Bpo])
                        S.op("pe", lambda e, pd=pd, p_=p_, c0=c0, first=first, last=last: e.matmul(
                            pd[:, c0:512], onesb[:], p_[:, c0:512], start=first, stop=last), reads=[Bk, Bp_], writes=[Bpd])

                    for kb in range(nkb):
                        r = kb - kb0
                        c0 = max(r, 0) * 128
                        first = (kb == 0)
                        last = (kb == nkb - 1)
                        pst, Bpst = PB.next(0, 6)
                        S.op("pe", lambda e, pst=pst, k_=k_, q_=q_, kb=kb, c0=c0: e.matmul(
                            pst[:, c0:512], k_[:, kb * 128:(kb + 1) * 128], q_[:, c0:512], start=True, stop=(h >= 6)),
                            reads=[Bk_, Bq_], writes=[Bpst])
                        if h < 6:
                            S.op("pe", lambda e, pst=pst, h=h, c0=c0: e.matmul(pst[:, c0:512], onesr[0:1, :], shrow[0:1, h, c0:512], start=False, stop=True),
                                 reads=[Bk, Bshrow], writes=[Bpst])
                        p_, Bp_ = pT.next()
                        bias_ap = alb[:, h, r + 60:r + 61]
                        S.op("act", lambda e, p_=p_, pst=pst, c0=c0, bias_ap=bias_ap: e.activation(
                            p_[:, c0:512], pst[:, c0:512], AF.Exp, scale=SM_SCALE, bias=bias_ap), reads=[Bpst, Bk], writes=[Bp_])
                        if r >= 0:
                            mc, Bmc = mcr.next()
                            S.op("dve", lambda e, mc=mc, kb=kb, r=r, h=h: e.tensor_tensor(mc[:], maskT[:, kb, r * 128:(r + 1) * 128], corr[:, h, :], ALU.mult),
                                 reads=[BmT, Bk], writes=[Bmc])
                            S.op("dve", lambda e, p_=p_, mc=mc, r=r: e.scalar_tensor_tensor(p_[:, r * 128:(r + 1) * 128], p_[:, r * 128:(r + 1) * 128], 3.0e38, mc[:],
                                                                                         op0=ALU.min, op1=ALU.mult), reads=[Bmc, Bp_], writes=[Bp_])
                            if c0 + 128 < 512:
                                S.op("dve", lambda e, p_=p_, kb=kb, c0=c0: e.scalar_tensor_tensor(p_[:, c0 + 128:512], p_[:, c0 + 128:512], 3.0e38, maskT[:, kb, c0 + 128:512],
                                                                                               op0=ALU.min, op1=ALU.mult), reads=[BmT, Bp_], writes=[Bp_])
                        else:
                            S.op("dve", lambda e, p_=p_, kb=kb: e.scalar_tensor_tensor(p_[:], p_[:], 3.0e38, maskT[:, kb, :], op0=ALU.min, op1=ALU.mult),
                                 reads=[BmT, Bp_], writes=[Bp_])
                        pend.append((kb, p_, Bp_, c0, first, last))
                        if len(pend) > 6:
                            stage2(*pend.pop(0))
                    while pend:
                        stage2(*pend.pop(0))
                    rc, Brc = rec.next()
                    S.op("dve", lambda e, rc=rc, pd=pd: e.reciprocal(rc[:], pd[:]), reads=[Bpd], writes=[Brc])
                    o_, Bo_ = ob.next()
                    S.op("dve", lambda e, o_=o_, po=po, rc=rc: e.tensor_tensor(o_[:], po[:], rc[:], ALU.mult), reads=[Bpo, Brc], writes=[Bo_])
                    S.dma("sp", A["BB"][h, :, g * 512:(g + 1) * 512], o_[:], reads=[Bo_], writes=[B["BB"]], nowaw=True)
                S.barrier()
                S.phase_end()
        for g in groups:
            do_group(g)
        S.es = es_outer

LN_EPS = 1e-5
DN_ALPHA = 2.0 ** 0.25
CAP = 256
NEXP = 32


def layer_norm_tile(S, G, pre, Bpre, out_t, Bout, gb, bb, Bgb, small, Bsmall, junk, Bjunk):
    s1, s2, mean, var, rstd, nmr = small
    S.op("act", lambda e: e.activation(junk[:], pre[:], AF.Identity, accum_out=s1[:]), reads=[Bpre], writes=[Bjunk, Bsmall])
    S.op("act", lambda e: e.activation(junk[:], pre[:], AF.Square, accum_out=s2[:]), reads=[Bpre], writes=[Bjunk, Bsmall])
    S.op("dve", lambda e: e.tensor_scalar(mean[:], s1[:], 1.0 / 2048.0, None, op0=ALU.mult), reads=[Bsmall], writes=[Bsmall])
    S.op("dve", lambda e: e.tensor_tensor(var[:], mean[:], mean[:], ALU.mult), reads=[Bsmall], writes=[Bsmall])
    S.op("dve", lambda e: e.scalar_tensor_tensor(var[:], s2[:], 1.0 / 2048.0, var[:], op0=ALU.mult, op1=ALU.subtract), reads=[Bsmall], writes=[Bsmall])
    S.op("act", lambda e: e.activation(rstd[:], var[:], AF.Sqrt, bias=G["cst"]["eps_ln"][:, 0:1]), reads=[Bsmall, G["Bcst"]], writes=[Bsmall])
    S.op("dve", lambda e: e.reciprocal(rstd[:], rstd[:]), reads=[Bsmall], writes=[Bsmall])
    S.op("dve", lambda e: e.scalar_tensor_tensor(nmr[:], mean[:], -1.0, rstd[:], op0=ALU.mult, op1=ALU.mult), reads=[Bsmall], writes=[Bsmall])
    S.op("act", lambda e: e.activation(pre[:], pre[:], AF.Identity, scale=rstd[:, 0:1], bias=nmr[:, 0:1]), reads=[Bpre, Bsmall], writes=[Bpre])
    S.op("dve", lambda e: e.tensor_tensor(pre[:], pre[:], gb[:], ALU.mult), reads=[Bpre, Bgb], writes=[Bpre])
    S.op("dve", lambda e: e.tensor_tensor(out_t[:], pre[:], bb[:], ALU.add), reads=[Bpre, Bgb], writes=[Bout])


def phase3a_merge(S, G):
    A = G["ap"]
    PB = G["pb"]
    B = G["B"]
    wa = S.sbuf("o_wa", [128, 8, 2048], BF16)
    wb = S.sbuf("o_wb", [128, 8, 2048], BF16)
    Bw = S.buf("o_w")
    S.dma("pool", wa[:], A["w_branch_a"].rearrange("(kc p) n -> p kc n", p=128), writes=[Bw], nowaw=True)
    S.dma("pool", wb[:], A["w_branch_b"].rearrange("(kc p) n -> p kc n", p=128), writes=[Bw], nowaw=True)
    bag = Rot(S, "o_ba", [128, 8, 512], BF16, 2)
    bbg = Rot(S, "o_bb", [128, 8, 512], BF16, 2)
    gtg = Rot(S, "o_gt", [128, 32, 512], BF16, 2)
    t1r = Rot(S, "o_t1", [128, 512], F32, 2)
    t2r = Rot(S, "o_t2", [128, 512], F32, 2)
    mgs = Rot(S, "o_mgs", [128, 512], BF16, 3)
    for g in range(4):
        ba_, Bba = bag.next()
        bb_, Bbb = bbg.next()
        gt_, Bgt = gtg.next()
        S.dma("sp", ba_[:], A["BA"][:, :, g * 512:(g + 1) * 512].rearrange("h p t -> p h t"), reads=[B["BA"]], writes=[Bba])
        S.dma("sp", bb_[:], A["BB"][:, :, g * 512:(g + 1) * 512].rearrange("h p t -> p h t"), reads=[B["BB"]], writes=[Bbb])
        S.dma("sp", gt_[:], A["GT"][:, :, g * 512:(g + 1) * 512].rearrange("c p t -> p c t"), reads=[B["GT"]], writes=[Bgt])
        for c in range(16):
            pa, Bpa = PB.next()
            for k in range(8):
                S.op("pe", lambda e, pa=pa, k=k, c=c, ba_=ba_: e.matmul(pa[:], wa[:, k, c * 128:(c + 1) * 128], ba_[:, k, :], start=(k == 0), stop=(k == 7)),
                     reads=[Bw, Bba], writes=[Bpa])
            pb2, Bpb2 = PB.next()
            for k in range(8):
                S.op("pe", lambda e, pb2=pb2, k=k, c=c, bb_=bb_: e.matmul(pb2[:], wb[:, k, c * 128:(c + 1) * 128], bb_[:, k, :], start=(k == 0), stop=(k == 7)),
                     reads=[Bw, Bbb], writes=[Bpb2])
            t1, Bt1 = t1r.next()
            t2, Bt2 = t2r.next()
            S.op("dve", lambda e, t1=t1, pa=pa, gt_=gt_, c=c: e.tensor_tensor(t1[:], pa[:], gt_[:, c, :], ALU.mult), reads=[Bpa, Bgt], writes=[Bt1])
            S.op("dve", lambda e, t2=t2, pb2=pb2, gt_=gt_, c=c: e.tensor_tensor(t2[:], pb2[:], gt_[:, 16 + c, :], ALU.mult), reads=[Bpb2, Bgt], writes=[Bt2])
            m_, Bm = mgs.next()
            S.op("dve", lambda e, t1=t1, t2=t2, m_=m_: e.tensor_tensor(m_[:], t1[:], t2[:], ALU.add), reads=[Bt1, Bt2], writes=[Bm])
            S.dma("sp", A["MG"][c, :, g * 512:(g + 1) * 512], m_[:], reads=[Bm], writes=[B["MG"]], nowaw=True)


def phase3b_out(S, G):
    A = G["ap"]
    PB = G["pb"]
    B = G["B"]
    P = G["persist"]
    ident_f = G["ident_f"]
    Bw = S.buf("o_w")
    wr = S.sbuf("o_wr", [128, 16, 36], F32)
    S.dma("sp", wr[:], A["wr"].rearrange("(kc p) n -> p kc n", p=128), writes=[Bw], nowaw=True)
    brr = S.sbuf("o_brr", [1, 36], F32)
    S.dma("sp", brr[:], A["brr"], writes=[Bw], nowaw=True)
    g1 = S.sbuf("o_g1", [128, 2048], F32)
    b1 = S.sbuf("o_b1", [128, 2048], F32)
    S.dma("sp", g1[:], A["ln1_gb"], writes=[Bw], nowaw=True)
    S.dma("sp", b1[:], A["ln1_bb"], writes=[Bw], nowaw=True)
    ltri = S.sbuf("o_ltri", [128, 128], BF16)
    S.dma("sp", ltri[:], A["ltri"], writes=[Bw], nowaw=True)
    e256 = S.sbuf("o_e256", [128, 32], F32)
    S.dma("sp", e256[:], A["e256"], writes=[Bw], nowaw=True)
    onesc = S.sbuf("o_onesc", [128, 1], BF16)
    S.op("dve", lambda e: e.memset(onesc[:], 1.0), writes=[Bw])
    onesr = S.sbuf("o_onesr", [1, 128], F32)
    S.op("dve", lambda e: e.memset(onesr[:], 1.0), writes=[Bw])
    cnt = S.sbuf("o_cnt", [1, 32], F32)
    Bcnt = S.buf("o_cnt")
    S.op("dve", lambda e: e.memset(cnt[:], 0.0), writes=[Bcnt])
    S.barrier()
    wo = Rot(S, "o_wo", [128, 16, 512], BF16, 2)
    mgr = Rot(S, "o_mg", [128, 16, 512], BF16, 2)
    pre = Rot(S, "o_pre", [128, 2048], F32, 5)
    h1t = Rot(S, "o_h1", [128, 2048], F32, 2)
    h1b = Rot(S, "o_h1b", [128, 2048], BF16, 2)
    junk = S.sbuf("o_junk", [128, 2048], BF16)
    Bjunk = S.buf("o_junk")
    hT = Rot(S, "o_hT", [128, 16, 128], F32, 1)
    smalls = [S.sbuf("o_sm%d" % i, [128, 1], F32) for i in range(6)]
    Bsmall = S.buf("o_small")
    rt = {n: S.sbuf("o_r_" + n, shp, F32) for n, shp in (
        ("L", [128, 36]), ("gmax", [128, 1]), ("ngmax", [128, 1]), ("ohg", [128, 4]), ("ge", [128, 4]), ("gsum", [128, 1]), ("gp", [128, 1]),
        ("e8", [128, 8]), ("m1", [128, 1]), ("oh1", [128, 8]), ("e8b", [128, 8]), ("m2", [128, 1]), ("oh2", [128, 8]), ("d", [128, 1]),
        ("sg", [128, 1]), ("A1", [128, 32]), ("A2", [128, 32]), ("At", [128, 32]), ("rk", [128, 32]), ("t", [128, 32]), ("i1", [128, 1]), ("i2", [128, 1]))}
    Abf = S.sbuf("o_Abf", [128, 32], BF16)
    Br = G["Br_persist"]

    def rop(fn, eng="dve"):
        S.op(eng, fn, reads=[Br, Bw], writes=[Br])

    for g in range(4):
        mg, Bmg = mgr.next()
        S.dma("sp", mg[:], A["MG"][:, :, g * 512:(g + 1) * 512].rearrange("c p t -> p c t"), reads=[B["MG"]], writes=[Bmg])
        prs = []
        for t in range(4):
            tt = g * 4 + t
            pr, Bpr = pre.next()
            S.dma("sp", pr[:], A["xs"][OWN0 + tt * 128:OWN0 + (tt + 1) * 128, :], writes=[Bpr])
            prs.append((pr, Bpr))
        for cb in range(4):
            w_, Bwo = wo.next()
            S.dma("pool", w_[:], A["w_out"][:, cb * 512:(cb + 1) * 512].rearrange("(kc p) n -> p kc n", p=128), writes=[Bwo])
            for t in range(4):
                pr, Bpr = prs[t]
                po, Bpo = PB.next()
                for c in range(16):
                    S.op("pe", lambda e, po=po, c=c, t=t, w_=w_, mg=mg: e.matmul(po[:], mg[:, c, t * 128:(t + 1) * 128], w_[:, c, :], start=(c == 0), stop=(c == 15)),
                         reads=[Bmg, Bwo], writes=[Bpo])
                S.op("dve", lambda e, pr=pr, po=po, cb=cb: e.scalar_tensor_tensor(pr[:, cb * 512:(cb + 1) * 512], pr[:, cb * 512:(cb + 1) * 512], DN_ALPHA, po[:],
                                                                                op0=ALU.mult, op1=ALU.add), reads=[Bpo, Bpr], writes=[Bpr])
        for t in range(4):
            tt = g * 4 + t
            pr, Bpr = prs[t]
            h_, Bh = h1t.next()
            layer_norm_tile(S, G, pr, Bpr, h_, Bh, g1, b1, Bw, smalls, Bsmall, junk, Bjunk)
            S.dma("sp", A["H1"][tt * 128:(tt + 1) * 128, :], h_[:], reads=[Bh], writes=[B["H1"]], nowaw=True)
            hb, Bhb = h1b.next()
            S.op("act", lambda e, hb=hb, h_=h_: e.activation(hb[:], h_[:], AF.Copy), reads=[Bh], writes=[Bhb])
            S.dma("sp", A["H1b"][tt * 128:(tt + 1) * 128, :], hb[:], reads=[Bhb], writes=[B["H1b"]], nowaw=True)
            hT_, BhT = hT.next()
            for q4 in range(4):
                pb, Bpb = PB.next()
                for j in range(4):
                    c = q4 * 4 + j
                    S.op("pe", lambda e, pb=pb, h_=h_, c=c, j=j: e.matmul(pb[:, j * 128:(j + 1) * 128], h_[:, c * 128:(c + 1) * 128], ident_f[:], start=True, stop=True),
                         reads=[Bh, G["Bidf"]], writes=[Bpb])
                S.op("act", lambda e, pb=pb, hT_=hT_, q4=q4: e.activation(hT_[:, q4 * 4:(q4 + 1) * 4, :], pb[:].rearrange("p (a b) -> p a b", a=4), AF.Copy),
                     reads=[Bpb], writes=[BhT])
            pl, Bpl = PB.next()
            for c in range(16):
                S.op("pe", lambda e, pl=pl, hT_=hT_, c=c: e.matmul(pl[:, 0:36], hT_[:, c, :], wr[:, c, :], start=(c == 0), stop=False), reads=[BhT, Bw], writes=[Bpl])
            S.op("pe", lambda e, pl=pl: e.matmul(pl[:, 0:36], onesr[0:1, :], brr[0:1, :], start=False, stop=True), reads=[Bw], writes=[Bpl])
            L = rt["L"]
            S.op("dve", lambda e, pl=pl: e.tensor_copy(L[:], pl[:, 0:36]), reads=[Bpl], writes=[Br])
            rop(lambda e: e.reduce_max(rt["gmax"][:], L[:, 0:4], AX.X))
            rop(lambda e: e.tensor_scalar(rt["ohg"][:], L[:, 0:4], rt["gmax"][:, 0:1], None, op0=ALU.is_equal))
            rop(lambda e: e.tensor_scalar(rt["ngmax"][:], rt["gmax"][:], -1.0, None, op0=ALU.mult))
            rop(lambda e: e.activation(rt["ge"][:], L[:, 0:4], AF.Exp, bias=rt["ngmax"][:, 0:1], accum_out=rt["gsum"][:]), "act")
            rop(lambda e: e.reciprocal(rt["gp"][:], rt["gsum"][:]))
            rop(lambda e: e.tensor_scalar(rt["e8"][:], L[:, 4:12], rt["ohg"][:, 0:1], None, op0=ALU.mult))
            for gg in range(1, 4):
                rop(lambda e, gg=gg: e.scalar_tensor_tensor(rt["e8"][:], L[:, 4 + 8 * gg:12 + 8 * gg], rt["ohg"][:, gg:gg + 1], rt["e8"][:], op0=ALU.mult, op1=ALU.add))
            rop(lambda e: e.reduce_max(rt["m1"][:], rt["e8"][:], AX.X))
            rop(lambda e: e.tensor_scalar(rt["oh1"][:], rt["e8"][:], rt["m1"][:, 0:1], None, op0=ALU.is_equal))
            rop(lambda e: e.scalar_tensor_tensor(rt["e8b"][:], rt["oh1"][:], -1.0e30, rt["e8"][:], op0=ALU.mult, op1=ALU.add))
            rop(lambda e: e.reduce_max(rt["m2"][:], rt["e8b"][:], AX.X))
            rop(lambda e: e.tensor_scalar(rt["oh2"][:], rt["e8b"][:], rt["m2"][:, 0:1], None, op0=ALU.is_equal))
            rop(lambda e: e.tensor_tensor(rt["d"][:], rt["m1"][:], rt["m2"][:], ALU.subtract))
            rop(lambda e: e.activation(rt["sg"][:], rt["d"][:], AF.Sigmoid), "act")
            rop(lambda e, tt=tt: e.tensor_tensor(P["wts"][:, tt, 0:1], rt["sg"][:], rt["gp"][:], ALU.mult))
            rop(lambda e, tt=tt: e.tensor_tensor(P["wts"][:, tt, 1:2], rt["gp"][:], P["wts"][:, tt, 0:1], ALU.subtract))
            for gg in range(4):
                rop(lambda e, gg=gg: e.tensor_scalar(rt["A1"][:, gg * 8:(gg + 1) * 8], rt["oh1"][:], rt["ohg"][:, gg:gg + 1], None, op0=ALU.mult))
                rop(lambda e, gg=gg: e.tensor_scalar(rt["A2"][:, gg * 8:(gg + 1) * 8], rt["oh2"][:], rt["ohg"][:, gg:gg + 1], None, op0=ALU.mult))
            rop(lambda e: e.tensor_tensor(rt["At"][:], rt["A1"][:], rt["A2"][:], ALU.add))
            rop(lambda e: e.tensor_copy(Abf[:], rt["At"][:]))
            pk, Bpk = PB.next()
            S.op("pe", lambda e, pk=pk: e.matmul(pk[:, 0:32], ltri[:], Abf[:], start=True, stop=False), reads=[Bw, Br], writes=[Bpk])
            S.op("pe", lambda e, pk=pk: e.matmul(pk[:, 0:32], onesr[0:1, :], cnt[0:1, :], start=False, stop=True), reads=[Bw, Bcnt], writes=[Bpk])
            pc, Bpc = PB.next()
            S.op("pe", lambda e, pc=pc: e.matmul(pc[0:1, 0:32], onesc[:], Abf[:], start=True, stop=True), reads=[Bw, Br], writes=[Bpc])
            S.op("dve", lambda e, pk=pk: e.tensor_copy(rt["rk"][:], pk[:, 0:32]), reads=[Bpk, Br], writes=[Br])
            S.op("dve", lambda e, pc=pc: e.tensor_tensor(cnt[:], cnt[:], pc[0:1, 0:32], ALU.add), reads=[Bpc, Bcnt], writes=[Bcnt])
            rop(lambda e: e.scalar_tensor_tensor(rt["t"][:], rt["rk"][:], 1.0, rt["At"][:], op0=ALU.add, op1=ALU.mult))
            rop(lambda e, tt=tt: e.tensor_scalar(P["RK"][:, tt, :], rt["t"][:], -1.0, None, op0=ALU.add))
            rop(lambda e: e.tensor_tensor(rt["rk"][:], rt["rk"][:], e256[:], ALU.add))
            rop(lambda e: e.tensor_tensor(rt["t"][:], rt["rk"][:], rt["A1"][:], ALU.mult))
            rop(lambda e: e.reduce_sum(rt["i1"][:], rt["t"][:], AX.X))
            rop(lambda e: e.tensor_tensor(rt["t"][:], rt["rk"][:], rt["A2"][:], ALU.mult))
            rop(lambda e: e.reduce_sum(rt["i2"][:], rt["t"][:], AX.X))
            rop(lambda e, tt=tt: e.tensor_copy(P["idx"][:, tt, 0:1], rt["i1"][:]))
            rop(lambda e, tt=tt: e.tensor_copy(P["idx"][:, tt, 1:2], rt["i2"][:]))


def phase4_moe(S, G, experts=range(NEXP)):
    A = G["ap"]
    PB = G["pb"]
    B = G["B"]
    P = G["persist"]
    h1b = S.sbuf("m_h1b", [128, 16, 2048], BF16)
    Bh1b = S.buf("m_h1b")
    S.dma("sp", h1b[:], A["H1b"].rearrange("(t p) d -> p t d", p=128), reads=[B["H1b"]], writes=[Bh1b])
    iot = S.sbuf("m_iota", [128, CAP], F32)
    Biot = S.buf("m_iota")
    S.dma("sp", iot[:], A["iota256"], writes=[Biot])
    wu = Rot(S, "m_w", [128, 16, 512], BF16, 4)
    wd = Rot(S, "m_wd", [128, 4, 2048], BF16, 2)
    sel = Rot(S, "m_sel", [128, 16, CAP], BF16, 1)
    xs_ = Rot(S, "m_xs", [128, 16, CAP], BF16, 1)
    hT = Rot(S, "m_hT", [128, 8, CAP], BF16, 2)
    sg = Rot(S, "m_sg", [128, CAP], F32, 3)
    yst = Rot(S, "m_y", [128, 1024], F32, 1)
    for e_ in experts:
        s_, Bs = sel.next()
        for tt in range(16):
            S.op("dve", lambda e, s_=s_, tt=tt, e_=e_: e.tensor_scalar(s_[:, tt, :], iot[:], P["RK"][:, tt, e_:e_ + 1], None, op0=ALU.is_equal),
                 reads=[Biot, G["Br_persist"]], writes=[Bs])
        x_, Bx = xs_.next()
        for kc in range(16):
            pb, Bpb = PB.next()
            for tt in range(16):
                S.op("pe", lambda e, pb=pb, tt=tt, kc=kc, s_=s_: e.matmul(pb[:, 0:CAP], h1b[:, tt, kc * 128:(kc + 1) * 128], s_[:, tt, :],
                                                                         start=(tt == 0), stop=(tt == 15)), reads=[Bh1b, Bs], writes=[Bpb])
            if kc % 2 == 0:
                S.op("act", lambda e, pb=pb, x_=x_, kc=kc: e.activation(x_[:, kc, :], pb[:, 0:CAP], AF.Copy), reads=[Bpb], writes=[Bx])
            else:
                S.op("dve", lambda e, pb=pb, x_=x_, kc=kc: e.tensor_copy(x_[:, kc, :], pb[:, 0:CAP]), reads=[Bpb], writes=[Bx])
        h_, Bh = hT.next()
        for half in range(2):
            wg_, Bwg = wu.next()
            S.dma("pool", wg_[:], A["w_gate"][e_, :, half * 512:(half + 1) * 512].rearrange("(kc p) n -> p kc n", p=128), writes=[Bwg])
            wu_, Bwu = wu.next()
            S.dma("pool", wu_[:], A["w_up"][e_, :, half * 512:(half + 1) * 512].rearrange("(kc p) n -> p kc n", p=128), writes=[Bwu])
            for f4 in range(4):
                f = half * 4 + f4
                pg, Bpg = PB.next()
                for kc in range(16):
                    S.op("pe", lambda e, pg=pg, kc=kc, f4=f4, wg_=wg_, x_=x_: e.matmul(pg[:, 0:CAP], wg_[:, kc, f4 * 128:(f4 + 1) * 128], x_[:, kc, :],
                                                                                    start=(kc == 0), stop=(kc == 15)), reads=[Bwg, Bx], writes=[Bpg])
                for kc in range(16):
                    S.op("pe", lambda e, pg=pg, kc=kc, f4=f4, wu_=wu_, x_=x_: e.matmul(pg[:, CAP:2 * CAP], wu_[:, kc, f4 * 128:(f4 + 1) * 128], x_[:, kc, :],
                                                                                    start=(kc == 0), stop=(kc == 15)), reads=[Bwu, Bx], writes=[Bpg])
                s1, Bs1 = sg.next()
                S.op("act", lambda e, s1=s1, pg=pg: e.activation(s1[:], pg[:, 0:CAP], AF.Silu), reads=[Bpg], writes=[Bs1])
                S.op("dve", lambda e, h_=h_, f=f, s1=s1, pg=pg: e.tensor_tensor(h_[:, f, :], s1[:], pg[:, CAP:2 * CAP], ALU.mult), reads=[Bs1, Bpg], writes=[Bh])
        wds = []
        for half in range(2):
            wd_, Bwd = wd.next()
            S.dma("pool", wd_[:], A["w_down"][e_, half * 512:(half + 1) * 512, :].rearrange("(fc p) n -> p fc n", p=128), writes=[Bwd])
            wds.append((wd_, Bwd))
        for rh in range(CAP // 128):
            for cbp in range(2):
                y_, By = yst.next()
                for c2 in range(2):
                    cb = cbp * 2 + c2
                    py, Bpy = PB.next()
                    for f in range(8):
                        wd_, Bwd = wds[f // 4]
                        S.op("pe", lambda e, py=py, f=f, rh=rh, cb=cb, wd_=wd_, h_=h_: e.matmul(py[:], h_[:, f, rh * 128:(rh + 1) * 128], wd_[:, f % 4, cb * 512:(cb + 1) * 512],
                                                                                             start=(f == 0), stop=(f == 7)), reads=[Bh, Bwd], writes=[Bpy])
                    if c2 == 0:
                        S.op("act", lambda e, y_=y_, py=py, c2=c2: e.activation(y_[:, c2 * 512:(c2 + 1) * 512], py[:], AF.Copy), reads=[Bpy], writes=[By])
                    else:
                        S.op("dve", lambda e, y_=y_, py=py, c2=c2: e.tensor_copy(y_[:, c2 * 512:(c2 + 1) * 512], py[:]), reads=[Bpy], writes=[By])
                S.dma("sp", A["Y"][e_ * CAP + rh * 128:e_ * CAP + (rh + 1) * 128, cbp * 1024:(cbp + 1) * 1024], y_[:], reads=[By], writes=[B["Y"]], nowaw=True)


def phase5_final(S, G):
    A = G["ap"]
    B = G["B"]
    P = G["persist"]
    g2 = S.sbuf("f_g2", [128, 2048], F32)
    b2 = S.sbuf("f_b2", [128, 2048], F32)
    Bw = S.buf("f_w")
    S.dma("sp", g2[:], A["ln2_gb"], writes=[Bw], nowaw=True)
    S.dma("sp", b2[:], A["ln2_bb"], writes=[Bw], nowaw=True)
    idxi = S.sbuf("f_idx", [128, 16, 2], U32)
    Bidx = S.buf("f_idx")
    S.op("dve", lambda e: e.tensor_copy(idxi[:], P["idx"][:]), reads=[G["Br_persist"]], writes=[Bidx])
    S.barrier()
    h1 = Rot(S, "f_h1", [128, 2048], F32, 2)
    y1 = Rot(S, "f_y1", [128, 2048], F32, 2)
    y2 = Rot(S, "f_y2", [128, 2048], F32, 2)
    ot = Rot(S, "f_ot", [128, 2048], F32, 2)
    junk = S.sbuf("f_junk", [128, 2048], BF16)
    Bjunk = S.buf("f_junk")
    smalls = [S.sbuf("f_sm%d" % i, [128, 1], F32) for i in range(6)]
    Bsmall = S.buf("f_small")
    for tt in range(16):
        h_, Bh = h1.next()
        S.dma("sp", h_[:], A["H1"][tt * 128:(tt + 1) * 128, :], reads=[B["H1"]], writes=[Bh])
        a_, Ba = y1.next()
        b_, Bb = y2.next()
        for (dst, Bd, k) in ((a_, Ba, 0), (b_, Bb, 1)):
            S.dma("pool", None, None, reads=[B["Y"], Bidx], writes=[Bd],
                  builder=lambda e, dst=dst, tt=tt, k=k: e.indirect_dma_start(
                      out=dst[:], out_offset=None, in_=A["Y"], in_offset=bass.IndirectOffsetOnAxis(ap=idxi[:, tt, k:k + 1], axis=0),
                      bounds_check=NEXP * CAP - 1, oob_is_err=False))
        S.op("dve", lambda e, a_=a_, tt=tt: e.tensor_scalar(a_[:], a_[:], P["wts"][:, tt, 0:1], None, op0=ALU.mult), reads=[Ba, G["Br_persist"]], writes=[Ba])
        S.op("dve", lambda e, a_=a_, b_=b_, tt=tt: e.scalar_tensor_tensor(a_[:], b_[:], P["wts"][:, tt, 1:2], a_[:], op0=ALU.mult, op1=ALU.add),
             reads=[Ba, Bb, G["Br_persist"]], writes=[Ba])
        S.op("dve", lambda e, a_=a_, h_=h_: e.scalar_tensor_tensor(a_[:], h_[:], DN_ALPHA, a_[:], op0=ALU.mult, op1=ALU.add), reads=[Ba, Bh], writes=[Ba])
        o_, Bo = ot.next()
        layer_norm_tile(S, G, a_, Ba, o_, Bo, g2, b2, Bw, smalls, Bsmall, junk, Bjunk)
        S.dma("sp", A["out"][tt * 128:(tt + 1) * 128, :], o_[:], reads=[Bo], writes=[B["out"]], nowaw=True)

from contextlib import ExitStack
from concourse.bass_utils import run_bass_kernel_spmd

BFM_IDX = {"af": 0, "aq": 8, "ag": 16, "bk": 24, "bq": 32, "iq": 40, "ik": 44, "g": 45}
NBFM = 77

SCRATCH = {
    "KT": ([8, 128, 8192], "bf16"), "VH": ([8, 128, 64, 128], "bf16"), "KIT": ([128, 8192], "bf16"),
    "HG_kdec": ([8, 128, 8192], "bf16"), "HG_kend": ([8192, 1024], "bf16"), "HG_v": ([8192, 1024], "bf16"),
    "HG_dec": ([128, 8, 128], "f32"), "HG_qdec": ([8, 128, 2048], "bf16"), "HG_gs": ([8, 128, 2048], "bf16"),
    "QT": ([8, 128, 2048], "bf16"), "QIT": ([4, 128, 2048], "bf16"), "WI": ([2048, 8], "f32"),
    "GT": ([32, 128, 2048], "bf16"), "BA": ([8, 128, 2048], "bf16"), "BB": ([8, 128, 2048], "bf16"),
    "MG": ([16, 128, 2048], "bf16"), "H1": ([2048, 2048], "f32"), "H1b": ([2048, 2048], "bf16"), "Y": ([NEXP * CAP, 2048], "f32"),
}

INPUTS = {
    "xs": ([8192, 2048], "f32"), "valid_tm": ([128, 64], "f32"), "w_in": ([2048, 11848], "f32"),
    "ident": ([128, 128], "f32"), "bfm": ([128, NBFM], "f32"), "brow": ([1, 2056], "f32"),
    "lbl": ([128, 2, 8], "f32"), "normg": ([128, 8], "f32"), "rmask": ([128, 512], "f32"), "bdmask": ([128, 128], "f32"),
    "alb": ([128, 8, 64], "f32"), "corr": ([128, 8, 128], "bf16"), "sel2": ([2, 128], "bf16"), "dtab": ([128, 8192], "bf16"),
    "qrel": ([1, 512], "f32"), "adm": ([16, 2, 8192], "bf16"),
    "w_branch_a": ([1024, 2048], "f32"), "w_branch_b": ([1024, 2048], "f32"), "w_out": ([2048, 2048], "f32"),
    "wr": ([2048, 36], "f32"), "brr": ([1, 36], "f32"),
    "ln1_gb": ([128, 2048], "f32"), "ln1_bb": ([128, 2048], "f32"), "ln2_gb": ([128, 2048], "f32"), "ln2_bb": ([128, 2048], "f32"),
    "ltri": ([128, 128], "bf16"), "e256": ([128, 32], "f32"), "iota256": ([128, CAP], "f32"),
    "w_gate": ([NEXP, 2048, 1024], "f32"), "w_up": ([NEXP, 2048, 1024], "f32"), "w_down": ([NEXP, 1024, 2048], "f32"),
}


def _dt(s):
    return {"f32": F32, "bf16": BF16, "u32": U32, "i32": I32}[s]


def host_consts(inputs):
    b_in = np.asarray(inputs["b_in"][0], np.float32)
    bfm = np.zeros((128, NBFM), np.float32)
    def put(idx, c0, n):
        for c in range(n):
            bfm[:, idx + c] = b_in[c0 + c * 128: c0 + (c + 1) * 128]
    put(BFM_IDX["af"], C_AF, 8); put(BFM_IDX["aq"], C_AQ, 8); put(BFM_IDX["ag"], C_AG, 8)
    put(BFM_IDX["bk"], C_BK, 8); put(BFM_IDX["bq"], C_BQ, 8); put(BFM_IDX["iq"], C_IQ, 4)
    put(BFM_IDX["g"], C_G, 32)
    bfm[0:64, BFM_IDX["ik"]] = b_in[C_IK:C_IK + 64]
    bfm[64:128, BFM_IDX["ik"]] = b_in[C_IK:C_IK + 64]
    brow = np.concatenate([b_in[C_AI:C_AI + 1024], b_in[C_BV:C_BV + 1024], b_in[C_IW:C_IW + 8]])[None, :].astype(np.float32)
    lbl = np.ascontiguousarray(np.asarray(inputs["hg_lb_logits"], np.float32).reshape(2, 8, 128).transpose(2, 0, 1))
    normg = np.ascontiguousarray(np.asarray(inputs["hg_norm_g"][0], np.float32).reshape(8, 128).T)
    rmask = np.ones((128, 512), np.float32)
    rmask[:, ::64] = 0.0
    ii = np.arange(128)
    bdmask = ((ii[:, None] // 64 == ii[None, :] // 64) & (ii[:, None] <= ii[None, :])).astype(np.float32)
    import ml_dtypes
    bf = ml_dtypes.bfloat16
    slopes = 2.0 ** -(np.arange(8) + 1.0)
    pp = np.arange(128)
    alb = (slopes[None, :, None] * (pp[:, None, None] + 128.0 * (np.arange(64)[None, None, :] - 60))).astype(np.float32)
    dsq = np.maximum(pp[:, None] - pp[None, :], 0).astype(np.float64)
    corr = np.exp(-2.0 * slopes[None, :, None] * dsq[:, None, :]).astype(bf)
    sel2 = np.zeros((2, 128), np.float32); sel2[0, :64] = 1; sel2[1, 64:] = 1
    dtab = np.abs(8064 + pp[:, None] - np.arange(8192)[None, :]).astype(bf)
    qrel = np.arange(512, dtype=np.float32)[None, :]
    extra = {}
    if "w_out" in inputs:
        extra["w_branch_a"] = np.ascontiguousarray(inputs["w_branch_a"][0]); extra["w_branch_b"] = np.ascontiguousarray(inputs["w_branch_b"][0])
        extra["w_out"] = np.ascontiguousarray(inputs["w_out"][0])
        extra["wr"] = np.ascontiguousarray(np.concatenate([inputs["w_group"][0], inputs["w_router"][0]], axis=1).astype(np.float32))
        extra["brr"] = np.concatenate([inputs["b_group"][0], inputs["b_router"][0]])[None, :].astype(np.float32)
        for nm in ("ln1_g", "ln1_b", "ln2_g", "ln2_b"):
            extra[nm + "b"] = np.ascontiguousarray(np.broadcast_to(np.asarray(inputs[nm][0], np.float32)[None, :], (128, 2048)))
        extra["ltri"] = (pp[:, None] < pp[None, :]).astype(bf)
        extra["e256"] = np.ascontiguousarray(np.broadcast_to((np.arange(32, dtype=np.float32) * CAP)[None, :], (128, 32)))
        extra["iota256"] = np.ascontiguousarray(np.broadcast_to(np.arange(CAP, dtype=np.float32)[None, :], (128, CAP)))
        extra["w_gate"] = np.ascontiguousarray(inputs["w_gate"][0]); extra["w_up"] = np.ascontiguousarray(inputs["w_up"][0])
        extra["w_down"] = np.ascontiguousarray(inputs["w_down"][0])
    return {**extra, "alb": alb, "corr": corr, "sel2": sel2.astype(bf), "dtab": dtab, "qrel": qrel, "bdmask": bdmask, "ident": np.eye(128, dtype=np.float32), "bfm": bfm, "brow": brow, "lbl": lbl, "normg": normg, "rmask": rmask,
            "w_in": np.ascontiguousarray(inputs["w_in"][0])}


def host_core_inputs(inputs, hc, core):
    b, j = core // 4, core % 4
    x = np.asarray(inputs["x"], np.float32)
    xs = np.zeros((8192, 2048), np.float32)
    npre = (3 - j) * 2048
    xs[npre:] = x[b, :(j + 1) * 2048]
    valid = np.zeros(8192, np.float32)
    valid[npre:] = 1.0
    d = dict(hc)
    d["xs"] = xs
    d["valid_tm"] = np.ascontiguousarray(valid.reshape(64, 128).T)
    import ml_dtypes
    chunk = np.arange(8192) // 64
    adm = np.full((16, 2, 8192), NEG_ADM, np.float32)
    for t in range(16):
        c_first = (OWN0 + t * 128) // 64
        adm[t, 0, (valid > 0) & (chunk <= c_first)] = 0.0
        adm[t, 1, (valid > 0) & (chunk <= c_first + 1)] = 0.0
    d["adm"] = adm.astype(ml_dtypes.bfloat16)
    return d


def build_program(phases=("p1a",), dump=(), p1_blocks=(0, 1, 2, 3), p2_groups=(0, 1, 2, 3), in_names=None):
    nc = bass.Bass("TRN2", target_bir_lowering=False)
    A = {}
    used_inputs = in_names if in_names is not None else list(INPUTS)
    for n in used_inputs:
        shp, dt = INPUTS[n]
        A[n] = nc.dram_tensor(n, shp, _dt(dt), kind="ExternalInput").ap()
    for n, (shp, dt) in SCRATCH.items():
        kind = "ExternalOutput" if n in dump else "Internal"
        A[n] = nc.dram_tensor(n, shp, _dt(dt), kind=kind).ap()
    A["out"] = nc.dram_tensor("out", [2048, 2048], F32, kind="ExternalOutput").ap()
    with ExitStack() as es:
        S = Sched(nc, es)
        G = {"ap": A, "pb": PBanks(S), "B": {n: S.buf(n, glob=True) for n in list(SCRATCH) + ["out"]}, "bfm_idx": BFM_IDX}
        G["persist"] = {"wts": S.sbuf("p_wts", [128, 16, 2], F32), "RK": S.sbuf("p_RK", [128, 16, 32], F32), "idx": S.sbuf("p_idx", [128, 16, 2], F32)}
        G["Br_persist"] = S.buf("persist", glob=True)
        cst = {}
        Bc = S.buf("cst", glob=True)
        G["cst"] = cst
        G["Bcst"] = Bc
        idf = S.sbuf("idf", [128, 128], F32)
        Bidf = S.buf("idf", glob=True)
        S.dma("sp", idf[:], A["ident"], writes=[Bidf])
        idb = S.sbuf("idb", [128, 128], BF16)
        Bident = S.buf("idb")
        S.op("dve", lambda e: e.tensor_copy(idb[:], idf[:]), reads=[Bidf], writes=[Bident])
        G["ident_bf"] = idb
        G["Bident"] = Bident
        G["ident_f"] = idf
        G["Bidf"] = Bidf
        for n in ("bfm", "brow", "valid_tm", "normg", "rmask", "bdmask"):
            shp, dt = INPUTS[n]
            cst[n] = S.sbuf("c_" + n, shp, _dt(dt))
            S.dma("sp", cst[n][:], A[n], writes=[Bc], nowaw=True)
        lbl = S.sbuf("c_lbl", [128, 2, 8], F32)
        Blbl = S.buf("lbl", glob=True)
        S.dma("sp", lbl[:], A["lbl"], writes=[Blbl])
        for n in ("lb", "oml", "noml", "lbd"):
            cst[n] = S.sbuf("c_" + n, [128, 8], F32)
        cst["ones_row"] = S.sbuf("c_ones_row", [1, 128], F32)
        S.op("dve", lambda e: e.memset(cst["ones_row"][:], 1.0), writes=[Bc])
        S.op("dve", lambda e: e.tensor_tensor(cst["lbd"][:], lbl[:, 0, :], lbl[:, 1, :], ALU.subtract), reads=[Blbl], writes=[Bc])
        S.op("act", lambda e: e.activation(cst["lb"][:], cst["lbd"][:], AF.Sigmoid), reads=[Bc], writes=[Bc])
        S.op("dve", lambda e: e.tensor_scalar(cst["oml"][:], cst["lb"][:], -1.0, 1.0, op0=ALU.mult, op1=ALU.add), reads=[Bc], writes=[Bc])
        S.op("dve", lambda e: e.tensor_scalar(cst["noml"][:], cst["oml"][:], -1.0, None, op0=ALU.mult), reads=[Bc], writes=[Bc])

        cst["eps_ln"] = S.sbuf("c_eps_ln", [128, 1], F32)
        S.op("dve", lambda e: e.memset(cst["eps_ln"][:], LN_EPS), writes=[Bc])
        cst["eps_rms"] = S.sbuf("c_eps_rms", [128, 1], F32)
        S.op("dve", lambda e: e.memset(cst["eps_rms"][:], RMS_EPS), writes=[Bc])
        S.barrier()
        if "p1a" in phases:
            with ExitStack() as pes:
                S.es = pes
                phase1a(S, G, blocks=p1_blocks)
                S.es = es
            S.barrier()
            S.phase_end()
        if "p1b" in phases:
            with ExitStack() as pes:
                S.es = pes
                phase1b(S, G)
                S.es = es
            S.barrier()
            S.phase_end()
        if "p2" in phases:
            phase2_dsa(S, G, groups=p2_groups)
            S.barrier()
            S.phase_end()
        for nm, fn in (("p3a", phase3a_merge), ("p3b", phase3b_out), ("p4", phase4_moe), ("p5", phase5_final)):
            if nm in phases:
                with ExitStack() as pes:
                    S.es = pes
                    fn(S, G)
                    S.es = es
                S.barrier()
                S.phase_end()
        outs = [G["B"][n] for n in dump] + ([G["B"]["out"]] if "p5" in phases else [])
        S.wait_all("sp", outs)
        print("instructions:", S.ninst, {k: len(v) for k, v in S.ops.items()})
        S.run()
    return nc


ALL_PHASES = ("p1a", "p1b", "p2", "p3a", "p3b", "p4", "p5")
_CACHE = {}


def kernel(**inputs):
    if "nc" not in _CACHE:
        _CACHE["nc"] = build_program(phases=ALL_PHASES)
    nc = _CACHE["nc"]
    hc = host_consts(inputs)
    in_maps = [host_core_inputs(inputs, hc, c) for c in range(8)]
    res = run_bass_kernel_spmd(nc, in_maps, core_ids=list(range(8)))
    out = np.zeros((2, 8192, 2048), np.float32)
    for c in range(8):
        b, j = c // 4, c % 4
        out[b, j * 2048:(j + 1) * 2048] = np.asarray(res.results[c]["out"])
    return out
```

```python
import numpy as np
import concourse.bass as bass
import concourse.mybir as mybir

F32 = mybir.dt.float32
BF16 = mybir.dt.bfloat16
U32 = mybir.dt.uint32
I32 = mybir.dt.int32
U8 = mybir.dt.uint8
AF = mybir.ActivationFunctionType
ALU = mybir.AluOpType
AX = mybir.AxisListType


class Buf:
    __slots__ = ("name", "lastw", "readers", "dsem", "dcount", "glob", "dkey")

    def __init__(self, name, glob=False):
        self.name = name
        self.glob = glob
        self.dkey = None
        self.lastw = None
        self.readers = {}
        self.dsem = None
        self.dcount = 0


class Sched:
    ENGS = ("pe", "act", "dve", "pool", "sp")
    SEM_LIMIT = 30000

    def __init__(self, nc, es):
        self.nc = nc
        self.es = es
        self.es_sem = es
        self.dbufs = []
        self.dstate = {}
        self.free_dsems = []
        self.local_dbufs = []
        self.sem = {}
        self.count = {}
        self.known = {}
        self.ops = {}
        self.epoch = {}
        for n in self.ENGS:
            self.sem[n] = es.enter_context(nc.semaphore("se_" + n))
            self.count[n] = 0
            self.epoch[n] = 0
            self.known[n] = {}
            self.ops[n] = []
        self.nbuf = 0
        self.ninst = 0

    def sbuf(self, name, shape, dtype):
        self.nbuf += 1
        name = "%s_u%d" % (name, self.nbuf)
        return self.es.enter_context(self.nc.sbuf_tensor(name, list(shape), dtype))

    def psum(self, name, shape, dtype):
        return self.es.enter_context(self.nc.psum_tensor(name, list(shape), dtype))

    def buf(self, name=None, glob=False):
        self.nbuf += 1
        return Buf("%s_b%d" % (name or "b", self.nbuf), glob)

    def bufs(self, n, name="b"):
        return [self.buf("%s%d" % (name, i)) for i in range(n)]

    def _waits(self, eng, reads, writes):
        need = {}

        def add(ev, skip_same):
            if ev is None:
                return
            key, sem, val, prod = ev
            if skip_same and prod == eng:
                return
            if self.known[eng].get(key, 0) >= val:
                return
            if key not in need or need[key][1] < val:
                need[key] = (sem, val)

        for b in reads:
            add(b.lastw, False)
        for b in writes:
            add(b.lastw, True)
            for ev in b.readers.values():
                add(ev, True)
        for key, (sem, val) in need.items():
            self.known[eng][key] = val
        return list(need.values())

    def op(self, eng, fn, reads=(), writes=()):
        waits = self._waits(eng, reads, writes)
        if self.count[eng] >= self.SEM_LIMIT:
            self.epoch[eng] += 1
            self.count[eng] = 0
            self.sem[eng] = self.es_sem.enter_context(self.nc.semaphore("se_%s_%d" % (eng, self.epoch[eng])))
        self.count[eng] += 1
        seq = self.count[eng]
        sem = self.sem[eng]
        key = "e_%s_%d" % (eng, self.epoch[eng])
        ev = (key, sem, seq, eng)
        for b in writes:
            b.lastw = ev
            b.readers = {}
        for b in reads:
            b.readers[key] = ev
        self.ninst += 1 + len(waits)

        def emit(e, fn=fn, waits=waits, sem=sem):
            for (s, v) in waits:
                e.wait_ge(s, v)
            fn(e).then_inc(sem, 1)

        self.ops[eng].append(emit)
        return ev

    def dma(self, q, out_ap, in_ap, reads=(), writes=(), nowaw=False, builder=None, **kw):
        waits = self._waits(q, reads, [] if nowaw else writes)
        tb = writes[0]
        if tb.dsem is None:
            if (not tb.glob) and self.free_dsems:
                tb.dsem, tb.dcount, tb.dkey = self.free_dsems.pop()
            else:
                tb.dsem = self.es_sem.enter_context(self.nc.semaphore("sd_" + tb.name))
                tb.dkey = "d_" + tb.name
            if not tb.glob:
                self.local_dbufs.append(tb)
        tb.dcount += 16
        self.dstate[tb.dkey] = (tb.dsem, tb.dcount)
        ev = (tb.dkey, tb.dsem, tb.dcount, None)
        for b in writes:
            b.lastw = ev
            if not nowaw:
                b.readers = {}
        for b in reads:
            b.readers[ev[0]] = ev
        self.ninst += 1 + len(waits)

        def emit(e, waits=waits, sem=tb.dsem, out_ap=out_ap, in_ap=in_ap, kw=kw, builder=builder):
            for (s, v) in waits:
                e.wait_ge(s, v)
            if builder is not None:
                builder(e).then_inc(sem, 16)
            else:
                e.dma_start(out=out_ap, in_=in_ap, **kw).then_inc(sem, 16)

        self.ops[q].append(emit)
        return ev

    def phase_end(self):
        for b in self.local_dbufs:
            self.free_dsems.append((b.dsem, b.dcount, b.dkey))
            b.dsem = None
        self.local_dbufs = []

    def raw(self, eng, fn):
        self.ops[eng].append(lambda e, fn=fn: fn(e))

    def wait_all(self, eng, bufs):
        waits = self._waits(eng, list(bufs), [])

        def emit(e, waits=waits):
            for (s, v) in waits:
                e.wait_ge(s, v)

        self.ops[eng].append(emit)

    def barrier(self):
        evs = []
        for n in self.ENGS:
            if self.count[n] > 0:
                evs.append(("e_%s_%d" % (n, self.epoch[n]), self.sem[n], self.count[n], n))
        for key, (sem, cnt) in self.dstate.items():
            evs.append((key, sem, cnt, None))
        for eng in self.ENGS:
            waits = []
            for (key, sem, val, prod) in evs:
                if prod == eng:
                    continue
                if self.known[eng].get(key, 0) >= val:
                    continue
                self.known[eng][key] = val
                waits.append((sem, val))

            def emit(e, waits=waits):
                for (s, v) in waits:
                    e.wait_ge(s, v)

            self.ops[eng].append(emit)

    def run(self):
        nc = self.nc
        ops = self.ops
        with nc.Block() as block:
            @block.tensor
            def _(e):
                for f in ops["pe"]:
                    f(e)

            @block.scalar
            def _(e):
                for f in ops["act"]:
                    f(e)

            @block.vector
            def _(e):
                for f in ops["dve"]:
                    f(e)

            @block.gpsimd
            def _(e):
                for f in ops["pool"]:
                    f(e)

            @block.sync
            def _(e):
                for f in ops["sp"]:
                    f(e)

NSLOT = 8192
NOWN = 2048
OWN0 = NSLOT - NOWN
D = 2048
KC = 16
C_AQ, C_AF, C_AI, C_AG, C_BQ, C_BK, C_BV, C_IQ, C_IK, C_IW, C_G = 0, 1024, 2048, 3072, 4096, 5120, 6144, 7168, 7680, 7744, 7752
W_SCALE = (8 ** -0.5) * (64 ** -0.5)


class Rot:
    def __init__(self, S, name, shape, dtype, n):
        self.t = [S.sbuf("%s%d" % (name, i), shape, dtype) for i in range(n)]
        self.b = [S.buf("%s%d" % (name, i)) for i in range(n)]
        self.i = 0
        self.n = n

    def next(self):
        k = self.i % self.n
        self.i += 1
        return self.t[k], self.b[k]


class PBanks:
    def __init__(self, S):
        self.t = [S.psum("pb%d" % i, [128, 512], F32) for i in range(8)]
        self.b = [S.buf("pb%d" % i) for i in range(8)]
        self.i = 0

    def next(self, lo=0, hi=8):
        n = hi - lo
        k = lo + (self.i % n)
        self.i += 1
        return self.t[k], self.b[k]


def phase1a(S, G, blocks=(0, 1, 2, 3)):
    A = G["ap"]
    PB = G["pb"]
    ident = G["ident_bf"]
    Bident = G["Bident"]
    w_in = A["w_in"]
    xT = S.sbuf("xT", [128, KC, 2048], BF16)
    BxT = S.bufs(16, "xT")
    xin = Rot(S, "xin", [128, 2048], BF16, 2)
    wt = Rot(S, "wt", [128, KC, 512], BF16, 2)
    f32t = Rot(S, "hgf", [128, 512], F32, 12)
    st16 = Rot(S, "st16", [128, 512], BF16, 8)
    stkT = Rot(S, "stkT", [128, 4, 128], BF16, 2)
    decst = S.sbuf("decst", [128, 8, 32], F32)
    Bdec = S.buf("decst")
    wist = Rot(S, "wist", [128, 8], F32, 2)
    cst = G["cst"]
    Bc = G["Bcst"]
    ones_row_b = S.sbuf("ones_row_b", [1, 128], BF16)
    brow_b = S.sbuf("brow_b", [1, 2056], BF16)
    S.op("dve", lambda e: e.memset(ones_row_b[:], 1.0), writes=[Bc])
    S.op("dve", lambda e: e.tensor_copy(brow_b[:], cst["brow"][:]), reads=[Bc], writes=[Bc])
    evq = [0]

    def evac_engine():
        evq[0] += 1
        return "act" if evq[0] % 2 else "dve"

    def copy_op(eng, out_ap, in_ap, reads, writes):
        if eng == "act":
            S.op("act", lambda e, out_ap=out_ap, in_ap=in_ap: e.activation(out_ap, in_ap, AF.Copy), reads=reads, writes=writes)
        else:
            S.op("dve", lambda e, out_ap=out_ap, in_ap=in_ap: e.tensor_copy(out_ap, in_ap), reads=reads, writes=writes)

    for blk in blocks:
        own = (blk == 3)
        s0 = blk * 2048
        for t in range(16):
            xi, Bxi = xin.next()
            S.dma("pool", xi[:], A["xs"][s0 + t * 128: s0 + (t + 1) * 128, :], writes=[Bxi])
            for q4 in range(4):
                pb, Bpb = PB.next()
                for j in range(4):
                    kc = q4 * 4 + j
                    S.op("pe", lambda e, pb=pb, xi=xi, kc=kc, j=j: e.matmul(
                        pb[:, j * 128:(j + 1) * 128], xi[:, kc * 128:(kc + 1) * 128], ident[:], start=True, stop=True),
                        reads=[Bxi, Bident], writes=[Bpb])
                copy_op(evac_engine(), xT[:, q4 * 4:(q4 + 1) * 4, t * 128:(t + 1) * 128],
                        pb[:].rearrange("p (a b) -> p a b", a=4), [Bpb], [BxT[t]])

        def load_w(cols):
            w, Bw = wt.next()
            off = 0
            for (c0, n) in cols:
                S.dma("pool", w[:, :, off:off + n], w_in[:, c0:c0 + n].rearrange("(kc p) n -> p kc n", p=128), writes=[Bw])
                off += n
            return w, Bw

        def fm_mm(w, Bw, m, g):
            pb, Bpb = PB.next()
            for kc in range(KC):
                S.op("pe", lambda e, pb=pb, w=w, kc=kc, m=m, g=g: e.matmul(
                    pb[:], w[:, kc, m * 128:(m + 1) * 128], xT[:, kc, g * 512:(g + 1) * 512], start=(kc == 0), stop=(kc == KC - 1)),
                    reads=[Bw] + BxT[g * 4:(g + 1) * 4], writes=[Bpb])
            return pb, Bpb

        def simple_fm(w, Bw, m, g, bias_ap, func, dst_ap, Bd, rows=128):
            pb, Bpb = fm_mm(w, Bw, m, g)
            st, Bst = st16.next()
            S.op("act", lambda e, st=st, pb=pb, func=func, bias_ap=bias_ap: e.activation(st[:], pb[:], func, bias=bias_ap), reads=[Bpb, Bc], writes=[Bst])
            S.dma("sp", dst_ap, st[0:rows, :], reads=[Bst], writes=[Bd], nowaw=True)

        tm_units = [("ai", C_AI), ("ai", C_AI + 512), ("bv", C_BV), ("bv", C_BV + 512)]
        for (kind, c0) in tm_units:
            w, Bw = load_w([(c0, 512)])
            for t in range(16):
                pb, Bpb = PB.next()
                for kc in range(KC):
                    S.op("pe", lambda e, pb=pb, w=w, kc=kc, t=t: e.matmul(
                        pb[:], xT[:, kc, t * 128:(t + 1) * 128], w[:, kc, :], start=(kc == 0), stop=False),
                        reads=[Bw, BxT[t]], writes=[Bpb])
                boff = (c0 - C_AI) if kind == "ai" else (1024 + c0 - C_BV)
                S.op("pe", lambda e, pb=pb, boff=boff: e.matmul(pb[:], ones_row_b[0:1, :], brow_b[0:1, boff:boff + 512],
                                                            start=False, stop=True), reads=[Bc], writes=[Bpb])
                st, Bst = st16.next()
                tt = blk * 16 + t
                if kind == "ai":
                    S.op("act", lambda e, st=st, pb=pb, tt=tt: e.activation(st[:], pb[:], AF.Identity, scale=cst["valid_tm"][:, tt:tt + 1]),
                         reads=[Bpb, Bc], writes=[Bst])
                    dst = A["HG_v"][s0 + t * 128:s0 + (t + 1) * 128, c0 - C_AI:c0 - C_AI + 512]
                else:
                    S.op("dve", lambda e, st=st, pb=pb: e.tensor_copy(st[:], pb[:]), reads=[Bpb], writes=[Bst])
                    h0 = (c0 - C_BV) // 128
                    dst = A["VH"][h0:h0 + 4, :, tt, :].rearrange("h p d -> p h d")
                if kind == "ai":
                    S.dma("sp", dst, st[:], reads=[Bst], writes=[G["B"]["HG_v"]], nowaw=True)
                else:
                    S.dma("sp", dst, st[:].rearrange("p (h d) -> p h d", h=4), reads=[Bst], writes=[G["B"]["VH"]], nowaw=True)
        if own:
            w, Bw = load_w([(C_IW, 8)])
            for t in range(16):
                pb, Bpb = PB.next()
                for kc in range(KC):
                    S.op("pe", lambda e, pb=pb, w=w, kc=kc, t=t: e.matmul(
                        pb[:, 0:8], xT[:, kc, t * 128:(t + 1) * 128], w[:, kc, 0:8], start=(kc == 0), stop=False),
                        reads=[Bw, BxT[t]], writes=[Bpb])
                S.op("pe", lambda e, pb=pb: e.matmul(pb[:, 0:8], ones_row_b[0:1, :], brow_b[0:1, 2048:2056],
                                                     start=False, stop=True), reads=[Bc], writes=[Bpb])
                st, Bst = wist.next()
                S.op("dve", lambda e, st=st, pb=pb: e.tensor_scalar(st[:], pb[:, 0:8], W_SCALE, None, op0=ALU.mult), reads=[Bpb], writes=[Bst])
                S.dma("sp", A["WI"][t * 128:(t + 1) * 128, :], st[:], reads=[Bst], writes=[G["B"]["WI"]], nowaw=True)

        for u in range(2):
            w, Bw = load_w([(C_BK + u * 512, 512)])
            for m in range(4):
                h = u * 4 + m
                for g in range(4):
                    simple_fm(w, Bw, m, g, cst["bfm"][:, G["bfm_idx"]["bk"] + h: G["bfm_idx"]["bk"] + h + 1], AF.Identity,
                              A["KT"][h, :, s0 + g * 512:s0 + (g + 1) * 512], G["B"]["KT"])
        w, Bw = load_w([(C_IK, 64), (C_IK, 64)])
        for g in range(4):
            simple_fm(w, Bw, 0, g, cst["bfm"][:, G["bfm_idx"]["ik"]: G["bfm_idx"]["ik"] + 1], AF.Identity,
                      A["KIT"][:, s0 + g * 512:s0 + (g + 1) * 512], G["B"]["KIT"])
        if own:
            for u in range(2):
                w, Bw = load_w([(C_BQ + u * 512, 512)])
                for m in range(4):
                    h = u * 4 + m
                    for g in range(4):
                        simple_fm(w, Bw, m, g, cst["bfm"][:, G["bfm_idx"]["bq"] + h: G["bfm_idx"]["bq"] + h + 1], AF.Identity,
                                  A["QT"][h, :, g * 512:(g + 1) * 512], G["B"]["QT"])
            w, Bw = load_w([(C_IQ, 512)])
            for m in range(4):
                for g in range(4):
                    simple_fm(w, Bw, m, g, cst["bfm"][:, G["bfm_idx"]["iq"] + m: G["bfm_idx"]["iq"] + m + 1], AF.Identity,
                              A["QIT"][m, :, g * 512:(g + 1) * 512], G["B"]["QIT"])
            for u in range(8):
                w, Bw = load_w([(C_G + u * 512, 512)])
                for m in range(4):
                    c = u * 4 + m
                    for g in range(4):
                        simple_fm(w, Bw, m, g, cst["bfm"][:, G["bfm_idx"]["g"] + c: G["bfm_idx"]["g"] + c + 1], AF.Sigmoid,
                                  A["GT"][c, :, g * 512:(g + 1) * 512], G["B"]["GT"])

        deferred = []
        for h in range(8):
            if own:
                w, Bw = load_w([(C_AF + h * 128, 128), (C_AQ + h * 128, 128), (C_AG + h * 128, 128)])
            else:
                if h % 4 == 0:
                    w4, Bw4 = load_w([(C_AF + h * 128, 512)])
                w, Bw = w4, Bw4
            mf = 0 if own else (h % 4)
            bi = G["bfm_idx"]
            oml_h = cst["oml"][:, h:h + 1]
            noml_h = cst["noml"][:, h:h + 1]
            lb_h = cst["lb"][:, h:h + 1]
            b_af = cst["bfm"][:, bi["af"] + h: bi["af"] + h + 1]
            b_aq = cst["bfm"][:, bi["aq"] + h: bi["aq"] + h + 1]
            b_ag = cst["bfm"][:, bi["ag"] + h: bi["ag"] + h + 1]
            for g in range(4):
                pb, Bpb = fm_mm(w, Bw, mf, g)
                while deferred:
                    deferred.pop(0)()
                sg, Bsg = f32t.next()
                S.op("act", lambda e, sg=sg, pb=pb, b_af=b_af: e.activation(sg[:], pb[:], AF.Sigmoid, bias=b_af),
                     reads=[Bpb, Bc], writes=[Bsg])
                lf, Blf = f32t.next()
                S.op("act", lambda e, lf=lf, sg=sg, oml_h=oml_h, lb_h=lb_h: e.activation(lf[:], sg[:], AF.Ln, scale=oml_h, bias=lb_h),
                     reads=[Bsg, Bc], writes=[Blf])
                cum, Bcum = f32t.next()
                S.op("dve", lambda e, cum=cum, lf=lf: e.tensor_tensor_scan(cum[:], cst["rmask"][:], lf[:], 0.0, ALU.mult, ALU.add),
                     reads=[Blf, Bc], writes=[Bcum])
                en, Ben = f32t.next()
                S.op("act", lambda e, en=en, cum=cum: e.activation(en[:], cum[:], AF.Exp, scale=-1.0), reads=[Bcum], writes=[Ben])
                kk, Bkk = f32t.next()
                S.op("dve", lambda e, kk=kk, sg=sg, noml_h=noml_h, oml_h=oml_h: e.tensor_scalar(kk[:], sg[:], noml_h, oml_h, op0=ALU.mult, op1=ALU.add),
                     reads=[Bsg, Bc], writes=[Bkk])
                kd, Bkd = st16.next()
                S.op("dve", lambda e, kd=kd, kk=kk, en=en: e.tensor_tensor(kd[:], kk[:], en[:], ALU.mult), reads=[Bkk, Ben], writes=[Bkd])
                S.dma("sp", A["HG_kdec"][h, :, s0 + g * 512:s0 + (g + 1) * 512], kd[:], reads=[Bkd], writes=[G["B"]["HG_kdec"]], nowaw=True)
                ex2, Bex2 = f32t.next()
                for c in range(8):
                    S.op("act", lambda e, ex2=ex2, cum=cum, c=c: e.activation(ex2[:, c * 64:(c + 1) * 64], cum[:, c * 64:(c + 1) * 64], AF.Exp,
                                                                           scale=-1.0, bias=cum[:, c * 64 + 63:c * 64 + 64]),
                         reads=[Bcum], writes=[Bex2])
                ke, Bke = st16.next()
                S.op("dve", lambda e, ke=ke, kk=kk, ex2=ex2: e.tensor_tensor(ke[:], kk[:], ex2[:], ALU.mult), reads=[Bkk, Bex2], writes=[Bke])
                dec_ap = decst[:, h, g * 8:(g + 1) * 8]
                S.op("act", lambda e, cum=cum, dec_ap=dec_ap: e.activation(dec_ap, cum[:].rearrange("p (c s) -> p c s", s=64)[:, :, 63], AF.Exp),
                     reads=[Bcum], writes=[Bdec])
                def kend_T(ke=ke, Bke=Bke, g=g, h=h, s0=s0):
                    pbt, Bpbt = PB.next()
                    for j in range(4):
                        S.op("pe", lambda e, pbt=pbt, ke=ke, j=j: e.matmul(pbt[:, j * 128:(j + 1) * 128], ke[:, j * 128:(j + 1) * 128], ident[:],
                                                                           start=True, stop=True), reads=[Bke, Bident], writes=[Bpbt])
                    kT, BkT = stkT.next()
                    S.op("dve", lambda e, kT=kT, pbt=pbt: e.tensor_copy(kT[:], pbt[:].rearrange("p (a b) -> p a b", a=4)), reads=[Bpbt], writes=[BkT])
                    S.dma("sp", A["HG_kend"][s0 + g * 512:s0 + (g + 1) * 512, h * 128:(h + 1) * 128].rearrange("(t p) d -> p t d", p=128),
                          kT[:], reads=[BkT], writes=[G["B"]["HG_kend"]], nowaw=True)
                deferred.append(kend_T)
                if own:
                    ec, Bec = f32t.next()
                    S.op("act", lambda e, ec=ec, cum=cum: e.activation(ec[:], cum[:], AF.Exp), reads=[Bcum], writes=[Bec])
                    pbq, Bpbq = fm_mm(w, Bw, 1, g)
                    qs, Bqs = f32t.next()
                    S.op("act", lambda e, qs=qs, pbq=pbq, b_aq=b_aq: e.activation(qs[:], pbq[:], AF.Silu, bias=b_aq),
                         reads=[Bpbq, Bc], writes=[Bqs])
                    qd, Bqd = st16.next()
                    S.op("dve", lambda e, qd=qd, qs=qs, ec=ec: e.tensor_tensor(qd[:], qs[:], ec[:], ALU.mult), reads=[Bqs, Bec], writes=[Bqd])
                    S.dma("sp", A["HG_qdec"][h, :, g * 512:(g + 1) * 512], qd[:], reads=[Bqd], writes=[G["B"]["HG_qdec"]], nowaw=True)
                    simple_fm(w, Bw, 2, g, b_ag, AF.Silu, A["HG_gs"][h, :, g * 512:(g + 1) * 512], G["B"]["HG_gs"])
        while deferred:
            deferred.pop(0)()
        S.dma("sp", A["HG_dec"][:, :, blk * 32:(blk + 1) * 32], decst[:], reads=[Bdec], writes=[G["B"]["HG_dec"]], nowaw=True)

RMS_EPS = 1e-6


def phase1b(S, G):
    A = G["ap"]
    PB = G["pb"]
    cst = G["cst"]
    Bc = G["Bcst"]
    B = G["B"]
    kend = S.sbuf("hs_kend", [128, 16, 1024], BF16)
    Bkend = S.buf("hs_kend")
    vv = S.sbuf("hs_v", [128, 16, 1024], BF16)
    Bvv = S.buf("hs_v")
    dec = S.sbuf("hs_dec", [128, 8, 128], F32)
    Bdec = S.buf("hs_dec")
    Sf = S.sbuf("hs_S", [128, 8, 128], F32)
    Sb = S.sbuf("hs_Sb", [128, 8, 128], BF16)
    BS = S.bufs(8, "hs_S")
    BSb = S.bufs(8, "hs_Sb")
    S.dma("sp", dec[:], A["HG_dec"], reads=[B["HG_dec"]], writes=[Bdec])
    S.op("dve", lambda e: e.memset(Sf[:], 0.0), writes=BS)
    S.op("dve", lambda e: e.memset(Sb[:], 0.0), writes=BSb)
    onesb = S.sbuf("hs_ones", [128, 128], BF16)
    Bones = S.buf("hs_ones")
    S.op("dve", lambda e: e.memset(onesb[:], 1.0 / 128.0), writes=[Bones])
    kdec = Rot(S, "hs_kdec", [128, 2048], BF16, 8)
    qdec = Rot(S, "hs_qdec", [128, 2048], BF16, 8)
    gs = Rot(S, "hs_gs", [128, 2048], BF16, 8)
    attm = Rot(S, "hs_attm", [128, 128], BF16, 8)
    sq = Rot(S, "hs_sq", [128, 128], BF16, 6)
    rs = Rot(S, "hs_rs", [128, 128], F32, 6)
    yy = Rot(S, "hs_y", [128, 128], F32, 6)
    bastr = Rot(S, "hs_bast", [128, 8, 128], BF16, 2)

    def state_update(t, h, half, ):
        p0 = half * 64
        chunk = None
        pk, Bpk = PB.next()
        S.op("pe", lambda e, pk=pk, t=t, h=h, p0=p0: e.matmul(pk[:, 0:128], kend[p0:p0 + 64, t, h * 128:(h + 1) * 128],
                                                            vv[p0:p0 + 64, t, h * 128:(h + 1) * 128], start=True, stop=True),
             reads=[Bkend, Bvv], writes=[Bpk])
        return pk, Bpk

    for blk in range(4):
        own = (blk == 3)
        s0 = blk * 2048
        S.dma("sp", kend[:], A["HG_kend"][s0:s0 + 2048, :].rearrange("(t p) c -> p t c", p=128), reads=[B["HG_kend"]], writes=[Bkend])
        S.dma("sp", vv[:], A["HG_v"][s0:s0 + 2048, :].rearrange("(t p) c -> p t c", p=128), reads=[B["HG_v"]], writes=[Bvv])
        if not own:
            for t in range(16):
                for half in range(2):
                    ch = blk * 32 + t * 2 + half
                    for h in range(8):
                        pk, Bpk = state_update(t, h, half)
                        S.op("dve", lambda e, pk=pk, h=h, ch=ch: e.scalar_tensor_tensor(Sf[:, h, :], Sf[:, h, :], dec[:, h, ch:ch + 1], pk[:, 0:128],
                                                                                      op0=ALU.mult, op1=ALU.add),
                             reads=[Bpk, Bdec, BS[h]], writes=[BS[h]])
            if blk == 2:
                for h in range(8):
                    S.op("act", lambda e, h=h: e.activation(Sb[:, h, :], Sf[:, h, :], AF.Copy), reads=[BS[h]], writes=[BSb[h]])
            continue
        kds, qds, ggs = [], [], []
        for h in range(8):
            kd, Bkd = kdec.next()
            qd, Bqd = qdec.next()
            gg, Bgg = gs.next()
            S.dma("sp", kd[:], A["HG_kdec"][h, :, s0:s0 + 2048], reads=[B["HG_kdec"]], writes=[Bkd])
            S.dma("sp", qd[:], A["HG_qdec"][h], reads=[B["HG_qdec"]], writes=[Bqd])
            S.dma("sp", gg[:], A["HG_gs"][h], reads=[B["HG_gs"]], writes=[Bgg])
            kds.append((kd, Bkd)); qds.append((qd, Bqd)); ggs.append((gg, Bgg))
        for t in range(16):
            tc = slice(t * 128, (t + 1) * 128)
            bt, Bbt = bastr.next()
            for h in range(8):
                kd, Bkd = kds[h]
                qd, Bqd = qds[h]
                gg, Bgg = ggs[h]
                pa, Bpa = PB.next()
                S.op("pe", lambda e, pa=pa, kd=kd, qd=qd, tc=tc: e.matmul(pa[:, 0:128], kd[:, tc], qd[:, tc], start=True, stop=True),
                     reads=[Bkd, Bqd], writes=[Bpa])
                am, Bam = attm.next()
                S.op("dve", lambda e, am=am, pa=pa: e.tensor_tensor(am[:], pa[:, 0:128], cst["bdmask"][:], ALU.mult), reads=[Bpa, Bc], writes=[Bam])
                po, Bpo = PB.next()
                S.op("pe", lambda e, po=po, am=am, t=t, h=h: e.matmul(po[:, 0:128], vv[:, t, h * 128:(h + 1) * 128], am[:], start=True, stop=False),
                     reads=[Bvv, Bam], writes=[Bpo])
                for half in range(2):
                    ch = blk * 32 + t * 2 + half
                    c0 = t * 128 + half * 64
                    S.op("pe", lambda e, po=po, qd=qd, h=h, c0=c0, half=half: e.matmul(po[:, half * 64:(half + 1) * 64], Sb[:, h, :], qd[:, c0:c0 + 64],
                                                                                  start=False, stop=(half == 1)),
                         reads=[BSb[h], Bqd], writes=[Bpo])
                    pk, Bpk = state_update(t, h, half)
                    S.op("dve", lambda e, pk=pk, h=h, ch=ch: e.scalar_tensor_tensor(Sf[:, h, :], Sf[:, h, :], dec[:, h, ch:ch + 1], pk[:, 0:128],
                                                                                  op0=ALU.mult, op1=ALU.add),
                         reads=[Bpk, Bdec, BS[h]], writes=[BS[h]])
                    S.op("act", lambda e, h=h: e.activation(Sb[:, h, :], Sf[:, h, :], AF.Copy), reads=[BS[h]], writes=[BSb[h]])
                q2, Bq2 = sq.next()
                S.op("act", lambda e, q2=q2, po=po: e.activation(q2[:], po[:, 0:128], AF.Square), reads=[Bpo], writes=[Bq2])
                pm, Bpm = PB.next()
                S.op("pe", lambda e, pm=pm, q2=q2: e.matmul(pm[:, 0:128], onesb[:], q2[:], start=True, stop=True), reads=[Bones, Bq2], writes=[Bpm])
                r1, Br1 = rs.next()
                S.op("act", lambda e, r1=r1, pm=pm: e.activation(r1[:], pm[:, 0:128], AF.Sqrt, bias=cst["eps_rms"][:, 0:1]), reads=[Bpm, Bc], writes=[Br1])
                S.op("dve", lambda e, r1=r1: e.reciprocal(r1[:], r1[:]), reads=[Br1], writes=[Br1])
                y1, By1 = yy.next()
                S.op("dve", lambda e, y1=y1, po=po, r1=r1: e.tensor_tensor(y1[:], po[:, 0:128], r1[:], ALU.mult), reads=[Bpo, Br1], writes=[By1])
                S.op("dve", lambda e, y1=y1, gg=gg, tc=tc, h=h, bt=bt: e.scalar_tensor_tensor(bt[:, h, :], y1[:], cst["normg"][:, h:h + 1], gg[:, tc],
                                                                                           op0=ALU.mult, op1=ALU.mult),
                     reads=[By1, Bgg, Bc], writes=[Bbt])
            S.dma("sp", A["BA"][:, :, tc].rearrange("h p t -> p h t"), bt[:], reads=[Bbt], writes=[B["BA"]], nowaw=True)

SM_SCALE = 128 ** -0.5
TOPK = 256
NBIS = 18
NTER = 9
BIS_WIN = 16.0
NEG_ADM = -30000.0


def phase2_dsa(S, G, groups=(0, 1, 2, 3)):
    A = G["ap"]
    PB = G["pb"]
    cst = G["cst"]
    Bc = G["Bcst"]
    B = G["B"]
    ident = G["ident_bf"]
    Bident = G["Bident"]
    es_outer = S.es
    with ExitStack() as pes:
        S.es = pes
        alb = S.sbuf("ds_alb", [128, 8, 64], F32)
        dtab = S.sbuf("ds_dtab", [128, 8192], BF16)
        qrel = S.sbuf("ds_qrel", [1, 512], F32)
        onesr = S.sbuf("ds_onesr", [1, 128], BF16)
        drow = S.sbuf("ds_drow", [1, 512], F32)
        Bdrow = S.buf("ds_drow")
        shrow = S.sbuf("ds_shrow", [1, 8, 512], BF16)
        Bshrow = S.buf("ds_shrow")
        dmc = S.sbuf("ds_dmc", [128, 1], F32)
        Bdmc = S.buf("ds_dmc")
        corr = S.sbuf("ds_corr", [128, 8, 128], BF16)
        sel2 = S.sbuf("ds_sel2", [2, 128], BF16)
        onesb = S.sbuf("ds_ones", [128, 128], BF16)
        Bk = S.buf("ds_const")
        S.dma("sp", alb[:], A["alb"], writes=[Bk], nowaw=True)
        S.dma("sp", corr[:], A["corr"], writes=[Bk], nowaw=True)
        S.dma("sp", sel2[:], A["sel2"], writes=[Bk], nowaw=True)
        S.op("dve", lambda e: e.memset(onesb[:], 1.0), writes=[Bk])
        S.op("dve", lambda e: e.memset(onesr[:], 1.0), writes=[Bk])
        S.dma("sp", dtab[:], A["dtab"], writes=[Bk], nowaw=True)
        S.dma("sp", qrel[:], A["qrel"], writes=[Bk], nowaw=True)
        kit = S.sbuf("ds_kit", [128, 8192], BF16)
        S.dma("sp", kit[:], A["KIT"], reads=[B["KIT"]], writes=[Bk], nowaw=True)
        wi = S.sbuf("ds_wi", [128, 16, 8], F32)
        S.dma("sp", wi[:], A["WI"].rearrange("(t p) h -> p t h", p=128), reads=[B["WI"]], writes=[Bk], nowaw=True)
        maskT = S.sbuf("ds_maskT", [128, 64, 512], U8)
        BmT = S.buf("ds_maskT")
        S.barrier()

        def do_group(g):
            Q0 = OWN0 + g * 512
            with ExitStack() as aes:
                S.es = aes
                qit = S.sbuf("ds_qit", [128, 4, 512], BF16)
                Bqit = S.buf("ds_qit")
                S.dma("sp", qit[:], A["QIT"][:, :, g * 512:(g + 1) * 512].rearrange("m p q -> p m q"), reads=[B["QIT"]], writes=[Bqit])
                sc2 = [S.sbuf("ds_sc", [128, 8192], F32) for _ in range(2)]
                Bsc2 = [S.buf("ds_sc"), S.buf("ds_sc")]
                mq = S.sbuf("ds_mq", [128, 8192], BF16)
                Bmq = S.buf("ds_mq")
                adm = Rot(S, "ds_adm", [2, 512], BF16, 3)
                junk2 = S.sbuf("ds_junk2", [128, 8192], U8)
                Bj2 = S.buf("ds_junk2")
                relu = Rot(S, "ds_relu", [128, 512], BF16, 8)
                dg2 = [S.sbuf("ds_dg", [128, 8, 128], BF16) for _ in range(2)]
                Bdg2 = [S.buf("ds_dg"), S.buf("ds_dg")]
                sm = {n: S.sbuf("ds_s_" + n, [128, 1], F32) for n in ("lo", "hi", "mid", "cnt", "ge", "d", "c0", "d3", "t1", "nt2", "s2", "g2")}
                Bsm = S.buf("ds_small")
                Bth = S.buf("ds_th")
                Bs2 = S.buf("ds_s2")

                def tparams(T):
                    tg = g * 4 + T
                    Qt = Q0 + T * 128
                    nk = Qt + 128
                    nb5 = (nk + 511) // 512
                    return tg, Qt, nk, nb5, nb5 * 512

                def score_blocks(T):
                    tg, Qt, nk, nb5, nkp = tparams(T)
                    sc, Bsc = sc2[T % 2], Bsc2[T % 2]
                    dg, Bdg = dg2[T % 2], Bdg2[T % 2]
                    out = []

                    def prep():
                        for h in range(8):
                            S.op("dve", lambda e, h=h: e.tensor_scalar(dg[:, h, :], ident[:], wi[:, tg, h:h + 1], None, op0=ALU.mult),
                                 reads=[Bident, Bk], writes=[Bdg])
                    out.append(prep)

                    def blk(kb5):
                        ks = slice(kb5 * 512, (kb5 + 1) * 512)
                        ad, Bad = adm.next()
                        S.dma("sp", ad[:], A["adm"][tg, :, ks], writes=[Bad])
                        rl = []
                        for hp in range(4):
                            for par in range(2):
                                pb, Bpb = PB.next()
                                p0 = par * 64
                                S.op("pe", lambda e, pb=pb, hp=hp, p0=p0: e.matmul(
                                    pb[:], qit[p0:p0 + 64, hp, T * 128:(T + 1) * 128], kit[p0:p0 + 64, ks], start=True, stop=True),
                                    reads=[Bqit, Bk], writes=[Bpb])
                                r, Br = relu.next()
                                if par == 0 or hp % 2 == 0:
                                    S.op("act", lambda e, r=r, pb=pb: e.activation(r[:], pb[:], AF.Relu), reads=[Bpb], writes=[Br])
                                else:
                                    S.op("dve", lambda e, r=r, pb=pb: e.tensor_scalar(r[:], pb[:], 0.0, None, op0=ALU.max), reads=[Bpb], writes=[Br])
                                rl.append((r, Br))
                        ps, Bps = PB.next()
                        for h in range(8):
                            r, Br = rl[h]
                            S.op("pe", lambda e, ps=ps, h=h, r=r: e.matmul(ps[:], dg[:, h, :], r[:], start=(h == 0), stop=False),
                                 reads=[Bdg, Br], writes=[Bps])
                        S.op("pe", lambda e, ps=ps, ad=ad: e.matmul(ps[:], sel2[0:2, :], ad[0:2, :], start=False, stop=True),
                             reads=[Bk, Bad], writes=[Bps])
                        S.op("act", lambda e, ps=ps: e.activation(sc[:, ks], ps[:], AF.Copy), reads=[Bps], writes=[Bsc])
                    for kb5 in range(nb5):
                        out.append(lambda kb5=kb5: blk(kb5))
                    return out

                def search_init(T):
                    tg, Qt, nk, nb5, nkp = tparams(T)
                    sc, Bsc = sc2[T % 2], Bsc2[T % 2]
                    scv = sc[:, 0:nkp]
                    S.op("dve", lambda e: e.reduce_max(sm["hi"][:], scv, AX.X), reads=[Bsc], writes=[Bsm])
                    S.op("dve", lambda e: e.tensor_scalar(sm["lo"][:], sm["hi"][:], -BIS_WIN, None, op0=ALU.add), reads=[Bsm], writes=[Bsm])
                    S.op("dve", lambda e: e.tensor_scalar(sm["hi"][:], sm["hi"][:], 1e-3, None, op0=ALU.add), reads=[Bsm], writes=[Bsm, Bth])
                    S.op("dve", lambda e: e.tensor_scalar(mq[:, 0:nkp], scv, sm["lo"][:, 0:1], None, op0=ALU.is_ge, op1=ALU.add,
                                                          accum_out=sm["c0"][:]), reads=[Bsc, Bsm], writes=[Bmq, Bsm])

                def search_round(T, it):
                    tg, Qt, nk, nb5, nkp = tparams(T)
                    sc, Bsc = sc2[T % 2], Bsc2[T % 2]
                    scv = sc[:, 0:nkp]
                    d3 = (BIS_WIN + 1e-3) / (3.0 ** (it + 1))
                    S.op("dve", lambda e: e.tensor_scalar(sm["t1"][:], sm["lo"][:], d3, None, op0=ALU.add), reads=[Bsm], writes=[Bsm])
                    S.op("dve", lambda e: e.tensor_scalar(sm["nt2"][:], sm["lo"][:], -1.0, -2.0 * d3, op0=ALU.mult, op1=ALU.add), reads=[Bsm], writes=[Bsm, Bth])
                    S.op("act", lambda e: e.activation(junk2[:, 0:nkp], scv, AF.Sign, bias=sm["nt2"][:, 0:1], accum_out=sm["s2"][:]),
                         reads=[Bsc, Bth], writes=[Bj2, Bs2])
                    S.op("dve", lambda e: e.tensor_scalar(mq[:, 0:nkp], scv, sm["t1"][:, 0:1], None, op0=ALU.is_ge, op1=ALU.add,
                                                          accum_out=sm["cnt"][:]), reads=[Bsc, Bsm], writes=[Bmq, Bsm])
                    S.op("dve", lambda e: e.tensor_scalar(sm["ge"][:], sm["cnt"][:], TOPK - 0.5, None, op0=ALU.is_ge), reads=[Bsm], writes=[Bsm])
                    S.op("dve", lambda e: e.scalar_tensor_tensor(sm["g2"][:], sm["s2"][:], 2.0 * (TOPK - 0.5) - nkp, sm["ge"][:], op0=ALU.is_ge, op1=ALU.add),
                         reads=[Bs2, Bsm], writes=[Bsm])
                    S.op("dve", lambda e: e.scalar_tensor_tensor(sm["lo"][:], sm["g2"][:], d3, sm["lo"][:], op0=ALU.mult, op1=ALU.add),
                         reads=[Bsm], writes=[Bsm])

                def finalize(T):
                    tg, Qt, nk, nb5, nkp = tparams(T)
                    sc, Bsc = sc2[T % 2], Bsc2[T % 2]
                    scv = sc[:, 0:nkp]
                    S.op("dve", lambda e: e.tensor_scalar(sm["ge"][:], sm["c0"][:], TOPK - 0.5, None, op0=ALU.is_ge), reads=[Bsm], writes=[Bsm])
                    S.op("dve", lambda e: e.tensor_scalar(sm["d"][:], sm["lo"][:], 1000.0, None, op0=ALU.add), reads=[Bsm], writes=[Bsm])
                    S.op("dve", lambda e: e.tensor_scalar(sm["lo"][:], sm["d"][:], sm["ge"][:, 0:1], -1000.0, op0=ALU.mult, op1=ALU.add),
                         reads=[Bsm], writes=[Bsm])
                    S.op("dve", lambda e: e.tensor_scalar(mq[:, 0:nkp], scv, sm["lo"][:, 0:1], None, op0=ALU.is_ge), reads=[Bsc, Bsm], writes=[Bmq])
                    S.op("dve", lambda e: e.scalar_tensor_tensor(sc[:, 0:nk], mq[:, 0:nk], -16384.0, dtab[:, 8192 - nk:8192], op0=ALU.mult, op1=ALU.add),
                         reads=[Bmq, Bk], writes=[Bsc])
                    S.op("dve", lambda e: e.tensor_reduce(dmc[:], sc[:, 0:nk], AX.X, ALU.min), reads=[Bsc], writes=[Bdmc])
                    S.op("dve", lambda e: e.tensor_scalar(dmc[:], dmc[:], 16384.0, None, op0=ALU.add), reads=[Bdmc], writes=[Bdmc])
                    pbd, Bpbd = PB.next()
                    S.op("pe", lambda e: e.matmul(pbd[0:1, 0:128], dmc[:, 0:1], G["ident_f"][:], start=True, stop=True),
                         reads=[Bdmc, G["Bidf"]], writes=[Bpbd])
                    S.op("dve", lambda e: e.tensor_copy(drow[0:1, T * 128:(T + 1) * 128], pbd[0:1, 0:128]), reads=[Bpbd], writes=[Bdrow])
                    nkb = nk // 128
                    for k4 in range(0, nkb, 4):
                        n4 = min(4, nkb - k4)
                        pb, Bpb = PB.next()
                        for j in range(n4):
                            kb = k4 + j
                            S.op("pe", lambda e, pb=pb, j=j, kb=kb: e.matmul(pb[:, j * 128:(j + 1) * 128], mq[:, kb * 128:(kb + 1) * 128], ident[:],
                                                                           start=True, stop=True), reads=[Bmq, Bident], writes=[Bpb])
                        dst = maskT[:, k4:k4 + n4, T * 128:(T + 1) * 128]
                        src = pb[:, 0:n4 * 128].rearrange("p (a b) -> p a b", a=n4)
                        if (k4 // 4) % 2 == 0:
                            S.op("act", lambda e, dst=dst, src=src: e.activation(dst, src, AF.Copy), reads=[Bpb], writes=[BmT])
                        else:
                            S.op("dve", lambda e, dst=dst, src=src: e.tensor_copy(dst, src), reads=[Bpb], writes=[BmT])

                for f_ in score_blocks(0):
                    f_()
                for T in range(4):
                    nxt = score_blocks(T + 1) if T < 3 else []
                    per = -(-len(nxt) // NTER)
                    search_init(T)
                    for it in range(NTER):
                        search_round(T, it)
                        for _ in range(per):
                            if nxt:
                                nxt.pop(0)()
                    while nxt:
                        nxt.pop(0)()
                    finalize(T)
                S.op("dve", lambda e: e.tensor_tensor(drow[:], drow[:], qrel[:], ALU.subtract), reads=[Bdrow, Bk], writes=[Bdrow])
                for h in range(8):
                    S.op("dve", lambda e, h=h: e.tensor_scalar(shrow[0:1, h, :], drow[:], (2.0 ** -(h + 1)) / SM_SCALE, None, op0=ALU.mult),
                         reads=[Bdrow], writes=[Bshrow])
                S.barrier()
                S.phase_end()
            with ExitStack() as bes:
                S.es = bes
                kt = Rot(S, "ds_kt", [128, 8192], BF16, 2)
                vh = Rot(S, "ds_vh", [128, 64, 128], BF16, 2)
                qt = Rot(S, "ds_qt", [128, 512], BF16, 2)
                pT = Rot(S, "ds_pT", [128, 512], BF16, 9)
                mcr = Rot(S, "ds_mc", [128, 128], BF16, 3)
                rec = Rot(S, "ds_rec", [128, 512], F32, 1)
                ob = Rot(S, "ds_ob", [128, 512], BF16, 1)
                nkb = (Q0 + 512) // 128
                kb0 = Q0 // 128
                for h in range(8):
                    k_, Bk_ = kt.next()
                    v_, Bv_ = vh.next()
                    q_, Bq_ = qt.next()
                    S.dma("sp", k_[:, 0:nkb * 128], A["KT"][h, :, 0:nkb * 128], reads=[B["KT"]], writes=[Bk_])
                    S.dma("sp", v_[:, 0:nkb, :], A["VH"][h, :, 0:nkb, :], reads=[B["VH"]], writes=[Bv_])
                    S.dma("sp", q_[:], A["QT"][h, :, g * 512:(g + 1) * 512], reads=[B["QT"]], writes=[Bq_])
                    po, Bpo = PB.t[6], PB.b[6]
                    pd, Bpd = PB.t[7], PB.b[7]
                    pend = []

                    def stage2(kb, p_, Bp_, c0, first, last, po=po, pd=pd, Bpo=Bpo, Bpd=Bpd, v_=v_, Bv_=Bv_):
                        S.op("pe", lambda e, po=po, v_=v_, p_=p_, kb=kb, c0=c0, first=first, last=last: e.matmul(
                            po[:, c0:512], v_[:, kb, :], p_[:, c0:512], start=first, stop=last), reads=[Bv_, Bp_], writes=[Bpo])
                        S.op("pe", lambda e, pd=pd, p_=p_, c0=c0, first=first, last=last: e.matmul(
                            pd[:, c0:512], onesb[:], p_[:, c0:512], start=first, stop=last), reads=[Bk, Bp_], writes=[Bpd])

                    for kb in range(nkb):
                        r = kb - kb0
                        c0 = max(r, 0) * 128
                        first = (kb == 0)
                        last = (kb == nkb - 1)
                        pst, Bpst = PB.next(0, 6)
                        S.op("pe", lambda e, pst=pst, k_=k_, q_=q_, kb=kb, c0=c0: e.matmul(
                            pst[:, c0:512], k_[:, kb * 128:(kb + 1) * 128], q_[:, c0:512], start=True, stop=(h >= 6)),
                            reads=[Bk_, Bq_], writes=[Bpst])
                        if h < 6:
                            S.op("pe", lambda e, pst=pst, h=h, c0=c0: e.matmul(pst[:, c0:512], onesr[0:1, :], shrow[0:1, h, c0:512], start=False, stop=True),
                                 reads=[Bk, Bshrow], writes=[Bpst])
                        p_, Bp_ = pT.next()
                        bias_ap = alb[:, h, r + 60:r + 61]
                        S.op("act", lambda e, p_=p_, pst=pst, c0=c0, bias_ap=bias_ap: e.activation(
                            p_[:, c0:512], pst[:, c0:512], AF.Exp, scale=SM_SCALE, bias=bias_ap), reads=[Bpst, Bk], writes=[Bp_])
                        if r >= 0:
                            mc, Bmc = mcr.next()
                            S.op("dve", lambda e, mc=mc, kb=kb, r=r, h=h: e.tensor_tensor(mc[:], maskT[:, kb, r * 128:(r + 1) * 128], corr[:, h, :], ALU.mult),
                                 reads=[BmT, Bk], writes=[Bmc])
                            S.op("dve", lambda e, p_=p_, mc=mc, r=r: e.scalar_tensor_tensor(p_[:, r * 128:(r + 1) * 128], p_[:, r * 128:(r + 1) * 128], 3.0e38, mc[:],
                                                                                         op0=ALU.min, op1=ALU.mult), reads=[Bmc, Bp_], writes=[Bp_])
                            if c0 + 128 < 512:
                                S.op("dve", lambda e, p_=p_, kb=kb, c0=c0: e.scalar_tensor_tensor(p_[:, c0 + 128:512], p_[:, c0 + 128:512], 3.0e38, maskT[:, kb, c0 + 128:512],
                                                                                               op0=ALU.min, op1=ALU.mult), reads=[BmT, Bp_], writes=[Bp_])
                        else:
                            S.op("dve", lambda e, p_=p_, kb=kb: e.scalar_tensor_tensor(p_[:], p_[:], 3.0e38, maskT[:, kb, :], op0=ALU.min, op1=ALU.mult),
                                 reads=[BmT, Bp_], writes=[Bp_])
                        pend.append((kb, p_, Bp_, c0, first, last))
                        if len(pend) > 6:
                            stage2(*pend.pop(0))
                    while pend:
                        stage2(*pend.pop(0))
                    rc, Brc = rec.next()
                    S.op("dve", lambda e, rc=rc, pd=pd: e.reciprocal(rc[:], pd[:]), reads=[Bpd], writes=[Brc])
                    o_, Bo_ = ob.next()
                    S.op("dve", lambda e, o_=o_, po=po, rc=rc: e.tensor_tensor(o_[:], po[:], rc[:], ALU.mult), reads=[Bpo, Brc], writes=[Bo_])
                    S.dma("sp", A["BB"][h, :, g * 512:(g + 1) * 512], o_[:], reads=[Bo_], writes=[B["BB"]], nowaw=True)
                S.barrier()
                S.phase_end()
        for g in groups:
            do_group(g)
        S.es = es_outer

LN_EPS = 1e-5
DN_ALPHA = 2.0 ** 0.25
CAP = 256
NEXP = 32


def layer_norm_tile(S, G, pre, Bpre, out_t, Bout, gb, bb, Bgb, small, Bsmall, junk, Bjunk):
    s1, s2, mean, var, rstd, nmr = small
    S.op("act", lambda e: e.activation(junk[:], pre[:], AF.Identity, accum_out=s1[:]), reads=[Bpre], writes=[Bjunk, Bsmall])
    S.op("act", lambda e: e.activation(junk[:], pre[:], AF.Square, accum_out=s2[:]), reads=[Bpre], writes=[Bjunk, Bsmall])
    S.op("dve", lambda e: e.tensor_scalar(mean[:], s1[:], 1.0 / 2048.0, None, op0=ALU.mult), reads=[Bsmall], writes=[Bsmall])
    S.op("dve", lambda e: e.tensor_tensor(var[:], mean[:], mean[:], ALU.mult), reads=[Bsmall], writes=[Bsmall])
    S.op("dve", lambda e: e.scalar_tensor_tensor(var[:], s2[:], 1.0 / 2048.0, var[:], op0=ALU.mult, op1=ALU.subtract), reads=[Bsmall], writes=[Bsmall])
    S.op("act", lambda e: e.activation(rstd[:], var[:], AF.Sqrt, bias=G["cst"]["eps_ln"][:, 0:1]), reads=[Bsmall, G["Bcst"]], writes=[Bsmall])
    S.op("dve", lambda e: e.reciprocal(rstd[:], rstd[:]), reads=[Bsmall], writes=[Bsmall])
    S.op("dve", lambda e: e.scalar_tensor_tensor(nmr[:], mean[:], -1.0, rstd[:], op0=ALU.mult, op1=ALU.mult), reads=[Bsmall], writes=[Bsmall])
    S.op("act", lambda e: e.activation(pre[:], pre[:], AF.Identity, scale=rstd[:, 0:1], bias=nmr[:, 0:1]), reads=[Bpre, Bsmall], writes=[Bpre])
    S.op("dve", lambda e: e.tensor_tensor(pre[:], pre[:], gb[:], ALU.mult), reads=[Bpre, Bgb], writes=[Bpre])
    S.op("dve", lambda e: e.tensor_tensor(out_t[:], pre[:], bb[:], ALU.add), reads=[Bpre, Bgb], writes=[Bout])


def phase3a_merge(S, G):
    A = G["ap"]
    PB = G["pb"]
    B = G["B"]
    wa = S.sbuf("o_wa", [128, 8, 2048], BF16)
    wb = S.sbuf("o_wb", [128, 8, 2048], BF16)
    Bw = S.buf("o_w")
    S.dma("pool", wa[:], A["w_branch_a"].rearrange("(kc p) n -> p kc n", p=128), writes=[Bw], nowaw=True)
    S.dma("pool", wb[:], A["w_branch_b"].rearrange("(kc p) n -> p kc n", p=128), writes=[Bw], nowaw=True)
    bag = Rot(S, "o_ba", [128, 8, 512], BF16, 2)
    bbg = Rot(S, "o_bb", [128, 8, 512], BF16, 2)
    gtg = Rot(S, "o_gt", [128, 32, 512], BF16, 2)
    t1r = Rot(S, "o_t1", [128, 512], F32, 2)
    t2r = Rot(S, "o_t2", [128, 512], F32, 2)
    mgs = Rot(S, "o_mgs", [128, 512], BF16, 3)
    for g in range(4):
        ba_, Bba = bag.next()
        bb_, Bbb = bbg.next()
        gt_, Bgt = gtg.next()
        S.dma("sp", ba_[:], A["BA"][:, :, g * 512:(g + 1) * 512].rearrange("h p t -> p h t"), reads=[B["BA"]], writes=[Bba])
        S.dma("sp", bb_[:], A["BB"][:, :, g * 512:(g + 1) * 512].rearrange("h p t -> p h t"), reads=[B["BB"]], writes=[Bbb])
        S.dma("sp", gt_[:], A["GT"][:, :, g * 512:(g + 1) * 512].rearrange("c p t -> p c t"), reads=[B["GT"]], writes=[Bgt])
        for c in range(16):
            pa, Bpa = PB.next()
            for k in range(8):
                S.op("pe", lambda e, pa=pa, k=k, c=c, ba_=ba_: e.matmul(pa[:], wa[:, k, c * 128:(c + 1) * 128], ba_[:, k, :], start=(k == 0), stop=(k == 7)),
                     reads=[Bw, Bba], writes=[Bpa])
            pb2, Bpb2 = PB.next()
            for k in range(8):
                S.op("pe", lambda e, pb2=pb2, k=k, c=c, bb_=bb_: e.matmul(pb2[:], wb[:, k, c * 128:(c + 1) * 128], bb_[:, k, :], start=(k == 0), stop=(k == 7)),
                     reads=[Bw, Bbb], writes=[Bpb2])
            t1, Bt1 = t1r.next()
            t2, Bt2 = t2r.next()
            S.op("dve", lambda e, t1=t1, pa=pa, gt_=gt_, c=c: e.tensor_tensor(t1[:], pa[:], gt_[:, c, :], ALU.mult), reads=[Bpa, Bgt], writes=[Bt1])
            S.op("dve", lambda e, t2=t2, pb2=pb2, gt_=gt_, c=c: e.tensor_tensor(t2[:], pb2[:], gt_[:, 16 + c, :], ALU.mult), reads=[Bpb2, Bgt], writes=[Bt2])
            m_, Bm = mgs.next()
            S.op("dve", lambda e, t1=t1, t2=t2, m_=m_: e.tensor_tensor(m_[:], t1[:], t2[:], ALU.add), reads=[Bt1, Bt2], writes=[Bm])
            S.dma("sp", A["MG"][c, :, g * 512:(g + 1) * 512], m_[:], reads=[Bm], writes=[B["MG"]], nowaw=True)


def phase3b_out(S, G):
    A = G["ap"]
    PB = G["pb"]
    B = G["B"]
    P = G["persist"]
    ident_f = G["ident_f"]
    Bw = S.buf("o_w")
    wr = S.sbuf("o_wr", [128, 16, 36], F32)
    S.dma("sp", wr[:], A["wr"].rearrange("(kc p) n -> p kc n", p=128), writes=[Bw], nowaw=True)
    brr = S.sbuf("o_brr", [1, 36], F32)
    S.dma("sp", brr[:], A["brr"], writes=[Bw], nowaw=True)
    g1 = S.sbuf("o_g1", [128, 2048], F32)
    b1 = S.sbuf("o_b1", [128, 2048], F32)
    S.dma("sp", g1[:], A["ln1_gb"], writes=[Bw], nowaw=True)
    S.dma("sp", b1[:], A["ln1_bb"], writes=[Bw], nowaw=True)
    ltri = S.sbuf("o_ltri", [128, 128], BF16)
    S.dma("sp", ltri[:], A["ltri"], writes=[Bw], nowaw=True)
    e256 = S.sbuf("o_e256", [128, 32], F32)
    S.dma("sp", e256[:], A["e256"], writes=[Bw], nowaw=True)
    onesc = S.sbuf("o_onesc", [128, 1], BF16)
    S.op("dve", lambda e: e.memset(onesc[:], 1.0), writes=[Bw])
    onesr = S.sbuf("o_onesr", [1, 128], F32)
    S.op("dve", lambda e: e.memset(onesr[:], 1.0), writes=[Bw])
    cnt = S.sbuf("o_cnt", [1, 32], F32)
    Bcnt = S.buf("o_cnt")
    S.op("dve", lambda e: e.memset(cnt[:], 0.0), writes=[Bcnt])
    S.barrier()
    wo = Rot(S, "o_wo", [128, 16, 512], BF16, 2)
    mgr = Rot(S, "o_mg", [128, 16, 512], BF16, 2)
    pre = Rot(S, "o_pre", [128, 2048], F32, 5)
    h1t = Rot(S, "o_h1", [128, 2048], F32, 2)
    h1b = Rot(S, "o_h1b", [128, 2048], BF16, 2)
    junk = S.sbuf("o_junk", [128, 2048], BF16)
    Bjunk = S.buf("o_junk")
    hT = Rot(S, "o_hT", [128, 16, 128], F32, 1)
    smalls = [S.sbuf("o_sm%d" % i, [128, 1], F32) for i in range(6)]
    Bsmall = S.buf("o_small")
    rt = {n: S.sbuf("o_r_" + n, shp, F32) for n, shp in (
        ("L", [128, 36]), ("gmax", [128, 1]), ("ngmax", [128, 1]), ("ohg", [128, 4]), ("ge", [128, 4]), ("gsum", [128, 1]), ("gp", [128, 1]),
        ("e8", [128, 8]), ("m1", [128, 1]), ("oh1", [128, 8]), ("e8b", [128, 8]), ("m2", [128, 1]), ("oh2", [128, 8]), ("d", [128, 1]),
        ("sg", [128, 1]), ("A1", [128, 32]), ("A2", [128, 32]), ("At", [128, 32]), ("rk", [128, 32]), ("t", [128, 32]), ("i1", [128, 1]), ("i2", [128, 1]))}
    Abf = S.sbuf("o_Abf", [128, 32], BF16)
    Br = G["Br_persist"]

    def rop(fn, eng="dve"):
        S.op(eng, fn, reads=[Br, Bw], writes=[Br])

    for g in range(4):
        mg, Bmg = mgr.next()
        S.dma("sp", mg[:], A["MG"][:, :, g * 512:(g + 1) * 512].rearrange("c p t -> p c t"), reads=[B["MG"]], writes=[Bmg])
        prs = []
        for t in range(4):
            tt = g * 4 + t
            pr, Bpr = pre.next()
            S.dma("sp", pr[:], A["xs"][OWN0 + tt * 128:OWN0 + (tt + 1) * 128, :], writes=[Bpr])
            prs.append((pr, Bpr))
        for cb in range(4):
            w_, Bwo = wo.next()
            S.dma("pool", w_[:], A["w_out"][:, cb * 512:(cb + 1) * 512].rearrange("(kc p) n -> p kc n", p=128), writes=[Bwo])
            for t in range(4):
                pr, Bpr = prs[t]
                po, Bpo = PB.next()
                for c in range(16):
                    S.op("pe", lambda e, po=po, c=c, t=t, w_=w_, mg=mg: e.matmul(po[:], mg[:, c, t * 128:(t + 1) * 128], w_[:, c, :], start=(c == 0), stop=(c == 15)),
                         reads=[Bmg, Bwo], writes=[Bpo])
                S.op("dve", lambda e, pr=pr, po=po, cb=cb: e.scalar_tensor_tensor(pr[:, cb * 512:(cb + 1) * 512], pr[:, cb * 512:(cb + 1) * 512], DN_ALPHA, po[:],
                                                                                op0=ALU.mult, op1=ALU.add), reads=[Bpo, Bpr], writes=[Bpr])
        for t in range(4):
            tt = g * 4 + t
            pr, Bpr = prs[t]
            h_, Bh = h1t.next()
            layer_norm_tile(S, G, pr, Bpr, h_, Bh, g1, b1, Bw, smalls, Bsmall, junk, Bjunk)
            S.dma("sp", A["H1"][tt * 128:(tt + 1) * 128, :], h_[:], reads=[Bh], writes=[B["H1"]], nowaw=True)
            hb, Bhb = h1b.next()
            S.op("act", lambda e, hb=hb, h_=h_: e.activation(hb[:], h_[:], AF.Copy), reads=[Bh], writes=[Bhb])
            S.dma("sp", A["H1b"][tt * 128:(tt + 1) * 128, :], hb[:], reads=[Bhb], writes=[B["H1b"]], nowaw=True)
            hT_, BhT = hT.next()
            for q4 in range(4):
                pb, Bpb = PB.next()
                for j in range(4):
                    c = q4 * 4 + j
                    S.op("pe", lambda e, pb=pb, h_=h_, c=c, j=j: e.matmul(pb[:, j * 128:(j + 1) * 128], h_[:, c * 128:(c + 1) * 128], ident_f[:], start=True, stop=True),
                         reads=[Bh, G["Bidf"]], writes=[Bpb])
                S.op("act", lambda e, pb=pb, hT_=hT_, q4=q4: e.activation(hT_[:, q4 * 4:(q4 + 1) * 4, :], pb[:].rearrange("p (a b) -> p a b", a=4), AF.Copy),
                     reads=[Bpb], writes=[BhT])
            pl, Bpl = PB.next()
            for c in range(16):
                S.op("pe", lambda e, pl=pl, hT_=hT_, c=c: e.matmul(pl[:, 0:36], hT_[:, c, :], wr[:, c, :], start=(c == 0), stop=False), reads=[BhT, Bw], writes=[Bpl])
            S.op("pe", lambda e, pl=pl: e.matmul(pl[:, 0:36], onesr[0:1, :], brr[0:1, :], start=False, stop=True), reads=[Bw], writes=[Bpl])
            L = rt["L"]
            S.op("dve", lambda e, pl=pl: e.tensor_copy(L[:], pl[:, 0:36]), reads=[Bpl], writes=[Br])
            rop(lambda e: e.reduce_max(rt["gmax"][:], L[:, 0:4], AX.X))
            rop(lambda e: e.tensor_scalar(rt["ohg"][:], L[:, 0:4], rt["gmax"][:, 0:1], None, op0=ALU.is_equal))
            rop(lambda e: e.tensor_scalar(rt["ngmax"][:], rt["gmax"][:], -1.0, None, op0=ALU.mult))
            rop(lambda e: e.activation(rt["ge"][:], L[:, 0:4], AF.Exp, bias=rt["ngmax"][:, 0:1], accum_out=rt["gsum"][:]), "act")
            rop(lambda e: e.reciprocal(rt["gp"][:], rt["gsum"][:]))
            rop(lambda e: e.tensor_scalar(rt["e8"][:], L[:, 4:12], rt["ohg"][:, 0:1], None, op0=ALU.mult))
            for gg in range(1, 4):
                rop(lambda e, gg=gg: e.scalar_tensor_tensor(rt["e8"][:], L[:, 4 + 8 * gg:12 + 8 * gg], rt["ohg"][:, gg:gg + 1], rt["e8"][:], op0=ALU.mult, op1=ALU.add))
            rop(lambda e: e.reduce_max(rt["m1"][:], rt["e8"][:], AX.X))
            rop(lambda e: e.tensor_scalar(rt["oh1"][:], rt["e8"][:], rt["m1"][:, 0:1], None, op0=ALU.is_equal))
            rop(lambda e: e.scalar_tensor_tensor(rt["e8b"][:], rt["oh1"][:], -1.0e30, rt["e8"][:], op0=ALU.mult, op1=ALU.add))
            rop(lambda e: e.reduce_max(rt["m2"][:], rt["e8b"][:], AX.X))
            rop(lambda e: e.tensor_scalar(rt["oh2"][:], rt["e8b"][:], rt["m2"][:, 0:1], None, op0=ALU.is_equal))
            rop(lambda e: e.tensor_tensor(rt["d"][:], rt["m1"][:], rt["m2"][:], ALU.subtract))
            rop(lambda e: e.activation(rt["sg"][:], rt["d"][:], AF.Sigmoid), "act")
            rop(lambda e, tt=tt: e.tensor_tensor(P["wts"][:, tt, 0:1], rt["sg"][:], rt["gp"][:], ALU.mult))
            rop(lambda e, tt=tt: e.tensor_tensor(P["wts"][:, tt, 1:2], rt["gp"][:], P["wts"][:, tt, 0:1], ALU.subtract))
            for gg in range(4):
                rop(lambda e, gg=gg: e.tensor_scalar(rt["A1"][:, gg * 8:(gg + 1) * 8], rt["oh1"][:], rt["ohg"][:, gg:gg + 1], None, op0=ALU.mult))
                rop(lambda e, gg=gg: e.tensor_scalar(rt["A2"][:, gg * 8:(gg + 1) * 8], rt["oh2"][:], rt["ohg"][:, gg:gg + 1], None, op0=ALU.mult))
            rop(lambda e: e.tensor_tensor(rt["At"][:], rt["A1"][:], rt["A2"][:], ALU.add))
            rop(lambda e: e.tensor_copy(Abf[:], rt["At"][:]))
            pk, Bpk = PB.next()
            S.op("pe", lambda e, pk=pk: e.matmul(pk[:, 0:32], ltri[:], Abf[:], start=True, stop=False), reads=[Bw, Br], writes=[Bpk])
            S.op("pe", lambda e, pk=pk: e.matmul(pk[:, 0:32], onesr[0:1, :], cnt[0:1, :], start=False, stop=True), reads=[Bw, Bcnt], writes=[Bpk])
            pc, Bpc = PB.next()
            S.op("pe", lambda e, pc=pc: e.matmul(pc[0:1, 0:32], onesc[:], Abf[:], start=True, stop=True), reads=[Bw, Br], writes=[Bpc])
            S.op("dve", lambda e, pk=pk: e.tensor_copy(rt["rk"][:], pk[:, 0:32]), reads=[Bpk, Br], writes=[Br])
            S.op("dve", lambda e, pc=pc: e.tensor_tensor(cnt[:], cnt[:], pc[0:1, 0:32], ALU.add), reads=[Bpc, Bcnt], writes=[Bcnt])
            rop(lambda e: e.scalar_tensor_tensor(rt["t"][:], rt["rk"][:], 1.0, rt["At"][:], op0=ALU.add, op1=ALU.mult))
            rop(lambda e, tt=tt: e.tensor_scalar(P["RK"][:, tt, :], rt["t"][:], -1.0, None, op0=ALU.add))
            rop(lambda e: e.tensor_tensor(rt["rk"][:], rt["rk"][:], e256[:], ALU.add))
            rop(lambda e: e.tensor_tensor(rt["t"][:], rt["rk"][:], rt["A1"][:], ALU.mult))
            rop(lambda e: e.reduce_sum(rt["i1"][:], rt["t"][:], AX.X))
            rop(lambda e: e.tensor_tensor(rt["t"][:], rt["rk"][:], rt["A2"][:], ALU.mult))
            rop(lambda e: e.reduce_sum(rt["i2"][:], rt["t"][:], AX.X))
            rop(lambda e, tt=tt: e.tensor_copy(P["idx"][:, tt, 0:1], rt["i1"][:]))
            rop(lambda e, tt=tt: e.tensor_copy(P["idx"][:, tt, 1:2], rt["i2"][:]))


def phase4_moe(S, G, experts=range(NEXP)):
    A = G["ap"]
    PB = G["pb"]
    B = G["B"]
    P = G["persist"]
    h1b = S.sbuf("m_h1b", [128, 16, 2048], BF16)
    Bh1b = S.buf("m_h1b")
    S.dma("sp", h1b[:], A["H1b"].rearrange("(t p) d -> p t d", p=128), reads=[B["H1b"]], writes=[Bh1b])
    iot = S.sbuf("m_iota", [128, CAP], F32)
    Biot = S.buf("m_iota")
    S.dma("sp", iot[:], A["iota256"], writes=[Biot])
    wu = Rot(S, "m_w", [128, 16, 512], BF16, 4)
    wd = Rot(S, "m_wd", [128, 4, 2048], BF16, 2)
    sel = Rot(S, "m_sel", [128, 16, CAP], BF16, 1)
    xs_ = Rot(S, "m_xs", [128, 16, CAP], BF16, 1)
    hT = Rot(S, "m_hT", [128, 8, CAP], BF16, 2)
    sg = Rot(S, "m_sg", [128, CAP], F32, 3)
    yst = Rot(S, "m_y", [128, 1024], F32, 1)
    for e_ in experts:
        s_, Bs = sel.next()
        for tt in range(16):
            S.op("dve", lambda e, s_=s_, tt=tt, e_=e_: e.tensor_scalar(s_[:, tt, :], iot[:], P["RK"][:, tt, e_:e_ + 1], None, op0=ALU.is_equal),
                 reads=[Biot, G["Br_persist"]], writes=[Bs])
        x_, Bx = xs_.next()
        for kc in range(16):
            pb, Bpb = PB.next()
            for tt in range(16):
                S.op("pe", lambda e, pb=pb, tt=tt, kc=kc, s_=s_: e.matmul(pb[:, 0:CAP], h1b[:, tt, kc * 128:(kc + 1) * 128], s_[:, tt, :],
                                                                         start=(tt == 0), stop=(tt == 15)), reads=[Bh1b, Bs], writes=[Bpb])
            if kc % 2 == 0:
                S.op("act", lambda e, pb=pb, x_=x_, kc=kc: e.activation(x_[:, kc, :], pb[:, 0:CAP], AF.Copy), reads=[Bpb], writes=[Bx])
            else:
                S.op("dve", lambda e, pb=pb, x_=x_, kc=kc: e.tensor_copy(x_[:, kc, :], pb[:, 0:CAP]), reads=[Bpb], writes=[Bx])
        h_, Bh = hT.next()
        for half in range(2):
            wg_, Bwg = wu.next()
            S.dma("pool", wg_[:], A["w_gate"][e_, :, half * 512:(half + 1) * 512].rearrange("(kc p) n -> p kc n", p=128), writes=[Bwg])
            wu_, Bwu = wu.next()
            S.dma("pool", wu_[:], A["w_up"][e_, :, half * 512:(half + 1) * 512].rearrange("(kc p) n -> p kc n", p=128), writes=[Bwu])
            for f4 in range(4):
                f = half * 4 + f4
                pg, Bpg = PB.next()
                for kc in range(16):
                    S.op("pe", lambda e, pg=pg, kc=kc, f4=f4, wg_=wg_, x_=x_: e.matmul(pg[:, 0:CAP], wg_[:, kc, f4 * 128:(f4 + 1) * 128], x_[:, kc, :],
                                                                                    start=(kc == 0), stop=(kc == 15)), reads=[Bwg, Bx], writes=[Bpg])
                for kc in range(16):
                    S.op("pe", lambda e, pg=pg, kc=kc, f4=f4, wu_=wu_, x_=x_: e.matmul(pg[:, CAP:2 * CAP], wu_[:, kc, f4 * 128:(f4 + 1) * 128], x_[:, kc, :],
                                                                                    start=(kc == 0), stop=(kc == 15)), reads=[Bwu, Bx], writes=[Bpg])
                s1, Bs1 = sg.next()
                S.op("act", lambda e, s1=s1, pg=pg: e.activation(s1[:], pg[:, 0:CAP], AF.Silu), reads=[Bpg], writes=[Bs1])
                S.op("dve", lambda e, h_=h_, f=f, s1=s1, pg=pg: e.tensor_tensor(h_[:, f, :], s1[:], pg[:, CAP:2 * CAP], ALU.mult), reads=[Bs1, Bpg], writes=[Bh])
        wds = []
        for half in range(2):
            wd_, Bwd = wd.next()
            S.dma("pool", wd_[:], A["w_down"][e_, half * 512:(half + 1) * 512, :].rearrange("(fc p) n -> p fc n", p=128), writes=[Bwd])
            wds.append((wd_, Bwd))
        for rh in range(CAP // 128):
            for cbp in range(2):
                y_, By = yst.next()
                for c2 in range(2):
                    cb = cbp * 2 + c2
                    py, Bpy = PB.next()
                    for f in range(8):
                        wd_, Bwd = wds[f // 4]
                        S.op("pe", lambda e, py=py, f=f, rh=rh, cb=cb, wd_=wd_, h_=h_: e.matmul(py[:], h_[:, f, rh * 128:(rh + 1) * 128], wd_[:, f % 4, cb * 512:(cb + 1) * 512],
                                                                                             start=(f == 0), stop=(f == 7)), reads=[Bh, Bwd], writes=[Bpy])
                    if c2 == 0:
                        S.op("act", lambda e, y_=y_, py=py, c2=c2: e.activation(y_[:, c2 * 512:(c2 + 1) * 512], py[:], AF.Copy), reads=[Bpy], writes=[By])
                    else:
                        S.op("dve", lambda e, y_=y_, py=py, c2=c2: e.tensor_copy(y_[:, c2 * 512:(c2 + 1) * 512], py[:]), reads=[Bpy], writes=[By])
                S.dma("sp", A["Y"][e_ * CAP + rh * 128:e_ * CAP + (rh + 1) * 128, cbp * 1024:(cbp + 1) * 1024], y_[:], reads=[By], writes=[B["Y"]], nowaw=True)


def phase5_final(S, G):
    A = G["ap"]
    B = G["B"]
    P = G["persist"]
    g2 = S.sbuf("f_g2", [128, 2048], F32)
    b2 = S.sbuf("f_b2", [128, 2048], F32)
    Bw = S.buf("f_w")
    S.dma("sp", g2[:], A["ln2_gb"], writes=[Bw], nowaw=True)
    S.dma("sp", b2[:], A["ln2_bb"], writes=[Bw], nowaw=True)
    idxi = S.sbuf("f_idx", [128, 16, 2], U32)
    Bidx = S.buf("f_idx")
    S.op("dve", lambda e: e.tensor_copy(idxi[:], P["idx"][:]), reads=[G["Br_persist"]], writes=[Bidx])
    S.barrier()
    h1 = Rot(S, "f_h1", [128, 2048], F32, 2)
    y1 = Rot(S, "f_y1", [128, 2048], F32, 2)
    y2 = Rot(S, "f_y2", [128, 2048], F32, 2)
    ot = Rot(S, "f_ot", [128, 2048], F32, 2)
    junk = S.sbuf("f_junk", [128, 2048], BF16)
    Bjunk = S.buf("f_junk")
    smalls = [S.sbuf("f_sm%d" % i, [128, 1], F32) for i in range(6)]
    Bsmall = S.buf("f_small")
    for tt in range(16):
        h_, Bh = h1.next()
        S.dma("sp", h_[:], A["H1"][tt * 128:(tt + 1) * 128, :], reads=[B["H1"]], writes=[Bh])
        a_, Ba = y1.next()
        b_, Bb = y2.next()
        for (dst, Bd, k) in ((a_, Ba, 0), (b_, Bb, 1)):
            S.dma("pool", None, None, reads=[B["Y"], Bidx], writes=[Bd],
                  builder=lambda e, dst=dst, tt=tt, k=k: e.indirect_dma_start(
                      out=dst[:], out_offset=None, in_=A["Y"], in_offset=bass.IndirectOffsetOnAxis(ap=idxi[:, tt, k:k + 1], axis=0),
                      bounds_check=NEXP * CAP - 1, oob_is_err=False))
        S.op("dve", lambda e, a_=a_, tt=tt: e.tensor_scalar(a_[:], a_[:], P["wts"][:, tt, 0:1], None, op0=ALU.mult), reads=[Ba, G["Br_persist"]], writes=[Ba])
        S.op("dve", lambda e, a_=a_, b_=b_, tt=tt: e.scalar_tensor_tensor(a_[:], b_[:], P["wts"][:, tt, 1:2], a_[:], op0=ALU.mult, op1=ALU.add),
             reads=[Ba, Bb, G["Br_persist"]], writes=[Ba])
        S.op("dve", lambda e, a_=a_, h_=h_: e.scalar_tensor_tensor(a_[:], h_[:], DN_ALPHA, a_[:], op0=ALU.mult, op1=ALU.add), reads=[Ba, Bh], writes=[Ba])
        o_, Bo = ot.next()
        layer_norm_tile(S, G, a_, Ba, o_, Bo, g2, b2, Bw, smalls, Bsmall, junk, Bjunk)
        S.dma("sp", A["out"][tt * 128:(tt + 1) * 128, :], o_[:], reads=[Bo], writes=[B["out"]], nowaw=True)

from contextlib import ExitStack
from concourse.bass_utils import run_bass_kernel_spmd

BFM_IDX = {"af": 0, "aq": 8, "ag": 16, "bk": 24, "bq": 32, "iq": 40, "ik": 44, "g": 45}
NBFM = 77

SCRATCH = {
    "KT": ([8, 128, 8192], "bf16"), "VH": ([8, 128, 64, 128], "bf16"), "KIT": ([128, 8192], "bf16"),
    "HG_kdec": ([8, 128, 8192], "bf16"), "HG_kend": ([8192, 1024], "bf16"), "HG_v": ([8192, 1024], "bf16"),
    "HG_dec": ([128, 8, 128], "f32"), "HG_qdec": ([8, 128, 2048], "bf16"), "HG_gs": ([8, 128, 2048], "bf16"),
    "QT": ([8, 128, 2048], "bf16"), "QIT": ([4, 128, 2048], "bf16"), "WI": ([2048, 8], "f32"),
    "GT": ([32, 128, 2048], "bf16"), "BA": ([8, 128, 2048], "bf16"), "BB": ([8, 128, 2048], "bf16"),
    "MG": ([16, 128, 2048], "bf16"), "H1": ([2048, 2048], "f32"), "H1b": ([2048, 2048], "bf16"), "Y": ([NEXP * CAP, 2048], "f32"),
}

INPUTS = {
    "xs": ([8192, 2048], "f32"), "valid_tm": ([128, 64], "f32"), "w_in": ([2048, 11848], "f32"),
    "ident": ([128, 128], "f32"), "bfm": ([128, NBFM], "f32"), "brow": ([1, 2056], "f32"),
    "lbl": ([128, 2, 8], "f32"), "normg": ([128, 8], "f32"), "rmask": ([128, 512], "f32"), "bdmask": ([128, 128], "f32"),
    "alb": ([128, 8, 64], "f32"), "corr": ([128, 8, 128], "bf16"), "sel2": ([2, 128], "bf16"), "dtab": ([128, 8192], "bf16"),
    "qrel": ([1, 512], "f32"), "adm": ([16, 2, 8192], "bf16"),
    "w_branch_a": ([1024, 2048], "f32"), "w_branch_b": ([1024, 2048], "f32"), "w_out": ([2048, 2048], "f32"),
    "wr": ([2048, 36], "f32"), "brr": ([1, 36], "f32"),
    "ln1_gb": ([128, 2048], "f32"), "ln1_bb": ([128, 2048], "f32"), "ln2_gb": ([128, 2048], "f32"), "ln2_bb": ([128, 2048], "f32"),
    "ltri": ([128, 128], "bf16"), "e256": ([128, 32], "f32"), "iota256": ([128, CAP], "f32"),
    "w_gate": ([NEXP, 2048, 1024], "f32"), "w_up": ([NEXP, 2048, 1024], "f32"), "w_down": ([NEXP, 1024, 2048], "f32"),
}


def _dt(s):
    return {"f32": F32, "bf16": BF16, "u32": U32, "i32": I32}[s]


def host_consts(inputs):
    b_in = np.asarray(inputs["b_in"][0], np.float32)
    bfm = np.zeros((128, NBFM), np.float32)
    def put(idx, c0, n):
        for c in range(n):
            bfm[:, idx + c] = b_in[c0 + c * 128: c0 + (c + 1) * 128]
    put(BFM_IDX["af"], C_AF, 8); put(BFM_IDX["aq"], C_AQ, 8); put(BFM_IDX["ag"], C_AG, 8)
    put(BFM_IDX["bk"], C_BK, 8); put(BFM_IDX["bq"], C_BQ, 8); put(BFM_IDX["iq"], C_IQ, 4)
    put(BFM_IDX["g"], C_G, 32)
    bfm[0:64, BFM_IDX["ik"]] = b_in[C_IK:C_IK + 64]
    bfm[64:128, BFM_IDX["ik"]] = b_in[C_IK:C_IK + 64]
    brow = np.concatenate([b_in[C_AI:C_AI + 1024], b_in[C_BV:C_BV + 1024], b_in[C_IW:C_IW + 8]])[None, :].astype(np.float32)
    lbl = np.ascontiguousarray(np.asarray(inputs["hg_lb_logits"], np.float32).reshape(2, 8, 128).transpose(2, 0, 1))
    normg = np.ascontiguousarray(np.asarray(inputs["hg_norm_g"][0], np.float32).reshape(8, 128).T)
    rmask = np.ones((128, 512), np.float32)
    rmask[:, ::64] = 0.0
    ii = np.arange(128)
    bdmask = ((ii[:, None] // 64 == ii[None, :] // 64) & (ii[:, None] <= ii[None, :])).astype(np.float32)
    import ml_dtypes
    bf = ml_dtypes.bfloat16
    slopes = 2.0 ** -(np.arange(8) + 1.0)
    pp = np.arange(128)
    alb = (slopes[None, :, None] * (pp[:, None, None] + 128.0 * (np.arange(64)[None, None, :] - 60))).astype(np.float32)
    dsq = np.maximum(pp[:, None] - pp[None, :], 0).astype(np.float64)
    corr = np.exp(-2.0 * slopes[None, :, None] * dsq[:, None, :]).astype(bf)
    sel2 = np.zeros((2, 128), np.float32); sel2[0, :64] = 1; sel2[1, 64:] = 1
    dtab = np.abs(8064 + pp[:, None] - np.arange(8192)[None, :]).astype(bf)
    qrel = np.arange(512, dtype=np.float32)[None, :]
    extra = {}
    if "w_out" in inputs:
        extra["w_branch_a"] = np.ascontiguousarray(inputs["w_branch_a"][0]); extra["w_branch_b"] = np.ascontiguousarray(inputs["w_branch_b"][0])
        extra["w_out"] = np.ascontiguousarray(inputs["w_out"][0])
        extra["wr"] = np.ascontiguousarray(np.concatenate([inputs["w_group"][0], inputs["w_router"][0]], axis=1).astype(np.float32))
        extra["brr"] = np.concatenate([inputs["b_group"][0], inputs["b_router"][0]])[None, :].astype(np.float32)
        for nm in ("ln1_g", "ln1_b", "ln2_g", "ln2_b"):
            extra[nm + "b"] = np.ascontiguousarray(np.broadcast_to(np.asarray(inputs[nm][0], np.float32)[None, :], (128, 2048)))
        extra["ltri"] = (pp[:, None] < pp[None, :]).astype(bf)
        extra["e256"] = np.ascontiguousarray(np.broadcast_to((np.arange(32, dtype=np.float32) * CAP)[None, :], (128, 32)))
        extra["iota256"] = np.ascontiguousarray(np.broadcast_to(np.arange(CAP, dtype=np.float32)[None, :], (128, CAP)))
        extra["w_gate"] = np.ascontiguousarray(inputs["w_gate"][0]); extra["w_up"] = np.ascontiguousarray(inputs["w_up"][0])
        extra["w_down"] = np.ascontiguousarray(inputs["w_down"][0])
    return {**extra, "alb": alb, "corr": corr, "sel2": sel2.astype(bf), "dtab": dtab, "qrel": qrel, "bdmask": bdmask, "ident": np.eye(128, dtype=np.float32), "bfm": bfm, "brow": brow, "lbl": lbl, "normg": normg, "rmask": rmask,
            "w_in": np.ascontiguousarray(inputs["w_in"][0])}


def host_core_inputs(inputs, hc, core):
    b, j = core // 4, core % 4
    x = np.asarray(inputs["x"], np.float32)
    xs = np.zeros((8192, 2048), np.float32)
    npre = (3 - j) * 2048
    xs[npre:] = x[b, :(j + 1) * 2048]
    valid = np.zeros(8192, np.float32)
    valid[npre:] = 1.0
    d = dict(hc)
    d["xs"] = xs
    d["valid_tm"] = np.ascontiguousarray(valid.reshape(64, 128).T)
    import ml_dtypes
    chunk = np.arange(8192) // 64
    adm = np.full((16, 2, 8192), NEG_ADM, np.float32)
    for t in range(16):
        c_first = (OWN0 + t * 128) // 64
        adm[t, 0, (valid > 0) & (chunk <= c_first)] = 0.0
        adm[t, 1, (valid > 0) & (chunk <= c_first + 1)] = 0.0
    d["adm"] = adm.astype(ml_dtypes.bfloat16)
    return d


def build_program(phases=("p1a",), dump=(), p1_blocks=(0, 1, 2, 3), p2_groups=(0, 1, 2, 3), in_names=None):
    nc = bass.Bass("TRN2", target_bir_lowering=False)
    A = {}
    used_inputs = in_names if in_names is not None else list(INPUTS)
    for n in used_inputs:
        shp, dt = INPUTS[n]
        A[n] = nc.dram_tensor(n, shp, _dt(dt), kind="ExternalInput").ap()
    for n, (shp, dt) in SCRATCH.items():
        kind = "ExternalOutput" if n in dump else "Internal"
        A[n] = nc.dram_tensor(n, shp, _dt(dt), kind=kind).ap()
    A["out"] = nc.dram_tensor("out", [2048, 2048], F32, kind="ExternalOutput").ap()
    with ExitStack() as es:
        S = Sched(nc, es)
        G = {"ap": A, "pb": PBanks(S), "B": {n: S.buf(n, glob=True) for n in list(SCRATCH) + ["out"]}, "bfm_idx": BFM_IDX}
        G["persist"] = {"wts": S.sbuf("p_wts", [128, 16, 2], F32), "RK": S.sbuf("p_RK", [128, 16, 32], F32), "idx": S.sbuf("p_idx", [128, 16, 2], F32)}
        G["Br_persist"] = S.buf("persist", glob=True)
        cst = {}
        Bc = S.buf("cst", glob=True)
        G["cst"] = cst
        G["Bcst"] = Bc
        idf = S.sbuf("idf", [128, 128], F32)
        Bidf = S.buf("idf", glob=True)
        S.dma("sp", idf[:], A["ident"], writes=[Bidf])
        idb = S.sbuf("idb", [128, 128], BF16)
        Bident = S.buf("idb")
        S.op("dve", lambda e: e.tensor_copy(idb[:], idf[:]), reads=[Bidf], writes=[Bident])
        G["ident_bf"] = idb
        G["Bident"] = Bident
        G["ident_f"] = idf
        G["Bidf"] = Bidf
        for n in ("bfm", "brow", "valid_tm", "normg", "rmask", "bdmask"):
            shp, dt = INPUTS[n]
            cst[n] = S.sbuf("c_" + n, shp, _dt(dt))
            S.dma("sp", cst[n][:], A[n], writes=[Bc], nowaw=True)
        lbl = S.sbuf("c_lbl", [128, 2, 8], F32)
        Blbl = S.buf("lbl", glob=True)
        S.dma("sp", lbl[:], A["lbl"], writes=[Blbl])
        for n in ("lb", "oml", "noml", "lbd"):
            cst[n] = S.sbuf("c_" + n, [128, 8], F32)
        cst["ones_row"] = S.sbuf("c_ones_row", [1, 128], F32)
        S.op("dve", lambda e: e.memset(cst["ones_row"][:], 1.0), writes=[Bc])
        S.op("dve", lambda e: e.tensor_tensor(cst["lbd"][:], lbl[:, 0, :], lbl[:, 1, :], ALU.subtract), reads=[Blbl], writes=[Bc])
        S.op("act", lambda e: e.activation(cst["lb"][:], cst["lbd"][:], AF.Sigmoid), reads=[Bc], writes=[Bc])
        S.op("dve", lambda e: e.tensor_scalar(cst["oml"][:], cst["lb"][:], -1.0, 1.0, op0=ALU.mult, op1=ALU.add), reads=[Bc], writes=[Bc])
        S.op("dve", lambda e: e.tensor_scalar(cst["noml"][:], cst["oml"][:], -1.0, None, op0=ALU.mult), reads=[Bc], writes=[Bc])

        cst["eps_ln"] = S.sbuf("c_eps_ln", [128, 1], F32)
        S.op("dve", lambda e: e.memset(cst["eps_ln"][:], LN_EPS), writes=[Bc])
        cst["eps_rms"] = S.sbuf("c_eps_rms", [128, 1], F32)
        S.op("dve", lambda e: e.memset(cst["eps_rms"][:], RMS_EPS), writes=[Bc])
        S.barrier()
        if "p1a" in phases:
            with ExitStack() as pes:
                S.es = pes
                phase1a(S, G, blocks=p1_blocks)
                S.es = es
            S.barrier()
            S.phase_end()
        if "p1b" in phases:
            with ExitStack() as pes:
                S.es = pes
                phase1b(S, G)
                S.es = es
            S.barrier()
            S.phase_end()
        if "p2" in phases:
            phase2_dsa(S, G, groups=p2_groups)
            S.barrier()
            S.phase_end()
        for nm, fn in (("p3a", phase3a_merge), ("p3b", phase3b_out), ("p4", phase4_moe), ("p5", phase5_final)):
            if nm in phases:
                with ExitStack() as pes:
                    S.es = pes
                    fn(S, G)
                    S.es = es
                S.barrier()
                S.phase_end()
        outs = [G["B"][n] for n in dump] + ([G["B"]["out"]] if "p5" in phases else [])
        S.wait_all("sp", outs)
        print("instructions:", S.ninst, {k: len(v) for k, v in S.ops.items()})
        S.run()
    return nc


ALL_PHASES = ("p1a", "p1b", "p2", "p3a", "p3b", "p4", "p5")
_CACHE = {}


def kernel(**inputs):
    if "nc" not in _CACHE:
        _CACHE["nc"] = build_program(phases=ALL_PHASES)
    nc = _CACHE["nc"]
    hc = host_consts(inputs)
    in_maps = [host_core_inputs(inputs, hc, c) for c in range(8)]
    res = run_bass_kernel_spmd(nc, in_maps, core_ids=list(range(8)))
    out = np.zeros((2, 8192, 2048), np.float32)
    for c in range(8):
        b, j = c // 4, c % 4
        out[b, j * 2048:(j + 1) * 2048] = np.asarray(res.results[c]["out"])
    return out
```

```python
import numpy as np
import concourse.bass as bass
import concourse.mybir as mybir

F32 = mybir.dt.float32
BF16 = mybir.dt.bfloat16
U32 = mybir.dt.uint32
I32 = mybir.dt.int32
U8 = mybir.dt.uint8
AF = mybir.ActivationFunctionType
ALU = mybir.AluOpType
AX = mybir.AxisListType


class Buf:
    __slots__ = ("name", "lastw", "readers", "dsem", "dcount", "glob", "dkey")

    def __init__(self, name, glob=False):
        self.name = name
        self.glob = glob
        self.dkey = None
        self.lastw = None
        self.readers = {}
        self.dsem = None
        self.dcount = 0


class Sched:
    ENGS = ("pe", "act", "dve", "pool", "sp")
    SEM_LIMIT = 30000

    def __init__(self, nc, es):
        self.nc = nc
        self.es = es
        self.es_sem = es
        self.dbufs = []
        self.dstate = {}
        self.free_dsems = []
        self.local_dbufs = []
        self.sem = {}
        self.count = {}
        self.known = {}
        self.ops = {}
        self.epoch = {}
        for n in self.ENGS:
            self.sem[n] = es.enter_context(nc.semaphore("se_" + n))
            self.count[n] = 0
            self.epoch[n] = 0
            self.known[n] = {}
            self.ops[n] = []
        self.nbuf = 0
        self.ninst = 0

    def sbuf(self, name, shape, dtype):
        self.nbuf += 1
        name = "%s_u%d" % (name, self.nbuf)
        return self.es.enter_context(self.nc.sbuf_tensor(name, list(shape), dtype))

    def psum(self, name, shape, dtype):
        return self.es.enter_context(self.nc.psum_tensor(name, list(shape), dtype))

    def buf(self, name=None, glob=False):
        self.nbuf += 1
        return Buf("%s_b%d" % (name or "b", self.nbuf), glob)

    def bufs(self, n, name="b"):
        return [self.buf("%s%d" % (name, i)) for i in range(n)]

    def _waits(self, eng, reads, writes):
        need = {}

        def add(ev, skip_same):
            if ev is None:
                return
            key, sem, val, prod = ev
            if skip_same and prod == eng:
                return
            if self.known[eng].get(key, 0) >= val:
                return
            if key not in need or need[key][1] < val:
                need[key] = (sem, val)

        for b in reads:
            add(b.lastw, False)
        for b in writes:
            add(b.lastw, True)
            for ev in b.readers.values():
                add(ev, True)
        for key, (sem, val) in need.items():
            self.known[eng][key] = val
        return list(need.values())

    def op(self, eng, fn, reads=(), writes=()):
        waits = self._waits(eng, reads, writes)
        if self.count[eng] >= self.SEM_LIMIT:
            self.epoch[eng] += 1
            self.count[eng] = 0
            self.sem[eng] = self.es_sem.enter_context(self.nc.semaphore("se_%s_%d" % (eng, self.epoch[eng])))
        self.count[eng] += 1
        seq = self.count[eng]
        sem = self.sem[eng]
        key = "e_%s_%d" % (eng, self.epoch[eng])
        ev = (key, sem, seq, eng)
        for b in writes:
            b.lastw = ev
            b.readers = {}
        for b in reads:
            b.readers[key] = ev
        self.ninst += 1 + len(waits)

        def emit(e, fn=fn, waits=waits, sem=sem):
            for (s, v) in waits:
                e.wait_ge(s, v)
            fn(e).then_inc(sem, 1)

        self.ops[eng].append(emit)
        return ev

    def dma(self, q, out_ap, in_ap, reads=(), writes=(), nowaw=False, builder=None, **kw):
        waits = self._waits(q, reads, [] if nowaw else writes)
        tb = writes[0]
        if tb.dsem is None:
            if (not tb.glob) and self.free_dsems:
                tb.dsem, tb.dcount, tb.dkey = self.free_dsems.pop()
            else:
                tb.dsem = self.es_sem.enter_context(self.nc.semaphore("sd_" + tb.name))
                tb.dkey = "d_" + tb.name
            if not tb.glob:
                self.local_dbufs.append(tb)
        tb.dcount += 16
        self.dstate[tb.dkey] = (tb.dsem, tb.dcount)
        ev = (tb.dkey, tb.dsem, tb.dcount, None)
        for b in writes:
            b.lastw = ev
            if not nowaw:
                b.readers = {}
        for b in reads:
            b.readers[ev[0]] = ev
        self.ninst += 1 + len(waits)

        def emit(e, waits=waits, sem=tb.dsem, out_ap=out_ap, in_ap=in_ap, kw=kw, builder=builder):
            for (s, v) in waits:
                e.wait_ge(s, v)
            if builder is not None:
                builder(e).then_inc(sem, 16)
            else:
                e.dma_start(out=out_ap, in_=in_ap, **kw).then_inc(sem, 16)

        self.ops[q].append(emit)
        return ev

    def phase_end(self):
        for b in self.local_dbufs:
            self.free_dsems.append((b.dsem, b.dcount, b.dkey))
            b.dsem = None
        self.local_dbufs = []

    def raw(self, eng, fn):
        self.ops[eng].append(lambda e, fn=fn: fn(e))

    def wait_all(self, eng, bufs):
        waits = self._waits(eng, list(bufs), [])

        def emit(e, waits=waits):
            for (s, v) in waits:
                e.wait_ge(s, v)

        self.ops[eng].append(emit)

    def barrier(self):
        evs = []
        for n in self.ENGS:
            if self.count[n] > 0:
                evs.append(("e_%s_%d" % (n, self.epoch[n]), self.sem[n], self.count[n], n))
        for key, (sem, cnt) in self.dstate.items():
            evs.append((key, sem, cnt, None))
        for eng in self.ENGS:
            waits = []
            for (key, sem, val, prod) in evs:
                if prod == eng:
                    continue
                if self.known[eng].get(key, 0) >= val:
                    continue
                self.known[eng][key] = val
                waits.append((sem, val))

            def emit(e, waits=waits):
                for (s, v) in waits:
                    e.wait_ge(s, v)

            self.ops[eng].append(emit)

    def run(self):
        nc = self.nc
        ops = self.ops
        with nc.Block() as block:
            @block.tensor
            def _(e):
                for f in ops["pe"]:
                    f(e)

            @block.scalar
            def _(e):
                for f in ops["act"]:
                    f(e)

            @block.vector
            def _(e):
                for f in ops["dve"]:
                    f(e)

            @block.gpsimd
            def _(e):
                for f in ops["pool"]:
                    f(e)

            @block.sync
            def _(e):
                for f in ops["sp"]:
                    f(e)

NSLOT = 8192
NOWN = 2048
OWN0 = NSLOT - NOWN
D = 2048
KC = 16
C_AQ, C_AF, C_AI, C_AG, C_BQ, C_BK, C_BV, C_IQ, C_IK, C_IW, C_G = 0, 1024, 2048, 3072, 4096, 5120, 6144, 7168, 7680, 7744, 7752
W_SCALE = (8 ** -0.5) * (64 ** -0.5)


class Rot:
    def __init__(self, S, name, shape, dtype, n):
        self.t = [S.sbuf("%s%d" % (name, i), shape, dtype) for i in range(n)]
        self.b = [S.buf("%s%d" % (name, i)) for i in range(n)]
        self.i = 0
        self.n = n

    def next(self):
        k = self.i % self.n
        self.i += 1
        return self.t[k], self.b[k]


class PBanks:
    def __init__(self, S):
        self.t = [S.psum("pb%d" % i, [128, 512], F32) for i in range(8)]
        self.b = [S.buf("pb%d" % i) for i in range(8)]
        self.i = 0

    def next(self, lo=0, hi=8):
        n = hi - lo
        k = lo + (self.i % n)
        self.i += 1
        return self.t[k], self.b[k]


def phase1a(S, G, blocks=(0, 1, 2, 3)):
    A = G["ap"]
    PB = G["pb"]
    ident = G["ident_bf"]
    Bident = G["Bident"]
    w_in = A["w_in"]
    xT = S.sbuf("xT", [128, KC, 2048], BF16)
    BxT = S.bufs(16, "xT")
    xin = Rot(S, "xin", [128, 2048], BF16, 2)
    wt = Rot(S, "wt", [128, KC, 512], BF16, 2)
    f32t = Rot(S, "hgf", [128, 512], F32, 12)
    st16 = Rot(S, "st16", [128, 512], BF16, 8)
    stkT = Rot(S, "stkT", [128, 4, 128], BF16, 2)
    decst = S.sbuf("decst", [128, 8, 32], F32)
    Bdec = S.buf("decst")
    wist = Rot(S, "wist", [128, 8], F32, 2)
    cst = G["cst"]
    Bc = G["Bcst"]
    ones_row_b = S.sbuf("ones_row_b", [1, 128], BF16)
    brow_b = S.sbuf("brow_b", [1, 2056], BF16)
    S.op("dve", lambda e: e.memset(ones_row_b[:], 1.0), writes=[Bc])
    S.op("dve", lambda e: e.tensor_copy(brow_b[:], cst["brow"][:]), reads=[Bc], writes=[Bc])
    evq = [0]

    def evac_engine():
        evq[0] += 1
        return "act" if evq[0] % 2 else "dve"

    def copy_op(eng, out_ap, in_ap, reads, writes):
        if eng == "act":
            S.op("act", lambda e, out_ap=out_ap, in_ap=in_ap: e.activation(out_ap, in_ap, AF.Copy), reads=reads, writes=writes)
        else:
            S.op("dve", lambda e, out_ap=out_ap, in_ap=in_ap: e.tensor_copy(out_ap, in_ap), reads=reads, writes=writes)

    for blk in blocks:
        own = (blk == 3)
        s0 = blk * 2048
        for t in range(16):
            xi, Bxi = xin.next()
            S.dma("pool", xi[:], A["xs"][s0 + t * 128: s0 + (t + 1) * 128, :], writes=[Bxi])
            for q4 in range(4):
                pb, Bpb = PB.next()
                for j in range(4):
                    kc = q4 * 4 + j
                    S.op("pe", lambda e, pb=pb, xi=xi, kc=kc, j=j: e.matmul(
                        pb[:, j * 128:(j + 1) * 128], xi[:, kc * 128:(kc + 1) * 128], ident[:], start=True, stop=True),
                        reads=[Bxi, Bident], writes=[Bpb])
                copy_op(evac_engine(), xT[:, q4 * 4:(q4 + 1) * 4, t * 128:(t + 1) * 128],
                        pb[:].rearrange("p (a b) -> p a b", a=4), [Bpb], [BxT[t]])

        def load_w(cols):
            w, Bw = wt.next()
            off = 0
            for (c0, n) in cols:
                S.dma("pool", w[:, :, off:off + n], w_in[:, c0:c0 + n].rearrange("(kc p) n -> p kc n", p=128), writes=[Bw])
                off += n
            return w, Bw

        def fm_mm(w, Bw, m, g):
            pb, Bpb = PB.next()
            for kc in range(KC):
                S.op("pe", lambda e, pb=pb, w=w, kc=kc, m=m, g=g: e.matmul(
                    pb[:], w[:, kc, m * 128:(m + 1) * 128], xT[:, kc, g * 512:(g + 1) * 512], start=(kc == 0), stop=(kc == KC - 1)),
                    reads=[Bw] + BxT[g * 4:(g + 1) * 4], writes=[Bpb])
            return pb, Bpb

        def simple_fm(w, Bw, m, g, bias_ap, func, dst_ap, Bd, rows=128):
            pb, Bpb = fm_mm(w, Bw, m, g)
            st, Bst = st16.next()
            S.op("act", lambda e, st=st, pb=pb, func=func, bias_ap=bias_ap: e.activation(st[:], pb[:], func, bias=bias_ap), reads=[Bpb, Bc], writes=[Bst])
            S.dma("sp", dst_ap, st[0:rows, :], reads=[Bst], writes=[Bd], nowaw=True)

        tm_units = [("ai", C_AI), ("ai", C_AI + 512), ("bv", C_BV), ("bv", C_BV + 512)]
        for (kind, c0) in tm_units:
            w, Bw = load_w([(c0, 512)])
            for t in range(16):
                pb, Bpb = PB.next()
                for kc in range(KC):
                    S.op("pe", lambda e, pb=pb, w=w, kc=kc, t=t: e.matmul(
                        pb[:], xT[:, kc, t * 128:(t + 1) * 128], w[:, kc, :], start=(kc == 0), stop=False),
                        reads=[Bw, BxT[t]], writes=[Bpb])
                boff = (c0 - C_AI) if kind == "ai" else (1024 + c0 - C_BV)
                S.op("pe", lambda e, pb=pb, boff=boff: e.matmul(pb[:], ones_row_b[0:1, :], brow_b[0:1, boff:boff + 512],
                                                            start=False, stop=True), reads=[Bc], writes=[Bpb])
                st, Bst = st16.next()
                tt = blk * 16 + t
                if kind == "ai":
                    S.op("act", lambda e, st=st, pb=pb, tt=tt: e.activation(st[:], pb[:], AF.Identity, scale=cst["valid_tm"][:, tt:tt + 1]),
                         reads=[Bpb, Bc], writes=[Bst])
                    dst = A["HG_v"][s0 + t * 128:s0 + (t + 1) * 128, c0 - C_AI:c0 - C_AI + 512]
                else:
                    S.op("dve", lambda e, st=st, pb=pb: e.tensor_copy(st[:], pb[:]), reads=[Bpb], writes=[Bst])
                    h0 = (c0 - C_BV) // 128
                    dst = A["VH"][h0:h0 + 4, :, tt, :].rearrange("h p d -> p h d")
                if kind == "ai":
                    S.dma("sp", dst, st[:], reads=[Bst], writes=[G["B"]["HG_v"]], nowaw=True)
                else:
                    S.dma("sp", dst, st[:].rearrange("p (h d) -> p h d", h=4), reads=[Bst], writes=[G["B"]["VH"]], nowaw=True)
        if own:
            w, Bw = load_w([(C_IW, 8)])
            for t in range(16):
                pb, Bpb = PB.next()
                for kc in range(KC):
                    S.op("pe", lambda e, pb=pb, w=w, kc=kc, t=t: e.matmul(
                        pb[:, 0:8], xT[:, kc, t * 128:(t + 1) * 128], w[:, kc, 0:8], start=(kc == 0), stop=False),
                        reads=[Bw, BxT[t]], writes=[Bpb])
                S.op("pe", lambda e, pb=pb: e.matmul(pb[:, 0:8], ones_row_b[0:1, :], brow_b[0:1, 2048:2056],
                                                     start=False, stop=True), reads=[Bc], writes=[Bpb])
                st, Bst = wist.next()
                S.op("dve", lambda e, st=st, pb=pb: e.tensor_scalar(st[:], pb[:, 0:8], W_SCALE, None, op0=ALU.mult), reads=[Bpb], writes=[Bst])
                S.dma("sp", A["WI"][t * 128:(t + 1) * 128, :], st[:], reads=[Bst], writes=[G["B"]["WI"]], nowaw=True)

        for u in range(2):
            w, Bw = load_w([(C_BK + u * 512, 512)])
            for m in range(4):
                h = u * 4 + m
                for g in range(4):
                    simple_fm(w, Bw, m, g, cst["bfm"][:, G["bfm_idx"]["bk"] + h: G["bfm_idx"]["bk"] + h + 1], AF.Identity,
                              A["KT"][h, :, s0 + g * 512:s0 + (g + 1) * 512], G["B"]["KT"])
        w, Bw = load_w([(C_IK, 64), (C_IK, 64)])
        for g in range(4):
            simple_fm(w, Bw, 0, g, cst["bfm"][:, G["bfm_idx"]["ik"]: G["bfm_idx"]["ik"] + 1], AF.Identity,
                      A["KIT"][:, s0 + g * 512:s0 + (g + 1) * 512], G["B"]["KIT"])
        if own:
            for u in range(2):
                w, Bw = load_w([(C_BQ + u * 512, 512)])
                for m in range(4):
                    h = u * 4 + m
                    for g in range(4):
                        simple_fm(w, Bw, m, g, cst["bfm"][:, G["bfm_idx"]["bq"] + h: G["bfm_idx"]["bq"] + h + 1], AF.Identity,
                                  A["QT"][h, :, g * 512:(g + 1) * 512], G["B"]["QT"])
            w, Bw = load_w([(C_IQ, 512)])
            for m in range(4):
                for g in range(4):
                    simple_fm(w, Bw, m, g, cst["bfm"][:, G["bfm_idx"]["iq"] + m: G["bfm_idx"]["iq"] + m + 1], AF.Identity,
                              A["QIT"][m, :, g * 512:(g + 1) * 512], G["B"]["QIT"])
            for u in range(8):
                w, Bw = load_w([(C_G + u * 512, 512)])
                for m in range(4):
                    c = u * 4 + m
                    for g in range(4):
                        simple_fm(w, Bw, m, g, cst["bfm"][:, G["bfm_idx"]["g"] + c: G["bfm_idx"]["g"] + c + 1], AF.Sigmoid,
                                  A["GT"][c, :, g * 512:(g + 1) * 512], G["B"]["GT"])

        deferred = []
        for h in range(8):
            if own:
                w, Bw = load_w([(C_AF + h * 128, 128), (C_AQ + h * 128, 128), (C_AG + h * 128, 128)])
            else:
                if h % 4 == 0:
                    w4, Bw4 = load_w([(C_AF + h * 128, 512)])
                w, Bw = w4, Bw4
            mf = 0 if own else (h % 4)
            bi = G["bfm_idx"]
            oml_h = cst["oml"][:, h:h + 1]
            noml_h = cst["noml"][:, h:h + 1]
            lb_h = cst["lb"][:, h:h + 1]
            b_af = cst["bfm"][:, bi["af"] + h: bi["af"] + h + 1]
            b_aq = cst["bfm"][:, bi["aq"] + h: bi["aq"] + h + 1]
            b_ag = cst["bfm"][:, bi["ag"] + h: bi["ag"] + h + 1]
            for g in range(4):
                pb, Bpb = fm_mm(w, Bw, mf, g)
                while deferred:
                    deferred.pop(0)()
                sg, Bsg = f32t.next()
                S.op("act", lambda e, sg=sg, pb=pb, b_af=b_af: e.activation(sg[:], pb[:], AF.Sigmoid, bias=b_af),
                     reads=[Bpb, Bc], writes=[Bsg])
                lf, Blf = f32t.next()
                S.op("act", lambda e, lf=lf, sg=sg, oml_h=oml_h, lb_h=lb_h: e.activation(lf[:], sg[:], AF.Ln, scale=oml_h, bias=lb_h),
                     reads=[Bsg, Bc], writes=[Blf])
                cum, Bcum = f32t.next()
                S.op("dve", lambda e, cum=cum, lf=lf: e.tensor_tensor_scan(cum[:], cst["rmask"][:], lf[:], 0.0, ALU.mult, ALU.add),
                     reads=[Blf, Bc], writes=[Bcum])
                en, Ben = f32t.next()
                S.op("act", lambda e, en=en, cum=cum: e.activation(en[:], cum[:], AF.Exp, scale=-1.0), reads=[Bcum], writes=[Ben])
                kk, Bkk = f32t.next()
                S.op("dve", lambda e, kk=kk, sg=sg, noml_h=noml_h, oml_h=oml_h: e.tensor_scalar(kk[:], sg[:], noml_h, oml_h, op0=ALU.mult, op1=ALU.add),
                     reads=[Bsg, Bc], writes=[Bkk])
                kd, Bkd = st16.next()
                S.op("dve", lambda e, kd=kd, kk=kk, en=en: e.tensor_tensor(kd[:], kk[:], en[:], ALU.mult), reads=[Bkk, Ben], writes=[Bkd])
                S.dma("sp", A["HG_kdec"][h, :, s0 + g * 512:s0 + (g + 1) * 512], kd[:], reads=[Bkd], writes=[G["B"]["HG_kdec"]], nowaw=True)
                ex2, Bex2 = f32t.next()
                for c in range(8):
                    S.op("act", lambda e, ex2=ex2, cum=cum, c=c: e.activation(ex2[:, c * 64:(c + 1) * 64], cum[:, c * 64:(c + 1) * 64], AF.Exp,
                                                                           scale=-1.0, bias=cum[:, c * 64 + 63:c * 64 + 64]),
                         reads=[Bcum], writes=[Bex2])
                ke, Bke = st16.next()
                S.op("dve", lambda e, ke=ke, kk=kk, ex2=ex2: e.tensor_tensor(ke[:], kk[:], ex2[:], ALU.mult), reads=[Bkk, Bex2], writes=[Bke])
                dec_ap = decst[:, h, g * 8:(g + 1) * 8]
                S.op("act", lambda e, cum=cum, dec_ap=dec_ap: e.activation(dec_ap, cum[:].rearrange("p (c s) -> p c s", s=64)[:, :, 63], AF.Exp),
                     reads=[Bcum], writes=[Bdec])
                def kend_T(ke=ke, Bke=Bke, g=g, h=h, s0=s0):
                    pbt, Bpbt = PB.next()
                    for j in range(4):
                        S.op("pe", lambda e, pbt=pbt, ke=ke, j=j: e.matmul(pbt[:, j * 128:(j + 1) * 128], ke[:, j * 128:(j + 1) * 128], ident[:],
                                                                           start=True, stop=True), reads=[Bke, Bident], writes=[Bpbt])
                    kT, BkT = stkT.next()
                    S.op("dve", lambda e, kT=kT, pbt=pbt: e.tensor_copy(kT[:], pbt[:].rearrange("p (a b) -> p a b", a=4)), reads=[Bpbt], writes=[BkT])
                    S.dma("sp", A["HG_kend"][s0 + g * 512:s0 + (g + 1) * 512, h * 128:(h + 1) * 128].rearrange("(t p) d -> p t d", p=128),
                          kT[:], reads=[BkT], writes=[G["B"]["HG_kend"]], nowaw=True)
                deferred.append(kend_T)
                if own:
                    ec, Bec = f32t.next()
                    S.op("act", lambda e, ec=ec, cum=cum: e.activation(ec[:], cum[:], AF.Exp), reads=[Bcum], writes=[Bec])
                    pbq, Bpbq = fm_mm(w, Bw, 1, g)
                    qs, Bqs = f32t.next()
                    S.op("act", lambda e, qs=qs, pbq=pbq, b_aq=b_aq: e.activation(qs[:], pbq[:], AF.Silu, bias=b_aq),
                         reads=[Bpbq, Bc], writes=[Bqs])
                    qd, Bqd = st16.next()
                    S.op("dve", lambda e, qd=qd, qs=qs, ec=ec: e.tensor_tensor(qd[:], qs[:], ec[:], ALU.mult), reads=[Bqs, Bec], writes=[Bqd])
                    S.dma("sp", A["HG_qdec"][h, :, g * 512:(g + 1) * 512], qd[:], reads=[Bqd], writes=[G["B"]["HG_qdec"]], nowaw=True)
                    simple_fm(w, Bw, 2, g, b_ag, AF.Silu, A["HG_gs"][h, :, g * 512:(g + 1) * 512], G["B"]["HG_gs"])
        while deferred:
            deferred.pop(0)()
        S.dma("sp", A["HG_dec"][:, :, blk * 32:(blk + 1) * 32], decst[:], reads=[Bdec], writes=[G["B"]["HG_dec"]], nowaw=True)

RMS_EPS = 1e-6


def phase1b(S, G):
    A = G["ap"]
    PB = G["pb"]
    cst = G["cst"]
    Bc = G["Bcst"]
    B = G["B"]
    kend = S.sbuf("hs_kend", [128, 16, 1024], BF16)
    Bkend = S.buf("hs_kend")
    vv = S.sbuf("hs_v", [128, 16, 1024], BF16)
    Bvv = S.buf("hs_v")
    dec = S.sbuf("hs_dec", [128, 8, 128], F32)
    Bdec = S.buf("hs_dec")
    Sf = S.sbuf("hs_S", [128, 8, 128], F32)
    Sb = S.sbuf("hs_Sb", [128, 8, 128], BF16)
    BS = S.bufs(8, "hs_S")
    BSb = S.bufs(8, "hs_Sb")
    S.dma("sp", dec[:], A["HG_dec"], reads=[B["HG_dec"]], writes=[Bdec])
    S.op("dve", lambda e: e.memset(Sf[:], 0.0), writes=BS)
    S.op("dve", lambda e: e.memset(Sb[:], 0.0), writes=BSb)
    onesb = S.sbuf("hs_ones", [128, 128], BF16)
    Bones = S.buf("hs_ones")
    S.op("dve", lambda e: e.memset(onesb[:], 1.0 / 128.0), writes=[Bones])
    kdec = Rot(S, "hs_kdec", [128, 2048], BF16, 8)
    qdec = Rot(S, "hs_qdec", [128, 2048], BF16, 8)
    gs = Rot(S, "hs_gs", [128, 2048], BF16, 8)
    attm = Rot(S, "hs_attm", [128, 128], BF16, 8)
    sq = Rot(S, "hs_sq", [128, 128], BF16, 6)
    rs = Rot(S, "hs_rs", [128, 128], F32, 6)
    yy = Rot(S, "hs_y", [128, 128], F32, 6)
    bastr = Rot(S, "hs_bast", [128, 8, 128], BF16, 2)

    def state_update(t, h, half, ):
        p0 = half * 64
        chunk = None
        pk, Bpk = PB.next()
        S.op("pe", lambda e, pk=pk, t=t, h=h, p0=p0: e.matmul(pk[:, 0:128], kend[p0:p0 + 64, t, h * 128:(h + 1) * 128],
                                                            vv[p0:p0 + 64, t, h * 128:(h + 1) * 128], start=True, stop=True),
             reads=[Bkend, Bvv], writes=[Bpk])
        return pk, Bpk

    for blk in range(4):
        own = (blk == 3)
        s0 = blk * 2048
        S.dma("sp", kend[:], A["HG_kend"][s0:s0 + 2048, :].rearrange("(t p) c -> p t c", p=128), reads=[B["HG_kend"]], writes=[Bkend])
        S.dma("sp", vv[:], A["HG_v"][s0:s0 + 2048, :].rearrange("(t p) c -> p t c", p=128), reads=[B["HG_v"]], writes=[Bvv])
        if not own:
            for t in range(16):
                for half in range(2):
                    ch = blk * 32 + t * 2 + half
                    for h in range(8):
                        pk, Bpk = state_update(t, h, half)
                        S.op("dve", lambda e, pk=pk, h=h, ch=ch: e.scalar_tensor_tensor(Sf[:, h, :], Sf[:, h, :], dec[:, h, ch:ch + 1], pk[:, 0:128],
                                                                                      op0=ALU.mult, op1=ALU.add),
                             reads=[Bpk, Bdec, BS[h]], writes=[BS[h]])
            if blk == 2:
                for h in range(8):
                    S.op("act", lambda e, h=h: e.activation(Sb[:, h, :], Sf[:, h, :], AF.Copy), reads=[BS[h]], writes=[BSb[h]])
            continue
        kds, qds, ggs = [], [], []
        for h in range(8):
            kd, Bkd = kdec.next()
            qd, Bqd = qdec.next()
            gg, Bgg = gs.next()
            S.dma("sp", kd[:], A["HG_kdec"][h, :, s0:s0 + 2048], reads=[B["HG_kdec"]], writes=[Bkd])
            S.dma("sp", qd[:], A["HG_qdec"][h], reads=[B["HG_qdec"]], writes=[Bqd])
            S.dma("sp", gg[:], A["HG_gs"][h], reads=[B["HG_gs"]], writes=[Bgg])
            kds.append((kd, Bkd)); qds.append((qd, Bqd)); ggs.append((gg, Bgg))
        for t in range(16):
            tc = slice(t * 128, (t + 1) * 128)
            bt, Bbt = bastr.next()
            for h in range(8):
                kd, Bkd = kds[h]
                qd, Bqd = qds[h]
                gg, Bgg = ggs[h]
                pa, Bpa = PB.next()
                S.op("pe", lambda e, pa=pa, kd=kd, qd=qd, tc=tc: e.matmul(pa[:, 0:128], kd[:, tc], qd[:, tc], start=True, stop=True),
                     reads=[Bkd, Bqd], writes=[Bpa])
                am, Bam = attm.next()
                S.op("dve", lambda e, am=am, pa=pa: e.tensor_tensor(am[:], pa[:, 0:128], cst["bdmask"][:], ALU.mult), reads=[Bpa, Bc], writes=[Bam])
                po, Bpo = PB.next()
                S.op("pe", lambda e, po=po, am=am, t=t, h=h: e.matmul(po[:, 0:128], vv[:, t, h * 128:(h + 1) * 128], am[:], start=True, stop=False),
                     reads=[Bvv, Bam], writes=[Bpo])
                for half in range(2):
                    ch = blk * 32 + t * 2 + half
                    c0 = t * 128 + half * 64
                    S.op("pe", lambda e, po=po, qd=qd, h=h, c0=c0, half=half: e.matmul(po[:, half * 64:(half + 1) * 64], Sb[:, h, :], qd[:, c0:c0 + 64],
                                                                                  start=False, stop=(half == 1)),
                         reads=[BSb[h], Bqd], writes=[Bpo])
                    pk, Bpk = state_update(t, h, half)
                    S.op("dve", lambda e, pk=pk, h=h, ch=ch: e.scalar_tensor_tensor(Sf[:, h, :], Sf[:, h, :], dec[:, h, ch:ch + 1], pk[:, 0:128],
                                                                                  op0=ALU.mult, op1=ALU.add),
                         reads=[Bpk, Bdec, BS[h]], writes=[BS[h]])
                    S.op("act", lambda e, h=h: e.activation(Sb[:, h, :], Sf[:, h, :], AF.Copy), reads=[BS[h]], writes=[BSb[h]])
                q2, Bq2 = sq.next()
                S.op("act", lambda e, q2=q2, po=po: e.activation(q2[:], po[:, 0:128], AF.Square), reads=[Bpo], writes=[Bq2])
                pm, Bpm = PB.next()
                S.op("pe", lambda e, pm=pm, q2=q2: e.matmul(pm[:, 0:128], onesb[:], q2[:], start=True, stop=True), reads=[Bones, Bq2], writes=[Bpm])
                r1, Br1 = rs.next()
                S.op("act", lambda e, r1=r1, pm=pm: e.activation(r1[:], pm[:, 0:128], AF.Sqrt, bias=cst["eps_rms"][:, 0:1]), reads=[Bpm, Bc], writes=[Br1])
                S.op("dve", lambda e, r1=r1: e.reciprocal(r1[:], r1[:]), reads=[Br1], writes=[Br1])
                y1, By1 = yy.next()
                S.op("dve", lambda e, y1=y1, po=po, r1=r1: e.tensor_tensor(y1[:], po[:, 0:128], r1[:], ALU.mult), reads=[Bpo, Br1], writes=[By1])
                S.op("dve", lambda e, y1=y1, gg=gg, tc=tc, h=h, bt=bt: e.scalar_tensor_tensor(bt[:, h, :], y1[:], cst["normg"][:, h:h + 1], gg[:, tc],
                                                                                           op0=ALU.mult, op1=ALU.mult),
                     reads=[By1, Bgg, Bc], writes=[Bbt])
            S.dma("sp", A["BA"][:, :, tc].rearrange("h p t -> p h t"), bt[:], reads=[Bbt], writes=[B["BA"]], nowaw=True)

SM_SCALE = 128 ** -0.5
TOPK = 256
NBIS = 18
NTER = 9
BIS_WIN = 16.0
NEG_ADM = -30000.0


def phase2_dsa(S, G, groups=(0, 1, 2, 3)):
    A = G["ap"]
    PB = G["pb"]
    cst = G["cst"]
    Bc = G["Bcst"]
    B = G["B"]
    ident = G["ident_bf"]
    Bident = G["Bident"]
    es_outer = S.es
    with ExitStack() as pes:
        S.es = pes
        alb = S.sbuf("ds_alb", [128, 8, 64], F32)
        dtab = S.sbuf("ds_dtab", [128, 8192], BF16)
        qrel = S.sbuf("ds_qrel", [1, 512], F32)
        onesr = S.sbuf("ds_onesr", [1, 128], BF16)
        drow = S.sbuf("ds_drow", [1, 512], F32)
        Bdrow = S.buf("ds_drow")
        shrow = S.sbuf("ds_shrow", [1, 8, 512], BF16)
        Bshrow = S.buf("ds_shrow")
        dmc = S.sbuf("ds_dmc", [128, 1], F32)
        Bdmc = S.buf("ds_dmc")
        corr = S.sbuf("ds_corr", [128, 8, 128], BF16)
        sel2 = S.sbuf("ds_sel2", [2, 128], BF16)
        onesb = S.sbuf("ds_ones", [128, 128], BF16)
        Bk = S.buf("ds_const")
        S.dma("sp", alb[:], A["alb"], writes=[Bk], nowaw=True)
        S.dma("sp", corr[:], A["corr"], writes=[Bk], nowaw=True)
        S.dma("sp", sel2[:], A["sel2"], writes=[Bk], nowaw=True)
        S.op("dve", lambda e: e.memset(onesb[:], 1.0), writes=[Bk])
        S.op("dve", lambda e: e.memset(onesr[:], 1.0), writes=[Bk])
        S.dma("sp", dtab[:], A["dtab"], writes=[Bk], nowaw=True)
        S.dma("sp", qrel[:], A["qrel"], writes=[Bk], nowaw=True)
        kit = S.sbuf("ds_kit", [128, 8192], BF16)
        S.dma("sp", kit[:], A["KIT"], reads=[B["KIT"]], writes=[Bk], nowaw=True)
        wi = S.sbuf("ds_wi", [128, 16, 8], F32)
        S.dma("sp", wi[:], A["WI"].rearrange("(t p) h -> p t h", p=128), reads=[B["WI"]], writes=[Bk], nowaw=True)
        maskT = S.sbuf("ds_maskT", [128, 64, 512], U8)
        BmT = S.buf("ds_maskT")
        S.barrier()

        def do_group(g):
            Q0 = OWN0 + g * 512
            with ExitStack() as aes:
                S.es = aes
                qit = S.sbuf("ds_qit", [128, 4, 512], BF16)
                Bqit = S.buf("ds_qit")
                S.dma("sp", qit[:], A["QIT"][:, :, g * 512:(g + 1) * 512].rearrange("m p q -> p m q"), reads=[B["QIT"]], writes=[Bqit])
                sc2 = [S.sbuf("ds_sc", [128, 8192], F32) for _ in range(2)]
                Bsc2 = [S.buf("ds_sc"), S.buf("ds_sc")]
                mq = S.sbuf("ds_mq", [128, 8192], BF16)
                Bmq = S.buf("ds_mq")
                adm = Rot(S, "ds_adm", [2, 512], BF16, 3)
                junk2 = S.sbuf("ds_junk2", [128, 8192], U8)
                Bj2 = S.buf("ds_junk2")
                relu = Rot(S, "ds_relu", [128, 512], BF16, 8)
                dg2 = [S.sbuf("ds_dg", [128, 8, 128], BF16) for _ in range(2)]
                Bdg2 = [S.buf("ds_dg"), S.buf("ds_dg")]
                sm = {n: S.sbuf("ds_s_" + n, [128, 1], F32) for n in ("lo", "hi", "mid", "cnt", "ge", "d", "c0", "d3", "t1", "nt2", "s2", "g2")}
                Bsm = S.buf("ds_small")
                Bth = S.buf("ds_th")
                Bs2 = S.buf("ds_s2")

                def tparams(T):
                    tg = g * 4 + T
                    Qt = Q0 + T * 128
                    nk = Qt + 128
                    nb5 = (nk + 511) // 512
                    return tg, Qt, nk, nb5, nb5 * 512

                def score_blocks(T):
                    tg, Qt, nk, nb5, nkp = tparams(T)
                    sc, Bsc = sc2[T % 2], Bsc2[T % 2]
                    dg, Bdg = dg2[T % 2], Bdg2[T % 2]
                    out = []

                    def prep():
                        for h in range(8):
                            S.op("dve", lambda e, h=h: e.tensor_scalar(dg[:, h, :], ident[:], wi[:, tg, h:h + 1], None, op0=ALU.mult),
                                 reads=[Bident, Bk], writes=[Bdg])
                    out.append(prep)

                    def blk(kb5):
                        ks = slice(kb5 * 512, (kb5 + 1) * 512)
                        ad, Bad = adm.next()
                        S.dma("sp", ad[:], A["adm"][tg, :, ks], writes=[Bad])
                        rl = []
                        for hp in range(4):
                            for par in range(2):
                                pb, Bpb = PB.next()
                                p0 = par * 64
                                S.op("pe", lambda e, pb=pb, hp=hp, p0=p0: e.matmul(
                                    pb[:], qit[p0:p0 + 64, hp, T * 128:(T + 1) * 128], kit[p0:p0 + 64, ks], start=True, stop=True),
                                    reads=[Bqit, Bk], writes=[Bpb])
                                r, Br = relu.next()
                                if par == 0 or hp % 2 == 0:
                                    S.op("act", lambda e, r=r, pb=pb: e.activation(r[:], pb[:], AF.Relu), reads=[Bpb], writes=[Br])
                                else:
                                    S.op("dve", lambda e, r=r, pb=pb: e.tensor_scalar(r[:], pb[:], 0.0, None, op0=ALU.max), reads=[Bpb], writes=[Br])
                                rl.append((r, Br))
                        ps, Bps = PB.next()
                        for h in range(8):
                            r, Br = rl[h]
                            S.op("pe", lambda e, ps=ps, h=h, r=r: e.matmul(ps[:], dg[:, h, :], r[:], start=(h == 0), stop=False),
                                 reads=[Bdg, Br], writes=[Bps])
                        S.op("pe", lambda e, ps=ps, ad=ad: e.matmul(ps[:], sel2[0:2, :], ad[0:2, :], start=False, stop=True),
                             reads=[Bk, Bad], writes=[Bps])
                        S.op("act", lambda e, ps=ps: e.activation(sc[:, ks], ps[:], AF.Copy), reads=[Bps], writes=[Bsc])
                    for kb5 in range(nb5):
                        out.append(lambda kb5=kb5: blk(kb5))
                    return out

                def search_init(T):
                    tg, Qt, nk, nb5, nkp = tparams(T)
                    sc, Bsc = sc2[T % 2], Bsc2[T % 2]
                    scv = sc[:, 0:nkp]
                    S.op("dve", lambda e: e.reduce_max(sm["hi"][:], scv, AX.X), reads=[Bsc], writes=[Bsm])
                    S.op("dve", lambda e: e.tensor_scalar(sm["lo"][:], sm["hi"][:], -BIS_WIN, None, op0=ALU.add), reads=[Bsm], writes=[Bsm])
                    S.op("dve", lambda e: e.tensor_scalar(sm["hi"][:], sm["hi"][:], 1e-3, None, op0=ALU.add), reads=[Bsm], writes=[Bsm, Bth])
                    S.op("dve", lambda e: e.tensor_scalar(mq[:, 0:nkp], scv, sm["lo"][:, 0:1], None, op0=ALU.is_ge, op1=ALU.add,
                                                          accum_out=sm["c0"][:]), reads=[Bsc, Bsm], writes=[Bmq, Bsm])

                def search_round(T, it):
                    tg, Qt, nk, nb5, nkp = tparams(T)
                    sc, Bsc = sc2[T % 2], Bsc2[T % 2]
                    scv = sc[:, 0:nkp]
                    d3 = (BIS_WIN + 1e-3) / (3.0 ** (it + 1))
                    S.op("dve", lambda e: e.tensor_scalar(sm["t1"][:], sm["lo"][:], d3, None, op0=ALU.add), reads=[Bsm], writes=[Bsm])
                    S.op("dve", lambda e: e.tensor_scalar(sm["nt2"][:], sm["lo"][:], -1.0, -2.0 * d3, op0=ALU.mult, op1=ALU.add), reads=[Bsm], writes=[Bsm, Bth])
                    S.op("act", lambda e: e.activation(junk2[:, 0:nkp], scv, AF.Sign, bias=sm["nt2"][:, 0:1], accum_out=sm["s2"][:]),
                         reads=[Bsc, Bth], writes=[Bj2, Bs2])
                    S.op("dve", lambda e: e.tensor_scalar(mq[:, 0:nkp], scv, sm["t1"][:, 0:1], None, op0=ALU.is_ge, op1=ALU.add,
                                                          accum_out=sm["cnt"][:]), reads=[Bsc, Bsm], writes=[Bmq, Bsm])
                    S.op("dve", lambda e: e.tensor_scalar(sm["ge"][:], sm["cnt"][:], TOPK - 0.5, None, op0=ALU.is_ge), reads=[Bsm], writes=[Bsm])
                    S.op("dve", lambda e: e.scalar_tensor_tensor(sm["g2"][:], sm["s2"][:], 2.0 * (TOPK - 0.5) - nkp, sm["ge"][:], op0=ALU.is_ge, op1=ALU.add),
                         reads=[Bs2, Bsm], writes=[Bsm])
                    S.op("dve", lambda e: e.scalar_tensor_tensor(sm["lo"][:], sm["g2"][:], d3, sm["lo"][:], op0=ALU.mult, op1=ALU.add),
                         reads=[Bsm], writes=[Bsm])

                def finalize(T):
                    tg, Qt, nk, nb5, nkp = tparams(T)
                    sc, Bsc = sc2[T % 2], Bsc2[T % 2]
                    scv = sc[:, 0:nkp]
                    S.op("dve", lambda e: e.tensor_scalar(sm["ge"][:], sm["c0"][:], TOPK - 0.5, None, op0=ALU.is_ge), reads=[Bsm], writes=[Bsm])
                    S.op("dve", lambda e: e.tensor_scalar(sm["d"][:], sm["lo"][:], 1000.0, None, op0=ALU.add), reads=[Bsm], writes=[Bsm])
                    S.op("dve", lambda e: e.tensor_scalar(sm["lo"][:], sm["d"][:], sm["ge"][:, 0:1], -1000.0, op0=ALU.mult, op1=ALU.add),
                         reads=[Bsm], writes=[Bsm])
                    S.op("dve", lambda e: e.tensor_scalar(mq[:, 0:nkp], scv, sm["lo"][:, 0:1], None, op0=ALU.is_ge), reads=[Bsc, Bsm], writes=[Bmq])
                    S.op("dve", lambda e: e.scalar_tensor_tensor(sc[:, 0:nk], mq[:, 0:nk], -16384.0, dtab[:, 8192 - nk:8192], op0=ALU.mult, op1=ALU.add),
                         reads=[Bmq, Bk], writes=[Bsc])
                    S.op("dve", lambda e: e.tensor_reduce(dmc[:], sc[:, 0:nk], AX.X, ALU.min), reads=[Bsc], writes=[Bdmc])
                    S.op("dve", lambda e: e.tensor_scalar(dmc[:], dmc[:], 16384.0, None, op0=ALU.add), reads=[Bdmc], writes=[Bdmc])
                    pbd, Bpbd = PB.next()
                    S.op("pe", lambda e: e.matmul(pbd[0:1, 0:128], dmc[:, 0:1], G["ident_f"][:], start=True, stop=True),
                         reads=[Bdmc, G["Bidf"]], writes=[Bpbd])
                    S.op("dve", lambda e: e.tensor_copy(drow[0:1, T * 128:(T + 1) * 128], pbd[0:1, 0:128]), reads=[Bpbd], writes=[Bdrow])
                    nkb = nk // 128
                    for k4 in range(0, nkb, 4):
                        n4 = min(4, nkb - k4)
                        pb, Bpb = PB.next()
                        for j in range(n4):
                            kb = k4 + j
                            S.op("pe", lambda e, pb=pb, j=j, kb=kb: e.matmul(pb[:, j * 128:(j + 1) * 128], mq[:, kb * 128:(kb + 1) * 128], ident[:],
                                                                           start=True, stop=True), reads=[Bmq, Bident], writes=[Bpb])
                        dst = maskT[:, k4:k4 + n4, T * 128:(T + 1) * 128]
                        src = pb[:, 0:n4 * 128].rearrange("p (a b) -> p a b", a=n4)
                        if (k4 // 4) % 2 == 0:
                            S.op("act", lambda e, dst=dst, src=src: e.activation(dst, src, AF.Copy), reads=[Bpb], writes=[BmT])
                        else:
                            S.op("dve", lambda e, dst=dst, src=src: e.tensor_copy(dst, src), reads=[Bpb], writes=[BmT])

                for f_ in score_blocks(0):
                    f_()
                for T in range(4):
                    nxt = score_blocks(T + 1) if T < 3 else []
                    per = -(-len(nxt) // NTER)
                    search_init(T)
                    for it in range(NTER):
                        search_round(T, it)
                        for _ in range(per):
                            if nxt:
                                nxt.pop(0)()
                    while nxt:
                        nxt.pop(0)()
                    finalize(T)
                S.op("dve", lambda e: e.tensor_tensor(drow[:], drow[:], qrel[:], ALU.subtract), reads=[Bdrow, Bk], writes=[Bdrow])
                for h in range(8):
                    S.op("dve", lambda e, h=h: e.tensor_scalar(shrow[0:1, h, :], drow[:], (2.0 ** -(h + 1)) / SM_SCALE, None, op0=ALU.mult),
                         reads=[Bdrow], writes=[Bshrow])
                S.barrier()
                S.phase_end()
            with ExitStack() as bes:
                S.es = bes
                kt = Rot(S, "ds_kt", [128, 8192], BF16, 2)
                vh = Rot(S, "ds_vh", [128, 64, 128], BF16, 2)
                qt = Rot(S, "ds_qt", [128, 512], BF16, 2)
                pT = Rot(S, "ds_pT", [128, 512], BF16, 11)
                mcr = Rot(S, "ds_mc", [128, 128], BF16, 3)
                rec = Rot(S, "ds_rec", [128, 512], F32, 1)
                ob = Rot(S, "ds_ob", [128, 512], BF16, 1)
                nkb = (Q0 + 512) // 128
                kb0 = Q0 // 128
                for h in range(8):
                    k_, Bk_ = kt.next()
                    v_, Bv_ = vh.next()
                    q_, Bq_ = qt.next()
                    S.dma("sp", k_[:, 0:nkb * 128], A["KT"][h, :, 0:nkb * 128], reads=[B["KT"]], writes=[Bk_])
                    S.dma("sp", v_[:, 0:nkb, :], A["VH"][h, :, 0:nkb, :], reads=[B["VH"]], writes=[Bv_])
                    S.dma("sp", q_[:], A["QT"][h, :, g * 512:(g + 1) * 512], reads=[B["QT"]], writes=[Bq_])
                    po, Bpo = PB.t[6], PB.b[6]
                    pd, Bpd = PB.t[7], PB.b[7]
                    pend = []

                    def stage2(kb, p_, Bp_, c0, first, last, po=po, pd=pd, Bpo=Bpo, Bpd=Bpd, v_=v_, Bv_=Bv_):
                        S.op("pe", lambda e, po=po, v_=v_, p_=p_, kb=kb, c0=c0, first=first, last=last: e.matmul(
                            po[:, c0:512], v_[:, kb, :], p_[:, c0:512], start=first, stop=last), reads=[Bv_, Bp_], writes=[Bpo])
                        S.op("pe", lambda e, pd=pd, p_=p_, c0=c0, first=first, last=last: e.matmul(
                            pd[:, c0:512], onesb[:], p_[:, c0:512], start=first, stop=last), reads=[Bk, Bp_], writes=[Bpd])

                    for kb in range(nkb):
                        r = kb - kb0
                        c0 = max(r, 0) * 128
                        first = (kb == 0)
                        last = (kb == nkb - 1)
                        pst, Bpst = PB.next(0, 6)
                        S.op("pe", lambda e, pst=pst, k_=k_, q_=q_, kb=kb, c0=c0: e.matmul(
                            pst[:, c0:512], k_[:, kb * 128:(kb + 1) * 128], q_[:, c0:512], start=True, stop=(h >= 6)),
                            reads=[Bk_, Bq_], writes=[Bpst])
                        if h < 6:
                            S.op("pe", lambda e, pst=pst, h=h, c0=c0: e.matmul(pst[:, c0:512], onesr[0:1, :], shrow[0:1, h, c0:512], start=False, stop=True),
                                 reads=[Bk, Bshrow], writes=[Bpst])
                        p_, Bp_ = pT.next()
                        bias_ap = alb[:, h, r + 60:r + 61]
                        S.op("act", lambda e, p_=p_, pst=pst, c0=c0, bias_ap=bias_ap: e.activation(
                            p_[:, c0:512], pst[:, c0:512], AF.Exp, scale=SM_SCALE, bias=bias_ap), reads=[Bpst, Bk], writes=[Bp_])
                        if r >= 0:
                            mc, Bmc = mcr.next()
                            S.op("dve", lambda e, mc=mc, kb=kb, r=r, h=h: e.tensor_tensor(mc[:], maskT[:, kb, r * 128:(r + 1) * 128], corr[:, h, :], ALU.mult),
                                 reads=[BmT, Bk], writes=[Bmc])
                            S.op("dve", lambda e, p_=p_, mc=mc, r=r: e.scalar_tensor_tensor(p_[:, r * 128:(r + 1) * 128], p_[:, r * 128:(r + 1) * 128], 3.0e38, mc[:],
                                                                                         op0=ALU.min, op1=ALU.mult), reads=[Bmc, Bp_], writes=[Bp_])
                            if c0 + 128 < 512:
                                S.op("dve", lambda e, p_=p_, kb=kb, c0=c0: e.scalar_tensor_tensor(p_[:, c0 + 128:512], p_[:, c0 + 128:512], 3.0e38, maskT[:, kb, c0 + 128:512],
                                                                                               op0=ALU.min, op1=ALU.mult), reads=[BmT, Bp_], writes=[Bp_])
                        else:
                            S.op("dve", lambda e, p_=p_, kb=kb: e.scalar_tensor_tensor(p_[:], p_[:], 3.0e38, maskT[:, kb, :], op0=ALU.min, op1=ALU.mult),
                                 reads=[BmT, Bp_], writes=[Bp_])
                        pend.append((kb, p_, Bp_, c0, first, last))
                        if len(pend) > 8:
                            stage2(*pend.pop(0))
                    while pend:
                        stage2(*pend.pop(0))
                    rc, Brc = rec.next()
                    S.op("dve", lambda e, rc=rc, pd=pd: e.reciprocal(rc[:], pd[:]), reads=[Bpd], writes=[Brc])
                    o_, Bo_ = ob.next()
                    S.op("dve", lambda e, o_=o_, po=po, rc=rc: e.tensor_tensor(o_[:], po[:], rc[:], ALU.mult), reads=[Bpo, Brc], writes=[Bo_])
                    S.dma("sp", A["BB"][h, :, g * 512:(g + 1) * 512], o_[:], reads=[Bo_], writes=[B["BB"]], nowaw=True)
                S.barrier()
                S.phase_end()
        for g in groups:
            do_group(g)
        S.es = es_outer

LN_EPS = 1e-5
DN_ALPHA = 2.0 ** 0.25
CAP = 256
NEXP = 32


def layer_norm_tile(S, G, pre, Bpre, out_t, Bout, gb, bb, Bgb, small, Bsmall, junk, Bjunk):
    s1, s2, mean, var, rstd, nmr = small
    S.op("act", lambda e: e.activation(junk[:], pre[:], AF.Identity, accum_out=s1[:]), reads=[Bpre], writes=[Bjunk, Bsmall])
    S.op("act", lambda e: e.activation(junk[:], pre[:], AF.Square, accum_out=s2[:]), reads=[Bpre], writes=[Bjunk, Bsmall])
    S.op("dve", lambda e: e.tensor_scalar(mean[:], s1[:], 1.0 / 2048.0, None, op0=ALU.mult), reads=[Bsmall], writes=[Bsmall])
    S.op("dve", lambda e: e.tensor_tensor(var[:], mean[:], mean[:], ALU.mult), reads=[Bsmall], writes=[Bsmall])
    S.op("dve", lambda e: e.scalar_tensor_tensor(var[:], s2[:], 1.0 / 2048.0, var[:], op0=ALU.mult, op1=ALU.subtract), reads=[Bsmall], writes=[Bsmall])
    S.op("act", lambda e: e.activation(rstd[:], var[:], AF.Sqrt, bias=G["cst"]["eps_ln"][:, 0:1]), reads=[Bsmall, G["Bcst"]], writes=[Bsmall])
    S.op("dve", lambda e: e.reciprocal(rstd[:], rstd[:]), reads=[Bsmall], writes=[Bsmall])
    S.op("dve", lambda e: e.scalar_tensor_tensor(nmr[:], mean[:], -1.0, rstd[:], op0=ALU.mult, op1=ALU.mult), reads=[Bsmall], writes=[Bsmall])
    S.op("act", lambda e: e.activation(pre[:], pre[:], AF.Identity, scale=rstd[:, 0:1], bias=nmr[:, 0:1]), reads=[Bpre, Bsmall], writes=[Bpre])
    S.op("dve", lambda e: e.tensor_tensor(pre[:], pre[:], gb[:], ALU.mult), reads=[Bpre, Bgb], writes=[Bpre])
    S.op("dve", lambda e: e.tensor_tensor(out_t[:], pre[:], bb[:], ALU.add), reads=[Bpre, Bgb], writes=[Bout])


def phase3a_merge(S, G):
    A = G["ap"]
    PB = G["pb"]
    B = G["B"]
    wa = S.sbuf("o_wa", [128, 8, 2048], BF16)
    wb = S.sbuf("o_wb", [128, 8, 2048], BF16)
    Bw = S.buf("o_w")
    S.dma("pool", wa[:], A["w_branch_a"].rearrange("(kc p) n -> p kc n", p=128), writes=[Bw], nowaw=True)
    S.dma("pool", wb[:], A["w_branch_b"].rearrange("(kc p) n -> p kc n", p=128), writes=[Bw], nowaw=True)
    bag = Rot(S, "o_ba", [128, 8, 512], BF16, 2)
    bbg = Rot(S, "o_bb", [128, 8, 512], BF16, 2)
    gtg = Rot(S, "o_gt", [128, 32, 512], BF16, 2)
    t1r = Rot(S, "o_t1", [128, 512], F32, 2)
    t2r = Rot(S, "o_t2", [128, 512], F32, 2)
    mgs = Rot(S, "o_mgs", [128, 512], BF16, 3)
    for g in range(4):
        ba_, Bba = bag.next()
        bb_, Bbb = bbg.next()
        gt_, Bgt = gtg.next()
        S.dma("sp", ba_[:], A["BA"][:, :, g * 512:(g + 1) * 512].rearrange("h p t -> p h t"), reads=[B["BA"]], writes=[Bba])
        S.dma("sp", bb_[:], A["BB"][:, :, g * 512:(g + 1) * 512].rearrange("h p t -> p h t"), reads=[B["BB"]], writes=[Bbb])
        S.dma("sp", gt_[:], A["GT"][:, :, g * 512:(g + 1) * 512].rearrange("c p t -> p c t"), reads=[B["GT"]], writes=[Bgt])
        for c in range(16):
            pa, Bpa = PB.next()
            for k in range(8):
                S.op("pe", lambda e, pa=pa, k=k, c=c, ba_=ba_: e.matmul(pa[:], wa[:, k, c * 128:(c + 1) * 128], ba_[:, k, :], start=(k == 0), stop=(k == 7)),
                     reads=[Bw, Bba], writes=[Bpa])
            pb2, Bpb2 = PB.next()
            for k in range(8):
                S.op("pe", lambda e, pb2=pb2, k=k, c=c, bb_=bb_: e.matmul(pb2[:], wb[:, k, c * 128:(c + 1) * 128], bb_[:, k, :], start=(k == 0), stop=(k == 7)),
                     reads=[Bw, Bbb], writes=[Bpb2])
            t1, Bt1 = t1r.next()
            t2, Bt2 = t2r.next()
            S.op("dve", lambda e, t1=t1, pa=pa, gt_=gt_, c=c: e.tensor_tensor(t1[:], pa[:], gt_[:, c, :], ALU.mult), reads=[Bpa, Bgt], writes=[Bt1])
            S.op("dve", lambda e, t2=t2, pb2=pb2, gt_=gt_, c=c: e.tensor_tensor(t2[:], pb2[:], gt_[:, 16 + c, :], ALU.mult), reads=[Bpb2, Bgt], writes=[Bt2])
            m_, Bm = mgs.next()
            S.op("dve", lambda e, t1=t1, t2=t2, m_=m_: e.tensor_tensor(m_[:], t1[:], t2[:], ALU.add), reads=[Bt1, Bt2], writes=[Bm])
            S.dma("sp", A["MG"][c, :, g * 512:(g + 1) * 512], m_[:], reads=[Bm], writes=[B["MG"]], nowaw=True)


def phase3b_out(S, G):
    A = G["ap"]
    PB = G["pb"]
    B = G["B"]
    P = G["persist"]
    ident_f = G["ident_f"]
    Bw = S.buf("o_w")
    wr = S.sbuf("o_wr", [128, 16, 36], F32)
    S.dma("sp", wr[:], A["wr"].rearrange("(kc p) n -> p kc n", p=128), writes=[Bw], nowaw=True)
    brr = S.sbuf("o_brr", [1, 36], F32)
    S.dma("sp", brr[:], A["brr"], writes=[Bw], nowaw=True)
    g1 = S.sbuf("o_g1", [128, 2048], F32)
    b1 = S.sbuf("o_b1", [128, 2048], F32)
    S.dma("sp", g1[:], A["ln1_gb"], writes=[Bw], nowaw=True)
    S.dma("sp", b1[:], A["ln1_bb"], writes=[Bw], nowaw=True)
    ltri = S.sbuf("o_ltri", [128, 128], BF16)
    S.dma("sp", ltri[:], A["ltri"], writes=[Bw], nowaw=True)
    e256 = S.sbuf("o_e256", [128, 32], F32)
    S.dma("sp", e256[:], A["e256"], writes=[Bw], nowaw=True)
    onesc = S.sbuf("o_onesc", [128, 1], BF16)
    S.op("dve", lambda e: e.memset(onesc[:], 1.0), writes=[Bw])
    onesr = S.sbuf("o_onesr", [1, 128], F32)
    S.op("dve", lambda e: e.memset(onesr[:], 1.0), writes=[Bw])
    cnt = S.sbuf("o_cnt", [1, 32], F32)
    Bcnt = S.buf("o_cnt")
    S.op("dve", lambda e: e.memset(cnt[:], 0.0), writes=[Bcnt])
    S.barrier()
    wo = Rot(S, "o_wo", [128, 16, 512], BF16, 2)
    mgr = Rot(S, "o_mg", [128, 16, 512], BF16, 2)
    pre = Rot(S, "o_pre", [128, 2048], F32, 5)
    h1t = Rot(S, "o_h1", [128, 2048], F32, 2)
    h1b = Rot(S, "o_h1b", [128, 2048], BF16, 2)
    junk = S.sbuf("o_junk", [128, 2048], BF16)
    Bjunk = S.buf("o_junk")
    hT = Rot(S, "o_hT", [128, 16, 128], F32, 1)
    smalls = [S.sbuf("o_sm%d" % i, [128, 1], F32) for i in range(6)]
    Bsmall = S.buf("o_small")
    rt = {n: S.sbuf("o_r_" + n, shp, F32) for n, shp in (
        ("L", [128, 36]), ("gmax", [128, 1]), ("ngmax", [128, 1]), ("ohg", [128, 4]), ("ge", [128, 4]), ("gsum", [128, 1]), ("gp", [128, 1]),
        ("e8", [128, 8]), ("m1", [128, 1]), ("oh1", [128, 8]), ("e8b", [128, 8]), ("m2", [128, 1]), ("oh2", [128, 8]), ("d", [128, 1]),
        ("sg", [128, 1]), ("A1", [128, 32]), ("A2", [128, 32]), ("At", [128, 32]), ("rk", [128, 32]), ("t", [128, 32]), ("i1", [128, 1]), ("i2", [128, 1]))}
    Abf = S.sbuf("o_Abf", [128, 32], BF16)
    Br = G["Br_persist"]

    def rop(fn, eng="dve"):
        S.op(eng, fn, reads=[Br, Bw], writes=[Br])

    for g in range(4):
        mg, Bmg = mgr.next()
        S.dma("sp", mg[:], A["MG"][:, :, g * 512:(g + 1) * 512].rearrange("c p t -> p c t"), reads=[B["MG"]], writes=[Bmg])
        prs = []
        for t in range(4):
            tt = g * 4 + t
            pr, Bpr = pre.next()
            S.dma("sp", pr[:], A["xs"][OWN0 + tt * 128:OWN0 + (tt + 1) * 128, :], writes=[Bpr])
            prs.append((pr, Bpr))
        for cb in range(4):
            w_, Bwo = wo.next()
            S.dma("pool", w_[:], A["w_out"][:, cb * 512:(cb + 1) * 512].rearrange("(kc p) n -> p kc n", p=128), writes=[Bwo])
            for t in range(4):
                pr, Bpr = prs[t]
                po, Bpo = PB.next()
                for c in range(16):
                    S.op("pe", lambda e, po=po, c=c, t=t, w_=w_, mg=mg: e.matmul(po[:], mg[:, c, t * 128:(t + 1) * 128], w_[:, c, :], start=(c == 0), stop=(c == 15)),
                         reads=[Bmg, Bwo], writes=[Bpo])
                S.op("dve", lambda e, pr=pr, po=po, cb=cb: e.scalar_tensor_tensor(pr[:, cb * 512:(cb + 1) * 512], pr[:, cb * 512:(cb + 1) * 512], DN_ALPHA, po[:],
                                                                                op0=ALU.mult, op1=ALU.add), reads=[Bpo, Bpr], writes=[Bpr])
        for t in range(4):
            tt = g * 4 + t
            pr, Bpr = prs[t]
            h_, Bh = h1t.next()
            layer_norm_tile(S, G, pr, Bpr, h_, Bh, g1, b1, Bw, smalls, Bsmall, junk, Bjunk)
            S.dma("sp", A["H1"][tt * 128:(tt + 1) * 128, :], h_[:], reads=[Bh], writes=[B["H1"]], nowaw=True)
            hb, Bhb = h1b.next()
            S.op("act", lambda e, hb=hb, h_=h_: e.activation(hb[:], h_[:], AF.Copy), reads=[Bh], writes=[Bhb])
            S.dma("sp", A["H1b"][tt * 128:(tt + 1) * 128, :], hb[:], reads=[Bhb], writes=[B["H1b"]], nowaw=True)
            hT_, BhT = hT.next()
            for q4 in range(4):
                pb, Bpb = PB.next()
                for j in range(4):
                    c = q4 * 4 + j
                    S.op("pe", lambda e, pb=pb, h_=h_, c=c, j=j: e.matmul(pb[:, j * 128:(j + 1) * 128], h_[:, c * 128:(c + 1) * 128], ident_f[:], start=True, stop=True),
                         reads=[Bh, G["Bidf"]], writes=[Bpb])
                S.op("act", lambda e, pb=pb, hT_=hT_, q4=q4: e.activation(hT_[:, q4 * 4:(q4 + 1) * 4, :], pb[:].rearrange("p (a b) -> p a b", a=4), AF.Copy),
                     reads=[Bpb], writes=[BhT])
            pl, Bpl = PB.next()
            for c in range(16):
                S.op("pe", lambda e, pl=pl, hT_=hT_, c=c: e.matmul(pl[:, 0:36], hT_[:, c, :], wr[:, c, :], start=(c == 0), stop=False), reads=[BhT, Bw], writes=[Bpl])
            S.op("pe", lambda e, pl=pl: e.matmul(pl[:, 0:36], onesr[0:1, :], brr[0:1, :], start=False, stop=True), reads=[Bw], writes=[Bpl])
            L = rt["L"]
            S.op("dve", lambda e, pl=pl: e.tensor_copy(L[:], pl[:, 0:36]), reads=[Bpl], writes=[Br])
            rop(lambda e: e.reduce_max(rt["gmax"][:], L[:, 0:4], AX.X))
            rop(lambda e: e.tensor_scalar(rt["ohg"][:], L[:, 0:4], rt["gmax"][:, 0:1], None, op0=ALU.is_equal))
            rop(lambda e: e.tensor_scalar(rt["ngmax"][:], rt["gmax"][:], -1.0, None, op0=ALU.mult))
            rop(lambda e: e.activation(rt["ge"][:], L[:, 0:4], AF.Exp, bias=rt["ngmax"][:, 0:1], accum_out=rt["gsum"][:]), "act")
            rop(lambda e: e.reciprocal(rt["gp"][:], rt["gsum"][:]))
            rop(lambda e: e.tensor_scalar(rt["e8"][:], L[:, 4:12], rt["ohg"][:, 0:1], None, op0=ALU.mult))
            for gg in range(1, 4):
                rop(lambda e, gg=gg: e.scalar_tensor_tensor(rt["e8"][:], L[:, 4 + 8 * gg:12 + 8 * gg], rt["ohg"][:, gg:gg + 1], rt["e8"][:], op0=ALU.mult, op1=ALU.add))
            rop(lambda e: e.reduce_max(rt["m1"][:], rt["e8"][:], AX.X))
            rop(lambda e: e.tensor_scalar(rt["oh1"][:], rt["e8"][:], rt["m1"][:, 0:1], None, op0=ALU.is_equal))
            rop(lambda e: e.scalar_tensor_tensor(rt["e8b"][:], rt["oh1"][:], -1.0e30, rt["e8"][:], op0=ALU.mult, op1=ALU.add))
            rop(lambda e: e.reduce_max(rt["m2"][:], rt["e8b"][:], AX.X))
            rop(lambda e: e.tensor_scalar(rt["oh2"][:], rt["e8b"][:], rt["m2"][:, 0:1], None, op0=ALU.is_equal))
            rop(lambda e: e.tensor_tensor(rt["d"][:], rt["m1"][:], rt["m2"][:], ALU.subtract))
            rop(lambda e: e.activation(rt["sg"][:], rt["d"][:], AF.Sigmoid), "act")
            rop(lambda e, tt=tt: e.tensor_tensor(P["wts"][:, tt, 0:1], rt["sg"][:], rt["gp"][:], ALU.mult))
            rop(lambda e, tt=tt: e.tensor_tensor(P["wts"][:, tt, 1:2], rt["gp"][:], P["wts"][:, tt, 0:1], ALU.subtract))
            for gg in range(4):
                rop(lambda e, gg=gg: e.tensor_scalar(rt["A1"][:, gg * 8:(gg + 1) * 8], rt["oh1"][:], rt["ohg"][:, gg:gg + 1], None, op0=ALU.mult))
                rop(lambda e, gg=gg: e.tensor_scalar(rt["A2"][:, gg * 8:(gg + 1) * 8], rt["oh2"][:], rt["ohg"][:, gg:gg + 1], None, op0=ALU.mult))
            rop(lambda e: e.tensor_tensor(rt["At"][:], rt["A1"][:], rt["A2"][:], ALU.add))
            rop(lambda e: e.tensor_copy(Abf[:], rt["At"][:]))
            pk, Bpk = PB.next()
            S.op("pe", lambda e, pk=pk: e.matmul(pk[:, 0:32], ltri[:], Abf[:], start=True, stop=False), reads=[Bw, Br], writes=[Bpk])
            S.op("pe", lambda e, pk=pk: e.matmul(pk[:, 0:32], onesr[0:1, :], cnt[0:1, :], start=False, stop=True), reads=[Bw, Bcnt], writes=[Bpk])
            pc, Bpc = PB.next()
            S.op("pe", lambda e, pc=pc: e.matmul(pc[0:1, 0:32], onesc[:], Abf[:], start=True, stop=True), reads=[Bw, Br], writes=[Bpc])
            S.op("dve", lambda e, pk=pk: e.tensor_copy(rt["rk"][:], pk[:, 0:32]), reads=[Bpk, Br], writes=[Br])
            S.op("dve", lambda e, pc=pc: e.tensor_tensor(cnt[:], cnt[:], pc[0:1, 0:32], ALU.add), reads=[Bpc, Bcnt], writes=[Bcnt])
            rop(lambda e: e.scalar_tensor_tensor(rt["t"][:], rt["rk"][:], 1.0, rt["At"][:], op0=ALU.add, op1=ALU.mult))
            rop(lambda e, tt=tt: e.tensor_scalar(P["RK"][:, tt, :], rt["t"][:], -1.0, None, op0=ALU.add))
            rop(lambda e: e.tensor_tensor(rt["rk"][:], rt["rk"][:], e256[:], ALU.add))
            rop(lambda e: e.tensor_tensor(rt["t"][:], rt["rk"][:], rt["A1"][:], ALU.mult))
            rop(lambda e: e.reduce_sum(rt["i1"][:], rt["t"][:], AX.X))
            rop(lambda e: e.tensor_tensor(rt["t"][:], rt["rk"][:], rt["A2"][:], ALU.mult))
            rop(lambda e: e.reduce_sum(rt["i2"][:], rt["t"][:], AX.X))
            rop(lambda e, tt=tt: e.tensor_copy(P["idx"][:, tt, 0:1], rt["i1"][:]))
            rop(lambda e, tt=tt: e.tensor_copy(P["idx"][:, tt, 1:2], rt["i2"][:]))


def phase4_moe(S, G, experts=range(NEXP)):
    A = G["ap"]
    PB = G["pb"]
    B = G["B"]
    P = G["persist"]
    h1b = S.sbuf("m_h1b", [128, 16, 2048], BF16)
    Bh1b = S.buf("m_h1b")
    S.dma("sp", h1b[:], A["H1b"].rearrange("(t p) d -> p t d", p=128), reads=[B["H1b"]], writes=[Bh1b])
    iot = S.sbuf("m_iota", [128, CAP], F32)
    Biot = S.buf("m_iota")
    S.dma("sp", iot[:], A["iota256"], writes=[Biot])
    wu = Rot(S, "m_w", [128, 16, 512], BF16, 4)
    wd = Rot(S, "m_wd", [128, 4, 2048], BF16, 2)
    sel = Rot(S, "m_sel", [128, 16, CAP], BF16, 1)
    xs_ = Rot(S, "m_xs", [128, 16, CAP], BF16, 1)
    hT = Rot(S, "m_hT", [128, 8, CAP], BF16, 2)
    sg = Rot(S, "m_sg", [128, CAP], F32, 3)
    yst = Rot(S, "m_y", [128, 1024], F32, 1)
    for e_ in experts:
        s_, Bs = sel.next()
        for tt in range(16):
            S.op("dve", lambda e, s_=s_, tt=tt, e_=e_: e.tensor_scalar(s_[:, tt, :], iot[:], P["RK"][:, tt, e_:e_ + 1], None, op0=ALU.is_equal),
                 reads=[Biot, G["Br_persist"]], writes=[Bs])
        x_, Bx = xs_.next()
        for kc in range(16):
            pb, Bpb = PB.next()
            for tt in range(16):
                S.op("pe", lambda e, pb=pb, tt=tt, kc=kc, s_=s_: e.matmul(pb[:, 0:CAP], h1b[:, tt, kc * 128:(kc + 1) * 128], s_[:, tt, :],
                                                                         start=(tt == 0), stop=(tt == 15)), reads=[Bh1b, Bs], writes=[Bpb])
            if kc % 2 == 0:
                S.op("act", lambda e, pb=pb, x_=x_, kc=kc: e.activation(x_[:, kc, :], pb[:, 0:CAP], AF.Copy), reads=[Bpb], writes=[Bx])
            else:
                S.op("dve", lambda e, pb=pb, x_=x_, kc=kc: e.tensor_copy(x_[:, kc, :], pb[:, 0:CAP]), reads=[Bpb], writes=[Bx])
        h_, Bh = hT.next()
        for half in range(2):
            wg_, Bwg = wu.next()
            S.dma("pool", wg_[:], A["w_gate"][e_, :, half * 512:(half + 1) * 512].rearrange("(kc p) n -> p kc n", p=128), writes=[Bwg])
            wu_, Bwu = wu.next()
            S.dma("pool", wu_[:], A["w_up"][e_, :, half * 512:(half + 1) * 512].rearrange("(kc p) n -> p kc n", p=128), writes=[Bwu])
            for f4 in range(4):
                f = half * 4 + f4
                pg, Bpg = PB.next()
                for kc in range(16):
                    S.op("pe", lambda e, pg=pg, kc=kc, f4=f4, wg_=wg_, x_=x_: e.matmul(pg[:, 0:CAP], wg_[:, kc, f4 * 128:(f4 + 1) * 128], x_[:, kc, :],
                                                                                    start=(kc == 0), stop=(kc == 15)), reads=[Bwg, Bx], writes=[Bpg])
                for kc in range(16):
                    S.op("pe", lambda e, pg=pg, kc=kc, f4=f4, wu_=wu_, x_=x_: e.matmul(pg[:, CAP:2 * CAP], wu_[:, kc, f4 * 128:(f4 + 1) * 128], x_[:, kc, :],
                                                                                    start=(kc == 0), stop=(kc == 15)), reads=[Bwu, Bx], writes=[Bpg])
                s1, Bs1 = sg.next()
                S.op("act", lambda e, s1=s1, pg=pg: e.activation(s1[:], pg[:, 0:CAP], AF.Silu), reads=[Bpg], writes=[Bs1])
                S.op("dve", lambda e, h_=h_, f=f, s1=s1, pg=pg: e.tensor_tensor(h_[:, f, :], s1[:], pg[:, CAP:2 * CAP], ALU.mult), reads=[Bs1, Bpg], writes=[Bh])
        wds = []
        for half in range(2):
            wd_, Bwd = wd.next()
            S.dma("pool", wd_[:], A["w_down"][e_, half * 512:(half + 1) * 512, :].rearrange("(fc p) n -> p fc n", p=128), writes=[Bwd])
            wds.append((wd_, Bwd))
        for rh in range(CAP // 128):
            for cbp in range(2):
                y_, By = yst.next()
                for c2 in range(2):
                    cb = cbp * 2 + c2
                    py, Bpy = PB.next()
                    for f in range(8):
                        wd_, Bwd = wds[f // 4]
                        S.op("pe", lambda e, py=py, f=f, rh=rh, cb=cb, wd_=wd_, h_=h_: e.matmul(py[:], h_[:, f, rh * 128:(rh + 1) * 128], wd_[:, f % 4, cb * 512:(cb + 1) * 512],
                                                                                             start=(f == 0), stop=(f == 7)), reads=[Bh, Bwd], writes=[Bpy])
                    if c2 == 0:
                        S.op("act", lambda e, y_=y_, py=py, c2=c2: e.activation(y_[:, c2 * 512:(c2 + 1) * 512], py[:], AF.Copy), reads=[Bpy], writes=[By])
                    else:
                        S.op("dve", lambda e, y_=y_, py=py, c2=c2: e.tensor_copy(y_[:, c2 * 512:(c2 + 1) * 512], py[:]), reads=[Bpy], writes=[By])
                S.dma("sp", A["Y"][e_ * CAP + rh * 128:e_ * CAP + (rh + 1) * 128, cbp * 1024:(cbp + 1) * 1024], y_[:], reads=[By], writes=[B["Y"]], nowaw=True)


def phase5_final(S, G):
    A = G["ap"]
    B = G["B"]
    P = G["persist"]
    g2 = S.sbuf("f_g2", [128, 2048], F32)
    b2 = S.sbuf("f_b2", [128, 2048], F32)
    Bw = S.buf("f_w")
    S.dma("sp", g2[:], A["ln2_gb"], writes=[Bw], nowaw=True)
    S.dma("sp", b2[:], A["ln2_bb"], writes=[Bw], nowaw=True)
    idxi = S.sbuf("f_idx", [128, 16, 2], U32)
    Bidx = S.buf("f_idx")
    S.op("dve", lambda e: e.tensor_copy(idxi[:], P["idx"][:]), reads=[G["Br_persist"]], writes=[Bidx])
    S.barrier()
    h1 = Rot(S, "f_h1", [128, 2048], F32, 2)
    y1 = Rot(S, "f_y1", [128, 2048], F32, 2)
    y2 = Rot(S, "f_y2", [128, 2048], F32, 2)
    ot = Rot(S, "f_ot", [128, 2048], F32, 2)
    junk = S.sbuf("f_junk", [128, 2048], BF16)
    Bjunk = S.buf("f_junk")
    smalls = [S.sbuf("f_sm%d" % i, [128, 1], F32) for i in range(6)]
    Bsmall = S.buf("f_small")
    for tt in range(16):
        h_, Bh = h1.next()
        S.dma("sp", h_[:], A["H1"][tt * 128:(tt + 1) * 128, :], reads=[B["H1"]], writes=[Bh])
        a_, Ba = y1.next()
        b_, Bb = y2.next()
        for (dst, Bd, k) in ((a_, Ba, 0), (b_, Bb, 1)):
            S.dma("pool", None, None, reads=[B["Y"], Bidx], writes=[Bd],
                  builder=lambda e, dst=dst, tt=tt, k=k: e.indirect_dma_start(
                      out=dst[:], out_offset=None, in_=A["Y"], in_offset=bass.IndirectOffsetOnAxis(ap=idxi[:, tt, k:k + 1], axis=0),
                      bounds_check=NEXP * CAP - 1, oob_is_err=False))
        S.op("dve", lambda e, a_=a_, tt=tt: e.tensor_scalar(a_[:], a_[:], P["wts"][:, tt, 0:1], None, op0=ALU.mult), reads=[Ba, G["Br_persist"]], writes=[Ba])
        S.op("dve", lambda e, a_=a_, b_=b_, tt=tt: e.scalar_tensor_tensor(a_[:], b_[:], P["wts"][:, tt, 1:2], a_[:], op0=ALU.mult, op1=ALU.add),
             reads=[Ba, Bb, G["Br_persist"]], writes=[Ba])
        S.op("dve", lambda e, a_=a_, h_=h_: e.scalar_tensor_tensor(a_[:], h_[:], DN_ALPHA, a_[:], op0=ALU.mult, op1=ALU.add), reads=[Ba, Bh], writes=[Ba])
        o_, Bo = ot.next()
        layer_norm_tile(S, G, a_, Ba, o_, Bo, g2, b2, Bw, smalls, Bsmall, junk, Bjunk)
        S.dma("sp", A["out"][tt * 128:(tt + 1) * 128, :], o_[:], reads=[Bo], writes=[B["out"]], nowaw=True)

from contextlib import ExitStack
from concourse.bass_utils import run_bass_kernel_spmd

BFM_IDX = {"af": 0, "aq": 8, "ag": 16, "bk": 24, "bq": 32, "iq": 40, "ik": 44, "g": 45}
NBFM = 77

SCRATCH = {
    "KT": ([8, 128, 8192], "bf16"), "VH": ([8, 128, 64, 128], "bf16"), "KIT": ([128, 8192], "bf16"),
    "HG_kdec": ([8, 128, 8192], "bf16"), "HG_kend": ([8192, 1024], "bf16"), "HG_v": ([8192, 1024], "bf16"),
    "HG_dec": ([128, 8, 128], "f32"), "HG_qdec": ([8, 128, 2048], "bf16"), "HG_gs": ([8, 128, 2048], "bf16"),
    "QT": ([8, 128, 2048], "bf16"), "QIT": ([4, 128, 2048], "bf16"), "WI": ([2048, 8], "f32"),
    "GT": ([32, 128, 2048], "bf16"), "BA": ([8, 128, 2048], "bf16"), "BB": ([8, 128, 2048], "bf16"),
    "MG": ([16, 128, 2048], "bf16"), "H1": ([2048, 2048], "f32"), "H1b": ([2048, 2048], "bf16"), "Y": ([NEXP * CAP, 2048], "f32"),
}

INPUTS = {
    "xs": ([8192, 2048], "f32"), "valid_tm": ([128, 64], "f32"), "w_in": ([2048, 11848], "f32"),
    "ident": ([128, 128], "f32"), "bfm": ([128, NBFM], "f32"), "brow": ([1, 2056], "f32"),
    "lbl": ([128, 2, 8], "f32"), "normg": ([128, 8], "f32"), "rmask": ([128, 512], "f32"), "bdmask": ([128, 128], "f32"),
    "alb": ([128, 8, 64], "f32"), "corr": ([128, 8, 128], "bf16"), "sel2": ([2, 128], "bf16"), "dtab": ([128, 8192], "bf16"),
    "qrel": ([1, 512], "f32"), "adm": ([16, 2, 8192], "bf16"),
    "w_branch_a": ([1024, 2048], "f32"), "w_branch_b": ([1024, 2048], "f32"), "w_out": ([2048, 2048], "f32"),
    "wr": ([2048, 36], "f32"), "brr": ([1, 36], "f32"),
    "ln1_gb": ([128, 2048], "f32"), "ln1_bb": ([128, 2048], "f32"), "ln2_gb": ([128, 2048], "f32"), "ln2_bb": ([128, 2048], "f32"),
    "ltri": ([128, 128], "bf16"), "e256": ([128, 32], "f32"), "iota256": ([128, CAP], "f32"),
    "w_gate": ([NEXP, 2048, 1024], "f32"), "w_up": ([NEXP, 2048, 1024], "f32"), "w_down": ([NEXP, 1024, 2048], "f32"),
}


def _dt(s):
    return {"f32": F32, "bf16": BF16, "u32": U32, "i32": I32}[s]


def host_consts(inputs):
    b_in = np.asarray(inputs["b_in"][0], np.float32)
    bfm = np.zeros((128, NBFM), np.float32)
    def put(idx, c0, n):
        for c in range(n):
            bfm[:, idx + c] = b_in[c0 + c * 128: c0 + (c + 1) * 128]
    put(BFM_IDX["af"], C_AF, 8); put(BFM_IDX["aq"], C_AQ, 8); put(BFM_IDX["ag"], C_AG, 8)
    put(BFM_IDX["bk"], C_BK, 8); put(BFM_IDX["bq"], C_BQ, 8); put(BFM_IDX["iq"], C_IQ, 4)
    put(BFM_IDX["g"], C_G, 32)
    bfm[0:64, BFM_IDX["ik"]] = b_in[C_IK:C_IK + 64]
    bfm[64:128, BFM_IDX["ik"]] = b_in[C_IK:C_IK + 64]
    brow = np.concatenate([b_in[C_AI:C_AI + 1024], b_in[C_BV:C_BV + 1024], b_in[C_IW:C_IW + 8]])[None, :].astype(np.float32)
    lbl = np.ascontiguousarray(np.asarray(inputs["hg_lb_logits"], np.float32).reshape(2, 8, 128).transpose(2, 0, 1))
    normg = np.ascontiguousarray(np.asarray(inputs["hg_norm_g"][0], np.float32).reshape(8, 128).T)
    rmask = np.ones((128, 512), np.float32)
    rmask[:, ::64] = 0.0
    ii = np.arange(128)
    bdmask = ((ii[:, None] // 64 == ii[None, :] // 64) & (ii[:, None] <= ii[None, :])).astype(np.float32)
    import ml_dtypes
    bf = ml_dtypes.bfloat16
    slopes = 2.0 ** -(np.arange(8) + 1.0)
    pp = np.arange(128)
    alb = (slopes[None, :, None] * (pp[:, None, None] + 128.0 * (np.arange(64)[None, None, :] - 60))).astype(np.float32)
    dsq = np.maximum(pp[:, None] - pp[None, :], 0).astype(np.float64)
    corr = np.exp(-2.0 * slopes[None, :, None] * dsq[:, None, :]).astype(bf)
    sel2 = np.zeros((2, 128), np.float32); sel2[0, :64] = 1; sel2[1, 64:] = 1
    dtab = np.abs(8064 + pp[:, None] - np.arange(8192)[None, :]).astype(bf)
    qrel = np.arange(512, dtype=np.float32)[None, :]
    extra = {}
    if "w_out" in inputs:
        extra["w_branch_a"] = np.ascontiguousarray(inputs["w_branch_a"][0]); extra["w_branch_b"] = np.ascontiguousarray(inputs["w_branch_b"][0])
        extra["w_out"] = np.ascontiguousarray(inputs["w_out"][0])
        extra["wr"] = np.ascontiguousarray(np.concatenate([inputs["w_group"][0], inputs["w_router"][0]], axis=1).astype(np.float32))
        extra["brr"] = np.concatenate([inputs["b_group"][0], inputs["b_router"][0]])[None, :].astype(np.float32)
        for nm in ("ln1_g", "ln1_b", "ln2_g", "ln2_b"):
            extra[nm + "b"] = np.ascontiguousarray(np.broadcast_to(np.asarray(inputs[nm][0], np.float32)[None, :], (128, 2048)))
        extra["ltri"] = (pp[:, None] < pp[None, :]).astype(bf)
        extra["e256"] = np.ascontiguousarray(np.broadcast_to((np.arange(32, dtype=np.float32) * CAP)[None, :], (128, 32)))
        extra["iota256"] = np.ascontiguousarray(np.broadcast_to(np.arange(CAP, dtype=np.float32)[None, :], (128, CAP)))
        extra["w_gate"] = np.ascontiguousarray(inputs["w_gate"][0]); extra["w_up"] = np.ascontiguousarray(inputs["w_up"][0])
        extra["w_down"] = np.ascontiguousarray(inputs["w_down"][0])
    return {**extra, "alb": alb, "corr": corr, "sel2": sel2.astype(bf), "dtab": dtab, "qrel": qrel, "bdmask": bdmask, "ident": np.eye(128, dtype=np.float32), "bfm": bfm, "brow": brow, "lbl": lbl, "normg": normg, "rmask": rmask,
            "w_in": np.ascontiguousarray(inputs["w_in"][0])}


def host_core_inputs(inputs, hc, core):
    b, j = core // 4, core % 4
    x = np.asarray(inputs["x"], np.float32)
    xs = np.zeros((8192, 2048), np.float32)
    npre = (3 - j) * 2048
    xs[npre:] = x[b, :(j + 1) * 2048]
    valid = np.zeros(8192, np.float32)
    valid[npre:] = 1.0
    d = dict(hc)
    d["xs"] = xs
    d["valid_tm"] = np.ascontiguousarray(valid.reshape(64, 128).T)
    import ml_dtypes
    chunk = np.arange(8192) // 64
    adm = np.full((16, 2, 8192), NEG_ADM, np.float32)
    for t in range(16):
        c_first = (OWN0 + t * 128) // 64
        adm[t, 0, (valid > 0) & (chunk <= c_first)] = 0.0
        adm[t, 1, (valid > 0) & (chunk <= c_first + 1)] = 0.0
    d["adm"] = adm.astype(ml_dtypes.bfloat16)
    return d


def build_program(phases=("p1a",), dump=(), p1_blocks=(0, 1, 2, 3), p2_groups=(0, 1, 2, 3), in_names=None):
    nc = bass.Bass("TRN2", target_bir_lowering=False)
    A = {}
    used_inputs = in_names if in_names is not None else list(INPUTS)
    for n in used_inputs:
        shp, dt = INPUTS[n]
        A[n] = nc.dram_tensor(n, shp, _dt(dt), kind="ExternalInput").ap()
    for n, (shp, dt) in SCRATCH.items():
        kind = "ExternalOutput" if n in dump else "Internal"
        A[n] = nc.dram_tensor(n, shp, _dt(dt), kind=kind).ap()
    A["out"] = nc.dram_tensor("out", [2048, 2048], F32, kind="ExternalOutput").ap()
    with ExitStack() as es:
        S = Sched(nc, es)
        G = {"ap": A, "pb": PBanks(S), "B": {n: S.buf(n, glob=True) for n in list(SCRATCH) + ["out"]}, "bfm_idx": BFM_IDX}
        G["persist"] = {"wts": S.sbuf("p_wts", [128, 16, 2], F32), "RK": S.sbuf("p_RK", [128, 16, 32], F32), "idx": S.sbuf("p_idx", [128, 16, 2], F32)}
        G["Br_persist"] = S.buf("persist", glob=True)
        cst = {}
        Bc = S.buf("cst", glob=True)
        G["cst"] = cst
        G["Bcst"] = Bc
        idf = S.sbuf("idf", [128, 128], F32)
        Bidf = S.buf("idf", glob=True)
        S.dma("sp", idf[:], A["ident"], writes=[Bidf])
        idb = S.sbuf("idb", [128, 128], BF16)
        Bident = S.buf("idb")
        S.op("dve", lambda e: e.tensor_copy(idb[:], idf[:]), reads=[Bidf], writes=[Bident])
        G["ident_bf"] = idb
        G["Bident"] = Bident
        G["ident_f"] = idf
        G["Bidf"] = Bidf
        for n in ("bfm", "brow", "valid_tm", "normg", "rmask", "bdmask"):
            shp, dt = INPUTS[n]
            cst[n] = S.sbuf("c_" + n, shp, _dt(dt))
            S.dma("sp", cst[n][:], A[n], writes=[Bc], nowaw=True)
        lbl = S.sbuf("c_lbl", [128, 2, 8], F32)
        Blbl = S.buf("lbl", glob=True)
        S.dma("sp", lbl[:], A["lbl"], writes=[Blbl])
        for n in ("lb", "oml", "noml", "lbd"):
            cst[n] = S.sbuf("c_" + n, [128, 8], F32)
        cst["ones_row"] = S.sbuf("c_ones_row", [1, 128], F32)
        S.op("dve", lambda e: e.memset(cst["ones_row"][:], 1.0), writes=[Bc])
        S.op("dve", lambda e: e.tensor_tensor(cst["lbd"][:], lbl[:, 0, :], lbl[:, 1, :], ALU.subtract), reads=[Blbl], writes=[Bc])
        S.op("act", lambda e: e.activation(cst["lb"][:], cst["lbd"][:], AF.Sigmoid), reads=[Bc], writes=[Bc])
        S.op("dve", lambda e: e.tensor_scalar(cst["oml"][:], cst["lb"][:], -1.0, 1.0, op0=ALU.mult, op1=ALU.add), reads=[Bc], writes=[Bc])
        S.op("dve", lambda e: e.tensor_scalar(cst["noml"][:], cst["oml"][:], -1.0, None, op0=ALU.mult), reads=[Bc], writes=[Bc])

        cst["eps_ln"] = S.sbuf("c_eps_ln", [128, 1], F32)
        S.op("dve", lambda e: e.memset(cst["eps_ln"][:], LN_EPS), writes=[Bc])
        cst["eps_rms"] = S.sbuf("c_eps_rms", [128, 1], F32)
        S.op("dve", lambda e: e.memset(cst["eps_rms"][:], RMS_EPS), writes=[Bc])
        S.barrier()
        if "p1a" in phases:
            with ExitStack() as pes:
                S.es = pes
                phase1a(S, G, blocks=p1_blocks)
                S.es = es
            S.barrier()
            S.phase_end()
        if "p1b" in phases:
            with ExitStack() as pes:
                S.es = pes
                phase1b(S, G)
                S.es = es
            S.barrier()
            S.phase_end()
        if "p2" in phases:
            phase2_dsa(S, G, groups=p2_groups)
            S.barrier()
            S.phase_end()
        for nm, fn in (("p3a", phase3a_merge), ("p3b", phase3b_out), ("p4", phase4_moe), ("p5", phase5_final)):
            if nm in phases:
                with ExitStack() as pes:
                    S.es = pes
                    fn(S, G)
                    S.es = es
                S.barrier()
                S.phase_end()
        outs = [G["B"][n] for n in dump] + ([G["B"]["out"]] if "p5" in phases else [])
        S.wait_all("sp", outs)
        print("instructions:", S.ninst, {k: len(v) for k, v in S.ops.items()})
        S.run()
    return nc


ALL_PHASES = ("p1a", "p1b", "p2", "p3a", "p3b", "p4", "p5")
_CACHE = {}


def kernel(**inputs):
    if "nc" not in _CACHE:
        _CACHE["nc"] = build_program(phases=ALL_PHASES)
    nc = _CACHE["nc"]
    hc = host_consts(inputs)
    in_maps = [host_core_inputs(inputs, hc, c) for c in range(8)]
    res = run_bass_kernel_spmd(nc, in_maps, core_ids=list(range(8)))
    out = np.zeros((2, 8192, 2048), np.float32)
    for c in range(8):
        b, j = c // 4, c % 4
        out[b, j * 2048:(j + 1) * 2048] = np.asarray(res.results[c]["out"])
    return out
```
